# Optimizing a Trainium2 kernel written in Bass

```python
import math
import jax
import jax.numpy as jnp
from jax import lax
import numpy as np

D_MODEL = 1024
BATCH = 32
SEQ = 2048
DEPTH = 1

CTX_LEN = 256
GRID_W = 64
MIX_W = D_MODEL
HY_W = MIX_W // 2
HG_W = MIX_W - HY_W
HY_ORDER = 2
HY_EMB = 33
HY_BANDS = (HY_EMB - 1) // 2
HY_FILTER_HIDDEN = 64
HY_DECAY_TARGET = 1e-2
HY_FAST_DECAY_PCT = 0.3
HY_SLOW_DECAY_PCT = 1.5
HG_HEAD_DIM = 128
HG_HEADS = HG_W // HG_HEAD_DIM
HG_SCALE = HG_HEAD_DIM ** -0.5
HG_CHUNK = 64
N_EXPERTS = 256
TOP_K = 8
N_GROUPS = 8
TOPK_GROUPS = 4
EXPERT_FF = 256
SHARED_FF = 256
ROUTED_SCALE = 2.5
MOE_BLOCK = 128
NORM_EPS = 1e-6
IN_COLS = 3 * HY_W + 5 * HG_W

kernel_name = 'hyena_hgrn2_moe_diffusion_block'


def rmsnorm(x, g):
    xf = x.astype(jnp.float32)
    y = xf * lax.rsqrt(jnp.mean(xf * xf, axis=-1, keepdims=True) + NORM_EPS)
    return (y * g.astype(jnp.float32)).astype(x.dtype)


def centred_conv3(u, w, b):
    pad = [(0, 0)] * (u.ndim - 2) + [(1, 1), (0, 0)]
    up = jnp.pad(u, pad)
    return w[0] * up[..., :-2, :] + w[1] * up[..., 1:-1, :] + w[2] * up[..., 2:, :] + b


def hyena_filter_spectrum(L, fw1, fb1, fw2, fb2, fw3, freq):
    f32 = jnp.float32
    pos = jnp.arange(L, dtype=f32)[:, None]
    t = pos / max(L - 1, 1)
    w = (2.0 * math.pi / L) * pos
    bands = jnp.linspace(1e-4, HY_BANDS - 1, HY_BANDS, dtype=f32)[None, :]
    feats = jnp.concatenate([t, jnp.cos(bands * w), -jnp.sin(bands * w)], axis=-1)
    fr = freq.astype(f32)
    h = jnp.sin(fr * (feats @ fw1.astype(f32) + fb1.astype(f32)))
    h = jnp.sin(fr * (h @ fw2.astype(f32) + fb2.astype(f32)))
    h = (h @ fw3.astype(f32)).reshape(L, HY_ORDER, 2, HY_W)
    max_decay = math.log(HY_DECAY_TARGET) / HY_FAST_DECAY_PCT
    min_decay = math.log(HY_DECAY_TARGET) / HY_SLOW_DECAY_PCT
    deltas = jnp.abs(jnp.linspace(min_decay, max_decay, HY_W, dtype=f32))
    h = h * jnp.exp(-t * deltas)[:, None, None, :]
    h = h / jnp.sum(jnp.abs(h), axis=(0, 2), keepdims=True)
    fwd, bwd = h[:, :, 0], h[:, :, 1]
    k_circ = jnp.concatenate([fwd[:1] + bwd[:1], fwd[1:], jnp.zeros_like(fwd[:1]), bwd[1:][::-1]], axis=0)
    return jnp.fft.rfft(k_circ, axis=0)


def hyena_group(p, rows, conv_w, conv_b, fw1, fb1, fw2, fb2, fw3, freq, d_skip):
    B, L, C3 = p.shape
    u = p.astype(jnp.float32)
    cw, cb = conv_w.astype(jnp.float32), conv_b.astype(jnp.float32)
    if rows is None:
        u = centred_conv3(u, cw, cb)
    else:
        u = centred_conv3(u.reshape(B, rows, GRID_W, C3), cw, cb).reshape(B, L, C3)
    x1, x2, v = jnp.split(u, 3, axis=-1)
    k_hat = hyena_filter_spectrum(L, fw1, fb1, fw2, fb2, fw3, freq)
    ds = d_skip.astype(jnp.float32)

    def long_conv(z, o):
        z_hat = jnp.fft.rfft(z, n=2 * L, axis=1)
        y = jnp.fft.irfft(z_hat * k_hat[:, o], n=2 * L, axis=1)[:, :L]
        return y + z * ds[o]

    z = x1 * long_conv(v, 0)
    y = x2 * long_conv(z, 1)
    return y.astype(p.dtype)


def gla_chunk(q, k, v, log_f, s0):
    B, H, L, DK = q.shape
    DV = v.shape[-1]
    N = L // HG_CHUNK
    q, k, log_f = (a.reshape(B, H, N, HG_CHUNK, DK) for a in (q, k, log_f))
    v = v.reshape(B, H, N, HG_CHUNK, DV)
    b = jnp.cumsum(log_f, axis=3)
    b_last = b[:, :, :, -1:, :]
    b_mid = b[:, :, :, HG_CHUNK // 2 - 1:HG_CHUNK // 2, :]
    qm = q * jnp.exp(b - b_mid)
    km = k * jnp.exp(b_mid - b)
    mask = jnp.tril(jnp.ones((HG_CHUNK, HG_CHUNK), dtype=bool))
    att = jnp.where(mask, jnp.einsum('bhncd,bhnsd->bhncs', qm, km), 0.0)
    o_intra = jnp.einsum('bhncs,bhnsv->bhncv', att, v)
    u = jnp.einsum('bhnsd,bhnsv->bhndv', k * jnp.exp(b_last - b), v)
    decay = jnp.exp(b_last[:, :, :, 0, :])

    def step(s, inp):
        dec, un = inp
        return dec[..., None] * s + un, s

    s_final, s_prev = lax.scan(step, s0, (jnp.moveaxis(decay, 2, 0), jnp.moveaxis(u, 2, 0)))
    s_prev = jnp.moveaxis(s_prev, 0, 2)
    o_inter = jnp.einsum('bhncd,bhndv->bhncv', q * jnp.exp(b), s_prev)
    return (o_intra + o_inter).reshape(B, H, L, DV), s_final


def hgrn2_group(p, lb_f, lb_b, norm_g, s0_f, s0_b):
    B, L, _ = p.shape
    pf = p.astype(jnp.float32)
    q, f_fwd, f_bwd, i, g = jnp.split(pf, 5, axis=-1)
    heads = lambda a: a.reshape(B, L, HG_HEADS, HG_HEAD_DIM).transpose(0, 2, 1, 3)
    q = heads(jax.nn.silu(q) * HG_SCALE)
    v = heads(i)

    def forget(fr, lb):
        lb = lb.astype(jnp.float32)
        log_f = jnp.log(lb + (1.0 - lb) * jax.nn.sigmoid(fr))
        k = (1.0 - lb) * jax.nn.sigmoid(-fr)
        return heads(log_f), heads(k)

    lf_f, k_f = forget(f_fwd, lb_f)
    lf_b, k_b = forget(f_bwd, lb_b)
    o_f, s_f = gla_chunk(q, k_f, v, lf_f, s0_f)
    rev = lambda a: jnp.flip(a, axis=2)
    o_b, s_b = gla_chunk(rev(q), rev(k_b), rev(v), rev(lf_b), s0_b)
    o = o_f + rev(o_b)
    o = o * lax.rsqrt(jnp.mean(o * o, axis=-1, keepdims=True) + NORM_EPS) * norm_g.astype(jnp.float32)
    o = o.transpose(0, 2, 1, 3).reshape(B, L, HG_W) * jax.nn.silu(g)
    return o.astype(p.dtype), s_f, s_b


def moe_ffn(h, w_router, router_bias, ew_gate, ew_up, ew_down, sw_gate, sw_up, sw_down):
    T, D = h.shape
    f32 = jnp.float32
    scores = jax.nn.sigmoid(h.astype(f32) @ w_router.astype(f32))
    biased = scores + router_bias.astype(f32)
    grp = biased.reshape(T, N_GROUPS, N_EXPERTS // N_GROUPS)
    grp_score = jnp.sum(lax.top_k(grp, 2)[0], axis=-1)
    _, top_grp = lax.top_k(grp_score, TOPK_GROUPS)
    grp_mask = jnp.sum(jax.nn.one_hot(top_grp, N_GROUPS, dtype=f32), axis=1) > 0
    expert_mask = jnp.repeat(grp_mask, N_EXPERTS // N_GROUPS, axis=1)
    _, top_e = lax.top_k(jnp.where(expert_mask, biased, -jnp.inf), TOP_K)
    w = jnp.take_along_axis(scores, top_e, axis=1)
    w = ROUTED_SCALE * w / jnp.sum(w, axis=-1, keepdims=True)
    n_assign = T * TOP_K
    flat_e = top_e.reshape(n_assign)
    flat_tok = jnp.repeat(jnp.arange(T, dtype=jnp.int32), TOP_K)
    order = jnp.argsort(flat_e)
    e_sorted = flat_e[order]
    counts = jnp.bincount(flat_e, length=N_EXPERTS)
    starts = jnp.cumsum(counts) - counts
    padded = (counts + MOE_BLOCK - 1) // MOE_BLOCK * MOE_BLOCK
    p_ends = jnp.cumsum(padded)
    p_starts = p_ends - padded
    dest = p_starts[e_sorted] + (jnp.arange(n_assign, dtype=jnp.int32) - starts[e_sorted])
    n_blocks = -(-n_assign // MOE_BLOCK) + N_EXPERTS
    slot_tok = jnp.full((n_blocks * MOE_BLOCK,), T, jnp.int32).at[dest].set(flat_tok[order])
    slot_w = jnp.zeros((n_blocks * MOE_BLOCK,), h.dtype).at[dest].set(w.reshape(n_assign)[order].astype(h.dtype))
    block_e = jnp.minimum(jnp.searchsorted(p_ends, jnp.arange(n_blocks, dtype=jnp.int32) * MOE_BLOCK, side='right'), N_EXPERTS - 1)
    h_pad = jnp.concatenate([h, jnp.zeros((1, D), h.dtype)], axis=0)

    def expert_block(acc, blk):
        tok, wt, e = blk
        xb = h_pad[tok]
        yb = (jax.nn.silu(xb @ ew_gate[e]) * (xb @ ew_up[e])) @ ew_down[e]
        return acc.at[tok].add(yb * wt[:, None]), None

    acc, _ = lax.scan(expert_block, jnp.zeros_like(h_pad),
                      (slot_tok.reshape(n_blocks, MOE_BLOCK), slot_w.reshape(n_blocks, MOE_BLOCK), block_e))
    shared = (jax.nn.silu(h @ sw_gate) * (h @ sw_up)) @ sw_down
    return acc[:T] + shared


def setup_inputs(seed: int = 0) -> dict:
    key = jax.random.key(seed)
    ks = jax.random.split(key, 30)
    nrm = lambda i, shape, scale: scale * jax.random.normal(ks[i], shape, jnp.float32)
    D, L_ = D_MODEL, DEPTH
    return {
        'x': nrm(0, (BATCH, SEQ, D), 1.0),
        'c': nrm(1, (BATCH, D), 1.0),
        'ctx': nrm(2, (BATCH, CTX_LEN, D), 1.0),
        'c_ctx': nrm(3, (D,), 1.0),
        'w_mod': nrm(4, (L_, D, 6 * D), 0.5 * D ** -0.5),
        'b_mod': nrm(5, (L_, 6 * D), 0.02),
        'norm1_g': 1.0 + nrm(6, (L_, D), 0.05),
        'norm2_g': 1.0 + nrm(7, (L_, D), 0.05),
        'w_in': nrm(8, (L_, D, IN_COLS), D ** -0.5),
        'w_out': nrm(9, (L_, MIX_W, D), MIX_W ** -0.5),
        'hy_conv_w': nrm(10, (L_, 3, 3 * HY_W), 0.5),
        'hy_conv_b': nrm(11, (L_, 3 * HY_W), 0.02),
        'hy_fw1': nrm(12, (L_, HY_EMB, HY_FILTER_HIDDEN), HY_EMB ** -0.5),
        'hy_fb1': nrm(13, (L_, HY_FILTER_HIDDEN), 0.1),
        'hy_fw2': nrm(14, (L_, HY_FILTER_HIDDEN, HY_FILTER_HIDDEN), HY_FILTER_HIDDEN ** -0.5),
        'hy_fb2': nrm(15, (L_, HY_FILTER_HIDDEN), 0.1),
        'hy_fw3': nrm(16, (L_, HY_FILTER_HIDDEN, HY_ORDER * 2 * HY_W), HY_FILTER_HIDDEN ** -0.5),
        'hy_freq': 1.0 + nrm(17, (L_, HY_FILTER_HIDDEN), 0.1),
        'hy_d': nrm(18, (L_, HY_ORDER, HY_W), 0.5),
        'hg_lb_logits': nrm(19, (2, L_ + 1, HG_W), 0.1),
        'hg_norm_g': 1.0 + nrm(20, (L_, HG_HEAD_DIM), 0.05),
        'w_router': nrm(21, (L_, D, N_EXPERTS), D ** -0.5),
        'router_bias': nrm(22, (L_, N_EXPERTS), 0.01),
        'ew_gate': nrm(23, (L_, N_EXPERTS, D, EXPERT_FF), D ** -0.5),
        'ew_up': nrm(24, (L_, N_EXPERTS, D, EXPERT_FF), D ** -0.5),
        'ew_down': nrm(25, (L_, N_EXPERTS, EXPERT_FF, D), EXPERT_FF ** -0.5),
        'sw_gate': nrm(26, (L_, D, SHARED_FF), D ** -0.5),
        'sw_up': nrm(27, (L_, D, SHARED_FF), D ** -0.5),
        'sw_down': nrm(28, (L_, SHARED_FF, D), SHARED_FF ** -0.5),
        'final_g': 1.0 + nrm(29, (D,), 0.05),
    }


def reference(x, c, ctx, c_ctx, w_mod, b_mod, norm1_g, norm2_g, w_in, w_out, hy_conv_w, hy_conv_b,
              hy_fw1, hy_fb1, hy_fw2, hy_fb2, hy_fw3, hy_freq, hy_d, hg_lb_logits, hg_norm_g,
              w_router, router_bias, ew_gate, ew_up, ew_down, sw_gate, sw_up, sw_down, final_g):
    B, L, D = x.shape
    rows = L // GRID_W
    lower_bounds = jnp.cumsum(jax.nn.softmax(hg_lb_logits.astype(jnp.float32), axis=1), axis=1)
    s_zero = jnp.zeros((B, HG_HEADS, HG_HEAD_DIM, HG_HEAD_DIM), jnp.float32)
    for l in range(DEPTH):
        mod_x = jax.nn.silu(c) @ w_mod[l] + b_mod[l]
        mod_c = jax.nn.silu(c_ctx) @ w_mod[l] + b_mod[l]
        sh1, sc1, g1, sh2, sc2, g2 = jnp.split(mod_x[:, None, :], 6, axis=-1)
        csh1, csc1, cg1, csh2, csc2, cg2 = jnp.split(mod_c, 6, axis=-1)
        hx = rmsnorm(x, norm1_g[l]) * (1 + sc1) + sh1
        hc = rmsnorm(ctx, norm1_g[l]) * (1 + csc1) + csh1
        px = hx @ w_in[l]
        pc = hc @ w_in[l]
        hy_params = (hy_conv_w[l], hy_conv_b[l], hy_fw1[l], hy_fb1[l], hy_fw2[l], hy_fb2[l],
                     hy_fw3[l], hy_freq[l], hy_d[l])
        lb_f, lb_b = lower_bounds[0, l], lower_bounds[1, l]
        yc_hg, s_f, s_b = hgrn2_group(pc[..., 3 * HY_W:], lb_f, lb_b, hg_norm_g[l], s_zero, s_zero)
        yx_hg, _, _ = hgrn2_group(px[..., 3 * HY_W:], lb_f, lb_b, hg_norm_g[l], s_f, s_b)
        yx_hy = hyena_group(px[..., :3 * HY_W], rows, *hy_params)
        x = x + g1 * (jnp.concatenate([yx_hy, yx_hg], axis=-1) @ w_out[l])
        moe_params = (w_router[l], router_bias[l], ew_gate[l], ew_up[l], ew_down[l],
                      sw_gate[l], sw_up[l], sw_down[l])
        hx2 = rmsnorm(x, norm2_g[l]) * (1 + sc2) + sh2
        if l == DEPTH - 1:
            x = x + g2 * moe_ffn(hx2.reshape(B * L, D), *moe_params).reshape(B, L, D)
        else:
            yc_hy = hyena_group(pc[..., :3 * HY_W], None, *hy_params)
            ctx = ctx + cg1 * (jnp.concatenate([yc_hy, yc_hg], axis=-1) @ w_out[l])
            hc2 = rmsnorm(ctx, norm2_g[l]) * (1 + csc2) + csh2
            out = moe_ffn(jnp.concatenate([hx2.reshape(B * L, D), hc2.reshape(-1, D)], axis=0), *moe_params)
            x = x + g2 * out[:B * L].reshape(B, L, D)
            ctx = ctx + cg2 * out[B * L:].reshape(ctx.shape)
    return rmsnorm(x, final_g)
```

```python
import os
from contextlib import ExitStack
from concourse.bass_utils import run_bass_kernel_spmd
import numpy as np
import concourse.bass as bass
import concourse.mybir as mybir

F32 = mybir.dt.float32
BF16 = mybir.dt.bfloat16
I32 = mybir.dt.int32
U32 = mybir.dt.uint32
AF = mybir.ActivationFunctionType
ALU = mybir.AluOpType
AX = mybir.AxisListType


class Buf:
    __slots__ = ("name", "w", "r")

    def __init__(self, name=""):
        self.name = name
        self.w = None
        self.r = []


class Prog:
    COMPUTE = ("pe", "act", "dve", "pool")
    NDMA = {"sp": 10, "act": 4, "pool": 8}

    def __init__(self, nc, stack, same_engine_sync=True):
        self.nc = nc
        self.same = same_engine_sync
        self.ops = {e: [] for e in ("pe", "act", "dve", "pool", "sp")}
        self.sem = {}
        self.cnt = {}
        for e in self.COMPUTE:
            self.sem[e] = stack.enter_context(nc.semaphore("s_" + e))
            self.cnt[e] = 0
        self.dsem = {}
        self.dcnt = {}
        self.drr = {}
        for q, n in self.NDMA.items():
            for i in range(n):
                k = "d_%s_%d" % (q, i)
                self.sem[k] = stack.enter_context(nc.semaphore(k))
                self.cnt[k] = 0
            self.drr[q] = 0
        self.known = {e: {} for e in self.ops}
        self.nwaits = 0
        self.pre = {}

    def _deps(self, eng, reads, writes, is_dma=False):
        need = {}

        def add(tok):
            if tok is None:
                return
            k, v = tok
            if need.get(k, 0) < v:
                need[k] = v
        for b in reads:
            add(b.w)
        for b in writes:
            add(b.w)
            for t in b.r:
                add(t)
        waits = []
        kn = self.known[eng]
        for k, v in need.items():
            if k == eng and not self.same and not is_dma:
                continue
            if k == "pe" and eng == "pe":
                continue
            if kn.get(k, 0) >= v:
                continue
            kn[k] = v
            waits.append((k, v))
        self.nwaits += len(waits)
        return waits

    def _commit(self, tok, reads, writes):
        for b in reads:
            b.r.append(tok)
            if len(b.r) > 64:
                m = {}
                for k, v in b.r:
                    if m.get(k, 0) < v:
                        m[k] = v
                b.r = list(m.items())
        for b in writes:
            b.w = tok
            b.r = []

    def op(self, eng, fn, reads=(), writes=()):
        waits = self._deps(eng, reads, writes)
        self.cnt[eng] += 1
        tok = (eng, self.cnt[eng])
        self.ops[eng].append((waits, fn, (eng, 1)))
        self._commit(tok, reads, writes)
        return tok

    def dma(self, q, fn, reads=(), writes=()):
        n = self.NDMA[q]
        i = self.drr[q]
        self.drr[q] = (i + 1) % n
        k = "d_%s_%d" % (q, i)
        waits = self._deps(q, reads, writes, is_dma=True)
        prev = self.cnt[k]
        kn = self.known[q]
        if prev > 0 and kn.get(k, 0) < prev:
            kn[k] = prev
            waits.append((k, prev))
        self.cnt[k] += 16
        tok = (k, self.cnt[k])
        self.ops[q].append((waits, fn, (k, 16)))
        self._commit(tok, reads, writes)
        return tok

    def barrier_tokens(self):
        toks = []
        for k, v in self.cnt.items():
            if v > 0:
                toks.append((k, v))
        return toks

    def barrier(self):
        toks = self.barrier_tokens()
        for e in self.ops:
            kn = self.known[e]
            waits = []
            for k, v in toks:
                if k == e and e == "pe":
                    continue
                if kn.get(k, 0) < v:
                    kn[k] = v
                    waits.append((k, v))
            if waits:
                self.ops[e].append((waits, None, None))

    def final_wait(self, eng="sp"):
        toks = self.barrier_tokens()
        self.ops[eng].append(([(k, v) for k, v in toks], None, None))

    def emit(self):
        nc = self.nc
        engmap = {"pe": "tensor", "act": "scalar", "dve": "vector", "pool": "gpsimd", "sp": "sync"}
        with nc.Block() as block:
            for e, attr in engmap.items():
                lst = self.ops[e]

                def body(engine, lst=lst, e=e):
                    if e in self.pre:
                        self.pre[e](engine)
                    for waits, fn, inc in lst:
                        for k, v in waits:
                            engine.wait_ge(self.sem[k], v)
                        if fn is not None:
                            ins = fn(engine)
                            ins.then_inc(self.sem[inc[0]], inc[1])
                getattr(block, attr)(body)


class Arena:
    def __init__(self, nc, stack, nwords, name="arena"):
        self.t = stack.enter_context(nc.sbuf_tensor(name, [128, nwords], F32))
        self.n = nwords
        self.off = 0
        self.marks = []

    def alloc(self, shape, dtype, parts=128):
        n = int(np.prod(shape))
        if dtype == BF16:
            words = (n + 1) // 2
        else:
            words = n
        assert self.off + words <= self.n, "arena overflow %d + %d > %d" % (self.off, words, self.n)
        a = self.t[0:parts, self.off:self.off + words]
        self.off += words
        if dtype != F32:
            a = a.bitcast(dtype)
        if dtype == BF16 and n % 2 == 1:
            a = a[:, 0:n]
        if len(shape) > 1:
            names = " ".join("d%d" % i for i in range(len(shape)))
            kw = {"d%d" % i: int(s) for i, s in enumerate(shape)}
            a = a.rearrange("p (%s) -> p %s" % (names, names), **kw)
        return a

    def mark(self):
        self.marks.append(self.off)

    def release(self):
        self.off = self.marks.pop()

D = 1024
KD = 8
L = 2048
CTX = 256
T = L + CTX
NT = T // 128
NLT = L // 128
NCH = T // 64
HGS = 128.0 ** -0.5
EPS = 1e-6
NE = 256
CAP = int(os.environ.get('KCAP', '384'))
OV = 128
NBLK = CAP // 128
RSCALE = 2.5


def mm(out, lhsT, rhs, start, stop):
    return lambda e: e.matmul(out, lhsT, rhs, start=start, stop=stop)


def tr(out, in_, ident):
    return lambda e: e.transpose(out=out, in_=in_, identity=ident)


def act(out, in_, func, **kw):
    return lambda e: e.activation(out=out, in_=in_, func=func, **kw)


def tt(out, a, b, op):
    return lambda e: e.tensor_tensor(out=out, in0=a, in1=b, op=op)


def ts(out, a, s1, s2, op0, op1=None):
    if op1 is None:
        return lambda e: e.tensor_scalar(out=out, in0=a, scalar1=s1, scalar2=None, op0=op0)
    return lambda e: e.tensor_scalar(out=out, in0=a, scalar1=s1, scalar2=s2, op0=op0, op1=op1)


def stt(out, a, s, b, op0, op1):
    return lambda e: e.scalar_tensor_tensor(out=out, in0=a, scalar=s, in1=b, op0=op0, op1=op1)


def cp(out, in_):
    return lambda e: e.tensor_copy(out=out, in_=in_)


def rsum(out, in_):
    return lambda e: e.reduce_sum(out=out, in_=in_, axis=AX.X)


def dma(out, in_):
    return lambda e: e.dma_start(out=out, in_=in_)


def bcast_rows(dram_ap_tensor, offset, n, parts=128):
    return bass.AP(tensor=dram_ap_tensor, offset=offset, ap=[[0, parts], [1, n]])


class K:
    def __init__(self, NB, dbg=(), upto=4):
        self.NB = NB
        self.upto = upto
        self.cut = int(os.environ.get('KCUT', '0'))
        self.dbg = set(dbg)
        self.nc = bass.Bass("TRN2", target_bir_lowering=False)
        self.outs = []

    def din(self, name, shape, dtype=F32):
        return self.nc.dram_tensor(name, list(shape), dtype, kind="ExternalInput").ap()

    def dscr(self, name, shape, dtype):
        return self.nc.dram_tensor(name, list(shape), dtype, kind="Internal").ap()

    def dout(self, name, shape, dtype=F32):
        self.outs.append(name)
        return self.nc.dram_tensor(name, list(shape), dtype, kind="ExternalOutput").ap()

    def bank(self):
        self.bi = (self.bi + 1) % len(self.rot)
        return self.rot[self.bi]

    def reserve(self, n):
        got = [self.rot.pop() for _ in range(n)]
        self.bi = 0
        return got

    def unreserve(self, got):
        self.rot.extend(got)

    def dump(self, name, ap, buf, shape, dtype=F32):
        if name not in self.dbg:
            return
        o = self.dout("dbg_" + name, shape, dtype)
        self.P.dma("sp", dma(o, ap), reads=[buf])

    def build(self):
        nc = self.nc
        NB = self.NB
        with ExitStack() as st:
            self.st = st
            P = self.P = Prog(nc, st, same_engine_sync=(os.environ.get('KSAME', '1') == '1'))
            A = self.A = Arena(nc, st, 51500)
            self.banks = [(st.enter_context(nc.psum_tensor("pb%d" % i, [128, 512], F32))[:, :], Buf("pb%d" % i)) for i in range(8)]
            self.bi = 0
            self.rot = list(self.banks)
            self.declare_io()
            self.consts()
            self.modulation()
            if self.upto >= 2:
                self.filters()
            if self.upto >= 0.5:
                for b in range(NB):
                    self.mixer_batch(b)
            if self.upto >= 2:
                self.hyena_all()
            if self.upto >= 3:
                self.route_init()
                for b in range(NB):
                    self.outproj_route(b)
                self.overflow_route()
                P.barrier()
                self.A.release()
            if self.upto >= 4:
                self.experts()
                self.combine()
            P.final_wait("sp")
            P.emit()
        return nc

    def declare_io(self):
        NB = self.NB
        d = self.din
        self.x_d = d("x", [NB * L, D])
        self.ctx_d = d("ctx", [NB * CTX, D])
        self.cT_d = d("cT", [128, KD, NB + 1])
        self.wmod_d = d("w_mod", [D, 6 * D]).rearrange("(k p) n -> p k n", p=128)
        self.bmod_d = d("b_mod", [1, 6 * D])
        self.n1g_d = d("norm1_g", [1, D])
        self.n2g_d = d("norm2_g", [1, D])
        self.fing_d = d("final_g", [1, D])
        self.win_d = d("w_in", [D, 4096]).rearrange("(k p) n -> p k n", p=128)
        self.wout_d = d("w_out", [D, D]).rearrange("(k p) n -> p k n", p=128)
        self.hcw_d = d("hy_conv_w", [3, 1536])
        self.hcb_d = d("hy_conv_b", [1, 1536])
        self.fw1_d = d("hy_fw1", [33, 64])
        self.fb1_d = d("hy_fb1", [64, 1])
        self.fw2_d = d("hy_fw2", [64, 64])
        self.fb2_d = d("hy_fb2", [64, 1])
        self.fw3_d = d("hy_fw3", [64, 2048])
        self.freq_d = d("hy_freq", [64, 1])
        self.hyd_d = d("hy_d", [1, 1024])
        self.lbT_d = d("lbT", [128, 16])
        self.hgng_d = d("hg_norm_g", [1, 128])
        self.wr_d = d("w_router", [D, NE]).rearrange("(k p) n -> p k n", p=128)
        self.rb_d = d("router_bias", [1, NE])
        if self.upto >= 4:
            self.ewg_d = d("ew_gate", [NE, 128, KD * 256])
            self.ewu_d = d("ew_up", [NE, 128, KD * 256])
            self.ewd_d = d("ew_down", [NE, 128, 2 * D])
        self.swg_d = d("sw_gate", [D, 256]).rearrange("(k p) n -> p k n", p=128)
        self.swu_d = d("sw_up", [D, 256]).rearrange("(k p) n -> p k n", p=128)
        self.swd_d = d("sw_down", [256, D]).rearrange("(k p) n -> p k n", p=128)
        self.featsT_d = d("featsT", [33, L])
        self.cst_d = d("cst", [128, 10, 128])
        self.deltas_d = d("deltas", [1, 512])
        self.tfrac_d = d("tfrac", [128, 16])
        self.iota_d = d("iota", [1, NE])
        self.pidx_d = d("pidx", [128, 1])
        self.out_d = self.dout("out", [NB * L, D])
        self.MOD_d = self.dscr("MODs", [NB + 1, 6 * D], F32)
        self.HT_d = self.dscr("HTs", [NB, 128, KD * L], BF16)
        self.YT_d = self.dscr("YTs", [NB, 8, 128, L], BF16)
        self.G_d = self.dscr("Gs", [2, 512, 4096], BF16)
        self.X1_d = self.dscr("X1s", [NB * L, D], F32)
        self.XS_d = self.dscr("XSs", [NE * CAP + OV * 128, D], BF16)
        self.Y_d = self.dscr("Ys", [NE * CAP + OV * 128, D], BF16)
        self.H2_d = self.dscr("H2s", [NB * L, D], BF16)

    def consts(self):
        P, A = self.P, self.A
        cst = A.alloc([10, 128], F32)
        self.b_cst = Buf("cst")
        P.dma("sp", dma(cst, self.cst_d), writes=[self.b_cst])
        self.identf = cst[:, 0, :]
        self.Jf = cst[:, 1, :]
        self.maskF = cst[:, 2, :]
        self.maskB = cst[:, 3, :]
        self.ustrict_f = cst[:, 6, :]
        self.ones_f = cst[:, 7, :]
        cb = A.alloc([10, 128], BF16)
        self.b_cb = Buf("cb")
        P.op("dve", cp(cb, cst), reads=[self.b_cst], writes=[self.b_cb])
        self.identb = cb[:, 0, :]
        self.Jb = cb[:, 1, :]
        self.Smb = cb[:, 4, :]
        self.Spb = cb[:, 5, :]
        self.ustrict_b = cb[:, 6, :]
        self.ones_b = cb[:, 7, :]
        self.SmRb = cb[:, 8, :]
        self.SpRb = cb[:, 9, :]
        ce = A.alloc([2], F32)
        self.b_eps = Buf("eps")
        P.op("pool", lambda e: e.memset(ce[:, 0:1], EPS), writes=[self.b_eps])
        P.op("pool", lambda e: e.memset(ce[:, 1:2], 1.0), writes=[self.b_eps])
        self.eps_ap = ce[:, 0:1]
        self.one_ap = ce[:, 1:2]
        self.n1g = A.alloc([D], F32)
        self.b_n1g = Buf()
        P.dma("sp", dma(self.n1g, bcast_rows(self.n1g_d.tensor, 0, D)), writes=[self.b_n1g])
        self.hgng = A.alloc([128], F32)
        self.b_hgng = Buf()
        P.dma("sp", dma(self.hgng, bcast_rows(self.hgng_d.tensor, 0, 128)), writes=[self.b_hgng])
        lbt = A.alloc([2, 2, 4], F32)
        b_lbt = Buf()
        P.dma("sp", dma(lbt, self.lbT_d.rearrange("p (a b c) -> p a b c", a=2, b=2)), writes=[b_lbt])
        self.lb = A.alloc([2, 4], F32)
        self.oml = A.alloc([2, 4], F32)
        self.noml = A.alloc([2, 4], F32)
        self.b_lb = Buf()
        P.op("dve", tt(self.lb, lbt[:, :, 0, :], lbt[:, :, 1, :], ALU.subtract), reads=[b_lbt], writes=[self.b_lb])
        P.op("act", act(self.lb, self.lb, AF.Sigmoid), reads=[self.b_lb], writes=[self.b_lb])
        P.op("dve", ts(self.oml, self.lb, -1.0, 1.0, ALU.mult, ALU.add), reads=[self.b_lb], writes=[self.b_lb])
        P.op("dve", ts(self.noml, self.lb, -1.0, None, ALU.add), reads=[self.b_lb], writes=[self.b_lb])

    def modulation(self):
        P, A, NB = self.P, self.A, self.NB
        A.mark()
        cT = A.alloc([KD, NB + 1], F32)
        b_cT = Buf()
        P.dma("sp", dma(cT, self.cT_d), writes=[b_cT])
        P.op("act", act(cT, cT, AF.Silu), reads=[b_cT], writes=[b_cT])
        bm = A.alloc([6 * D], F32, parts=1)
        b_bm = Buf()
        P.dma("sp", dma(bm, self.bmod_d), writes=[b_bm])
        modsb = A.alloc([6 * D], F32)
        b_mod = Buf()
        wms = [(A.alloc([KD, 512], F32), Buf()) for _ in range(2)]
        for ci in range(12):
            wm, bw = wms[ci % 2]
            P.dma("sp" if ci % 2 == 0 else "act", dma(wm, self.wmod_d[:, :, ci * 512:(ci + 1) * 512]), writes=[bw])
            ps, pb = self.bank()
            for k in range(KD):
                P.op("pe", mm(ps[0:NB + 1, :], cT[:, k, :], wm[:, k, :], k == 0, False), reads=[b_cT, bw], writes=[pb])
            P.op("pe", mm(ps[0:NB + 1, :], self.ones_f[0:1, 0:NB + 1], bm[0:1, ci * 512:(ci + 1) * 512], False, True),
                 reads=[self.b_cst, b_bm], writes=[pb])
            P.op("act", act(modsb[0:NB + 1, ci * 512:(ci + 1) * 512], ps[0:NB + 1, :], AF.Copy), reads=[pb], writes=[b_mod])
        self.b_MOD = Buf("MOD")
        P.dma("sp", dma(self.MOD_d, modsb[0:NB + 1, :]), reads=[b_mod], writes=[self.b_MOD])
        self.dump("mod", modsb[0:NB + 1, :], b_mod, [NB + 1, 6 * D])
        P.barrier()
        A.release()
        self.CA = A.alloc([D], F32)
        self.CB = A.alloc([D], F32)
        self.b_CA = Buf()
        self.b_CB = Buf()
        mt = self.MOD_d.tensor
        P.dma("sp", dma(self.CB, bcast_rows(mt, NB * 6 * D + 0 * D, D)), reads=[self.b_MOD], writes=[self.b_CB])
        P.dma("sp", dma(self.CA, bcast_rows(mt, NB * 6 * D + 1 * D, D)), reads=[self.b_MOD], writes=[self.b_CA])
        P.op("dve", stt(self.CA, self.CA, 1.0, self.n1g, ALU.add, ALU.mult), reads=[self.b_CA, self.b_n1g], writes=[self.b_CA])

    def norm_mod_tile(self, i, src, Abc, bA, Bbc, bB, dst, b_dst, hb_out=None):
        P = self.P
        xt, bx = self.xt[i % 2]
        P.dma("sp", dma(xt, src), writes=[bx])
        self.norm_mod_sb(i, xt, bx, Abc, bA, Bbc, bB, dst, b_dst)

    def norm_mod_sb(self, i, xt, bx, Abc, bA, Bbc, bB, dst, b_dst, hf_out=None, hb_out=None):
        P = self.P
        junk, bj = self.junk
        ss, bss = self.ssb[i % 2]
        hb, bhb = (self.hb[i % 2] if hb_out is None else hb_out)
        P.op("act", act(junk, xt, AF.Square), reads=[bx], writes=[bj])
        P.op("dve", rsum(ss, junk), reads=[bj], writes=[bss])
        P.op("act", act(ss, ss, AF.Sqrt, scale=1.0 / D, bias=self.eps_ap), reads=[bss, self.b_eps], writes=[bss])
        P.op("dve", lambda e: e.reciprocal(out=ss, in_=ss), reads=[bss], writes=[bss])
        P.op("dve", stt(junk, xt, ss, Abc, ALU.mult, ALU.mult), reads=[bx, bss, bA], writes=[bj])
        if hf_out is not None:
            hf, bhf = hf_out
            P.op("dve", tt(hf, junk, Bbc, ALU.add), reads=[bj, bB], writes=[bhf])
            P.op("pool", cp(hb, hf), reads=[bhf], writes=[bhb])
        else:
            P.op("pool", tt(hb, junk, Bbc, ALU.add), reads=[bj, bB], writes=[bhb])
        ps, pb = self.bank()
        psb = ps.bitcast(BF16)
        for k in range(KD):
            P.op("pe", tr(psb[:, k * 128:(k + 1) * 128], hb[:, k * 128:(k + 1) * 128], self.identb),
                 reads=[bhb, self.b_cb], writes=[pb])
        P.op("act", act(dst, psb.rearrange("p (k t) -> p k t", k=KD), AF.Copy), reads=[pb], writes=[b_dst])

    def mixer_batch(self, b):
        P, A, NB = self.P, self.A, self.NB
        A.mark()
        mt = self.MOD_d.tensor
        A1 = A.alloc([D], F32)
        B1 = A.alloc([D], F32)
        bA1, bB1 = Buf(), Buf()
        P.dma("sp", dma(B1, bcast_rows(mt, b * 6 * D + 0 * D, D)), reads=[self.b_MOD], writes=[bB1])
        P.dma("sp", dma(A1, bcast_rows(mt, b * 6 * D + 1 * D, D)), reads=[self.b_MOD], writes=[bA1])
        P.op("dve", stt(A1, A1, 1.0, self.n1g, ALU.add, ALU.mult), reads=[bA1, self.b_n1g], writes=[bA1])
        hT = A.alloc([KD, T], BF16)
        b_hT = Buf("hT")
        yT = A.alloc([4, L], BF16)
        b_yT = Buf("yT")
        A.mark()
        self.xt = [(A.alloc([D], F32), Buf()) for _ in range(2)]
        self.junk = (A.alloc([D], F32), Buf())
        self.ssb = [(A.alloc([1], F32), Buf()) for _ in range(2)]
        self.hb = [(A.alloc([D], BF16), Buf()) for _ in range(2)]
        for j in range(NT):
            if j < 2:
                src = self.ctx_d[b * CTX + j * 128: b * CTX + (j + 1) * 128, :]
                self.norm_mod_tile(j, src, self.CA, self.b_CA, self.CB, self.b_CB, hT[:, :, j * 128:(j + 1) * 128], b_hT)
            else:
                src = self.x_d[b * L + (j - 2) * 128: b * L + (j - 1) * 128, :]
                self.norm_mod_tile(j, src, A1, bA1, B1, bB1, hT[:, :, j * 128:(j + 1) * 128], b_hT)
        self.b_HT = getattr(self, "b_HT", None) or Buf("HT")
        P.dma("sp", dma(self.HT_d[b].rearrange("p (k t) -> p k t", k=KD), hT[:, :, CTX:T]), reads=[b_hT], writes=[self.b_HT])
        if b == 0:
            self.dump("hT", hT, b_hT, [128, KD, T], BF16)
        P.barrier()
        A.release()
        if self.upto < 1:
            A.release()
            return
        self.hgrn2(b, hT, b_hT, yT, b_yT)
        self.b_YT = getattr(self, "b_YT", None) or Buf("YT")
        for hh in range(4):
            P.dma("sp", dma(self.YT_d[b, 4 + hh], yT[:, hh, :]), reads=[b_yT], writes=[self.b_YT])
        if b == 0:
            self.dump("yT_hg", yT, b_yT, [128, 4, L], BF16)
        P.barrier()
        A.release()

    def hgrn2(self, b, hT, b_hT, yT, b_yT):
        P, A = self.P, self.A
        A.mark()
        f32b = lambda: (A.alloc([T], F32), Buf())
        bf16b = lambda: (A.alloc([T], BF16), Buf())
        self.rs = A.alloc([T], F32)
        self.b_rs = Buf()
        P.op("pool", lambda e: e.memset(self.rs, 1.0), writes=[self.b_rs])
        rs3 = self.rs.rearrange("p (a b) -> p a b", b=64)
        P.op("pool", lambda e: e.memset(rs3[:, :, 0:1], 0.0), writes=[self.b_rs])
        t1, b_t1 = f32b()
        kk, b_kk = f32b()
        bb, b_bb = f32b()
        t2, b_t2 = f32b()
        qs, b_qs = bf16b()
        qm, b_qm = bf16b()
        km, b_km = bf16b()
        qbE, b_qbE = bf16b()
        qbO, b_qbO = bf16b()
        kdT, b_kdT = bf16b()
        P.op("pool", lambda e: e.memset(qbE, 0.0), writes=[b_qbE])
        P.op("pool", lambda e: e.memset(qbO, 0.0), writes=[b_qbO])
        kdTokE = A.alloc([NT, 128], BF16)
        kdTokO = A.alloc([NT, 128], BF16)
        b_kdTok = Buf()
        P.op("pool", lambda e: e.memset(kdTokE[64:128], 0.0), writes=[b_kdTok])
        P.op("pool", lambda e: e.memset(kdTokO[0:64], 0.0), writes=[b_kdTok])
        V = A.alloc([NT, 128], BF16)
        b_V = Buf()
        gsn = A.alloc([NLT, 128], BF16)
        b_gsn = Buf()
        of = A.alloc([NLT, 128], F32)
        b_of = Buf()
        dec = A.alloc([NCH], F32)
        b_dec = Buf()
        ws = [(A.alloc([KD, 128], BF16), Buf()) for _ in range(5)]
        Sf = [(A.alloc([128], F32), Buf()) for _ in range(2)]
        Sb = [(A.alloc([128], BF16), Buf()) for _ in range(2)]
        attT = [(A.alloc([128], BF16), Buf()) for _ in range(2)]
        otl = [(A.alloc([128], F32), Buf()) for _ in range(2)]
        oj = [(A.alloc([128], F32), Buf()) for _ in range(2)]
        ossb = [(A.alloc([1], F32), Buf()) for _ in range(2)]
        ytl = [(A.alloc([128], BF16), Buf()) for _ in range(2)]
        gtmp = [(A.alloc([128], F32), Buf()) for _ in range(2)]
        chunks = [(t0, min(512, T - t0)) for t0 in range(0, T, 512)]
        v3 = lambda ap: ap.rearrange("p (a b) -> p a b", b=64)
        for hh in range(4):
            cols = [1536 + s * 512 + hh * 128 for s in range(5)]
            for s in range(5):
                w, bw = ws[s]
                P.dma("pool", dma(w, self.win_d[:, :, cols[s]:cols[s] + 128]), writes=[bw])
            wq, wf, wb_, wi, wg = ws
            for j in range(NT):
                ps, pb = self.bank()
                for k in range(KD):
                    P.op("pe", mm(ps[:, 0:128], hT[:, k, j * 128:(j + 1) * 128], wi[0][:, k, :], k == 0, k == KD - 1),
                         reads=[b_hT, wi[1]], writes=[pb])
                P.op("act", act(V[:, j, :], ps[:, 0:128], AF.Copy), reads=[pb], writes=[b_V])
                if j >= 2:
                    ps2, pb2 = self.bank()
                    for k in range(KD):
                        P.op("pe", mm(ps2[:, 0:128], hT[:, k, j * 128:(j + 1) * 128], wg[0][:, k, :], k == 0, k == KD - 1),
                             reads=[b_hT, wg[1]], writes=[pb2])
                    gt, bgt = gtmp[j % 2]
                    P.op("act", act(gt, ps2[:, 0:128], AF.Silu), reads=[pb2], writes=[bgt])
                    P.op("dve", tt(gsn[:, j - 2, :], gt, self.hgng, ALU.mult), reads=[bgt, self.b_hgng], writes=[b_gsn])
            for (t0, n) in chunks:
                ps, pb = self.bank()
                for k in range(KD):
                    P.op("pe", mm(ps[:, 0:n], wq[0][:, k, :], hT[:, k, t0:t0 + n], k == 0, k == KD - 1),
                         reads=[b_hT, wq[1]], writes=[pb])
                P.op("act", act(qs[:, t0:t0 + n], ps[:, 0:n], AF.Silu), reads=[pb], writes=[b_qs])
            for d in range(2):
                wgate = wf if d == 0 else wb_
                for (t0, n) in chunks:
                    ps, pb = self.bank()
                    for k in range(KD):
                        P.op("pe", mm(ps[:, 0:n], wgate[0][:, k, :], hT[:, k, t0:t0 + n], k == 0, k == KD - 1),
                             reads=[b_hT, wgate[1]], writes=[pb])
                    P.op("act", act(t1[:, t0:t0 + n], ps[:, 0:n], AF.Sigmoid), reads=[pb], writes=[b_t1])
                P.op("dve", ts(kk, t1, self.noml[:, d, hh:hh + 1], self.oml[:, d, hh:hh + 1], ALU.mult, ALU.add),
                     reads=[b_t1, self.b_lb], writes=[b_kk])
                P.op("act", act(t1, kk, AF.Ln, scale=-1.0, bias=self.one_ap), reads=[b_kk, self.b_eps], writes=[b_t1])
                P.op("dve", lambda e: e.tensor_tensor_scan(out=bb, data0=self.rs, data1=t1, initial=0.0, op0=ALU.mult, op1=ALU.add),
                     reads=[self.b_rs, b_t1], writes=[b_bb])
                bb3, t13 = v3(bb), v3(t1)
                if d == 1:
                    P.op("dve", tt(t1, t1, bb, ALU.subtract), reads=[b_t1, b_bb], writes=[b_t1])
                    P.op("dve", tt(bb3, t13, bb3[:, :, 63:64].to_broadcast([128, NCH, 64]), ALU.add), reads=[b_t1, b_bb], writes=[b_bb])
                mid = 31 if d == 0 else 32
                last = 63 if d == 0 else 0
                P.op("dve", tt(t13, bb3, bb3[:, :, mid:mid + 1].to_broadcast([128, NCH, 64]), ALU.subtract), reads=[b_bb], writes=[b_t1])
                P.op("act", act(t2, t1, AF.Exp), reads=[b_t1], writes=[b_t2])
                P.op("dve", stt(qm, qs, HGS, t2, ALU.mult, ALU.mult), reads=[b_qs, b_t2], writes=[b_qm])
                P.op("act", act(t2, t1, AF.Exp, scale=-1.0), reads=[b_t1, b_qm], writes=[b_t2])
                P.op("dve", tt(km, kk, t2, ALU.mult), reads=[b_kk, b_t2], writes=[b_km])
                P.op("act", act(t2, bb, AF.Exp), reads=[b_bb, b_km], writes=[b_t2])
                t23, qs3, qbE3, qbO3 = v3(t2), v3(qs), v3(qbE), v3(qbO)
                P.op("dve", stt(qbE3[:, 0::2, :], qs3[:, 0::2, :], HGS, t23[:, 0::2, :], ALU.mult, ALU.mult),
                     reads=[b_qs, b_t2], writes=[b_qbE])
                P.op("dve", stt(qbO3[:, 1::2, :], qs3[:, 1::2, :], HGS, t23[:, 1::2, :], ALU.mult, ALU.mult),
                     reads=[b_qs, b_t2], writes=[b_qbO])
                P.op("dve", tt(t13, bb3[:, :, last:last + 1].to_broadcast([128, NCH, 64]), bb3, ALU.subtract), reads=[b_bb, b_t2], writes=[b_t1])
                P.op("act", act(t1, t1, AF.Exp), reads=[b_t1], writes=[b_t1])
                P.op("dve", tt(kdT, kk, t1, ALU.mult), reads=[b_kk, b_t1], writes=[b_kdT])
                P.op("act", act(dec, bb3[:, :, last], AF.Exp), reads=[b_bb], writes=[b_dec])
                for j0 in range(0, NT, 8):
                    nj = min(8, NT - j0)
                    ps, pb = self.bank()
                    psb = ps.bitcast(BF16)
                    for jj in range(nj):
                        j = j0 + jj
                        P.op("pe", tr(psb[:, jj * 128:(jj + 1) * 128], kdT[:, j * 128:(j + 1) * 128], self.identb),
                             reads=[b_kdT, self.b_cb], writes=[pb])
                    P.op("act", act(kdTokE[0:64, j0:j0 + nj, :], psb[0:64, 0:nj * 128].rearrange("p (a b) -> p a b", b=128), AF.Copy),
                         reads=[pb], writes=[b_kdTok])
                    P.op("act", act(kdTokO[64:128, j0:j0 + nj, :], psb[64:128, 0:nj * 128].rearrange("p (a b) -> p a b", b=128), AF.Copy),
                         reads=[pb], writes=[b_kdTok])
                si = 0
                P.op("pool", lambda e, s=Sf[0][0]: e.memset(s, 0.0), writes=[Sf[0][1]])
                P.op("pool", lambda e, s=Sb[0][0]: e.memset(s, 0.0), writes=[Sb[0][1]])
                order = list(range(NT)) if d == 0 else [1, 0] + list(range(NT - 1, 1, -1))
                mask = self.maskF if d == 0 else self.maskB
                for j in order:
                    lat = j >= 2
                    tsl = slice(j * 128, (j + 1) * 128)
                    halves = [0, 1] if d == 0 else [1, 0]
                    if lat:
                        psA, pbA = self.bank()
                        P.op("pe", mm(psA[:, 0:128], km[:, tsl], qm[:, tsl], True, True), reads=[b_km, b_qm], writes=[pbA])
                        at, bat = attT[j % 2]
                        P.op("dve", tt(at, psA[:, 0:128], mask, ALU.mult), reads=[pbA, self.b_cst], writes=[bat])
                        psO, pbO = self.bank()
                        P.op("pe", mm(psO[:, 0:128], at, V[:, j, :], True, False), reads=[bat, b_V], writes=[pbO])
                    for hi, h in enumerate(halves):
                        if lat:
                            qbx, b_qbx = (qbE, b_qbE) if h == 0 else (qbO, b_qbO)
                            P.op("pe", mm(psO[:, 0:128], qbx[:, tsl], Sb[si][0], False, hi == 1),
                                 reads=[b_qbx, Sb[si][1]], writes=[pbO])
                        psU, pbU = self.bank()
                        kdx = kdTokE if h == 0 else kdTokO
                        P.op("pe", mm(psU[:, 0:128], kdx[:, j, :], V[:, j, :], True, True), reads=[b_kdTok, b_V], writes=[pbU])
                        ch = 2 * j + h
                        P.op("dve", stt(Sf[1 - si][0], Sf[si][0], dec[:, ch:ch + 1], psU[:, 0:128], ALU.mult, ALU.add),
                             reads=[Sf[si][1], b_dec, pbU], writes=[Sf[1 - si][1]])
                        P.op("act", act(Sb[1 - si][0], Sf[1 - si][0], AF.Copy), reads=[Sf[1 - si][1]], writes=[Sb[1 - si][1]])
                        si = 1 - si
                    if lat:
                        if d == 0:
                            P.op("act", act(of[:, j - 2, :], psO[:, 0:128], AF.Copy), reads=[pbO], writes=[b_of])
                        else:
                            o, bo = oj[j % 2]
                            P.op("dve", tt(o, psO[:, 0:128], of[:, j - 2, :], ALU.add), reads=[pbO, b_of], writes=[bo])
                            jk, bjk = otl[j % 2]
                            oss, boss = ossb[j % 2]
                            P.op("act", act(jk, o, AF.Square), reads=[bo], writes=[bjk])
                            P.op("dve", rsum(oss, jk), reads=[bjk], writes=[boss])
                            P.op("act", act(oss, oss, AF.Sqrt, scale=1.0 / 128, bias=self.eps_ap), reads=[boss, self.b_eps], writes=[boss])
                            P.op("dve", lambda e, oss=oss: e.reciprocal(out=oss, in_=oss), reads=[boss], writes=[boss])
                            yt, byt = ytl[j % 2]
                            P.op("dve", stt(yt, o, oss, gsn[:, j - 2, :], ALU.mult, ALU.mult), reads=[bo, boss, b_gsn], writes=[byt])
                            psT, pbT = self.bank()
                            psTb = psT.bitcast(BF16)
                            P.op("pe", tr(psTb[:, 0:128], yt, self.identb), reads=[byt, self.b_cb], writes=[pbT])
                            P.op("act", act(yT[:, hh, (j - 2) * 128:(j - 1) * 128], psTb[:, 0:128], AF.Copy), reads=[pbT], writes=[b_yT])
        P.barrier()
        A.release()

    def sin_da(self, out, arg, tmpa, tmpb, bufs):
        P = self.P
        b_out, b_arg, b_ta, b_tb = bufs
        P.op("act", act(tmpa, arg, AF.Sin, scale=0.5), reads=[b_arg], writes=[b_ta])
        P.op("act", act(tmpb, arg, AF.Sin, scale=0.25), reads=[b_arg], writes=[b_tb])
        P.op("dve", tt(tmpb, tmpb, tmpb, ALU.mult), reads=[b_tb], writes=[b_tb])
        P.op("dve", ts(tmpb, tmpb, -2.0, 1.0, ALU.mult, ALU.add), reads=[b_tb], writes=[b_tb])
        P.op("dve", stt(out, tmpa, 2.0, tmpb, ALU.mult, ALU.mult), reads=[b_ta, b_tb], writes=[b_out])

    def filters(self):
        P, A = self.P, self.A
        A.mark()
        ld = lambda shape, src, parts: (A.alloc(shape, F32, parts=parts), Buf())
        featsT, b_ft = ld([L], None, 33)
        P.dma("sp", dma(featsT, self.featsT_d), writes=[b_ft])
        fw1, b_fw1 = ld([64], None, 33)
        P.dma("sp", dma(fw1, self.fw1_d), writes=[b_fw1])
        fw2, b_fw2 = ld([64], None, 64)
        P.dma("sp", dma(fw2, self.fw2_d), writes=[b_fw2])
        fw3, b_fw3 = ld([2048], None, 64)
        P.dma("sp", dma(fw3, self.fw3_d), writes=[b_fw3])
        sm, b_sm = ld([8], None, 64)
        P.dma("sp", dma(sm[:, 0:1], self.fb1_d), writes=[b_sm])
        P.dma("sp", dma(sm[:, 1:2], self.fb2_d), writes=[b_sm])
        P.dma("sp", dma(sm[:, 2:3], self.freq_d), writes=[b_sm])
        P.op("dve", ts(sm[:, 3:5], sm[:, 0:2], sm[:, 2:3], None, ALU.mult), reads=[b_sm], writes=[b_sm])
        dl, b_dl = ld([512], None, 128)
        P.dma("sp", dma(dl, bcast_rows(self.deltas_d.tensor, 0, 512)), writes=[b_dl])
        tf, b_tf = ld([16], None, 128)
        P.dma("sp", dma(tf, self.tfrac_d), writes=[b_tf])
        dsk, b_dsk = ld([2, 4], None, 128)
        P.dma("sp", lambda e: e.dma_start(out=dsk, in_=self.hyd_d.rearrange("a (o g c) -> c (a o) g", o=2, g=4),
                                          allow_slow_non_contiguous=True), writes=[b_dsk])
        h1, b_h1 = ld([L], None, 64)
        h2, b_h2 = ld([L], None, 64)
        ta, b_ta = ld([L], None, 64)
        tb, b_tb = ld([L], None, 64)
        ar, b_ar = ld([L], None, 64)
        for layer in range(2):
            src, b_src, K_, w, b_w = (featsT, b_ft, 33, fw1, b_fw1) if layer == 0 else (h1, b_h1, 64, fw2, b_fw2)
            for q in range(4):
                ps, pb = self.bank()
                P.op("pe", mm(ps[0:64, :], w[0:K_, :], src[0:K_, q * 512:(q + 1) * 512], True, True), reads=[b_w, b_src], writes=[pb])
                P.op("act", act(ar[:, q * 512:(q + 1) * 512], ps[0:64, :], AF.Identity, scale=sm[:, 2:3], bias=sm[:, 3 + layer:4 + layer]),
                     reads=[pb, b_sm], writes=[b_ar])
            dst, b_dst = (h1, b_h1) if layer == 0 else (h2, b_h2)
            self.sin_da(dst, ar, ta, tb, (b_dst, b_ar, b_ta, b_tb))
        rinv = A.alloc([2, 512], F32)
        b_rinv = Buf()
        win = [(A.alloc([512], F32), Buf()) for _ in range(2)]
        winr = [(A.alloc([2, 512], F32), Buf()) for _ in range(2)]
        hw = [(A.alloc([512], F32), Buf()) for _ in range(2)]
        hn = [(A.alloc([512], BF16), Buf()) for _ in range(2)]
        GT = A.alloc([4, 2, 4096], BF16)
        b_GT = Buf("GT")
        P.op("pool", lambda e: e.memset(GT, 0.0), writes=[b_GT])
        accs = self.reserve(2)
        for i in range(16):
            wi_, bwi = win[i % 2]
            P.op("act", act(wi_, dl, AF.Exp, scale=tf[:, i:i + 1]), reads=[b_dl, b_tf], writes=[bwi])
            for o in range(2):
                for dr in range(2):
                    q = o * 2 + dr
                    ps, pb = self.bank()
                    P.op("pe", mm(ps[:, :], h2[0:64, i * 128:(i + 1) * 128], fw3[0:64, q * 512:(q + 1) * 512], True, True),
                         reads=[b_h2, b_fw3], writes=[pb])
                    hwt, bhw = hw[q % 2]
                    P.op("dve", tt(hwt, ps, wi_, ALU.mult), reads=[pb, bwi], writes=[bhw])
                    P.op("act", act(hwt, hwt, AF.Abs), reads=[bhw], writes=[bhw])
                    first = (i == 0 and dr == 0)
                    lastf = (i == 15 and dr == 1)
                    P.op("pe", mm(accs[o][0][:, :], self.ones_f, hwt, first, lastf), reads=[self.b_cst, bhw], writes=[accs[o][1]])
        for o in range(2):
            P.op("dve", lambda e, o=o: e.reciprocal(out=rinv[:, o, :], in_=accs[o][0][:, :]), reads=[accs[o][1]], writes=[b_rinv])
        self.unreserve(accs)
        GTv = GT
        for i in range(16):
            wi_, bwi = win[i % 2]
            wr, bwr = winr[i % 2]
            P.op("act", act(wi_, dl, AF.Exp, scale=tf[:, i:i + 1]), reads=[b_dl, b_tf], writes=[bwi])
            P.op("dve", tt(wr, rinv, wi_.unsqueeze(1).to_broadcast([128, 2, 512]), ALU.mult), reads=[bwi, b_rinv], writes=[bwr])
            for o in range(2):
                for dr in ([1, 0] if o == 0 else [0, 1]):
                    q = o * 2 + dr
                    useJ = (o == 0 and dr == 1) or (o == 1 and dr == 0)
                    ps, pb = self.bank()
                    P.op("pe", mm(ps[:, :], h2[0:64, i * 128:(i + 1) * 128], fw3[0:64, q * 512:(q + 1) * 512], True, True),
                         reads=[b_h2, b_fw3], writes=[pb])
                    hnt, bhn = hn[q % 2]
                    P.op("dve", tt(hnt, ps, wr[:, o, :], ALU.mult), reads=[pb, bwr], writes=[bhn])
                    pt, pbt = self.bank()
                    for cg in range(4):
                        P.op("pe", mm(pt[:, cg * 128:(cg + 1) * 128], hnt[:, cg * 128:(cg + 1) * 128], self.Jb if useJ else self.identb, True, True),
                             reads=[bhn, self.b_cb], writes=[pbt])
                    pt3 = pt.rearrange("p (g m) -> p g m", g=4)
                    if o == 0:
                        start = (1921 - 128 * i) if useJ else (2048 + 128 * i)
                    else:
                        start = (1920 - 128 * i) if useJ else (2047 + 128 * i)
                    if i == 0 and not useJ:
                        P.op("act", act(GTv[:, :, o, start + 1:start + 128], pt3[:, :, 1:128], AF.Copy), reads=[pbt], writes=[b_GT])
                        ctr, b_ctr = self.ctr_tmp = getattr(self, "ctr_tmp", None) or (A.alloc([4], F32), Buf())
                        P.op("dve", tt(ctr, pt3[:, :, 0], dsk[:, o, :], ALU.add), reads=[pbt, b_dsk], writes=[b_ctr])
                        P.op("dve", tt(GTv[:, :, o, start], GTv[:, :, o, start], ctr, ALU.add), reads=[b_ctr, b_GT], writes=[b_GT])
                    else:
                        P.op("act", act(GTv[:, :, o, start:start + 128], pt3, AF.Copy), reads=[pbt], writes=[b_GT])
        self.b_G = Buf("G")
        for o in range(2):
            for cg in range(4):
                P.dma("sp", dma(self.G_d[o, cg * 128:(cg + 1) * 128, :], GTv[:, cg, o, :]), reads=[b_GT], writes=[self.b_G])
        if "G" in self.dbg:
            for o in range(2):
                og = self.dout("dbg_G%d" % o, [128, 4, 4096], BF16)
                P.dma("sp", dma(og, GTv[:, :, o, :]), reads=[b_GT])
        P.barrier()
        A.release()

    def hyena_all(self):
        P, A, NB = self.P, self.A, self.NB
        NBI = NB * 16
        A.mark()
        bf = lambda: A.alloc([128, NB, 16], BF16)
        X1, X2r, Vr, Z = bf(), bf(), bf(), bf()
        b_in = [Buf() for _ in range(16)]
        b_Z = [Buf() for _ in range(16)]
        hTl = A.alloc([KD, L], BF16)
        b_hTl = Buf()
        W3 = A.alloc([KD, 384], BF16)
        b_W3 = Buf()
        wc = A.alloc([3, 384], F32)
        bc = A.alloc([384], F32)
        b_wc = Buf()
        pw = [(A.alloc([3, 384], BF16), Buf()) for _ in range(2)]
        NTZ = 3
        TZ = [[(A.alloc([3968], BF16), Buf()) for _ in range(NTZ)] for _ in range(2)]
        yTc = [(A.alloc([512], BF16), Buf()) for _ in range(2)]
        gt = self.G_d.tensor
        dq = ["sp", "act"]
        tzc = [0, 0]
        for cg in range(4):
            for s in range(3):
                c0 = s * 512 + cg * 128
                P.dma("pool", dma(W3[:, :, s * 128:(s + 1) * 128], self.win_d[:, :, c0:c0 + 128]), writes=[b_W3])
                for kk in range(3):
                    P.dma("sp", dma(wc[:, kk, s * 128:(s + 1) * 128], bcast_rows(self.hcw_d.tensor, kk * 1536 + c0, 128)), writes=[b_wc])
                P.dma("sp", dma(bc[:, s * 128:(s + 1) * 128], bcast_rows(self.hcb_d.tensor, c0, 128)), writes=[b_wc])
            for b in range(NB):
                P.dma("sp", dma(hTl, self.HT_d[b].rearrange("p (k t) -> p k t", k=KD)), reads=[self.b_HT], writes=[b_hTl])
                for i in range(NLT):
                    ps, pb = self.bank()
                    for k in range(KD):
                        P.op("pe", mm(ps[:, 0:384], hTl[:, k, i * 128:(i + 1) * 128], W3[:, k, :], k == 0, k == KD - 1),
                             reads=[b_hTl, b_W3], writes=[pb])
                    pwt, bpw = pw[i % 2]
                    for kk in range(3):
                        P.op("dve", tt(pwt[:, kk, :], ps[:, 0:384], wc[:, kk, :], ALU.mult), reads=[pb, b_wc], writes=[bpw])
                    pa, pba = self.bank()
                    pr, pbr = self.bank()
                    mats = [(self.Smb, self.SmRb), (self.identb, self.Jb), (self.Spb, self.SpRb)]
                    for kk in range(3):
                        P.op("pe", mm(pa[:, 0:128], mats[kk][0], pwt[:, kk, 0:128], kk == 0, kk == 2), reads=[bpw, self.b_cb], writes=[pba])
                    for kk in range(3):
                        P.op("pe", mm(pr[:, 0:256], mats[kk][1], pwt[:, kk, 128:384], kk == 0, kk == 2), reads=[bpw, self.b_cb], writes=[pbr])
                    P.op("dve", tt(X1[:, :, b, i], pa[:, 0:128], bc[:, 0:128], ALU.add), reads=[pba, b_wc], writes=b_in)
                    P.op("dve", tt(X2r[:, :, b, i], pr[:, 0:128], bc[:, 128:256], ALU.add), reads=[pbr, b_wc], writes=b_in)
                    P.op("dve", tt(Vr[:, :, b, i], pr[:, 128:256], bc[:, 256:384], ALU.add), reads=[pbr, b_wc], writes=b_in)
            deltas = [0] + [s * m for m in range(1, 16) for s in (1, -1)]

            def conv(o, g, src):
                ps, pb = self.bank()
                for cc in range(8):
                    c = g * 8 + cc
                    ch = cg * 128 + c
                    tz, btz = TZ[o][tzc[o] % NTZ]
                    tzc[o] += 1
                    off = (o * 512 + ch) * 4096 + (1 if o == 0 else 0)
                    P.dma(dq[(c + o) % 2], dma(tz, bass.AP(tensor=gt, offset=off, ap=[[1, 128], [1, 3968]])), reads=[self.b_G], writes=[btz])
                    psv = ps[:, cc * NBI:(cc + 1) * NBI].rearrange("p (b i) -> p b i", b=NB)
                    for di, dl_ in enumerate(deltas):
                        ilo, ihi = max(0, -dl_), min(16, 16 - dl_)
                        blk = (dl_ + 15) if o == 0 else (-dl_ + 15)
                        P.op("pe", mm(psv[:, :, ilo + dl_:ihi + dl_], tz[:, blk * 128:(blk + 1) * 128], src[:, c, :, ilo:ihi],
                                      di == 0, di == len(deltas) - 1),
                             reads=[btz, (b_in[g] if o == 0 else b_Z[g])], writes=[pb])
                return ps, pb

            def evac1(g, ps, pb):
                P.op("dve", tt(Z[:, g * 8:(g + 1) * 8, :, :].rearrange("p c b i -> p (c b i)"), ps[:, 0:8 * NBI],
                               X1[:, g * 8:(g + 1) * 8, :, :].rearrange("p c b i -> p (c b i)"), ALU.mult),
                     reads=[pb, b_in[g]], writes=[b_Z[g]])

            def evac2(g, ps, pb):
                P.op("dve", tt(Z[:, g * 8:(g + 1) * 8, :, :].rearrange("p c b i -> p (c b i)"), ps[:, 0:8 * NBI],
                               X2r[:, g * 8:(g + 1) * 8, :, :].rearrange("p c b i -> p (c b i)"), ALU.mult),
                     reads=[pb, b_in[g]], writes=[b_Z[g]])

            p1 = conv(0, 0, Vr)
            for g in range(16):
                evac1(g, *p1)
                if g + 1 < 16:
                    p1 = conv(0, g + 1, Vr)
                p2 = conv(1, g, Z)
                evac2(g, *p2)
            for b in range(NB):
                for i0 in range(0, NLT, 4):
                    ps, pb = self.bank()
                    for ii in range(4):
                        P.op("pe", mm(ps[:, ii * 128:(ii + 1) * 128], Z[:, :, b, i0 + ii], self.Jb, True, True), reads=b_Z + [self.b_cb], writes=[pb])
                    yt, byt = yTc[(i0 // 4) % 2]
                    P.op("act", act(yt, ps, AF.Copy), reads=[pb], writes=[byt])
                    P.dma("sp", dma(self.YT_d[b, cg, :, i0 * 128:(i0 + 4) * 128], yt), reads=[byt], writes=[self.b_YT])
                    if b == 0 and "yT_hy" in self.dbg:
                        if not hasattr(self, "dbg_hy"):
                            self.dbg_hy = self.dout("dbg_yT_hy", [128, 4, L], BF16)
                        P.dma("sp", dma(self.dbg_hy[:, cg, i0 * 128:(i0 + 4) * 128], yt), reads=[byt])
        P.barrier()
        A.release()

    def route_init(self):
        P, A, NB = self.P, self.A, self.NB
        NTT = NB * NLT
        self.destAll = A.alloc([NTT, 8], I32)
        self.wAll = A.alloc([NTT, 8], F32)
        self.b_dw = Buf("destw")
        self.cnt = A.alloc([NE], F32)
        self.b_cnt = Buf("cnt")
        P.op("pool", lambda e: e.memset(self.cnt, 0.0), writes=[self.b_cnt])
        self.b_rc = Buf("routeconst")
        self.E8All = A.alloc([NTT, 8], F32)
        self.P8All = A.alloc([NTT, 8], F32)
        self.W8raw = A.alloc([NTT, 8], F32)
        self.DestF = A.alloc([NTT, 8], F32)
        self.b_ov = Buf("ovstate")
        self.IDXG = A.alloc([OV], I32)
        self.b_idx = Buf("ovidx")
        self.b_H2 = Buf("H2")
        A.mark()
        self.iota3 = A.alloc([NE], F32)
        self.rbias = A.alloc([NE], F32)
        self.n2g = A.alloc([D], F32)
        P.dma("sp", dma(self.iota3, bcast_rows(self.iota_d.tensor, 0, NE)), writes=[self.b_rc])
        P.dma("sp", dma(self.rbias, bcast_rows(self.rb_d.tensor, 0, NE)), writes=[self.b_rc])
        P.dma("sp", dma(self.n2g, bcast_rows(self.n2g_d.tensor, 0, D)), writes=[self.b_rc])
        self.wr = A.alloc([KD, NE], F32)
        P.dma("sp", dma(self.wr, self.wr_d), writes=[self.b_rc])
        self.wout = A.alloc([KD, D], BF16)
        self.swgu = A.alloc([KD, 512], BF16)
        self.swd = A.alloc([2, D], BF16)
        self.b_wts = Buf("wts")
        for k in range(KD):
            P.dma("pool", dma(self.wout[:, k, :], self.wout_d[:, k, :]), writes=[self.b_wts])
        P.dma("pool", dma(self.swgu[:, :, 0:256], self.swg_d), writes=[self.b_wts])
        P.dma("pool", dma(self.swgu[:, :, 256:512], self.swu_d), writes=[self.b_wts])
        P.dma("pool", dma(self.swd, self.swd_d), writes=[self.b_wts])
        self.b_X1 = Buf("X1")
        self.b_XS = Buf("XS")

        def pre_pool(engine):
            self.bc_reg = engine.to_reg(NE * CAP - 1)
            self.bc_reg2 = engine.to_reg(NE * CAP + OV * 128 - 1)
        P.pre["pool"] = pre_pool

    def outproj_route(self, b):
        P, A, NB = self.P, self.A, self.NB
        A.mark()
        mt = self.MOD_d.tensor
        G1 = A.alloc([D], F32)
        A2 = A.alloc([D], F32)
        B2 = A.alloc([D], F32)
        G2 = A.alloc([D], F32)
        b_m = Buf()
        P.dma("sp", dma(G1, bcast_rows(mt, b * 6 * D + 2 * D, D)), reads=[self.b_MOD], writes=[b_m])
        P.dma("sp", dma(B2, bcast_rows(mt, b * 6 * D + 3 * D, D)), reads=[self.b_MOD], writes=[b_m])
        P.dma("sp", dma(A2, bcast_rows(mt, b * 6 * D + 4 * D, D)), reads=[self.b_MOD], writes=[b_m])
        P.dma("sp", dma(G2, bcast_rows(mt, b * 6 * D + 5 * D, D)), reads=[self.b_MOD], writes=[b_m])
        P.op("dve", stt(A2, A2, 1.0, self.n2g, ALU.add, ALU.mult), reads=[b_m, self.b_rc], writes=[b_m])
        self.junk = (A.alloc([D], F32), Buf())
        self.ssb = [(A.alloc([1], F32), Buf()) for _ in range(2)]
        yT4 = [(A.alloc([KD, 512], BF16), Buf()) for _ in range(2)]
        xts = [(A.alloc([D], F32), Buf()) for _ in range(2)]
        x1s = [(A.alloc([D], F32), Buf()) for _ in range(2)]
        tmp = (A.alloc([D], F32), Buf())
        hfs = [(A.alloc([D], F32), Buf()) for _ in range(2)]
        hbs = [(A.alloc([D], BF16), Buf()) for _ in range(2)]
        h2Tb = [(A.alloc([KD, 128], BF16), Buf()) for _ in range(2)]
        h2Tf = [(A.alloc([KD, 128], F32), Buf()) for _ in range(2)]
        sc = (A.alloc([NE], F32), Buf())
        bia = (A.alloc([NE], F32), Buf())
        msk = (A.alloc([NE], F32), Buf())
        sel = (A.alloc([NE], F32), Buf())
        selb = (A.alloc([NE], BF16), Buf())
        pos = (A.alloc([NE], F32), Buf())
        oh = (A.alloc([8, NE], F32), Buf())
        sm = (A.alloc([160], F32), Buf())
        dsc = [(A.alloc([8], I32), Buf()) for _ in range(2)]
        sg = (A.alloc([256], F32), Buf())
        hmid = (A.alloc([2, 128], BF16), Buf())
        smv = sm[0]
        b_s = sm[1]
        m8g = smv[:, 0:64].rearrange("p (g e) -> p g e", g=8)
        gs_ = smv[:, 64:72]
        m8 = smv[:, 72:80]
        gmask = smv[:, 80:88]
        gm1 = smv[:, 88:96]
        m8b = smv[:, 96:104]
        w8 = smv[:, 104:112]
        e8f = smv[:, 112:120]
        posk = smv[:, 120:128]
        dest = smv[:, 128:136]
        valid = smv[:, 136:144]
        t8 = smv[:, 144:152]
        ws1 = smv[:, 152:153]
        e8 = A.alloc([8], U32)
        for i in range(NLT):
            ti = b * NLT + i
            tt_ = i % 4
            yt, byt = yT4[(i // 4) % 2]
            if tt_ == 0:
                P.dma("sp", dma(yt, self.YT_d[b].rearrange("k p t -> p k t")[:, :, i * 128:(i + 4) * 128]), reads=[self.b_YT], writes=[byt])
            xt, bx = xts[i % 2]
            P.dma("act", dma(xt, self.x_d[b * L + i * 128: b * L + (i + 1) * 128, :]), writes=[bx])
            pss = [self.bank(), self.bank()]
            for n in range(2):
                for k in range(KD):
                    P.op("pe", mm(pss[n][0][:, :], yt[:, k, tt_ * 128:(tt_ + 1) * 128], self.wout[:, k, n * 512:(n + 1) * 512], k == 0, k == KD - 1),
                         reads=[byt, self.b_wts], writes=[pss[n][1]])
            for n in range(2):
                P.op("dve", tt(tmp[0][:, n * 512:(n + 1) * 512], pss[n][0][:, :], G1[:, n * 512:(n + 1) * 512], ALU.mult),
                     reads=[pss[n][1], b_m], writes=[tmp[1]])
            x1, bx1 = x1s[i % 2]
            P.op("pool", tt(x1, tmp[0], xt, ALU.add), reads=[tmp[1], bx], writes=[bx1])
            if b == 0 and "x1" in self.dbg:
                if not hasattr(self, "dbg_x1"):
                    self.dbg_x1 = self.dout("dbg_x1", [L, D])
                P.dma("sp", dma(self.dbg_x1[i * 128:(i + 1) * 128, :], x1), reads=[bx1])
            hf, bhf = hfs[i % 2]
            hb, bhb = hbs[i % 2]
            hT2, bhT2 = h2Tb[i % 2]
            self.norm_mod_sb(i, x1, bx1, A2, b_m, B2, b_m, hT2, bhT2, hf_out=(hf, bhf), hb_out=(hb, bhb))
            if b == 0 and "hx2" in self.dbg:
                if not hasattr(self, "dbg_hx2"):
                    self.dbg_hx2 = self.dout("dbg_hx2", [L, D])
                P.dma("sp", dma(self.dbg_hx2[i * 128:(i + 1) * 128, :], hf), reads=[bhf])
            if self.cut == 1:
                continue
            pf = [self.bank(), self.bank()]
            for k in range(KD):
                P.op("pe", tr(pf[k // 4][0][:, (k % 4) * 128:(k % 4 + 1) * 128], hf[:, k * 128:(k + 1) * 128], self.identf),
                     reads=[bhf, self.b_cst], writes=[pf[k // 4][1]])
            hTf, bhTf = h2Tf[i % 2]
            P.op("act", act(hTf[:, 0:4, :], pf[0][0].rearrange("p (k t) -> p k t", k=4), AF.Copy), reads=[pf[0][1]], writes=[bhTf])
            P.op("dve", cp(hTf[:, 4:8, :], pf[1][0].rearrange("p (k t) -> p k t", k=4)), reads=[pf[1][1]], writes=[bhTf])
            pr, pbr = self.bank()
            for k in range(KD):
                P.op("pe", mm(pr[:, 0:NE], hTf[:, k, :], self.wr[:, k, :], k == 0, k == KD - 1), reads=[bhTf, self.b_rc], writes=[pbr])
            P.op("act", act(sc[0], pr[:, 0:NE], AF.Sigmoid), reads=[pbr], writes=[sc[1]])
            if self.cut == 2:
                continue
            P.op("dve", tt(bia[0], sc[0], self.rbias, ALU.add), reads=[sc[1], self.b_rc], writes=[bia[1]])
            bia3 = bia[0].rearrange("p (g e) -> p g e", g=8)
            for g in range(8):
                P.op("dve", lambda e, g=g: e.max(out=m8g[:, g, :], in_=bia3[:, g, :]), reads=[bia[1]], writes=[b_s])
            P.op("dve", tt(gs_, m8g[:, :, 0], m8g[:, :, 1], ALU.add), reads=[b_s], writes=[b_s])
            P.op("dve", lambda e: e.max(out=m8, in_=gs_), reads=[b_s], writes=[b_s])
            P.op("dve", ts(gmask, gs_, m8[:, 3:4], None, ALU.is_ge), reads=[b_s], writes=[b_s])
            P.op("dve", ts(gm1, gmask, -1.0, None, ALU.add), reads=[b_s], writes=[b_s])
            msk3 = msk[0].rearrange("p (g e) -> p g e", g=8)
            P.op("dve", tt(msk3, bia3, gmask.unsqueeze(2).to_broadcast([128, 8, 32]), ALU.mult), reads=[bia[1], b_s], writes=[msk[1]])
            P.op("dve", tt(msk3, msk3, gm1.unsqueeze(2).to_broadcast([128, 8, 32]), ALU.add), reads=[msk[1], b_s], writes=[msk[1]])
            P.op("dve", lambda e: e.max(out=m8b, in_=msk[0]), reads=[msk[1]], writes=[b_s])
            P.op("dve", ts(sel[0], msk[0], m8b[:, 7:8], None, ALU.is_ge), reads=[msk[1], b_s], writes=[sel[1]])
            P.op("pool", cp(selb[0], sel[0]), reads=[sel[1]], writes=[selb[1]])
            P.op("dve", tt(msk[0], sc[0], sel[0], ALU.mult), reads=[sc[1], sel[1]], writes=[msk[1]])
            P.op("dve", lambda e: e.max(out=w8, in_=msk[0]), reads=[msk[1]], writes=[b_s])
            P.op("dve", lambda e: e.max_index(out=e8, in_max=w8, in_values=msk[0]), reads=[msk[1], b_s], writes=[b_s])
            P.op("dve", cp(e8f, e8), reads=[b_s], writes=[b_s])
            P.op("dve", rsum(ws1, w8), reads=[b_s], writes=[b_s])
            P.op("dve", lambda e: e.reciprocal(out=ws1, in_=ws1), reads=[b_s], writes=[b_s])
            P.op("dve", ts(w8, w8, ws1, RSCALE, ALU.mult, ALU.mult), reads=[b_s], writes=[b_s])
            if self.cut == 3:
                continue
            pp, pbp = self.bank()
            P.op("pe", mm(pp[:, 0:NE], self.ustrict_b, selb[0], True, True), reads=[selb[1], self.b_cb], writes=[pbp])
            pc, pbc = self.bank()
            P.op("pe", mm(pc[:, 0:NE], self.ones_b, selb[0], True, True), reads=[selb[1], self.b_cb], writes=[pbc])
            P.op("dve", tt(pos[0], pp[:, 0:NE], self.cnt, ALU.add), reads=[pbp, self.b_cnt], writes=[pos[1]])
            P.op("dve", tt(self.cnt, self.cnt, pc[:, 0:NE], ALU.add), reads=[pbc, pos[1]], writes=[self.b_cnt])
            P.op("dve", tt(oh[0], self.iota3.unsqueeze(1).to_broadcast([128, 8, NE]), e8f.unsqueeze(2).to_broadcast([128, 8, NE]), ALU.is_equal),
                 reads=[self.b_rc, b_s], writes=[oh[1]])
            P.op("dve", tt(oh[0], oh[0], pos[0].unsqueeze(1).to_broadcast([128, 8, NE]), ALU.mult), reads=[oh[1], pos[1]], writes=[oh[1]])
            P.op("dve", rsum(posk, oh[0]), reads=[oh[1]], writes=[b_s])
            P.op("dve", stt(dest, e8f, float(CAP), posk, ALU.mult, ALU.add), reads=[b_s], writes=[b_s])
            P.op("dve", ts(valid, posk, float(CAP), None, ALU.is_lt), reads=[b_s], writes=[b_s])
            P.op("dve", ts(t8, valid, -1.0e6, 1.0e6, ALU.mult, ALU.add), reads=[b_s], writes=[b_s])
            P.op("dve", tt(t8, t8, dest, ALU.add), reads=[b_s], writes=[b_s])
            if self.cut == 4:
                continue
            di, bdi = dsc[i % 2]
            P.op("dve", cp(di, t8), reads=[b_s], writes=[bdi])
            P.op("dve", tt(self.DestF[:, ti, :], dest, valid, ALU.mult), reads=[b_s], writes=[self.b_ov])
            P.op("dve", tt(self.wAll[:, ti, :], w8, valid, ALU.mult), reads=[b_s], writes=[self.b_dw])
            P.op("dve", cp(self.E8All[:, ti, :], e8f), reads=[b_s], writes=[self.b_ov])
            P.op("dve", cp(self.P8All[:, ti, :], posk), reads=[b_s], writes=[self.b_ov])
            P.op("dve", cp(self.W8raw[:, ti, :], w8), reads=[b_s], writes=[self.b_ov])
            P.dma("act", dma(self.H2_d[ti * 128:(ti + 1) * 128, :], hb), reads=[bhb], writes=[self.b_H2])
            if self.cut == 5:
                continue
            for k in range(8):
                P.dma("pool", lambda e, k=k, di=di, hb=hb: e.indirect_dma_start(
                    out=self.XS_d, out_offset=bass.IndirectOffsetOnAxis(ap=di[:, k:k + 1], axis=0), in_=hb, in_offset=None,
                    bounds_check=self.bc_reg, oob_is_err=False), reads=[bdi, bhb], writes=[self.b_XS])
            if self.cut == 6:
                continue
            ph, pbh = self.bank()
            for fc in range(4):
                for k in range(KD):
                    P.op("pe", mm(ph[:, fc * 128:(fc + 1) * 128], self.swgu[:, k, fc * 128:(fc + 1) * 128], hT2[:, k, :], k == 0, k == KD - 1),
                         reads=[bhT2, self.b_wts], writes=[pbh])
            P.op("act", act(sg[0], ph[:, 0:256], AF.Silu), reads=[pbh], writes=[sg[1]])
            P.op("dve", tt(hmid[0].rearrange("p a b -> p (a b)"), sg[0], ph[:, 256:512], ALU.mult), reads=[sg[1], pbh], writes=[hmid[1]])
            if self.cut == 7:
                continue
            pd = [self.bank(), self.bank()]
            for n in range(2):
                for fc in range(2):
                    P.op("pe", mm(pd[n][0][:, :], hmid[0][:, fc, :], self.swd[:, fc, n * 512:(n + 1) * 512], fc == 0, fc == 1),
                         reads=[hmid[1], self.b_wts], writes=[pd[n][1]])
            if self.cut == 8:
                continue
            for n in range(2):
                P.op("dve", tt(tmp[0][:, n * 512:(n + 1) * 512], pd[n][0][:, :], G2[:, n * 512:(n + 1) * 512], ALU.mult),
                     reads=[pd[n][1], b_m], writes=[tmp[1]])
            if self.cut == 9:
                continue
            P.op("dve", tt(x1, tmp[0], x1, ALU.add), reads=[tmp[1], bx1], writes=[bx1])
            if self.cut == 10:
                continue
            P.dma("sp", dma(self.X1_d[ti * 128:(ti + 1) * 128, :], x1), reads=[bx1], writes=[self.b_X1])
        P.barrier()
        A.release()

    def overflow_route(self):
        P, A, NB = self.P, self.A, self.NB
        NTT = NB * NLT
        A.mark()
        pidx = A.alloc([1], F32)
        b_c = Buf()
        P.dma("sp", dma(pidx, self.pidx_d), writes=[b_c])
        thr = A.alloc([1], F32)
        P.op("dve", ts(thr, pidx, 128.0, None, ALU.mult), reads=[b_c], writes=[b_c])
        ovc = A.alloc([NE], F32)
        b_o = Buf()
        P.op("dve", ts(ovc, self.cnt, -float(CAP), 0.0, ALU.add, ALU.max), reads=[self.b_cnt], writes=[b_o])
        cmpb = A.alloc([NE], BF16)
        P.op("dve", ts(cmpb, ovc, thr, None, ALU.is_gt), reads=[b_o, b_c], writes=[b_o])
        ps, pb = self.bank()
        P.op("pe", mm(ps[:, 0:NE], self.ones_b, cmpb, True, True), reads=[b_o, self.b_cb], writes=[pb])
        ovblk = A.alloc([NE], F32)
        ovend = A.alloc([NE], F32)
        ovs = A.alloc([NE], F32)
        onesr = A.alloc([NE], F32)
        b_e = Buf()
        P.op("act", act(ovblk, ps[:, 0:NE], AF.Copy), reads=[pb], writes=[b_e])
        P.op("pool", lambda e: e.memset(onesr, 1.0), writes=[b_e])
        P.op("dve", lambda e: e.tensor_tensor_scan(out=ovend, data0=onesr, data1=ovblk, initial=0.0, op0=ALU.mult, op1=ALU.add),
             reads=[b_e], writes=[b_e])
        P.op("dve", tt(ovs, ovend, ovblk, ALU.subtract), reads=[b_e], writes=[b_e])
        P.op("dve", ts(ovs, ovs, 128.0, None, ALU.mult), reads=[b_e], writes=[b_e])
        beRow = A.alloc([OV], F32)
        b_b = Buf()
        cmp3 = A.alloc([16, NE], F32)
        for c0 in range(0, OV, 16):
            P.op("dve", tt(cmp3, ovend.unsqueeze(1).to_broadcast([128, 16, NE]),
                           self.iota3[:, c0:c0 + 16].unsqueeze(2).to_broadcast([128, 16, NE]), ALU.is_le), reads=[b_e, self.b_rc], writes=[b_b])
            P.op("dve", rsum(beRow[:, c0:c0 + 16], cmp3), reads=[b_b], writes=[b_b])
        P.op("dve", ts(beRow, beRow, float(NE - 1), None, ALU.min), reads=[b_b], writes=[b_b])
        self.dump("be", beRow, b_b, [128, OV])
        idxf = A.alloc([OV], F32)
        P.op("dve", ts(idxf, beRow, 128.0, pidx, ALU.mult, ALU.add), reads=[b_b, b_c], writes=[b_b])
        P.op("dve", cp(self.IDXG, idxf), reads=[b_b], writes=[self.b_idx])
        oh = A.alloc([8, NE], F32)
        b_oh = Buf()
        sm = A.alloc([64], F32)
        b_s = Buf()
        ob8, ovslot, isov, ok, t8 = (sm[:, 8 * q:8 * q + 8] for q in range(5))
        di_ = [(A.alloc([8], I32), Buf()) for _ in range(2)]
        hbs = [(A.alloc([D], BF16), Buf()) for _ in range(2)]
        for ti in range(NTT):
            hb, bhb = hbs[ti % 2]
            P.dma("sp", dma(hb, self.H2_d[ti * 128:(ti + 1) * 128, :]), reads=[self.b_H2], writes=[bhb])
            P.op("dve", tt(oh, self.iota3.unsqueeze(1).to_broadcast([128, 8, NE]),
                           self.E8All[:, ti, :].unsqueeze(2).to_broadcast([128, 8, NE]), ALU.is_equal), reads=[self.b_rc, self.b_ov], writes=[b_oh])
            P.op("dve", tt(oh, oh, ovs.unsqueeze(1).to_broadcast([128, 8, NE]), ALU.mult), reads=[b_oh, b_e], writes=[b_oh])
            P.op("dve", rsum(ob8, oh), reads=[b_oh], writes=[b_s])
            P.op("dve", stt(ovslot, self.P8All[:, ti, :], -float(CAP), ob8, ALU.add, ALU.add), reads=[b_s, self.b_ov], writes=[b_s])
            P.op("dve", ts(isov, self.P8All[:, ti, :], float(CAP), None, ALU.is_ge), reads=[self.b_ov], writes=[b_s])
            P.op("dve", ts(ok, ovslot, float(OV * 128), None, ALU.is_lt), reads=[b_s], writes=[b_s])
            P.op("dve", tt(ok, ok, isov, ALU.mult), reads=[b_s], writes=[b_s])
            P.op("dve", ts(ovslot, ovslot, float(NE * CAP), None, ALU.add), reads=[b_s], writes=[b_s])
            P.op("dve", ts(t8, ok, -1.0e6, 1.0e6, ALU.mult, ALU.add), reads=[b_s], writes=[b_s])
            P.op("dve", tt(t8, t8, ovslot, ALU.add), reads=[b_s], writes=[b_s])
            di, bdi = di_[ti % 2]
            P.op("dve", cp(di, t8), reads=[b_s], writes=[bdi])
            P.op("dve", tt(ovslot, ovslot, ok, ALU.mult), reads=[b_s], writes=[b_s])
            P.op("dve", tt(t8, self.DestF[:, ti, :], ovslot, ALU.add), reads=[b_s, self.b_ov], writes=[b_s])
            P.op("dve", cp(self.destAll[:, ti, :], t8), reads=[b_s], writes=[self.b_dw])
            P.op("dve", tt(t8, self.W8raw[:, ti, :], ok, ALU.mult), reads=[b_s, self.b_ov], writes=[b_s])
            P.op("dve", tt(self.wAll[:, ti, :], self.wAll[:, ti, :], t8, ALU.add), reads=[b_s, self.b_dw], writes=[self.b_dw])
            for k in range(8):
                P.dma("pool", lambda e, k=k, di=di, hb=hb: e.indirect_dma_start(
                    out=self.XS_d, out_offset=bass.IndirectOffsetOnAxis(ap=di[:, k:k + 1], axis=0), in_=hb, in_offset=None,
                    bounds_check=self.bc_reg2, oob_is_err=False), reads=[bdi, bhb], writes=[self.b_XS])
        P.barrier()
        A.release()

    def experts(self):
        P, A = self.P, self.A
        self.dump("cnt", self.cnt, self.b_cnt, [128, NE])
        A.mark()
        xs4 = [(A.alloc([NBLK, D], BF16), Buf()) for _ in range(3)]
        xT = [(A.alloc([KD, CAP], BF16), Buf()) for _ in range(2)]
        wguf = [(A.alloc([KD, 512], F32), Buf()) for _ in range(3)]
        wdf = [(A.alloc([2, D], F32), Buf()) for _ in range(3)]
        wgub = [(A.alloc([KD, 512], BF16), Buf()) for _ in range(2)]
        wdb = [(A.alloc([2, D], BF16), Buf()) for _ in range(2)]
        sg = [(A.alloc([2, CAP], F32), Buf()) for _ in range(1)]
        hm = [(A.alloc([2, CAP], BF16), Buf()) for _ in range(2)]
        yo = [(A.alloc([D], BF16), Buf()) for _ in range(3)]
        self.b_Y = Buf("Y")

        def loads(e_):
            x4, bx4 = xs4[e_ % 3]
            P.dma("sp", dma(x4, self.XS_d[e_ * CAP:(e_ + 1) * CAP, :].rearrange("(s p) d -> p s d", p=128)), reads=[self.b_XS], writes=[bx4])
            wg, bwg = wguf[e_ % 3]
            wd, bwd = wdf[e_ % 3]
            P.dma("sp", dma(wg[:, :, 0:256], self.ewg_d[e_].rearrange("p (k f) -> p k f", k=KD)), writes=[bwg])
            P.dma("sp", dma(wg[:, :, 256:512], self.ewu_d[e_].rearrange("p (k f) -> p k f", k=KD)), writes=[bwg])
            P.dma("sp", dma(wd, self.ewd_d[e_].rearrange("p (k n) -> p k n", k=2)), writes=[bwd])

        def casts(e_):
            wg, bwg = wguf[e_ % 3]
            wd, bwd = wdf[e_ % 3]
            wgb, bwgb = wgub[e_ % 2]
            wdb_, bwdb = wdb[e_ % 2]
            P.op("dve", cp(wgb[:, 0:3, :], wg[:, 0:3, :]), reads=[bwg], writes=[bwgb])
            P.op("act", act(wgb[:, 3:6, :], wg[:, 3:6, :], AF.Copy), reads=[bwg], writes=[bwgb])
            P.op("pool", cp(wgb[:, 6:8, :], wg[:, 6:8, :]), reads=[bwg], writes=[bwgb])
            P.op("act", act(wdb_[:, 0, :], wd[:, 0, :], AF.Copy), reads=[bwd], writes=[bwdb])
            P.op("dve", cp(wdb_[:, 1, :], wd[:, 1, :]), reads=[bwd], writes=[bwdb])

        def transposes(e_):
            x4, bx4 = xs4[e_ % 3]
            xt_, bxt = xT[e_ % 2]
            for sb in range(NBLK):
                ps, pb = self.bank()
                psb = ps.bitcast(BF16)
                for k in range(KD):
                    P.op("pe", tr(psb[:, k * 128:(k + 1) * 128], x4[:, sb, k * 128:(k + 1) * 128], self.identb), reads=[bx4, self.b_cb], writes=[pb])
                src = psb.rearrange("p (k t) -> p k t", k=KD)
                P.op("dve", cp(xt_[:, :, sb * 128:(sb + 1) * 128], src), reads=[pb], writes=[bxt])

        loads(0)
        loads(1)
        casts(0)
        transposes(0)
        yc = 0
        for e_ in range(NE):
            if e_ + 2 < NE:
                loads(e_ + 2)
            if e_ + 1 < NE:
                casts(e_ + 1)
            wgb, bwgb = wgub[e_ % 2]
            wdb_, bwdb = wdb[e_ % 2]
            xt_, bxt = xT[e_ % 2]
            pg = [self.bank() for _ in range(4)]
            for fc in range(4):
                for k in range(KD):
                    P.op("pe", mm(pg[fc][0][:, 0:CAP], wgb[:, k, fc * 128:(fc + 1) * 128], xt_[:, k, :], k == 0, k == KD - 1),
                         reads=[bwgb, bxt], writes=[pg[fc][1]])
            sg_, bsg = sg[0]
            hm_, bhm = hm[e_ % 2]
            for fc in range(2):
                P.op("act", act(sg_[:, fc, :], pg[fc][0][:, 0:CAP], AF.Silu), reads=[pg[fc][1]], writes=[bsg])
                P.op("dve", tt(hm_[:, fc, :], sg_[:, fc, :], pg[2 + fc][0][:, 0:CAP], ALU.mult), reads=[bsg, pg[2 + fc][1]], writes=[bhm])
            if e_ + 1 < NE:
                transposes(e_ + 1)
            for sb in range(NBLK):
                pd = [self.bank(), self.bank()]
                for n in range(2):
                    for fc in range(2):
                        P.op("pe", mm(pd[n][0][:, :], hm_[:, fc, sb * 128:(sb + 1) * 128], wdb_[:, fc, n * 512:(n + 1) * 512], fc == 0, fc == 1),
                             reads=[bhm, bwdb], writes=[pd[n][1]])
                y_, by = yo[yc % 3]
                yc += 1
                P.op("act", act(y_[:, 0:512], pd[0][0][:, :], AF.Copy), reads=[pd[0][1]], writes=[by])
                P.op("act", act(y_[:, 512:1024], pd[1][0][:, :], AF.Copy), reads=[pd[1][1]], writes=[by])
                r0 = e_ * CAP + sb * 128
                P.dma("act", dma(self.Y_d[r0:r0 + 128, :], y_), reads=[by], writes=[self.b_Y])
        if OV > 0:
            ewg_rows = self.ewg_d.rearrange("e p n -> (e p) n")
            ewu_rows = self.ewu_d.rearrange("e p n -> (e p) n")
            ewd_rows = self.ewd_d.rearrange("e p n -> (e p) n")
            ovw = [(A.alloc([KD * 256], BF16), A.alloc([KD * 256], BF16), A.alloc([2 * D], BF16), Buf()) for _ in range(2)]

            def ovloads(j):
                x4, bx4 = xs4[j % 3]
                r0 = NE * CAP + j * 128
                P.dma("sp", dma(x4[:, 0, :], self.XS_d[r0:r0 + 128, :]), reads=[self.b_XS], writes=[bx4])
                og, ou, od, bw = ovw[j % 2]
                off = lambda j=j: bass.IndirectOffsetOnAxis(ap=self.IDXG[:, j:j + 1], axis=0)
                P.dma("pool", lambda e, og=og, off=off: e.indirect_dma_start(out=og, out_offset=None, in_=ewg_rows, in_offset=off()),
                      reads=[self.b_idx], writes=[bw])
                P.dma("pool", lambda e, ou=ou, off=off: e.indirect_dma_start(out=ou, out_offset=None, in_=ewu_rows, in_offset=off()),
                      reads=[self.b_idx], writes=[bw])
                P.dma("pool", lambda e, od=od, off=off: e.indirect_dma_start(out=od, out_offset=None, in_=ewd_rows, in_offset=off()),
                      reads=[self.b_idx], writes=[bw])

            ovloads(0)
            for j in range(OV):
                if j + 1 < OV:
                    ovloads(j + 1)
                x4, bx4 = xs4[j % 3]
                og, ou, od, bw = ovw[j % 2]
                og3 = og.rearrange("p (k f) -> p k f", k=KD)
                ou3 = ou.rearrange("p (k f) -> p k f", k=KD)
                od3 = od.rearrange("p (k n) -> p k n", k=2)
                xt_, bxt = xT[j % 2]
                ps, pb = self.bank()
                psb = ps.bitcast(BF16)
                for k in range(KD):
                    P.op("pe", tr(psb[:, k * 128:(k + 1) * 128], x4[:, 0, k * 128:(k + 1) * 128], self.identb), reads=[bx4, self.b_cb], writes=[pb])
                P.op("dve", cp(xt_[:, :, 0:128], psb.rearrange("p (k t) -> p k t", k=KD)), reads=[pb], writes=[bxt])
                pg, pbg = self.bank()
                for fc in range(4):
                    wsrc = og3 if fc < 2 else ou3
                    f0 = (fc % 2) * 128
                    for k in range(KD):
                        P.op("pe", mm(pg[:, fc * 128:(fc + 1) * 128], wsrc[:, k, f0:f0 + 128], xt_[:, k, 0:128], k == 0, k == KD - 1),
                             reads=[bw, bxt], writes=[pbg])
                sg_, bsg = sg[0]
                hm_, bhm = hm[j % 2]
                for fc in range(2):
                    P.op("act", act(sg_[:, fc, 0:128], pg[:, fc * 128:(fc + 1) * 128], AF.Silu), reads=[pbg], writes=[bsg])
                    P.op("dve", tt(hm_[:, fc, 0:128], sg_[:, fc, 0:128], pg[:, (2 + fc) * 128:(3 + fc) * 128], ALU.mult), reads=[bsg, pbg], writes=[bhm])
                pd = [self.bank(), self.bank()]
                for n in range(2):
                    for fc in range(2):
                        P.op("pe", mm(pd[n][0][:, :], hm_[:, fc, 0:128], od3[:, fc, n * 512:(n + 1) * 512], fc == 0, fc == 1),
                             reads=[bhm, bw], writes=[pd[n][1]])
                y_, by = yo[yc % 3]
                yc += 1
                P.op("act", act(y_[:, 0:512], pd[0][0][:, :], AF.Copy), reads=[pd[0][1]], writes=[by])
                P.op("act", act(y_[:, 512:1024], pd[1][0][:, :], AF.Copy), reads=[pd[1][1]], writes=[by])
                r0 = NE * CAP + j * 128
                P.dma("act", dma(self.Y_d[r0:r0 + 128, :], y_), reads=[by], writes=[self.b_Y])
        P.barrier()
        A.release()

    def combine(self):
        P, A, NB = self.P, self.A, self.NB
        A.mark()
        mt = self.MOD_d.tensor
        fing = A.alloc([D], F32)
        b_fg = Buf()
        P.dma("sp", dma(fing, bcast_rows(self.fing_d.tensor, 0, D)), writes=[b_fg])
        G2s = [(A.alloc([D], F32), Buf()) for _ in range(2)]
        base = [(A.alloc([D], F32), Buf()) for _ in range(2)]
        yk = [(A.alloc([D], BF16), Buf()) for _ in range(8)]
        acc = [(A.alloc([D], F32), Buf()) for _ in range(2)]
        junk = (A.alloc([D], F32), Buf())
        pre = [(A.alloc([D], F32), Buf()) for _ in range(2)]
        ssb = [(A.alloc([1], F32), Buf()) for _ in range(2)]
        ot = [(A.alloc([D], F32), Buf()) for _ in range(2)]
        for b in range(NB):
            G2, bG2 = G2s[b % 2]
            P.dma("sp", dma(G2, bcast_rows(mt, b * 6 * D + 5 * D, D)), reads=[self.b_MOD], writes=[bG2])
            for i in range(NLT):
                ti = b * NLT + i
                bs, bbs = base[i % 2]
                P.dma("sp", dma(bs, self.X1_d[ti * 128:(ti + 1) * 128, :]), reads=[self.b_X1], writes=[bbs])
                ac, bac = acc[i % 2]
                for k in range(8):
                    y_, by = yk[k]
                    P.dma("pool", lambda e, k=k, y_=y_, ti=ti: e.indirect_dma_start(
                        out=y_, out_offset=None, in_=self.Y_d, in_offset=bass.IndirectOffsetOnAxis(ap=self.destAll[:, ti, k:k + 1], axis=0)),
                        reads=[self.b_dw, self.b_Y], writes=[by])
                    if k == 0:
                        P.op("dve", ts(ac, y_, self.wAll[:, ti, 0:1], None, ALU.mult), reads=[by, self.b_dw], writes=[bac])
                    else:
                        P.op("dve", stt(ac, y_, self.wAll[:, ti, k:k + 1], ac, ALU.mult, ALU.add), reads=[by, self.b_dw, bac], writes=[bac])
                pa_, bpa = pre[0]
                pb_, bpb = pre[1]
                P.op("pool", tt(pa_, ac, G2, ALU.mult), reads=[bac, bG2], writes=[bpa])
                P.op("pool", tt(pb_, pa_, bs, ALU.add), reads=[bpa, bbs], writes=[bpb])
                ac, bac = pb_, bpb
                ss, bss = ssb[i % 2]
                P.op("act", act(junk[0], ac, AF.Square), reads=[bac], writes=[junk[1]])
                P.op("dve", rsum(ss, junk[0]), reads=[junk[1]], writes=[bss])
                P.op("act", act(ss, ss, AF.Sqrt, scale=1.0 / D, bias=self.eps_ap), reads=[bss, self.b_eps], writes=[bss])
                P.op("dve", lambda e, ss=ss: e.reciprocal(out=ss, in_=ss), reads=[bss], writes=[bss])
                o_, bo = ot[i % 2]
                P.op("dve", stt(o_, ac, ss, fing, ALU.mult, ALU.mult), reads=[bac, bss, b_fg], writes=[bo])
                P.dma("sp", dma(self.out_d[ti * 128:(ti + 1) * 128, :], o_), reads=[bo])
        A.release()


def const_tables():
    import math
    ident = np.eye(128, dtype=np.float32)
    J = ident[::-1].copy()
    s = np.arange(128)[:, None]
    c = np.arange(128)[None, :]
    same = (s // 64) == (c // 64)
    maskF = (same & (s <= c)).astype(np.float32)
    maskB = (same & (s >= c)).astype(np.float32)
    Sm = ((c == s + 1) & ((c % 64) != 0)).astype(np.float32)
    Sp = ((c == s - 1) & ((c % 64) != 63)).astype(np.float32)
    ustrict = (s < c).astype(np.float32)
    ones = np.ones((128, 128), np.float32)
    cst = np.stack([ident, J, maskF, maskB, Sm, Sp, ustrict, ones, Sm[:, ::-1], Sp[:, ::-1]], axis=1).astype(np.float32)
    f32 = np.float32
    pos = np.arange(L, dtype=f32)[:, None]
    t = pos / f32(L - 1)
    w = f32(2.0 * math.pi / L) * pos
    bands = np.linspace(1e-4, 15, 16, dtype=f32)[None, :]
    feats = np.concatenate([t, np.cos(bands * w), -np.sin(bands * w)], axis=-1).astype(f32)
    max_decay = math.log(1e-2) / 0.3
    min_decay = math.log(1e-2) / 1.5
    deltas = np.abs(np.linspace(min_decay, max_decay, 512, dtype=f32))[None, :].astype(f32)
    tfrac = (-(np.arange(L, dtype=f32) / f32(L - 1))).reshape(16, 128).T.copy()
    iota = np.arange(NE, dtype=f32)[None, :]
    pidx = np.arange(128, dtype=f32)[:, None].copy()
    return dict(cst=cst, featsT=np.ascontiguousarray(feats.T), deltas=deltas, tfrac=tfrac.astype(f32), iota=iota, pidx=pidx)


def prep_core(inp, core, NB, tables):
    f = lambda a: np.ascontiguousarray(a, dtype=np.float32)
    b0 = core * NB
    m = dict(tables)
    m["x"] = f(inp["x"][b0:b0 + NB].reshape(NB * L, D))
    m["ctx"] = f(inp["ctx"][b0:b0 + NB].reshape(NB * CTX, D))
    cc = np.concatenate([inp["c"][b0:b0 + NB], inp["c_ctx"][None, :]], axis=0)
    m["cT"] = f(cc.T.reshape(KD, 128, NB + 1).transpose(1, 0, 2))
    return m


def shared_inputs(inp):
    f = lambda a: np.ascontiguousarray(a, dtype=np.float32)
    m = {}
    m["w_mod"] = f(inp["w_mod"][0])
    m["b_mod"] = f(inp["b_mod"][0][None, :])
    m["norm1_g"] = f(inp["norm1_g"][0][None, :])
    m["norm2_g"] = f(inp["norm2_g"][0][None, :])
    m["final_g"] = f(inp["final_g"][None, :])
    m["w_in"] = f(inp["w_in"][0])
    m["w_out"] = f(inp["w_out"][0])
    m["hy_conv_w"] = f(inp["hy_conv_w"][0])
    m["hy_conv_b"] = f(inp["hy_conv_b"][0][None, :])
    m["hy_fw1"] = f(inp["hy_fw1"][0])
    m["hy_fb1"] = f(inp["hy_fb1"][0][:, None])
    m["hy_fw2"] = f(inp["hy_fw2"][0])
    m["hy_fb2"] = f(inp["hy_fb2"][0][:, None])
    m["hy_fw3"] = f(inp["hy_fw3"][0])
    m["hy_freq"] = f(inp["hy_freq"][0][:, None])
    m["hy_d"] = f(inp["hy_d"][0].reshape(1, 1024))
    lg = inp["hg_lb_logits"].reshape(2, 2, 4, 128)
    m["lbT"] = f(lg.transpose(3, 0, 1, 2).reshape(128, 16))
    m["hg_norm_g"] = f(inp["hg_norm_g"][0][None, :])
    m["w_router"] = f(inp["w_router"][0])
    m["router_bias"] = f(inp["router_bias"][0][None, :])
    m["ew_gate"] = f(np.asarray(inp["ew_gate"][0]).reshape(NE, KD, 128, 256).transpose(0, 2, 1, 3).reshape(NE, 128, KD * 256))
    m["ew_up"] = f(np.asarray(inp["ew_up"][0]).reshape(NE, KD, 128, 256).transpose(0, 2, 1, 3).reshape(NE, 128, KD * 256))
    m["ew_down"] = f(np.asarray(inp["ew_down"][0]).reshape(NE, 2, 128, D).transpose(0, 2, 1, 3).reshape(NE, 128, 2 * D))
    m["sw_gate"] = f(inp["sw_gate"][0])
    m["sw_up"] = f(inp["sw_up"][0])
    m["sw_down"] = f(inp["sw_down"][0])
    return m


def kernel(**inputs):
    NB = 4
    ncores = 8
    k = K(NB)
    nc = k.build()
    tables = const_tables()
    sh = shared_inputs(inputs)
    in_maps = []
    for c in range(ncores):
        m = prep_core(inputs, c, NB, tables)
        m.update(sh)
        in_maps.append(m)
    res = run_bass_kernel_spmd(nc, in_maps, core_ids=list(range(ncores)))
    outs = [np.asarray(r["out"], dtype=np.float32).reshape(NB, L, D) for r in res.results]
    return np.concatenate(outs, axis=0)
```

```python
import os
from contextlib import ExitStack
from concourse.bass_utils import run_bass_kernel_spmd
import numpy as np
import concourse.bass as bass
import concourse.mybir as mybir

F32 = mybir.dt.float32
BF16 = mybir.dt.bfloat16
I32 = mybir.dt.int32
U32 = mybir.dt.uint32
AF = mybir.ActivationFunctionType
ALU = mybir.AluOpType
AX = mybir.AxisListType


class Buf:
    __slots__ = ("name", "w", "r")

    def __init__(self, name=""):
        self.name = name
        self.w = None
        self.r = []


class Prog:
    COMPUTE = ("pe", "act", "dve", "pool")
    NDMA = {"sp": 10, "act": 4, "pool": 8}

    def __init__(self, nc, stack, same_engine_sync=True):
        self.nc = nc
        self.same = same_engine_sync
        self.ops = {e: [] for e in ("pe", "act", "dve", "pool", "sp")}
        self.sem = {}
        self.cnt = {}
        for e in self.COMPUTE:
            self.sem[e] = stack.enter_context(nc.semaphore("s_" + e))
            self.cnt[e] = 0
        self.dsem = {}
        self.dcnt = {}
        self.drr = {}
        for q, n in self.NDMA.items():
            for i in range(n):
                k = "d_%s_%d" % (q, i)
                self.sem[k] = stack.enter_context(nc.semaphore(k))
                self.cnt[k] = 0
            self.drr[q] = 0
        self.known = {e: {} for e in self.ops}
        self.nwaits = 0
        self.pre = {}

    def _deps(self, eng, reads, writes, is_dma=False):
        need = {}

        def add(tok):
            if tok is None:
                return
            k, v = tok
            if need.get(k, 0) < v:
                need[k] = v
        for b in reads:
            add(b.w)
        for b in writes:
            add(b.w)
            for t in b.r:
                add(t)
        waits = []
        kn = self.known[eng]
        for k, v in need.items():
            if k == eng and not self.same and not is_dma:
                continue
            if k == "pe" and eng == "pe":
                continue
            if kn.get(k, 0) >= v:
                continue
            kn[k] = v
            waits.append((k, v))
        self.nwaits += len(waits)
        return waits

    def _commit(self, tok, reads, writes):
        for b in reads:
            b.r.append(tok)
            if len(b.r) > 64:
                m = {}
                for k, v in b.r:
                    if m.get(k, 0) < v:
                        m[k] = v
                b.r = list(m.items())
        for b in writes:
            b.w = tok
            b.r = []

    def op(self, eng, fn, reads=(), writes=()):
        waits = self._deps(eng, reads, writes)
        self.cnt[eng] += 1
        tok = (eng, self.cnt[eng])
        self.ops[eng].append((waits, fn, (eng, 1)))
        self._commit(tok, reads, writes)
        return tok

    def dma(self, q, fn, reads=(), writes=()):
        n = self.NDMA[q]
        i = self.drr[q]
        self.drr[q] = (i + 1) % n
        k = "d_%s_%d" % (q, i)
        waits = self._deps(q, reads, writes, is_dma=True)
        prev = self.cnt[k]
        kn = self.known[q]
        if prev > 0 and kn.get(k, 0) < prev:
            kn[k] = prev
            waits.append((k, prev))
        self.cnt[k] += 16
        tok = (k, self.cnt[k])
        self.ops[q].append((waits, fn, (k, 16)))
        self._commit(tok, reads, writes)
        return tok

    def barrier_tokens(self):
        toks = []
        for k, v in self.cnt.items():
            if v > 0:
                toks.append((k, v))
        return toks

    def barrier(self):
        toks = self.barrier_tokens()
        for e in self.ops:
            kn = self.known[e]
            waits = []
            for k, v in toks:
                if k == e and e == "pe":
                    continue
                if kn.get(k, 0) < v:
                    kn[k] = v
                    waits.append((k, v))
            if waits:
                self.ops[e].append((waits, None, None))

    def final_wait(self, eng="sp"):
        toks = self.barrier_tokens()
        self.ops[eng].append(([(k, v) for k, v in toks], None, None))

    def emit(self):
        nc = self.nc
        engmap = {"pe": "tensor", "act": "scalar", "dve": "vector", "pool": "gpsimd", "sp": "sync"}
        with nc.Block() as block:
            for e, attr in engmap.items():
                lst = self.ops[e]

                def body(engine, lst=lst, e=e):
                    if e in self.pre:
                        self.pre[e](engine)
                    for waits, fn, inc in lst:
                        for k, v in waits:
                            engine.wait_ge(self.sem[k], v)
                        if fn is not None:
                            ins = fn(engine)
                            ins.then_inc(self.sem[inc[0]], inc[1])
                getattr(block, attr)(body)


class Arena:
    def __init__(self, nc, stack, nwords, name="arena"):
        self.t = stack.enter_context(nc.sbuf_tensor(name, [128, nwords], F32))
        self.n = nwords
        self.off = 0
        self.marks = []

    def alloc(self, shape, dtype, parts=128):
        n = int(np.prod(shape))
        if dtype == BF16:
            words = (n + 1) // 2
        else:
            words = n
        assert self.off + words <= self.n, "arena overflow %d + %d > %d" % (self.off, words, self.n)
        a = self.t[0:parts, self.off:self.off + words]
        self.off += words
        if dtype != F32:
            a = a.bitcast(dtype)
        if dtype == BF16 and n % 2 == 1:
            a = a[:, 0:n]
        if len(shape) > 1:
            names = " ".join("d%d" % i for i in range(len(shape)))
            kw = {"d%d" % i: int(s) for i, s in enumerate(shape)}
            a = a.rearrange("p (%s) -> p %s" % (names, names), **kw)
        return a

    def mark(self):
        self.marks.append(self.off)

    def release(self):
        self.off = self.marks.pop()

D = 1024
KD = 8
L = 2048
CTX = 256
T = L + CTX
NT = T // 128
NLT = L // 128
NCH = T // 64
HGS = 128.0 ** -0.5
EPS = 1e-6
NE = 256
CAP = int(os.environ.get('KCAP', '384'))
OV = 128
NBLK = CAP // 128
RSCALE = 2.5


def mm(out, lhsT, rhs, start, stop):
    return lambda e: e.matmul(out, lhsT, rhs, start=start, stop=stop)


def tr(out, in_, ident):
    return lambda e: e.transpose(out=out, in_=in_, identity=ident)


def act(out, in_, func, **kw):
    return lambda e: e.activation(out=out, in_=in_, func=func, **kw)


def tt(out, a, b, op):
    return lambda e: e.tensor_tensor(out=out, in0=a, in1=b, op=op)


def ts(out, a, s1, s2, op0, op1=None):
    if op1 is None:
        return lambda e: e.tensor_scalar(out=out, in0=a, scalar1=s1, scalar2=None, op0=op0)
    return lambda e: e.tensor_scalar(out=out, in0=a, scalar1=s1, scalar2=s2, op0=op0, op1=op1)


def stt(out, a, s, b, op0, op1):
    return lambda e: e.scalar_tensor_tensor(out=out, in0=a, scalar=s, in1=b, op0=op0, op1=op1)


def cp(out, in_):
    return lambda e: e.tensor_copy(out=out, in_=in_)


def rsum(out, in_):
    return lambda e: e.reduce_sum(out=out, in_=in_, axis=AX.X)


def dma(out, in_):
    return lambda e: e.dma_start(out=out, in_=in_)


def bcast_rows(dram_ap_tensor, offset, n, parts=128):
    return bass.AP(tensor=dram_ap_tensor, offset=offset, ap=[[0, parts], [1, n]])


class K:
    def __init__(self, NB, dbg=(), upto=4):
        self.NB = NB
        self.upto = upto
        self.cut = int(os.environ.get('KCUT', '0'))
        self.dbg = set(dbg)
        self.nc = bass.Bass("TRN2", target_bir_lowering=False)
        self.outs = []

    def din(self, name, shape, dtype=F32):
        return self.nc.dram_tensor(name, list(shape), dtype, kind="ExternalInput").ap()

    def dscr(self, name, shape, dtype):
        return self.nc.dram_tensor(name, list(shape), dtype, kind="Internal").ap()

    def dout(self, name, shape, dtype=F32):
        self.outs.append(name)
        return self.nc.dram_tensor(name, list(shape), dtype, kind="ExternalOutput").ap()

    def bank(self):
        self.bi = (self.bi + 1) % len(self.rot)
        return self.rot[self.bi]

    def reserve(self, n):
        got = [self.rot.pop() for _ in range(n)]
        self.bi = 0
        return got

    def unreserve(self, got):
        self.rot.extend(got)

    def dump(self, name, ap, buf, shape, dtype=F32):
        if name not in self.dbg:
            return
        o = self.dout("dbg_" + name, shape, dtype)
        self.P.dma("sp", dma(o, ap), reads=[buf])

    def build(self):
        nc = self.nc
        NB = self.NB
        with ExitStack() as st:
            self.st = st
            P = self.P = Prog(nc, st, same_engine_sync=(os.environ.get('KSAME', '1') == '1'))
            A = self.A = Arena(nc, st, 51500)
            self.banks = [(st.enter_context(nc.psum_tensor("pb%d" % i, [128, 512], F32))[:, :], Buf("pb%d" % i)) for i in range(8)]
            self.bi = 0
            self.rot = list(self.banks)
            self.declare_io()
            self.consts()
            self.modulation()
            if self.upto >= 2:
                self.filters()
            if self.upto >= 0.5:
                for b in range(NB):
                    self.mixer_batch(b)
            if self.upto >= 2:
                self.hyena_all()
            if self.upto >= 3:
                self.route_init()
                for b in range(NB):
                    self.outproj_route(b)
                self.overflow_route()
                P.barrier()
                self.A.release()
            if self.upto >= 4:
                self.experts()
                self.combine()
            P.final_wait("sp")
            P.emit()
        return nc

    def declare_io(self):
        NB = self.NB
        d = self.din
        self.x_d = d("x", [NB * L, D])
        self.ctx_d = d("ctx", [NB * CTX, D])
        self.cT_d = d("cT", [128, KD, NB + 1])
        self.wmod_d = d("w_mod", [D, 6 * D]).rearrange("(k p) n -> p k n", p=128)
        self.bmod_d = d("b_mod", [1, 6 * D])
        self.n1g_d = d("norm1_g", [1, D])
        self.n2g_d = d("norm2_g", [1, D])
        self.fing_d = d("final_g", [1, D])
        self.win_d = d("w_in", [D, 4096]).rearrange("(k p) n -> p k n", p=128)
        self.wout_d = d("w_out", [D, D]).rearrange("(k p) n -> p k n", p=128)
        self.hcw_d = d("hy_conv_w", [3, 1536])
        self.hcb_d = d("hy_conv_b", [1, 1536])
        self.fw1_d = d("hy_fw1", [33, 64])
        self.fb1_d = d("hy_fb1", [64, 1])
        self.fw2_d = d("hy_fw2", [64, 64])
        self.fb2_d = d("hy_fb2", [64, 1])
        self.fw3_d = d("hy_fw3", [64, 2048])
        self.freq_d = d("hy_freq", [64, 1])
        self.hyd_d = d("hy_d", [1, 1024])
        self.lbT_d = d("lbT", [128, 16])
        self.hgng_d = d("hg_norm_g", [1, 128])
        self.wr_d = d("w_router", [D, NE]).rearrange("(k p) n -> p k n", p=128)
        self.rb_d = d("router_bias", [1, NE])
        if self.upto >= 4:
            self.ewg_d = d("ew_gate", [NE, 128, KD * 256])
            self.ewu_d = d("ew_up", [NE, 128, KD * 256])
            self.ewd_d = d("ew_down", [NE, 128, 2 * D])
        self.swg_d = d("sw_gate", [D, 256]).rearrange("(k p) n -> p k n", p=128)
        self.swu_d = d("sw_up", [D, 256]).rearrange("(k p) n -> p k n", p=128)
        self.swd_d = d("sw_down", [256, D]).rearrange("(k p) n -> p k n", p=128)
        self.featsT_d = d("featsT", [33, L])
        self.cst_d = d("cst", [128, 10, 128])
        self.deltas_d = d("deltas", [1, 512])
        self.tfrac_d = d("tfrac", [128, 16])
        self.iota_d = d("iota", [1, NE])
        self.pidx_d = d("pidx", [128, 1])
        self.out_d = self.dout("out", [NB * L, D])
        self.MOD_d = self.dscr("MODs", [NB + 1, 6 * D], F32)
        self.HT_d = self.dscr("HTs", [NB, 128, KD * L], BF16)
        self.YT_d = self.dscr("YTs", [NB, 8, 128, L], BF16)
        self.G_d = self.dscr("Gs", [2, 512, 4096], BF16)
        self.X1_d = self.dscr("X1s", [NB * L, D], F32)
        self.XS_d = self.dscr("XSs", [NE * CAP + OV * 128, D], BF16)
        self.Y_d = self.dscr("Ys", [NE * CAP + OV * 128, D], BF16)
        self.H2_d = self.dscr("H2s", [NB * L, D], BF16)

    def consts(self):
        P, A = self.P, self.A
        cst = A.alloc([10, 128], F32)
        self.b_cst = Buf("cst")
        P.dma("sp", dma(cst, self.cst_d), writes=[self.b_cst])
        self.identf = cst[:, 0, :]
        self.Jf = cst[:, 1, :]
        self.maskF = cst[:, 2, :]
        self.maskB = cst[:, 3, :]
        self.ustrict_f = cst[:, 6, :]
        self.ones_f = cst[:, 7, :]
        cb = A.alloc([10, 128], BF16)
        self.b_cb = Buf("cb")
        P.op("dve", cp(cb, cst), reads=[self.b_cst], writes=[self.b_cb])
        self.identb = cb[:, 0, :]
        self.Jb = cb[:, 1, :]
        self.Smb = cb[:, 4, :]
        self.Spb = cb[:, 5, :]
        self.ustrict_b = cb[:, 6, :]
        self.ones_b = cb[:, 7, :]
        self.SmRb = cb[:, 8, :]
        self.SpRb = cb[:, 9, :]
        ce = A.alloc([2], F32)
        self.b_eps = Buf("eps")
        P.op("pool", lambda e: e.memset(ce[:, 0:1], EPS), writes=[self.b_eps])
        P.op("pool", lambda e: e.memset(ce[:, 1:2], 1.0), writes=[self.b_eps])
        self.eps_ap = ce[:, 0:1]
        self.one_ap = ce[:, 1:2]
        self.n1g = A.alloc([D], F32)
        self.b_n1g = Buf()
        P.dma("sp", dma(self.n1g, bcast_rows(self.n1g_d.tensor, 0, D)), writes=[self.b_n1g])
        self.hgng = A.alloc([128], F32)
        self.b_hgng = Buf()
        P.dma("sp", dma(self.hgng, bcast_rows(self.hgng_d.tensor, 0, 128)), writes=[self.b_hgng])
        lbt = A.alloc([2, 2, 4], F32)
        b_lbt = Buf()
        P.dma("sp", dma(lbt, self.lbT_d.rearrange("p (a b c) -> p a b c", a=2, b=2)), writes=[b_lbt])
        self.lb = A.alloc([2, 4], F32)
        self.oml = A.alloc([2, 4], F32)
        self.noml = A.alloc([2, 4], F32)
        self.b_lb = Buf()
        P.op("dve", tt(self.lb, lbt[:, :, 0, :], lbt[:, :, 1, :], ALU.subtract), reads=[b_lbt], writes=[self.b_lb])
        P.op("act", act(self.lb, self.lb, AF.Sigmoid), reads=[self.b_lb], writes=[self.b_lb])
        P.op("dve", ts(self.oml, self.lb, -1.0, 1.0, ALU.mult, ALU.add), reads=[self.b_lb], writes=[self.b_lb])
        P.op("dve", ts(self.noml, self.lb, -1.0, None, ALU.add), reads=[self.b_lb], writes=[self.b_lb])

    def modulation(self):
        P, A, NB = self.P, self.A, self.NB
        A.mark()
        cT = A.alloc([KD, NB + 1], F32)
        b_cT = Buf()
        P.dma("sp", dma(cT, self.cT_d), writes=[b_cT])
        P.op("act", act(cT, cT, AF.Silu), reads=[b_cT], writes=[b_cT])
        bm = A.alloc([6 * D], F32, parts=1)
        b_bm = Buf()
        P.dma("sp", dma(bm, self.bmod_d), writes=[b_bm])
        modsb = A.alloc([6 * D], F32)
        b_mod = Buf()
        wms = [(A.alloc([KD, 512], F32), Buf()) for _ in range(2)]
        for ci in range(12):
            wm, bw = wms[ci % 2]
            P.dma("sp" if ci % 2 == 0 else "act", dma(wm, self.wmod_d[:, :, ci * 512:(ci + 1) * 512]), writes=[bw])
            ps, pb = self.bank()
            for k in range(KD):
                P.op("pe", mm(ps[0:NB + 1, :], cT[:, k, :], wm[:, k, :], k == 0, False), reads=[b_cT, bw], writes=[pb])
            P.op("pe", mm(ps[0:NB + 1, :], self.ones_f[0:1, 0:NB + 1], bm[0:1, ci * 512:(ci + 1) * 512], False, True),
                 reads=[self.b_cst, b_bm], writes=[pb])
            P.op("act", act(modsb[0:NB + 1, ci * 512:(ci + 1) * 512], ps[0:NB + 1, :], AF.Copy), reads=[pb], writes=[b_mod])
        self.b_MOD = Buf("MOD")
        P.dma("sp", dma(self.MOD_d, modsb[0:NB + 1, :]), reads=[b_mod], writes=[self.b_MOD])
        self.dump("mod", modsb[0:NB + 1, :], b_mod, [NB + 1, 6 * D])
        P.barrier()
        A.release()
        self.CA = A.alloc([D], F32)
        self.CB = A.alloc([D], F32)
        self.b_CA = Buf()
        self.b_CB = Buf()
        mt = self.MOD_d.tensor
        P.dma("sp", dma(self.CB, bcast_rows(mt, NB * 6 * D + 0 * D, D)), reads=[self.b_MOD], writes=[self.b_CB])
        P.dma("sp", dma(self.CA, bcast_rows(mt, NB * 6 * D + 1 * D, D)), reads=[self.b_MOD], writes=[self.b_CA])
        P.op("dve", stt(self.CA, self.CA, 1.0, self.n1g, ALU.add, ALU.mult), reads=[self.b_CA, self.b_n1g], writes=[self.b_CA])

    def norm_mod_tile(self, i, src, Abc, bA, Bbc, bB, dst, b_dst, hb_out=None):
        P = self.P
        xt, bx = self.xt[i % 2]
        P.dma("sp", dma(xt, src), writes=[bx])
        self.norm_mod_sb(i, xt, bx, Abc, bA, Bbc, bB, dst, b_dst)

    def norm_mod_sb(self, i, xt, bx, Abc, bA, Bbc, bB, dst, b_dst, hf_out=None, hb_out=None):
        P = self.P
        junk, bj = self.junk
        ss, bss = self.ssb[i % 2]
        hb, bhb = (self.hb[i % 2] if hb_out is None else hb_out)
        P.op("act", act(junk, xt, AF.Square), reads=[bx], writes=[bj])
        P.op("dve", rsum(ss, junk), reads=[bj], writes=[bss])
        P.op("act", act(ss, ss, AF.Sqrt, scale=1.0 / D, bias=self.eps_ap), reads=[bss, self.b_eps], writes=[bss])
        P.op("dve", lambda e: e.reciprocal(out=ss, in_=ss), reads=[bss], writes=[bss])
        P.op("dve", stt(junk, xt, ss, Abc, ALU.mult, ALU.mult), reads=[bx, bss, bA], writes=[bj])
        if hf_out is not None:
            hf, bhf = hf_out
            P.op("dve", tt(hf, junk, Bbc, ALU.add), reads=[bj, bB], writes=[bhf])
            P.op("pool", cp(hb, hf), reads=[bhf], writes=[bhb])
        else:
            P.op("pool", tt(hb, junk, Bbc, ALU.add), reads=[bj, bB], writes=[bhb])
        ps, pb = self.bank()
        psb = ps.bitcast(BF16)
        for k in range(KD):
            P.op("pe", tr(psb[:, k * 128:(k + 1) * 128], hb[:, k * 128:(k + 1) * 128], self.identb),
                 reads=[bhb, self.b_cb], writes=[pb])
        P.op("act", act(dst, psb.rearrange("p (k t) -> p k t", k=KD), AF.Copy), reads=[pb], writes=[b_dst])

    def mixer_batch(self, b):
        P, A, NB = self.P, self.A, self.NB
        A.mark()
        mt = self.MOD_d.tensor
        A1 = A.alloc([D], F32)
        B1 = A.alloc([D], F32)
        bA1, bB1 = Buf(), Buf()
        P.dma("sp", dma(B1, bcast_rows(mt, b * 6 * D + 0 * D, D)), reads=[self.b_MOD], writes=[bB1])
        P.dma("sp", dma(A1, bcast_rows(mt, b * 6 * D + 1 * D, D)), reads=[self.b_MOD], writes=[bA1])
        P.op("dve", stt(A1, A1, 1.0, self.n1g, ALU.add, ALU.mult), reads=[bA1, self.b_n1g], writes=[bA1])
        hT = A.alloc([KD, T], BF16)
        b_hT = Buf("hT")
        yT = A.alloc([4, L], BF16)
        b_yT = Buf("yT")
        A.mark()
        self.xt = [(A.alloc([D], F32), Buf()) for _ in range(2)]
        self.junk = (A.alloc([D], F32), Buf())
        self.ssb = [(A.alloc([1], F32), Buf()) for _ in range(2)]
        self.hb = [(A.alloc([D], BF16), Buf()) for _ in range(2)]
        for j in range(NT):
            if j < 2:
                src = self.ctx_d[b * CTX + j * 128: b * CTX + (j + 1) * 128, :]
                self.norm_mod_tile(j, src, self.CA, self.b_CA, self.CB, self.b_CB, hT[:, :, j * 128:(j + 1) * 128], b_hT)
            else:
                src = self.x_d[b * L + (j - 2) * 128: b * L + (j - 1) * 128, :]
                self.norm_mod_tile(j, src, A1, bA1, B1, bB1, hT[:, :, j * 128:(j + 1) * 128], b_hT)
        self.b_HT = getattr(self, "b_HT", None) or Buf("HT")
        P.dma("sp", dma(self.HT_d[b].rearrange("p (k t) -> p k t", k=KD), hT[:, :, CTX:T]), reads=[b_hT], writes=[self.b_HT])
        if b == 0:
            self.dump("hT", hT, b_hT, [128, KD, T], BF16)
        P.barrier()
        A.release()
        if self.upto < 1:
            A.release()
            return
        self.hgrn2(b, hT, b_hT, yT, b_yT)
        self.b_YT = getattr(self, "b_YT", None) or Buf("YT")
        for hh in range(4):
            P.dma("sp", dma(self.YT_d[b, 4 + hh], yT[:, hh, :]), reads=[b_yT], writes=[self.b_YT])
        if b == 0:
            self.dump("yT_hg", yT, b_yT, [128, 4, L], BF16)
        P.barrier()
        A.release()

    def hgrn2(self, b, hT, b_hT, yT, b_yT):
        P, A = self.P, self.A
        A.mark()
        f32b = lambda: (A.alloc([T], F32), Buf())
        bf16b = lambda: (A.alloc([T], BF16), Buf())
        self.rs = A.alloc([T], F32)
        self.b_rs = Buf()
        P.op("pool", lambda e: e.memset(self.rs, 1.0), writes=[self.b_rs])
        rs3 = self.rs.rearrange("p (a b) -> p a b", b=64)
        P.op("pool", lambda e: e.memset(rs3[:, :, 0:1], 0.0), writes=[self.b_rs])
        t1, b_t1 = f32b()
        kk, b_kk = f32b()
        bb, b_bb = f32b()
        t2, b_t2 = f32b()
        qs, b_qs = bf16b()
        qm, b_qm = bf16b()
        km, b_km = bf16b()
        qbE, b_qbE = bf16b()
        qbO, b_qbO = bf16b()
        kdT, b_kdT = bf16b()
        P.op("pool", lambda e: e.memset(qbE, 0.0), writes=[b_qbE])
        P.op("pool", lambda e: e.memset(qbO, 0.0), writes=[b_qbO])
        kdTokE = A.alloc([NT, 128], BF16)
        kdTokO = A.alloc([NT, 128], BF16)
        b_kdTok = Buf()
        P.op("pool", lambda e: e.memset(kdTokE[64:128], 0.0), writes=[b_kdTok])
        P.op("pool", lambda e: e.memset(kdTokO[0:64], 0.0), writes=[b_kdTok])
        V = A.alloc([NT, 128], BF16)
        b_V = Buf()
        gsn = A.alloc([NLT, 128], BF16)
        b_gsn = Buf()
        of = A.alloc([NLT, 128], F32)
        b_of = Buf()
        dec = A.alloc([NCH], F32)
        b_dec = Buf()
        ws = [(A.alloc([KD, 128], BF16), Buf()) for _ in range(5)]
        Sf = [(A.alloc([128], F32), Buf()) for _ in range(2)]
        Sb = [(A.alloc([128], BF16), Buf()) for _ in range(2)]
        attT = [(A.alloc([128], BF16), Buf()) for _ in range(2)]
        otl = [(A.alloc([128], F32), Buf()) for _ in range(2)]
        oj = [(A.alloc([128], F32), Buf()) for _ in range(2)]
        ossb = [(A.alloc([1], F32), Buf()) for _ in range(2)]
        ytl = [(A.alloc([128], BF16), Buf()) for _ in range(2)]
        gtmp = [(A.alloc([128], F32), Buf()) for _ in range(2)]
        chunks = [(t0, min(512, T - t0)) for t0 in range(0, T, 512)]
        v3 = lambda ap: ap.rearrange("p (a b) -> p a b", b=64)
        for hh in range(4):
            cols = [1536 + s * 512 + hh * 128 for s in range(5)]
            for s in range(5):
                w, bw = ws[s]
                P.dma("pool", dma(w, self.win_d[:, :, cols[s]:cols[s] + 128]), writes=[bw])
            wq, wf, wb_, wi, wg = ws
            for j in range(NT):
                ps, pb = self.bank()
                for k in range(KD):
                    P.op("pe", mm(ps[:, 0:128], hT[:, k, j * 128:(j + 1) * 128], wi[0][:, k, :], k == 0, k == KD - 1),
                         reads=[b_hT, wi[1]], writes=[pb])
                P.op("act", act(V[:, j, :], ps[:, 0:128], AF.Copy), reads=[pb], writes=[b_V])
                if j >= 2:
                    ps2, pb2 = self.bank()
                    for k in range(KD):
                        P.op("pe", mm(ps2[:, 0:128], hT[:, k, j * 128:(j + 1) * 128], wg[0][:, k, :], k == 0, k == KD - 1),
                             reads=[b_hT, wg[1]], writes=[pb2])
                    gt, bgt = gtmp[j % 2]
                    P.op("act", act(gt, ps2[:, 0:128], AF.Silu), reads=[pb2], writes=[bgt])
                    P.op("dve", tt(gsn[:, j - 2, :], gt, self.hgng, ALU.mult), reads=[bgt, self.b_hgng], writes=[b_gsn])
            for (t0, n) in chunks:
                ps, pb = self.bank()
                for k in range(KD):
                    P.op("pe", mm(ps[:, 0:n], wq[0][:, k, :], hT[:, k, t0:t0 + n], k == 0, k == KD - 1),
                         reads=[b_hT, wq[1]], writes=[pb])
                P.op("act", act(qs[:, t0:t0 + n], ps[:, 0:n], AF.Silu), reads=[pb], writes=[b_qs])
            for d in range(2):
                wgate = wf if d == 0 else wb_
                for (t0, n) in chunks:
                    ps, pb = self.bank()
                    for k in range(KD):
                        P.op("pe", mm(ps[:, 0:n], wgate[0][:, k, :], hT[:, k, t0:t0 + n], k == 0, k == KD - 1),
                             reads=[b_hT, wgate[1]], writes=[pb])
                    P.op("act", act(t1[:, t0:t0 + n], ps[:, 0:n], AF.Sigmoid), reads=[pb], writes=[b_t1])
                P.op("dve", ts(kk, t1, self.noml[:, d, hh:hh + 1], self.oml[:, d, hh:hh + 1], ALU.mult, ALU.add),
                     reads=[b_t1, self.b_lb], writes=[b_kk])
                P.op("act", act(t1, kk, AF.Ln, scale=-1.0, bias=self.one_ap), reads=[b_kk, self.b_eps], writes=[b_t1])
                P.op("dve", lambda e: e.tensor_tensor_scan(out=bb, data0=self.rs, data1=t1, initial=0.0, op0=ALU.mult, op1=ALU.add),
                     reads=[self.b_rs, b_t1], writes=[b_bb])
                bb3, t13 = v3(bb), v3(t1)
                if d == 1:
                    P.op("dve", tt(t1, t1, bb, ALU.subtract), reads=[b_t1, b_bb], writes=[b_t1])
                    P.op("dve", tt(bb3, t13, bb3[:, :, 63:64].to_broadcast([128, NCH, 64]), ALU.add), reads=[b_t1, b_bb], writes=[b_bb])
                mid = 31 if d == 0 else 32
                last = 63 if d == 0 else 0
                P.op("dve", tt(t13, bb3, bb3[:, :, mid:mid + 1].to_broadcast([128, NCH, 64]), ALU.subtract), reads=[b_bb], writes=[b_t1])
                P.op("act", act(t2, t1, AF.Exp), reads=[b_t1], writes=[b_t2])
                P.op("dve", stt(qm, qs, HGS, t2, ALU.mult, ALU.mult), reads=[b_qs, b_t2], writes=[b_qm])
                P.op("act", act(t2, t1, AF.Exp, scale=-1.0), reads=[b_t1, b_qm], writes=[b_t2])
                P.op("dve", tt(km, kk, t2, ALU.mult), reads=[b_kk, b_t2], writes=[b_km])
                P.op("act", act(t2, bb, AF.Exp), reads=[b_bb, b_km], writes=[b_t2])
                t23, qs3, qbE3, qbO3 = v3(t2), v3(qs), v3(qbE), v3(qbO)
                P.op("dve", stt(qbE3[:, 0::2, :], qs3[:, 0::2, :], HGS, t23[:, 0::2, :], ALU.mult, ALU.mult),
                     reads=[b_qs, b_t2], writes=[b_qbE])
                P.op("dve", stt(qbO3[:, 1::2, :], qs3[:, 1::2, :], HGS, t23[:, 1::2, :], ALU.mult, ALU.mult),
                     reads=[b_qs, b_t2], writes=[b_qbO])
                P.op("dve", tt(t13, bb3[:, :, last:last + 1].to_broadcast([128, NCH, 64]), bb3, ALU.subtract), reads=[b_bb, b_t2], writes=[b_t1])
                P.op("act", act(t1, t1, AF.Exp), reads=[b_t1], writes=[b_t1])
                P.op("dve", tt(kdT, kk, t1, ALU.mult), reads=[b_kk, b_t1], writes=[b_kdT])
                P.op("act", act(dec, bb3[:, :, last], AF.Exp), reads=[b_bb], writes=[b_dec])
                for j0 in range(0, NT, 8):
                    nj = min(8, NT - j0)
                    ps, pb = self.bank()
                    psb = ps.bitcast(BF16)
                    for jj in range(nj):
                        j = j0 + jj
                        P.op("pe", tr(psb[:, jj * 128:(jj + 1) * 128], kdT[:, j * 128:(j + 1) * 128], self.identb),
                             reads=[b_kdT, self.b_cb], writes=[pb])
                    P.op("act", act(kdTokE[0:64, j0:j0 + nj, :], psb[0:64, 0:nj * 128].rearrange("p (a b) -> p a b", b=128), AF.Copy),
                         reads=[pb], writes=[b_kdTok])
                    P.op("act", act(kdTokO[64:128, j0:j0 + nj, :], psb[64:128, 0:nj * 128].rearrange("p (a b) -> p a b", b=128), AF.Copy),
                         reads=[pb], writes=[b_kdTok])
                si = 0
                P.op("pool", lambda e, s=Sf[0][0]: e.memset(s, 0.0), writes=[Sf[0][1]])
                P.op("pool", lambda e, s=Sb[0][0]: e.memset(s, 0.0), writes=[Sb[0][1]])
                order = list(range(NT)) if d == 0 else [1, 0] + list(range(NT - 1, 1, -1))
                mask = self.maskF if d == 0 else self.maskB
                for j in order:
                    lat = j >= 2
                    tsl = slice(j * 128, (j + 1) * 128)
                    halves = [0, 1] if d == 0 else [1, 0]
                    if lat:
                        psA, pbA = self.bank()
                        P.op("pe", mm(psA[:, 0:128], km[:, tsl], qm[:, tsl], True, True), reads=[b_km, b_qm], writes=[pbA])
                        at, bat = attT[j % 2]
                        P.op("dve", tt(at, psA[:, 0:128], mask, ALU.mult), reads=[pbA, self.b_cst], writes=[bat])
                        psO, pbO = self.bank()
                        P.op("pe", mm(psO[:, 0:128], at, V[:, j, :], True, False), reads=[bat, b_V], writes=[pbO])
                    for hi, h in enumerate(halves):
                        if lat:
                            qbx, b_qbx = (qbE, b_qbE) if h == 0 else (qbO, b_qbO)
                            P.op("pe", mm(psO[:, 0:128], qbx[:, tsl], Sb[si][0], False, hi == 1),
                                 reads=[b_qbx, Sb[si][1]], writes=[pbO])
                        psU, pbU = self.bank()
                        kdx = kdTokE if h == 0 else kdTokO
                        P.op("pe", mm(psU[:, 0:128], kdx[:, j, :], V[:, j, :], True, True), reads=[b_kdTok, b_V], writes=[pbU])
                        ch = 2 * j + h
                        P.op("dve", stt(Sb[1 - si][0], Sf[si][0], dec[:, ch:ch + 1], psU[:, 0:128], ALU.mult, ALU.add),
                             reads=[Sf[si][1], b_dec, pbU], writes=[Sb[1 - si][1]])
                        P.op("dve", stt(Sf[1 - si][0], Sf[si][0], dec[:, ch:ch + 1], psU[:, 0:128], ALU.mult, ALU.add),
                             reads=[Sf[si][1], b_dec, pbU], writes=[Sf[1 - si][1]])
                        si = 1 - si
                    if lat:
                        if d == 0:
                            P.op("act", act(of[:, j - 2, :], psO[:, 0:128], AF.Copy), reads=[pbO], writes=[b_of])
                        else:
                            o, bo = oj[j % 2]
                            P.op("dve", tt(o, psO[:, 0:128], of[:, j - 2, :], ALU.add), reads=[pbO, b_of], writes=[bo])
                            jk, bjk = otl[j % 2]
                            oss, boss = ossb[j % 2]
                            P.op("act", act(jk, o, AF.Square), reads=[bo], writes=[bjk])
                            P.op("dve", rsum(oss, jk), reads=[bjk], writes=[boss])
                            P.op("act", act(oss, oss, AF.Sqrt, scale=1.0 / 128, bias=self.eps_ap), reads=[boss, self.b_eps], writes=[boss])
                            P.op("dve", lambda e, oss=oss: e.reciprocal(out=oss, in_=oss), reads=[boss], writes=[boss])
                            yt, byt = ytl[j % 2]
                            P.op("dve", stt(yt, o, oss, gsn[:, j - 2, :], ALU.mult, ALU.mult), reads=[bo, boss, b_gsn], writes=[byt])
                            psT, pbT = self.bank()
                            psTb = psT.bitcast(BF16)
                            P.op("pe", tr(psTb[:, 0:128], yt, self.identb), reads=[byt, self.b_cb], writes=[pbT])
                            P.op("act", act(yT[:, hh, (j - 2) * 128:(j - 1) * 128], psTb[:, 0:128], AF.Copy), reads=[pbT], writes=[b_yT])
        P.barrier()
        A.release()

    def sin_da(self, out, arg, tmpa, tmpb, bufs):
        P = self.P
        b_out, b_arg, b_ta, b_tb = bufs
        P.op("act", act(tmpa, arg, AF.Sin, scale=0.5), reads=[b_arg], writes=[b_ta])
        P.op("act", act(tmpb, arg, AF.Sin, scale=0.25), reads=[b_arg], writes=[b_tb])
        P.op("dve", tt(tmpb, tmpb, tmpb, ALU.mult), reads=[b_tb], writes=[b_tb])
        P.op("dve", ts(tmpb, tmpb, -2.0, 1.0, ALU.mult, ALU.add), reads=[b_tb], writes=[b_tb])
        P.op("dve", stt(out, tmpa, 2.0, tmpb, ALU.mult, ALU.mult), reads=[b_ta, b_tb], writes=[b_out])

    def filters(self):
        P, A = self.P, self.A
        A.mark()
        ld = lambda shape, src, parts: (A.alloc(shape, F32, parts=parts), Buf())
        featsT, b_ft = ld([L], None, 33)
        P.dma("sp", dma(featsT, self.featsT_d), writes=[b_ft])
        fw1, b_fw1 = ld([64], None, 33)
        P.dma("sp", dma(fw1, self.fw1_d), writes=[b_fw1])
        fw2, b_fw2 = ld([64], None, 64)
        P.dma("sp", dma(fw2, self.fw2_d), writes=[b_fw2])
        fw3, b_fw3 = ld([2048], None, 64)
        P.dma("sp", dma(fw3, self.fw3_d), writes=[b_fw3])
        sm, b_sm = ld([8], None, 64)
        P.dma("sp", dma(sm[:, 0:1], self.fb1_d), writes=[b_sm])
        P.dma("sp", dma(sm[:, 1:2], self.fb2_d), writes=[b_sm])
        P.dma("sp", dma(sm[:, 2:3], self.freq_d), writes=[b_sm])
        P.op("dve", ts(sm[:, 3:5], sm[:, 0:2], sm[:, 2:3], None, ALU.mult), reads=[b_sm], writes=[b_sm])
        dl, b_dl = ld([512], None, 128)
        P.dma("sp", dma(dl, bcast_rows(self.deltas_d.tensor, 0, 512)), writes=[b_dl])
        tf, b_tf = ld([16], None, 128)
        P.dma("sp", dma(tf, self.tfrac_d), writes=[b_tf])
        dsk, b_dsk = ld([2, 4], None, 128)
        P.dma("sp", lambda e: e.dma_start(out=dsk, in_=self.hyd_d.rearrange("a (o g c) -> c (a o) g", o=2, g=4),
                                          allow_slow_non_contiguous=True), writes=[b_dsk])
        h1, b_h1 = ld([L], None, 64)
        h2, b_h2 = ld([L], None, 64)
        ta, b_ta = ld([L], None, 64)
        tb, b_tb = ld([L], None, 64)
        ar, b_ar = ld([L], None, 64)
        for layer in range(2):
            src, b_src, K_, w, b_w = (featsT, b_ft, 33, fw1, b_fw1) if layer == 0 else (h1, b_h1, 64, fw2, b_fw2)
            for q in range(4):
                ps, pb = self.bank()
                P.op("pe", mm(ps[0:64, :], w[0:K_, :], src[0:K_, q * 512:(q + 1) * 512], True, True), reads=[b_w, b_src], writes=[pb])
                P.op("act", act(ar[:, q * 512:(q + 1) * 512], ps[0:64, :], AF.Identity, scale=sm[:, 2:3], bias=sm[:, 3 + layer:4 + layer]),
                     reads=[pb, b_sm], writes=[b_ar])
            dst, b_dst = (h1, b_h1) if layer == 0 else (h2, b_h2)
            self.sin_da(dst, ar, ta, tb, (b_dst, b_ar, b_ta, b_tb))
        rinv = A.alloc([2, 512], F32)
        b_rinv = Buf()
        win = [(A.alloc([512], F32), Buf()) for _ in range(2)]
        winr = [(A.alloc([2, 512], F32), Buf()) for _ in range(2)]
        hw = [(A.alloc([512], F32), Buf()) for _ in range(2)]
        hn = [(A.alloc([512], BF16), Buf()) for _ in range(2)]
        GT = A.alloc([4, 2, 4096], BF16)
        b_GT = Buf("GT")
        P.op("pool", lambda e: e.memset(GT, 0.0), writes=[b_GT])
        accs = self.reserve(2)
        for i in range(16):
            wi_, bwi = win[i % 2]
            P.op("act", act(wi_, dl, AF.Exp, scale=tf[:, i:i + 1]), reads=[b_dl, b_tf], writes=[bwi])
            for o in range(2):
                for dr in range(2):
                    q = o * 2 + dr
                    ps, pb = self.bank()
                    P.op("pe", mm(ps[:, :], h2[0:64, i * 128:(i + 1) * 128], fw3[0:64, q * 512:(q + 1) * 512], True, True),
                         reads=[b_h2, b_fw3], writes=[pb])
                    hwt, bhw = hw[q % 2]
                    P.op("dve", tt(hwt, ps, wi_, ALU.mult), reads=[pb, bwi], writes=[bhw])
                    P.op("act", act(hwt, hwt, AF.Abs), reads=[bhw], writes=[bhw])
                    first = (i == 0 and dr == 0)
                    lastf = (i == 15 and dr == 1)
                    P.op("pe", mm(accs[o][0][:, :], self.ones_f, hwt, first, lastf), reads=[self.b_cst, bhw], writes=[accs[o][1]])
        for o in range(2):
            P.op("dve", lambda e, o=o: e.reciprocal(out=rinv[:, o, :], in_=accs[o][0][:, :]), reads=[accs[o][1]], writes=[b_rinv])
        self.unreserve(accs)
        GTv = GT
        for i in range(16):
            wi_, bwi = win[i % 2]
            wr, bwr = winr[i % 2]
            P.op("act", act(wi_, dl, AF.Exp, scale=tf[:, i:i + 1]), reads=[b_dl, b_tf], writes=[bwi])
            P.op("dve", tt(wr, rinv, wi_.unsqueeze(1).to_broadcast([128, 2, 512]), ALU.mult), reads=[bwi, b_rinv], writes=[bwr])
            for o in range(2):
                for dr in ([1, 0] if o == 0 else [0, 1]):
                    q = o * 2 + dr
                    useJ = (o == 0 and dr == 1) or (o == 1 and dr == 0)
                    ps, pb = self.bank()
                    P.op("pe", mm(ps[:, :], h2[0:64, i * 128:(i + 1) * 128], fw3[0:64, q * 512:(q + 1) * 512], True, True),
                         reads=[b_h2, b_fw3], writes=[pb])
                    hnt, bhn = hn[q % 2]
                    P.op("dve", tt(hnt, ps, wr[:, o, :], ALU.mult), reads=[pb, bwr], writes=[bhn])
                    pt, pbt = self.bank()
                    for cg in range(4):
                        P.op("pe", mm(pt[:, cg * 128:(cg + 1) * 128], hnt[:, cg * 128:(cg + 1) * 128], self.Jb if useJ else self.identb, True, True),
                             reads=[bhn, self.b_cb], writes=[pbt])
                    pt3 = pt.rearrange("p (g m) -> p g m", g=4)
                    if o == 0:
                        start = (1921 - 128 * i) if useJ else (2048 + 128 * i)
                    else:
                        start = (1920 - 128 * i) if useJ else (2047 + 128 * i)
                    if i == 0 and not useJ:
                        P.op("act", act(GTv[:, :, o, start + 1:start + 128], pt3[:, :, 1:128], AF.Copy), reads=[pbt], writes=[b_GT])
                        ctr, b_ctr = self.ctr_tmp = getattr(self, "ctr_tmp", None) or (A.alloc([4], F32), Buf())
                        P.op("dve", tt(ctr, pt3[:, :, 0], dsk[:, o, :], ALU.add), reads=[pbt, b_dsk], writes=[b_ctr])
                        P.op("dve", tt(GTv[:, :, o, start], GTv[:, :, o, start], ctr, ALU.add), reads=[b_ctr, b_GT], writes=[b_GT])
                    else:
                        P.op("act", act(GTv[:, :, o, start:start + 128], pt3, AF.Copy), reads=[pbt], writes=[b_GT])
        self.b_G = Buf("G")
        for o in range(2):
            for cg in range(4):
                P.dma("sp", dma(self.G_d[o, cg * 128:(cg + 1) * 128, :], GTv[:, cg, o, :]), reads=[b_GT], writes=[self.b_G])
        if "G" in self.dbg:
            for o in range(2):
                og = self.dout("dbg_G%d" % o, [128, 4, 4096], BF16)
                P.dma("sp", dma(og, GTv[:, :, o, :]), reads=[b_GT])
        P.barrier()
        A.release()

    def hyena_all(self):
        P, A, NB = self.P, self.A, self.NB
        NBI = NB * 16
        A.mark()
        bf = lambda: A.alloc([128, NB, 16], BF16)
        X1, X2r, Vr, Z = bf(), bf(), bf(), bf()
        b_in = [Buf() for _ in range(16)]
        b_Z = [Buf() for _ in range(16)]
        hTl = A.alloc([KD, L], BF16)
        b_hTl = Buf()
        W3 = A.alloc([KD, 384], BF16)
        b_W3 = Buf()
        wc = A.alloc([3, 384], F32)
        bc = A.alloc([384], F32)
        b_wc = Buf()
        pw = [(A.alloc([3, 384], BF16), Buf()) for _ in range(2)]
        NTZ = 3
        TZ = [[(A.alloc([3968], BF16), Buf()) for _ in range(NTZ)] for _ in range(2)]
        yTc = [(A.alloc([512], BF16), Buf()) for _ in range(2)]
        gt = self.G_d.tensor
        dq = ["sp", "act"]
        tzc = [0, 0]
        for cg in range(4):
            for s in range(3):
                c0 = s * 512 + cg * 128
                P.dma("pool", dma(W3[:, :, s * 128:(s + 1) * 128], self.win_d[:, :, c0:c0 + 128]), writes=[b_W3])
                for kk in range(3):
                    P.dma("sp", dma(wc[:, kk, s * 128:(s + 1) * 128], bcast_rows(self.hcw_d.tensor, kk * 1536 + c0, 128)), writes=[b_wc])
                P.dma("sp", dma(bc[:, s * 128:(s + 1) * 128], bcast_rows(self.hcb_d.tensor, c0, 128)), writes=[b_wc])
            for b in range(NB):
                P.dma("sp", dma(hTl, self.HT_d[b].rearrange("p (k t) -> p k t", k=KD)), reads=[self.b_HT], writes=[b_hTl])
                for i in range(NLT):
                    ps, pb = self.bank()
                    for k in range(KD):
                        P.op("pe", mm(ps[:, 0:384], hTl[:, k, i * 128:(i + 1) * 128], W3[:, k, :], k == 0, k == KD - 1),
                             reads=[b_hTl, b_W3], writes=[pb])
                    pwt, bpw = pw[i % 2]
                    for kk in range(3):
                        P.op("dve", tt(pwt[:, kk, :], ps[:, 0:384], wc[:, kk, :], ALU.mult), reads=[pb, b_wc], writes=[bpw])
                    pa, pba = self.bank()
                    pr, pbr = self.bank()
                    mats = [(self.Smb, self.SmRb), (self.identb, self.Jb), (self.Spb, self.SpRb)]
                    for kk in range(3):
                        P.op("pe", mm(pa[:, 0:128], mats[kk][0], pwt[:, kk, 0:128], kk == 0, kk == 2), reads=[bpw, self.b_cb], writes=[pba])
                    for kk in range(3):
                        P.op("pe", mm(pr[:, 0:256], mats[kk][1], pwt[:, kk, 128:384], kk == 0, kk == 2), reads=[bpw, self.b_cb], writes=[pbr])
                    P.op("dve", tt(X1[:, :, b, i], pa[:, 0:128], bc[:, 0:128], ALU.add), reads=[pba, b_wc], writes=b_in)
                    P.op("dve", tt(X2r[:, :, b, i], pr[:, 0:128], bc[:, 128:256], ALU.add), reads=[pbr, b_wc], writes=b_in)
                    P.op("dve", tt(Vr[:, :, b, i], pr[:, 128:256], bc[:, 256:384], ALU.add), reads=[pbr, b_wc], writes=b_in)
            deltas = [0] + [s * m for m in range(1, 16) for s in (1, -1)]

            def conv(o, g, src):
                ps, pb = self.bank()
                for cc in range(8):
                    c = g * 8 + cc
                    ch = cg * 128 + c
                    tz, btz = TZ[o][tzc[o] % NTZ]
                    tzc[o] += 1
                    off = (o * 512 + ch) * 4096 + (1 if o == 0 else 0)
                    P.dma(dq[(c + o) % 2], dma(tz, bass.AP(tensor=gt, offset=off, ap=[[1, 128], [1, 3968]])), reads=[self.b_G], writes=[btz])
                    psv = ps[:, cc * NBI:(cc + 1) * NBI].rearrange("p (b i) -> p b i", b=NB)
                    for di, dl_ in enumerate(deltas):
                        ilo, ihi = max(0, -dl_), min(16, 16 - dl_)
                        blk = (dl_ + 15) if o == 0 else (-dl_ + 15)
                        P.op("pe", mm(psv[:, :, ilo + dl_:ihi + dl_], tz[:, blk * 128:(blk + 1) * 128], src[:, c, :, ilo:ihi],
                                      di == 0, di == len(deltas) - 1),
                             reads=[btz, (b_in[g] if o == 0 else b_Z[g])], writes=[pb])
                return ps, pb

            def evac1(g, ps, pb):
                P.op("dve", tt(Z[:, g * 8:(g + 1) * 8, :, :].rearrange("p c b i -> p (c b i)"), ps[:, 0:8 * NBI],
                               X1[:, g * 8:(g + 1) * 8, :, :].rearrange("p c b i -> p (c b i)"), ALU.mult),
                     reads=[pb, b_in[g]], writes=[b_Z[g]])

            def evac2(g, ps, pb):
                P.op("dve", tt(Z[:, g * 8:(g + 1) * 8, :, :].rearrange("p c b i -> p (c b i)"), ps[:, 0:8 * NBI],
                               X2r[:, g * 8:(g + 1) * 8, :, :].rearrange("p c b i -> p (c b i)"), ALU.mult),
                     reads=[pb, b_in[g]], writes=[b_Z[g]])

            p1 = conv(0, 0, Vr)
            for g in range(16):
                evac1(g, *p1)
                if g + 1 < 16:
                    p1 = conv(0, g + 1, Vr)
                p2 = conv(1, g, Z)
                evac2(g, *p2)
            for b in range(NB):
                for i0 in range(0, NLT, 4):
                    ps, pb = self.bank()
                    for ii in range(4):
                        P.op("pe", mm(ps[:, ii * 128:(ii + 1) * 128], Z[:, :, b, i0 + ii], self.Jb, True, True), reads=b_Z + [self.b_cb], writes=[pb])
                    yt, byt = yTc[(i0 // 4) % 2]
                    P.op("act", act(yt, ps, AF.Copy), reads=[pb], writes=[byt])
                    P.dma("sp", dma(self.YT_d[b, cg, :, i0 * 128:(i0 + 4) * 128], yt), reads=[byt], writes=[self.b_YT])
                    if b == 0 and "yT_hy" in self.dbg:
                        if not hasattr(self, "dbg_hy"):
                            self.dbg_hy = self.dout("dbg_yT_hy", [128, 4, L], BF16)
                        P.dma("sp", dma(self.dbg_hy[:, cg, i0 * 128:(i0 + 4) * 128], yt), reads=[byt])
        P.barrier()
        A.release()

    def route_init(self):
        P, A, NB = self.P, self.A, self.NB
        NTT = NB * NLT
        self.destAll = A.alloc([NTT, 8], I32)
        self.wAll = A.alloc([NTT, 8], F32)
        self.b_dw = Buf("destw")
        self.cnt = A.alloc([NE], F32)
        self.b_cnt = Buf("cnt")
        P.op("pool", lambda e: e.memset(self.cnt, 0.0), writes=[self.b_cnt])
        self.b_rc = Buf("routeconst")
        self.E8All = A.alloc([NTT, 8], F32)
        self.P8All = A.alloc([NTT, 8], F32)
        self.W8raw = A.alloc([NTT, 8], F32)
        self.DestF = A.alloc([NTT, 8], F32)
        self.b_ov = Buf("ovstate")
        self.IDXG = A.alloc([OV], I32)
        self.b_idx = Buf("ovidx")
        self.b_H2 = Buf("H2")
        A.mark()
        self.iota3 = A.alloc([NE], F32)
        self.rbias = A.alloc([NE], F32)
        self.n2g = A.alloc([D], F32)
        P.dma("sp", dma(self.iota3, bcast_rows(self.iota_d.tensor, 0, NE)), writes=[self.b_rc])
        P.dma("sp", dma(self.rbias, bcast_rows(self.rb_d.tensor, 0, NE)), writes=[self.b_rc])
        P.dma("sp", dma(self.n2g, bcast_rows(self.n2g_d.tensor, 0, D)), writes=[self.b_rc])
        self.wr = A.alloc([KD, NE], F32)
        P.dma("sp", dma(self.wr, self.wr_d), writes=[self.b_rc])
        self.wout = A.alloc([KD, D], BF16)
        self.swgu = A.alloc([KD, 512], BF16)
        self.swd = A.alloc([2, D], BF16)
        self.b_wts = Buf("wts")
        for k in range(KD):
            P.dma("pool", dma(self.wout[:, k, :], self.wout_d[:, k, :]), writes=[self.b_wts])
        P.dma("pool", dma(self.swgu[:, :, 0:256], self.swg_d), writes=[self.b_wts])
        P.dma("pool", dma(self.swgu[:, :, 256:512], self.swu_d), writes=[self.b_wts])
        P.dma("pool", dma(self.swd, self.swd_d), writes=[self.b_wts])
        self.b_X1 = Buf("X1")
        self.b_XS = Buf("XS")

        def pre_pool(engine):
            self.bc_reg = engine.to_reg(NE * CAP - 1)
            self.bc_reg2 = engine.to_reg(NE * CAP + OV * 128 - 1)
        P.pre["pool"] = pre_pool

    def outproj_route(self, b):
        P, A, NB = self.P, self.A, self.NB
        A.mark()
        mt = self.MOD_d.tensor
        G1 = A.alloc([D], F32)
        A2 = A.alloc([D], F32)
        B2 = A.alloc([D], F32)
        G2 = A.alloc([D], F32)
        b_m = Buf()
        P.dma("sp", dma(G1, bcast_rows(mt, b * 6 * D + 2 * D, D)), reads=[self.b_MOD], writes=[b_m])
        P.dma("sp", dma(B2, bcast_rows(mt, b * 6 * D + 3 * D, D)), reads=[self.b_MOD], writes=[b_m])
        P.dma("sp", dma(A2, bcast_rows(mt, b * 6 * D + 4 * D, D)), reads=[self.b_MOD], writes=[b_m])
        P.dma("sp", dma(G2, bcast_rows(mt, b * 6 * D + 5 * D, D)), reads=[self.b_MOD], writes=[b_m])
        P.op("dve", stt(A2, A2, 1.0, self.n2g, ALU.add, ALU.mult), reads=[b_m, self.b_rc], writes=[b_m])
        self.junk = (A.alloc([D], F32), Buf())
        self.ssb = [(A.alloc([1], F32), Buf()) for _ in range(2)]
        yT4 = [(A.alloc([KD, 512], BF16), Buf()) for _ in range(2)]
        xts = [(A.alloc([D], F32), Buf()) for _ in range(2)]
        x1s = [(A.alloc([D], F32), Buf()) for _ in range(2)]
        tmpA = (A.alloc([D], F32), Buf())
        tmpB = (A.alloc([D], F32), Buf())
        hfs = [(A.alloc([D], F32), Buf()) for _ in range(2)]
        hbs = [(A.alloc([D], BF16), Buf()) for _ in range(2)]
        h2Tb = [(A.alloc([KD, 128], BF16), Buf()) for _ in range(2)]
        h2Tf = [(A.alloc([KD, 128], F32), Buf()) for _ in range(2)]
        scs = [(A.alloc([NE], F32), Buf()) for _ in range(2)]
        bias_ = [(A.alloc([NE], F32), Buf()) for _ in range(2)]
        msk = (A.alloc([NE], F32), Buf())
        sel = (A.alloc([NE], F32), Buf())
        selb = (A.alloc([NE], BF16), Buf())
        pos = (A.alloc([NE], F32), Buf())
        oh = (A.alloc([8, NE], F32), Buf())
        sm = (A.alloc([160], F32), Buf())
        dsc = [(A.alloc([8], I32), Buf()) for _ in range(2)]
        sg = (A.alloc([256], F32), Buf())
        hmid = (A.alloc([2, 128], BF16), Buf())
        smv = sm[0]
        b_s = sm[1]
        m8g = smv[:, 0:64].rearrange("p (g e) -> p g e", g=8)
        gs_ = smv[:, 64:72]
        m8 = smv[:, 72:80]
        gmask = smv[:, 80:88]
        gm1 = smv[:, 88:96]
        m8b = smv[:, 96:104]
        w8 = smv[:, 104:112]
        e8f = smv[:, 112:120]
        posk = smv[:, 120:128]
        dest = smv[:, 128:136]
        valid = smv[:, 136:144]
        t8 = smv[:, 144:152]
        ws1 = smv[:, 152:153]
        e8 = A.alloc([8], U32)
        def stage1(i):
            ti = b * NLT + i
            sc, bia = scs[i % 2], bias_[i % 2]
            tmp = tmpA
            tt_ = i % 4
            yt, byt = yT4[(i // 4) % 2]
            if tt_ == 0:
                P.dma("sp", dma(yt, self.YT_d[b].rearrange("k p t -> p k t")[:, :, i * 128:(i + 4) * 128]), reads=[self.b_YT], writes=[byt])
            xt, bx = xts[i % 2]
            P.dma("act", dma(xt, self.x_d[b * L + i * 128: b * L + (i + 1) * 128, :]), writes=[bx])
            pss = [self.bank(), self.bank()]
            for n in range(2):
                for k in range(KD):
                    P.op("pe", mm(pss[n][0][:, :], yt[:, k, tt_ * 128:(tt_ + 1) * 128], self.wout[:, k, n * 512:(n + 1) * 512], k == 0, k == KD - 1),
                         reads=[byt, self.b_wts], writes=[pss[n][1]])
            for n in range(2):
                P.op("dve", tt(tmp[0][:, n * 512:(n + 1) * 512], pss[n][0][:, :], G1[:, n * 512:(n + 1) * 512], ALU.mult),
                     reads=[pss[n][1], b_m], writes=[tmp[1]])
            x1, bx1 = x1s[i % 2]
            P.op("pool", tt(x1, tmp[0], xt, ALU.add), reads=[tmp[1], bx], writes=[bx1])
            if b == 0 and "x1" in self.dbg:
                if not hasattr(self, "dbg_x1"):
                    self.dbg_x1 = self.dout("dbg_x1", [L, D])
                P.dma("sp", dma(self.dbg_x1[i * 128:(i + 1) * 128, :], x1), reads=[bx1])
            hf, bhf = hfs[i % 2]
            hb, bhb = hbs[i % 2]
            hT2, bhT2 = h2Tb[i % 2]
            self.norm_mod_sb(i, x1, bx1, A2, b_m, B2, b_m, hT2, bhT2, hf_out=(hf, bhf), hb_out=(hb, bhb))
            if b == 0 and "hx2" in self.dbg:
                if not hasattr(self, "dbg_hx2"):
                    self.dbg_hx2 = self.dout("dbg_hx2", [L, D])
                P.dma("sp", dma(self.dbg_hx2[i * 128:(i + 1) * 128, :], hf), reads=[bhf])
            pf = [self.bank(), self.bank()]
            for k in range(KD):
                P.op("pe", tr(pf[k // 4][0][:, (k % 4) * 128:(k % 4 + 1) * 128], hf[:, k * 128:(k + 1) * 128], self.identf),
                     reads=[bhf, self.b_cst], writes=[pf[k // 4][1]])
            hTf, bhTf = h2Tf[i % 2]
            P.op("act", act(hTf[:, 0:4, :], pf[0][0].rearrange("p (k t) -> p k t", k=4), AF.Copy), reads=[pf[0][1]], writes=[bhTf])
            P.op("dve", cp(hTf[:, 4:8, :], pf[1][0].rearrange("p (k t) -> p k t", k=4)), reads=[pf[1][1]], writes=[bhTf])
            pr, pbr = self.bank()
            for k in range(KD):
                P.op("pe", mm(pr[:, 0:NE], hTf[:, k, :], self.wr[:, k, :], k == 0, k == KD - 1), reads=[bhTf, self.b_rc], writes=[pbr])
            P.op("act", act(sc[0], pr[:, 0:NE], AF.Sigmoid), reads=[pbr], writes=[sc[1]])
            P.op("dve", tt(bia[0], sc[0], self.rbias, ALU.add), reads=[sc[1], self.b_rc], writes=[bia[1]])
        def stage2(i):
            ti = b * NLT + i
            x1, bx1 = x1s[i % 2]
            hf, bhf = hfs[i % 2]
            hb, bhb = hbs[i % 2]
            hT2, bhT2 = h2Tb[i % 2]
            sc, bia = scs[i % 2], bias_[i % 2]
            tmp = tmpB
            bia3 = bia[0].rearrange("p (g e) -> p g e", g=8)
            for g in range(8):
                P.op("dve", lambda e, g=g: e.max(out=m8g[:, g, :], in_=bia3[:, g, :]), reads=[bia[1]], writes=[b_s])
            P.op("dve", tt(gs_, m8g[:, :, 0], m8g[:, :, 1], ALU.add), reads=[b_s], writes=[b_s])
            P.op("dve", lambda e: e.max(out=m8, in_=gs_), reads=[b_s], writes=[b_s])
            P.op("dve", ts(gmask, gs_, m8[:, 3:4], None, ALU.is_ge), reads=[b_s], writes=[b_s])
            P.op("dve", ts(gm1, gmask, -1.0, None, ALU.add), reads=[b_s], writes=[b_s])
            msk3 = msk[0].rearrange("p (g e) -> p g e", g=8)
            P.op("dve", tt(msk3, bia3, gmask.unsqueeze(2).to_broadcast([128, 8, 32]), ALU.mult), reads=[bia[1], b_s], writes=[msk[1]])
            P.op("dve", tt(msk3, msk3, gm1.unsqueeze(2).to_broadcast([128, 8, 32]), ALU.add), reads=[msk[1], b_s], writes=[msk[1]])
            P.op("dve", lambda e: e.max(out=m8b, in_=msk[0]), reads=[msk[1]], writes=[b_s])
            P.op("dve", ts(sel[0], msk[0], m8b[:, 7:8], None, ALU.is_ge), reads=[msk[1], b_s], writes=[sel[1]])
            P.op("pool", cp(selb[0], sel[0]), reads=[sel[1]], writes=[selb[1]])
            P.op("dve", tt(msk[0], sc[0], sel[0], ALU.mult), reads=[sc[1], sel[1]], writes=[msk[1]])
            P.op("dve", lambda e: e.max(out=w8, in_=msk[0]), reads=[msk[1]], writes=[b_s])
            P.op("dve", lambda e: e.max_index(out=e8, in_max=w8, in_values=msk[0]), reads=[msk[1], b_s], writes=[b_s])
            P.op("dve", cp(e8f, e8), reads=[b_s], writes=[b_s])
            P.op("dve", rsum(ws1, w8), reads=[b_s], writes=[b_s])
            P.op("dve", lambda e: e.reciprocal(out=ws1, in_=ws1), reads=[b_s], writes=[b_s])
            P.op("dve", ts(w8, w8, ws1, RSCALE, ALU.mult, ALU.mult), reads=[b_s], writes=[b_s])
            pp, pbp = self.bank()
            P.op("pe", mm(pp[:, 0:NE], self.ustrict_b, selb[0], True, True), reads=[selb[1], self.b_cb], writes=[pbp])
            pc, pbc = self.bank()
            P.op("pe", mm(pc[:, 0:NE], self.ones_b, selb[0], True, True), reads=[selb[1], self.b_cb], writes=[pbc])
            P.op("dve", tt(pos[0], pp[:, 0:NE], self.cnt, ALU.add), reads=[pbp, self.b_cnt], writes=[pos[1]])
            P.op("dve", tt(self.cnt, self.cnt, pc[:, 0:NE], ALU.add), reads=[pbc, pos[1]], writes=[self.b_cnt])
            P.op("dve", tt(oh[0], self.iota3.unsqueeze(1).to_broadcast([128, 8, NE]), e8f.unsqueeze(2).to_broadcast([128, 8, NE]), ALU.is_equal),
                 reads=[self.b_rc, b_s], writes=[oh[1]])
            P.op("dve", tt(oh[0], oh[0], pos[0].unsqueeze(1).to_broadcast([128, 8, NE]), ALU.mult), reads=[oh[1], pos[1]], writes=[oh[1]])
            P.op("dve", rsum(posk, oh[0]), reads=[oh[1]], writes=[b_s])
            P.op("dve", stt(dest, e8f, float(CAP), posk, ALU.mult, ALU.add), reads=[b_s], writes=[b_s])
            P.op("dve", ts(valid, posk, float(CAP), None, ALU.is_lt), reads=[b_s], writes=[b_s])
            P.op("dve", ts(t8, valid, -1.0e6, 1.0e6, ALU.mult, ALU.add), reads=[b_s], writes=[b_s])
            P.op("dve", tt(t8, t8, dest, ALU.add), reads=[b_s], writes=[b_s])
            di, bdi = dsc[i % 2]
            P.op("dve", cp(di, t8), reads=[b_s], writes=[bdi])
            P.op("dve", tt(self.DestF[:, ti, :], dest, valid, ALU.mult), reads=[b_s], writes=[self.b_ov])
            P.op("dve", tt(self.wAll[:, ti, :], w8, valid, ALU.mult), reads=[b_s], writes=[self.b_dw])
            P.op("dve", cp(self.E8All[:, ti, :], e8f), reads=[b_s], writes=[self.b_ov])
            P.op("dve", cp(self.P8All[:, ti, :], posk), reads=[b_s], writes=[self.b_ov])
            P.op("dve", cp(self.W8raw[:, ti, :], w8), reads=[b_s], writes=[self.b_ov])
            P.dma("act", dma(self.H2_d[ti * 128:(ti + 1) * 128, :], hb), reads=[bhb], writes=[self.b_H2])
            for k in range(8):
                P.dma("pool", lambda e, k=k, di=di, hb=hb: e.indirect_dma_start(
                    out=self.XS_d, out_offset=bass.IndirectOffsetOnAxis(ap=di[:, k:k + 1], axis=0), in_=hb, in_offset=None,
                    bounds_check=self.bc_reg, oob_is_err=False), reads=[bdi, bhb], writes=[self.b_XS])
            ph, pbh = self.bank()
            for fc in range(4):
                for k in range(KD):
                    P.op("pe", mm(ph[:, fc * 128:(fc + 1) * 128], self.swgu[:, k, fc * 128:(fc + 1) * 128], hT2[:, k, :], k == 0, k == KD - 1),
                         reads=[bhT2, self.b_wts], writes=[pbh])
            P.op("act", act(sg[0], ph[:, 0:256], AF.Silu), reads=[pbh], writes=[sg[1]])
            P.op("dve", tt(hmid[0].rearrange("p a b -> p (a b)"), sg[0], ph[:, 256:512], ALU.mult), reads=[sg[1], pbh], writes=[hmid[1]])
            pd = [self.bank(), self.bank()]
            for n in range(2):
                for fc in range(2):
                    P.op("pe", mm(pd[n][0][:, :], hmid[0][:, fc, :], self.swd[:, fc, n * 512:(n + 1) * 512], fc == 0, fc == 1),
                         reads=[hmid[1], self.b_wts], writes=[pd[n][1]])
            for n in range(2):
                P.op("dve", tt(tmp[0][:, n * 512:(n + 1) * 512], pd[n][0][:, :], G2[:, n * 512:(n + 1) * 512], ALU.mult),
                     reads=[pd[n][1], b_m], writes=[tmp[1]])
            P.op("dve", tt(x1, tmp[0], x1, ALU.add), reads=[tmp[1], bx1], writes=[bx1])
            P.dma("sp", dma(self.X1_d[ti * 128:(ti + 1) * 128, :], x1), reads=[bx1], writes=[self.b_X1])
        stage1(0)
        for i in range(NLT):
            if i + 1 < NLT:
                stage1(i + 1)
            stage2(i)
        P.barrier()
        A.release()

    def overflow_route(self):
        P, A, NB = self.P, self.A, self.NB
        NTT = NB * NLT
        A.mark()
        pidx = A.alloc([1], F32)
        b_c = Buf()
        P.dma("sp", dma(pidx, self.pidx_d), writes=[b_c])
        thr = A.alloc([1], F32)
        P.op("dve", ts(thr, pidx, 128.0, None, ALU.mult), reads=[b_c], writes=[b_c])
        ovc = A.alloc([NE], F32)
        b_o = Buf()
        P.op("dve", ts(ovc, self.cnt, -float(CAP), 0.0, ALU.add, ALU.max), reads=[self.b_cnt], writes=[b_o])
        cmpb = A.alloc([NE], BF16)
        P.op("dve", ts(cmpb, ovc, thr, None, ALU.is_gt), reads=[b_o, b_c], writes=[b_o])
        ps, pb = self.bank()
        P.op("pe", mm(ps[:, 0:NE], self.ones_b, cmpb, True, True), reads=[b_o, self.b_cb], writes=[pb])
        ovblk = A.alloc([NE], F32)
        ovend = A.alloc([NE], F32)
        ovs = A.alloc([NE], F32)
        onesr = A.alloc([NE], F32)
        b_e = Buf()
        P.op("act", act(ovblk, ps[:, 0:NE], AF.Copy), reads=[pb], writes=[b_e])
        P.op("pool", lambda e: e.memset(onesr, 1.0), writes=[b_e])
        P.op("dve", lambda e: e.tensor_tensor_scan(out=ovend, data0=onesr, data1=ovblk, initial=0.0, op0=ALU.mult, op1=ALU.add),
             reads=[b_e], writes=[b_e])
        P.op("dve", tt(ovs, ovend, ovblk, ALU.subtract), reads=[b_e], writes=[b_e])
        P.op("dve", ts(ovs, ovs, 128.0, None, ALU.mult), reads=[b_e], writes=[b_e])
        beRow = A.alloc([OV], F32)
        b_b = Buf()
        cmp3 = A.alloc([16, NE], F32)
        for c0 in range(0, OV, 16):
            P.op("dve", tt(cmp3, ovend.unsqueeze(1).to_broadcast([128, 16, NE]),
                           self.iota3[:, c0:c0 + 16].unsqueeze(2).to_broadcast([128, 16, NE]), ALU.is_le), reads=[b_e, self.b_rc], writes=[b_b])
            P.op("dve", rsum(beRow[:, c0:c0 + 16], cmp3), reads=[b_b], writes=[b_b])
        P.op("dve", ts(beRow, beRow, float(NE - 1), None, ALU.min), reads=[b_b], writes=[b_b])
        self.dump("be", beRow, b_b, [128, OV])
        idxf = A.alloc([OV], F32)
        P.op("dve", ts(idxf, beRow, 128.0, pidx, ALU.mult, ALU.add), reads=[b_b, b_c], writes=[b_b])
        P.op("dve", cp(self.IDXG, idxf), reads=[b_b], writes=[self.b_idx])
        oh = A.alloc([8, NE], F32)
        b_oh = Buf()
        sm = A.alloc([64], F32)
        b_s = Buf()
        ob8, ovslot, isov, ok, t8 = (sm[:, 8 * q:8 * q + 8] for q in range(5))
        di_ = [(A.alloc([8], I32), Buf()) for _ in range(2)]
        hbs = [(A.alloc([D], BF16), Buf()) for _ in range(2)]
        for ti in range(NTT):
            hb, bhb = hbs[ti % 2]
            P.dma("sp", dma(hb, self.H2_d[ti * 128:(ti + 1) * 128, :]), reads=[self.b_H2], writes=[bhb])
            P.op("dve", tt(oh, self.iota3.unsqueeze(1).to_broadcast([128, 8, NE]),
                           self.E8All[:, ti, :].unsqueeze(2).to_broadcast([128, 8, NE]), ALU.is_equal), reads=[self.b_rc, self.b_ov], writes=[b_oh])
            P.op("dve", tt(oh, oh, ovs.unsqueeze(1).to_broadcast([128, 8, NE]), ALU.mult), reads=[b_oh, b_e], writes=[b_oh])
            P.op("dve", rsum(ob8, oh), reads=[b_oh], writes=[b_s])
            P.op("dve", stt(ovslot, self.P8All[:, ti, :], -float(CAP), ob8, ALU.add, ALU.add), reads=[b_s, self.b_ov], writes=[b_s])
            P.op("dve", ts(isov, self.P8All[:, ti, :], float(CAP), None, ALU.is_ge), reads=[self.b_ov], writes=[b_s])
            P.op("dve", ts(ok, ovslot, float(OV * 128), None, ALU.is_lt), reads=[b_s], writes=[b_s])
            P.op("dve", tt(ok, ok, isov, ALU.mult), reads=[b_s], writes=[b_s])
            P.op("dve", ts(ovslot, ovslot, float(NE * CAP), None, ALU.add), reads=[b_s], writes=[b_s])
            P.op("dve", ts(t8, ok, -1.0e6, 1.0e6, ALU.mult, ALU.add), reads=[b_s], writes=[b_s])
            P.op("dve", tt(t8, t8, ovslot, ALU.add), reads=[b_s], writes=[b_s])
            di, bdi = di_[ti % 2]
            P.op("dve", cp(di, t8), reads=[b_s], writes=[bdi])
            P.op("dve", tt(ovslot, ovslot, ok, ALU.mult), reads=[b_s], writes=[b_s])
            P.op("dve", tt(t8, self.DestF[:, ti, :], ovslot, ALU.add), reads=[b_s, self.b_ov], writes=[b_s])
            P.op("dve", cp(self.destAll[:, ti, :], t8), reads=[b_s], writes=[self.b_dw])
            P.op("dve", tt(t8, self.W8raw[:, ti, :], ok, ALU.mult), reads=[b_s, self.b_ov], writes=[b_s])
            P.op("dve", tt(self.wAll[:, ti, :], self.wAll[:, ti, :], t8, ALU.add), reads=[b_s, self.b_dw], writes=[self.b_dw])
            for k in range(8):
                P.dma("pool", lambda e, k=k, di=di, hb=hb: e.indirect_dma_start(
                    out=self.XS_d, out_offset=bass.IndirectOffsetOnAxis(ap=di[:, k:k + 1], axis=0), in_=hb, in_offset=None,
                    bounds_check=self.bc_reg2, oob_is_err=False), reads=[bdi, bhb], writes=[self.b_XS])
        P.barrier()
        A.release()

    def experts(self):
        P, A = self.P, self.A
        self.dump("cnt", self.cnt, self.b_cnt, [128, NE])
        A.mark()
        xs4 = [(A.alloc([NBLK, D], BF16), Buf()) for _ in range(3)]
        xT = [(A.alloc([KD, CAP], BF16), Buf()) for _ in range(2)]
        wguf = [(A.alloc([KD, 512], F32), Buf()) for _ in range(3)]
        wdf = [(A.alloc([2, D], F32), Buf()) for _ in range(3)]
        wgub = [(A.alloc([KD, 512], BF16), Buf()) for _ in range(2)]
        wdb = [(A.alloc([2, D], BF16), Buf()) for _ in range(2)]
        sg = [(A.alloc([2, CAP], F32), Buf()) for _ in range(1)]
        hm = [(A.alloc([2, CAP], BF16), Buf()) for _ in range(2)]
        yo = [(A.alloc([D], BF16), Buf()) for _ in range(3)]
        self.b_Y = Buf("Y")

        def loads(e_):
            x4, bx4 = xs4[e_ % 3]
            P.dma("sp", dma(x4, self.XS_d[e_ * CAP:(e_ + 1) * CAP, :].rearrange("(s p) d -> p s d", p=128)), reads=[self.b_XS], writes=[bx4])
            wg, bwg = wguf[e_ % 3]
            wd, bwd = wdf[e_ % 3]
            P.dma("sp", dma(wg[:, :, 0:256], self.ewg_d[e_].rearrange("p (k f) -> p k f", k=KD)), writes=[bwg])
            P.dma("sp", dma(wg[:, :, 256:512], self.ewu_d[e_].rearrange("p (k f) -> p k f", k=KD)), writes=[bwg])
            P.dma("sp", dma(wd, self.ewd_d[e_].rearrange("p (k n) -> p k n", k=2)), writes=[bwd])

        def casts(e_):
            wg, bwg = wguf[e_ % 3]
            wd, bwd = wdf[e_ % 3]
            wgb, bwgb = wgub[e_ % 2]
            wdb_, bwdb = wdb[e_ % 2]
            P.op("dve", cp(wgb[:, 0:3, :], wg[:, 0:3, :]), reads=[bwg], writes=[bwgb])
            P.op("act", act(wgb[:, 3:6, :], wg[:, 3:6, :], AF.Copy), reads=[bwg], writes=[bwgb])
            P.op("pool", cp(wgb[:, 6:8, :], wg[:, 6:8, :]), reads=[bwg], writes=[bwgb])
            P.op("act", act(wdb_[:, 0, :], wd[:, 0, :], AF.Copy), reads=[bwd], writes=[bwdb])
            P.op("dve", cp(wdb_[:, 1, :], wd[:, 1, :]), reads=[bwd], writes=[bwdb])

        def transposes(e_):
            x4, bx4 = xs4[e_ % 3]
            xt_, bxt = xT[e_ % 2]
            for sb in range(NBLK):
                ps, pb = self.bank()
                psb = ps.bitcast(BF16)
                for k in range(KD):
                    P.op("pe", tr(psb[:, k * 128:(k + 1) * 128], x4[:, sb, k * 128:(k + 1) * 128], self.identb), reads=[bx4, self.b_cb], writes=[pb])
                src = psb.rearrange("p (k t) -> p k t", k=KD)
                P.op("dve", cp(xt_[:, :, sb * 128:(sb + 1) * 128], src), reads=[pb], writes=[bxt])

        loads(0)
        loads(1)
        casts(0)
        transposes(0)
        yc = 0
        for e_ in range(NE):
            if e_ + 2 < NE:
                loads(e_ + 2)
            if e_ + 1 < NE:
                casts(e_ + 1)
            wgb, bwgb = wgub[e_ % 2]
            wdb_, bwdb = wdb[e_ % 2]
            xt_, bxt = xT[e_ % 2]
            pg = [self.bank() for _ in range(4)]
            for fc in range(4):
                for k in range(KD):
                    P.op("pe", mm(pg[fc][0][:, 0:CAP], wgb[:, k, fc * 128:(fc + 1) * 128], xt_[:, k, :], k == 0, k == KD - 1),
                         reads=[bwgb, bxt], writes=[pg[fc][1]])
            sg_, bsg = sg[0]
            hm_, bhm = hm[e_ % 2]
            for fc in range(2):
                P.op("act", act(sg_[:, fc, :], pg[fc][0][:, 0:CAP], AF.Silu), reads=[pg[fc][1]], writes=[bsg])
                P.op("dve", tt(hm_[:, fc, :], sg_[:, fc, :], pg[2 + fc][0][:, 0:CAP], ALU.mult), reads=[bsg, pg[2 + fc][1]], writes=[bhm])
            if e_ + 1 < NE:
                transposes(e_ + 1)
            for sb in range(NBLK):
                pd = [self.bank(), self.bank()]
                for n in range(2):
                    for fc in range(2):
                        P.op("pe", mm(pd[n][0][:, :], hm_[:, fc, sb * 128:(sb + 1) * 128], wdb_[:, fc, n * 512:(n + 1) * 512], fc == 0, fc == 1),
                             reads=[bhm, bwdb], writes=[pd[n][1]])
                y_, by = yo[yc % 3]
                yc += 1
                P.op("act", act(y_[:, 0:512], pd[0][0][:, :], AF.Copy), reads=[pd[0][1]], writes=[by])
                P.op("act", act(y_[:, 512:1024], pd[1][0][:, :], AF.Copy), reads=[pd[1][1]], writes=[by])
                r0 = e_ * CAP + sb * 128
                P.dma("act", dma(self.Y_d[r0:r0 + 128, :], y_), reads=[by], writes=[self.b_Y])
        if OV > 0:
            ewg_rows = self.ewg_d.rearrange("e p n -> (e p) n")
            ewu_rows = self.ewu_d.rearrange("e p n -> (e p) n")
            ewd_rows = self.ewd_d.rearrange("e p n -> (e p) n")
            ovw = [(A.alloc([KD * 256], BF16), A.alloc([KD * 256], BF16), A.alloc([2 * D], BF16), Buf()) for _ in range(2)]

            def ovloads(j):
                x4, bx4 = xs4[j % 3]
                r0 = NE * CAP + j * 128
                P.dma("sp", dma(x4[:, 0, :], self.XS_d[r0:r0 + 128, :]), reads=[self.b_XS], writes=[bx4])
                og, ou, od, bw = ovw[j % 2]
                off = lambda j=j: bass.IndirectOffsetOnAxis(ap=self.IDXG[:, j:j + 1], axis=0)
                P.dma("pool", lambda e, og=og, off=off: e.indirect_dma_start(out=og, out_offset=None, in_=ewg_rows, in_offset=off()),
                      reads=[self.b_idx], writes=[bw])
                P.dma("pool", lambda e, ou=ou, off=off: e.indirect_dma_start(out=ou, out_offset=None, in_=ewu_rows, in_offset=off()),
                      reads=[self.b_idx], writes=[bw])
                P.dma("pool", lambda e, od=od, off=off: e.indirect_dma_start(out=od, out_offset=None, in_=ewd_rows, in_offset=off()),
                      reads=[self.b_idx], writes=[bw])

            ovloads(0)
            for j in range(OV):
                if j + 1 < OV:
                    ovloads(j + 1)
                x4, bx4 = xs4[j % 3]
                og, ou, od, bw = ovw[j % 2]
                og3 = og.rearrange("p (k f) -> p k f", k=KD)
                ou3 = ou.rearrange("p (k f) -> p k f", k=KD)
                od3 = od.rearrange("p (k n) -> p k n", k=2)
                xt_, bxt = xT[j % 2]
                ps, pb = self.bank()
                psb = ps.bitcast(BF16)
                for k in range(KD):
                    P.op("pe", tr(psb[:, k * 128:(k + 1) * 128], x4[:, 0, k * 128:(k + 1) * 128], self.identb), reads=[bx4, self.b_cb], writes=[pb])
                P.op("dve", cp(xt_[:, :, 0:128], psb.rearrange("p (k t) -> p k t", k=KD)), reads=[pb], writes=[bxt])
                pg, pbg = self.bank()
                for fc in range(4):
                    wsrc = og3 if fc < 2 else ou3
                    f0 = (fc % 2) * 128
                    for k in range(KD):
                        P.op("pe", mm(pg[:, fc * 128:(fc + 1) * 128], wsrc[:, k, f0:f0 + 128], xt_[:, k, 0:128], k == 0, k == KD - 1),
                             reads=[bw, bxt], writes=[pbg])
                sg_, bsg = sg[0]
                hm_, bhm = hm[j % 2]
                for fc in range(2):
                    P.op("act", act(sg_[:, fc, 0:128], pg[:, fc * 128:(fc + 1) * 128], AF.Silu), reads=[pbg], writes=[bsg])
                    P.op("dve", tt(hm_[:, fc, 0:128], sg_[:, fc, 0:128], pg[:, (2 + fc) * 128:(3 + fc) * 128], ALU.mult), reads=[bsg, pbg], writes=[bhm])
                pd = [self.bank(), self.bank()]
                for n in range(2):
                    for fc in range(2):
                        P.op("pe", mm(pd[n][0][:, :], hm_[:, fc, 0:128], od3[:, fc, n * 512:(n + 1) * 512], fc == 0, fc == 1),
                             reads=[bhm, bw], writes=[pd[n][1]])
                y_, by = yo[yc % 3]
                yc += 1
                P.op("act", act(y_[:, 0:512], pd[0][0][:, :], AF.Copy), reads=[pd[0][1]], writes=[by])
                P.op("act", act(y_[:, 512:1024], pd[1][0][:, :], AF.Copy), reads=[pd[1][1]], writes=[by])
                r0 = NE * CAP + j * 128
                P.dma("act", dma(self.Y_d[r0:r0 + 128, :], y_), reads=[by], writes=[self.b_Y])
        P.barrier()
        A.release()

    def combine(self):
        P, A, NB = self.P, self.A, self.NB
        A.mark()
        mt = self.MOD_d.tensor
        fing = A.alloc([D], F32)
        b_fg = Buf()
        P.dma("sp", dma(fing, bcast_rows(self.fing_d.tensor, 0, D)), writes=[b_fg])
        G2s = [(A.alloc([D], F32), Buf()) for _ in range(2)]
        base = [(A.alloc([D], F32), Buf()) for _ in range(2)]
        yk = [(A.alloc([D], BF16), Buf()) for _ in range(8)]
        acc = [(A.alloc([D], F32), Buf()) for _ in range(2)]
        junk = (A.alloc([D], F32), Buf())
        pre = [(A.alloc([D], F32), Buf()) for _ in range(2)]
        ssb = [(A.alloc([1], F32), Buf()) for _ in range(2)]
        ot = [(A.alloc([D], F32), Buf()) for _ in range(2)]
        for b in range(NB):
            G2, bG2 = G2s[b % 2]
            P.dma("sp", dma(G2, bcast_rows(mt, b * 6 * D + 5 * D, D)), reads=[self.b_MOD], writes=[bG2])
            for i in range(NLT):
                ti = b * NLT + i
                bs, bbs = base[i % 2]
                P.dma("sp", dma(bs, self.X1_d[ti * 128:(ti + 1) * 128, :]), reads=[self.b_X1], writes=[bbs])
                ac, bac = acc[i % 2]
                for k in range(8):
                    y_, by = yk[k]
                    P.dma("pool", lambda e, k=k, y_=y_, ti=ti: e.indirect_dma_start(
                        out=y_, out_offset=None, in_=self.Y_d, in_offset=bass.IndirectOffsetOnAxis(ap=self.destAll[:, ti, k:k + 1], axis=0)),
                        reads=[self.b_dw, self.b_Y], writes=[by])
                    if k == 0:
                        P.op("dve", ts(ac, y_, self.wAll[:, ti, 0:1], None, ALU.mult), reads=[by, self.b_dw], writes=[bac])
                    else:
                        P.op("dve", stt(ac, y_, self.wAll[:, ti, k:k + 1], ac, ALU.mult, ALU.add), reads=[by, self.b_dw, bac], writes=[bac])
                pa_, bpa = pre[0]
                pb_, bpb = pre[1]
                P.op("pool", tt(pa_, ac, G2, ALU.mult), reads=[bac, bG2], writes=[bpa])
                P.op("pool", tt(pb_, pa_, bs, ALU.add), reads=[bpa, bbs], writes=[bpb])
                ac, bac = pb_, bpb
                ss, bss = ssb[i % 2]
                P.op("act", act(junk[0], ac, AF.Square), reads=[bac], writes=[junk[1]])
                P.op("dve", rsum(ss, junk[0]), reads=[junk[1]], writes=[bss])
                P.op("act", act(ss, ss, AF.Sqrt, scale=1.0 / D, bias=self.eps_ap), reads=[bss, self.b_eps], writes=[bss])
                P.op("dve", lambda e, ss=ss: e.reciprocal(out=ss, in_=ss), reads=[bss], writes=[bss])
                o_, bo = ot[i % 2]
                P.op("dve", stt(o_, ac, ss, fing, ALU.mult, ALU.mult), reads=[bac, bss, b_fg], writes=[bo])
                P.dma("sp", dma(self.out_d[ti * 128:(ti + 1) * 128, :], o_), reads=[bo])
        A.release()


def const_tables():
    import math
    ident = np.eye(128, dtype=np.float32)
    J = ident[::-1].copy()
    s = np.arange(128)[:, None]
    c = np.arange(128)[None, :]
    same = (s // 64) == (c // 64)
    maskF = (same & (s <= c)).astype(np.float32)
    maskB = (same & (s >= c)).astype(np.float32)
    Sm = ((c == s + 1) & ((c % 64) != 0)).astype(np.float32)
    Sp = ((c == s - 1) & ((c % 64) != 63)).astype(np.float32)
    ustrict = (s < c).astype(np.float32)
    ones = np.ones((128, 128), np.float32)
    cst = np.stack([ident, J, maskF, maskB, Sm, Sp, ustrict, ones, Sm[:, ::-1], Sp[:, ::-1]], axis=1).astype(np.float32)
    f32 = np.float32
    pos = np.arange(L, dtype=f32)[:, None]
    t = pos / f32(L - 1)
    w = f32(2.0 * math.pi / L) * pos
    bands = np.linspace(1e-4, 15, 16, dtype=f32)[None, :]
    feats = np.concatenate([t, np.cos(bands * w), -np.sin(bands * w)], axis=-1).astype(f32)
    max_decay = math.log(1e-2) / 0.3
    min_decay = math.log(1e-2) / 1.5
    deltas = np.abs(np.linspace(min_decay, max_decay, 512, dtype=f32))[None, :].astype(f32)
    tfrac = (-(np.arange(L, dtype=f32) / f32(L - 1))).reshape(16, 128).T.copy()
    iota = np.arange(NE, dtype=f32)[None, :]
    pidx = np.arange(128, dtype=f32)[:, None].copy()
    return dict(cst=cst, featsT=np.ascontiguousarray(feats.T), deltas=deltas, tfrac=tfrac.astype(f32), iota=iota, pidx=pidx)


def prep_core(inp, core, NB, tables):
    f = lambda a: np.ascontiguousarray(a, dtype=np.float32)
    b0 = core * NB
    m = dict(tables)
    m["x"] = f(inp["x"][b0:b0 + NB].reshape(NB * L, D))
    m["ctx"] = f(inp["ctx"][b0:b0 + NB].reshape(NB * CTX, D))
    cc = np.concatenate([inp["c"][b0:b0 + NB], inp["c_ctx"][None, :]], axis=0)
    m["cT"] = f(cc.T.reshape(KD, 128, NB + 1).transpose(1, 0, 2))
    return m


def shared_inputs(inp):
    f = lambda a: np.ascontiguousarray(a, dtype=np.float32)
    m = {}
    m["w_mod"] = f(inp["w_mod"][0])
    m["b_mod"] = f(inp["b_mod"][0][None, :])
    m["norm1_g"] = f(inp["norm1_g"][0][None, :])
    m["norm2_g"] = f(inp["norm2_g"][0][None, :])
    m["final_g"] = f(inp["final_g"][None, :])
    m["w_in"] = f(inp["w_in"][0])
    m["w_out"] = f(inp["w_out"][0])
    m["hy_conv_w"] = f(inp["hy_conv_w"][0])
    m["hy_conv_b"] = f(inp["hy_conv_b"][0][None, :])
    m["hy_fw1"] = f(inp["hy_fw1"][0])
    m["hy_fb1"] = f(inp["hy_fb1"][0][:, None])
    m["hy_fw2"] = f(inp["hy_fw2"][0])
    m["hy_fb2"] = f(inp["hy_fb2"][0][:, None])
    m["hy_fw3"] = f(inp["hy_fw3"][0])
    m["hy_freq"] = f(inp["hy_freq"][0][:, None])
    m["hy_d"] = f(inp["hy_d"][0].reshape(1, 1024))
    lg = inp["hg_lb_logits"].reshape(2, 2, 4, 128)
    m["lbT"] = f(lg.transpose(3, 0, 1, 2).reshape(128, 16))
    m["hg_norm_g"] = f(inp["hg_norm_g"][0][None, :])
    m["w_router"] = f(inp["w_router"][0])
    m["router_bias"] = f(inp["router_bias"][0][None, :])
    m["ew_gate"] = f(np.asarray(inp["ew_gate"][0]).reshape(NE, KD, 128, 256).transpose(0, 2, 1, 3).reshape(NE, 128, KD * 256))
    m["ew_up"] = f(np.asarray(inp["ew_up"][0]).reshape(NE, KD, 128, 256).transpose(0, 2, 1, 3).reshape(NE, 128, KD * 256))
    m["ew_down"] = f(np.asarray(inp["ew_down"][0]).reshape(NE, 2, 128, D).transpose(0, 2, 1, 3).reshape(NE, 128, 2 * D))
    m["sw_gate"] = f(inp["sw_gate"][0])
    m["sw_up"] = f(inp["sw_up"][0])
    m["sw_down"] = f(inp["sw_down"][0])
    return m


def kernel(**inputs):
    NB = 4
    ncores = 8
    k = K(NB)
    nc = k.build()
    tables = const_tables()
    sh = shared_inputs(inputs)
    in_maps = []
    for c in range(ncores):
        m = prep_core(inputs, c, NB, tables)
        m.update(sh)
        in_maps.append(m)
    res = run_bass_kernel_spmd(nc, in_maps, core_ids=list(range(ncores)))
    outs = [np.asarray(r["out"], dtype=np.float32).reshape(NB, L, D) for r in res.results]
    return np.concatenate(outs, axis=0)
```

```python
import os
from contextlib import ExitStack
from concourse.bass_utils import run_bass_kernel_spmd
import numpy as np
import concourse.bass as bass
import concourse.mybir as mybir

F32 = mybir.dt.float32
BF16 = mybir.dt.bfloat16
I32 = mybir.dt.int32
U32 = mybir.dt.uint32
AF = mybir.ActivationFunctionType
ALU = mybir.AluOpType
AX = mybir.AxisListType


class Buf:
    __slots__ = ("name", "w", "r")

    def __init__(self, name=""):
        self.name = name
        self.w = None
        self.r = []


class Prog:
    COMPUTE = ("pe", "act", "dve", "pool")
    NDMA = {"sp": 10, "act": 4, "pool": 8}

    def __init__(self, nc, stack, same_engine_sync=True):
        self.nc = nc
        self.same = same_engine_sync
        self.ops = {e: [] for e in ("pe", "act", "dve", "pool", "sp")}
        self.sem = {}
        self.cnt = {}
        for e in self.COMPUTE:
            self.sem[e] = stack.enter_context(nc.semaphore("s_" + e))
            self.cnt[e] = 0
        self.dsem = {}
        self.dcnt = {}
        self.drr = {}
        for q, n in self.NDMA.items():
            for i in range(n):
                k = "d_%s_%d" % (q, i)
                self.sem[k] = stack.enter_context(nc.semaphore(k))
                self.cnt[k] = 0
            self.drr[q] = 0
        self.known = {e: {} for e in self.ops}
        self.nwaits = 0
        self.pre = {}

    def _deps(self, eng, reads, writes, is_dma=False):
        need = {}

        def add(tok):
            if tok is None:
                return
            k, v = tok
            if need.get(k, 0) < v:
                need[k] = v
        for b in reads:
            add(b.w)
        for b in writes:
            add(b.w)
            for t in b.r:
                add(t)
        waits = []
        kn = self.known[eng]
        for k, v in need.items():
            if k == eng and not self.same and not is_dma:
                continue
            if k == "pe" and eng == "pe":
                continue
            if kn.get(k, 0) >= v:
                continue
            kn[k] = v
            waits.append((k, v))
        self.nwaits += len(waits)
        return waits

    def _commit(self, tok, reads, writes):
        for b in reads:
            b.r.append(tok)
            if len(b.r) > 64:
                m = {}
                for k, v in b.r:
                    if m.get(k, 0) < v:
                        m[k] = v
                b.r = list(m.items())
        for b in writes:
            b.w = tok
            b.r = []

    def op(self, eng, fn, reads=(), writes=()):
        waits = self._deps(eng, reads, writes)
        self.cnt[eng] += 1
        tok = (eng, self.cnt[eng])
        self.ops[eng].append((waits, fn, (eng, 1)))
        self._commit(tok, reads, writes)
        return tok

    def dma(self, q, fn, reads=(), writes=()):
        n = self.NDMA[q]
        i = self.drr[q]
        self.drr[q] = (i + 1) % n
        k = "d_%s_%d" % (q, i)
        waits = self._deps(q, reads, writes, is_dma=True)
        prev = self.cnt[k]
        kn = self.known[q]
        if prev > 0 and kn.get(k, 0) < prev:
            kn[k] = prev
            waits.append((k, prev))
        self.cnt[k] += 16
        tok = (k, self.cnt[k])
        self.ops[q].append((waits, fn, (k, 16)))
        self._commit(tok, reads, writes)
        return tok

    def barrier_tokens(self):
        toks = []
        for k, v in self.cnt.items():
            if v > 0:
                toks.append((k, v))
        return toks

    def barrier(self):
        toks = self.barrier_tokens()
        for e in self.ops:
            kn = self.known[e]
            waits = []
            for k, v in toks:
                if k == e and e == "pe":
                    continue
                if kn.get(k, 0) < v:
                    kn[k] = v
                    waits.append((k, v))
            if waits:
                self.ops[e].append((waits, None, None))

    def final_wait(self, eng="sp"):
        toks = self.barrier_tokens()
        self.ops[eng].append(([(k, v) for k, v in toks], None, None))

    def emit(self):
        nc = self.nc
        engmap = {"pe": "tensor", "act": "scalar", "dve": "vector", "pool": "gpsimd", "sp": "sync"}
        with nc.Block() as block:
            for e, attr in engmap.items():
                lst = self.ops[e]

                def body(engine, lst=lst, e=e):
                    if e in self.pre:
                        self.pre[e](engine)
                    for waits, fn, inc in lst:
                        for k, v in waits:
                            engine.wait_ge(self.sem[k], v)
                        if fn is not None:
                            ins = fn(engine)
                            ins.then_inc(self.sem[inc[0]], inc[1])
                getattr(block, attr)(body)


class Arena:
    def __init__(self, nc, stack, nwords, name="arena"):
        self.t = stack.enter_context(nc.sbuf_tensor(name, [128, nwords], F32))
        self.n = nwords
        self.off = 0
        self.marks = []

    def alloc(self, shape, dtype, parts=128):
        n = int(np.prod(shape))
        if dtype == BF16:
            words = (n + 1) // 2
        else:
            words = n
        assert self.off + words <= self.n, "arena overflow %d + %d > %d" % (self.off, words, self.n)
        a = self.t[0:parts, self.off:self.off + words]
        self.off += words
        if dtype != F32:
            a = a.bitcast(dtype)
        if dtype == BF16 and n % 2 == 1:
            a = a[:, 0:n]
        if len(shape) > 1:
            names = " ".join("d%d" % i for i in range(len(shape)))
            kw = {"d%d" % i: int(s) for i, s in enumerate(shape)}
            a = a.rearrange("p (%s) -> p %s" % (names, names), **kw)
        return a

    def mark(self):
        self.marks.append(self.off)

    def release(self):
        self.off = self.marks.pop()

D = 1024
KD = 8
L = 2048
CTX = 256
T = L + CTX
NT = T // 128
NLT = L // 128
NCH = T // 64
HGS = 128.0 ** -0.5
EPS = 1e-6
NE = 256
CAP = int(os.environ.get('KCAP', '384'))
OV = 128
NBLK = CAP // 128
RSCALE = 2.5


def mm(out, lhsT, rhs, start, stop):
    return lambda e: e.matmul(out, lhsT, rhs, start=start, stop=stop)


def tr(out, in_, ident):
    return lambda e: e.transpose(out=out, in_=in_, identity=ident)


def act(out, in_, func, **kw):
    return lambda e: e.activation(out=out, in_=in_, func=func, **kw)


def tt(out, a, b, op):
    return lambda e: e.tensor_tensor(out=out, in0=a, in1=b, op=op)


def ts(out, a, s1, s2, op0, op1=None):
    if op1 is None:
        return lambda e: e.tensor_scalar(out=out, in0=a, scalar1=s1, scalar2=None, op0=op0)
    return lambda e: e.tensor_scalar(out=out, in0=a, scalar1=s1, scalar2=s2, op0=op0, op1=op1)


def stt(out, a, s, b, op0, op1):
    return lambda e: e.scalar_tensor_tensor(out=out, in0=a, scalar=s, in1=b, op0=op0, op1=op1)


def cp(out, in_):
    return lambda e: e.tensor_copy(out=out, in_=in_)


def rsum(out, in_):
    return lambda e: e.reduce_sum(out=out, in_=in_, axis=AX.X)


def dma(out, in_):
    return lambda e: e.dma_start(out=out, in_=in_)


def bcast_rows(dram_ap_tensor, offset, n, parts=128):
    return bass.AP(tensor=dram_ap_tensor, offset=offset, ap=[[0, parts], [1, n]])


class K:
    def __init__(self, NB, dbg=(), upto=4):
        self.NB = NB
        self.upto = upto
        self.cut = int(os.environ.get('KCUT', '0'))
        self.dbg = set(dbg)
        self.nc = bass.Bass("TRN2", target_bir_lowering=False)
        self.outs = []

    def din(self, name, shape, dtype=F32):
        return self.nc.dram_tensor(name, list(shape), dtype, kind="ExternalInput").ap()

    def dscr(self, name, shape, dtype):
        return self.nc.dram_tensor(name, list(shape), dtype, kind="Internal").ap()

    def dout(self, name, shape, dtype=F32):
        self.outs.append(name)
        return self.nc.dram_tensor(name, list(shape), dtype, kind="ExternalOutput").ap()

    def bank(self):
        self.bi = (self.bi + 1) % len(self.rot)
        return self.rot[self.bi]

    def reserve(self, n):
        got = [self.rot.pop() for _ in range(n)]
        self.bi = 0
        return got

    def unreserve(self, got):
        self.rot.extend(got)

    def dump(self, name, ap, buf, shape, dtype=F32):
        if name not in self.dbg:
            return
        o = self.dout("dbg_" + name, shape, dtype)
        self.P.dma("sp", dma(o, ap), reads=[buf])

    def build(self):
        nc = self.nc
        NB = self.NB
        with ExitStack() as st:
            self.st = st
            P = self.P = Prog(nc, st, same_engine_sync=(os.environ.get('KSAME', '1') == '1'))
            A = self.A = Arena(nc, st, 51500)
            self.banks = [(st.enter_context(nc.psum_tensor("pb%d" % i, [128, 512], F32))[:, :], Buf("pb%d" % i)) for i in range(8)]
            self.bi = 0
            self.rot = list(self.banks)
            self.declare_io()
            self.consts()
            self.modulation()
            if self.upto >= 2:
                self.filters()
            if self.upto >= 0.5:
                for b in range(NB):
                    self.mixer_batch(b)
            if self.upto >= 2:
                self.hyena_all()
            if self.upto >= 3:
                self.route_init()
                for b in range(NB):
                    self.outproj_route(b)
                self.overflow_route()
                P.barrier()
                self.A.release()
            if self.upto >= 4:
                self.experts()
                self.combine()
            P.final_wait("sp")
            P.emit()
        return nc

    def declare_io(self):
        NB = self.NB
        d = self.din
        self.x_d = d("x", [NB * L, D])
        self.ctx_d = d("ctx", [NB * CTX, D])
        self.cT_d = d("cT", [128, KD, NB + 1])
        self.wmod_d = d("w_mod", [D, 6 * D]).rearrange("(k p) n -> p k n", p=128)
        self.bmod_d = d("b_mod", [1, 6 * D])
        self.n1g_d = d("norm1_g", [1, D])
        self.n2g_d = d("norm2_g", [1, D])
        self.fing_d = d("final_g", [1, D])
        self.win_d = d("w_in", [D, 4096]).rearrange("(k p) n -> p k n", p=128)
        self.wout_d = d("w_out", [D, D]).rearrange("(k p) n -> p k n", p=128)
        self.hcw_d = d("hy_conv_w", [3, 1536])
        self.hcb_d = d("hy_conv_b", [1, 1536])
        self.fw1_d = d("hy_fw1", [33, 64])
        self.fb1_d = d("hy_fb1", [64, 1])
        self.fw2_d = d("hy_fw2", [64, 64])
        self.fb2_d = d("hy_fb2", [64, 1])
        self.fw3_d = d("hy_fw3", [64, 2048])
        self.freq_d = d("hy_freq", [64, 1])
        self.hyd_d = d("hy_d", [1, 1024])
        self.lbT_d = d("lbT", [128, 16])
        self.hgng_d = d("hg_norm_g", [1, 128])
        self.wr_d = d("w_router", [D, NE]).rearrange("(k p) n -> p k n", p=128)
        self.rb_d = d("router_bias", [1, NE])
        if self.upto >= 4:
            self.ewg_d = d("ew_gate", [NE, 128, KD * 256])
            self.ewu_d = d("ew_up", [NE, 128, KD * 256])
            self.ewd_d = d("ew_down", [NE, 128, 2 * D])
        self.swg_d = d("sw_gate", [D, 256]).rearrange("(k p) n -> p k n", p=128)
        self.swu_d = d("sw_up", [D, 256]).rearrange("(k p) n -> p k n", p=128)
        self.swd_d = d("sw_down", [256, D]).rearrange("(k p) n -> p k n", p=128)
        self.featsT_d = d("featsT", [33, L])
        self.cst_d = d("cst", [128, 10, 128])
        self.deltas_d = d("deltas", [1, 512])
        self.tfrac_d = d("tfrac", [128, 16])
        self.iota_d = d("iota", [1, NE])
        self.pidx_d = d("pidx", [128, 1])
        self.out_d = self.dout("out", [NB * L, D])
        self.MOD_d = self.dscr("MODs", [NB + 1, 6 * D], F32)
        self.HT_d = self.dscr("HTs", [NB, 128, KD * L], BF16)
        self.YT_d = self.dscr("YTs", [NB, 8, 128, L], BF16)
        self.G_d = self.dscr("Gs", [2, 512, 4096], BF16)
        self.X1_d = self.dscr("X1s", [NB * L, D], F32)
        self.XS_d = self.dscr("XSs", [NE * CAP + OV * 128, D], BF16)
        self.Y_d = self.dscr("Ys", [NE * CAP + OV * 128, D], BF16)
        self.H2_d = self.dscr("H2s", [NB * L, D], BF16)

    def consts(self):
        P, A = self.P, self.A
        cst = A.alloc([10, 128], F32)
        self.b_cst = Buf("cst")
        P.dma("sp", dma(cst, self.cst_d), writes=[self.b_cst])
        self.identf = cst[:, 0, :]
        self.Jf = cst[:, 1, :]
        self.maskF = cst[:, 2, :]
        self.maskB = cst[:, 3, :]
        self.ustrict_f = cst[:, 6, :]
        self.ones_f = cst[:, 7, :]
        cb = A.alloc([10, 128], BF16)
        self.b_cb = Buf("cb")
        P.op("dve", cp(cb, cst), reads=[self.b_cst], writes=[self.b_cb])
        self.identb = cb[:, 0, :]
        self.Jb = cb[:, 1, :]
        self.Smb = cb[:, 4, :]
        self.Spb = cb[:, 5, :]
        self.ustrict_b = cb[:, 6, :]
        self.ones_b = cb[:, 7, :]
        self.SmRb = cb[:, 8, :]
        self.SpRb = cb[:, 9, :]
        ce = A.alloc([2], F32)
        self.b_eps = Buf("eps")
        P.op("pool", lambda e: e.memset(ce[:, 0:1], EPS), writes=[self.b_eps])
        P.op("pool", lambda e: e.memset(ce[:, 1:2], 1.0), writes=[self.b_eps])
        self.eps_ap = ce[:, 0:1]
        self.one_ap = ce[:, 1:2]
        self.n1g = A.alloc([D], F32)
        self.b_n1g = Buf()
        P.dma("sp", dma(self.n1g, bcast_rows(self.n1g_d.tensor, 0, D)), writes=[self.b_n1g])
        self.hgng = A.alloc([128], F32)
        self.b_hgng = Buf()
        P.dma("sp", dma(self.hgng, bcast_rows(self.hgng_d.tensor, 0, 128)), writes=[self.b_hgng])
        lbt = A.alloc([2, 2, 4], F32)
        b_lbt = Buf()
        P.dma("sp", dma(lbt, self.lbT_d.rearrange("p (a b c) -> p a b c", a=2, b=2)), writes=[b_lbt])
        self.lb = A.alloc([2, 4], F32)
        self.oml = A.alloc([2, 4], F32)
        self.noml = A.alloc([2, 4], F32)
        self.b_lb = Buf()
        P.op("dve", tt(self.lb, lbt[:, :, 0, :], lbt[:, :, 1, :], ALU.subtract), reads=[b_lbt], writes=[self.b_lb])
        P.op("act", act(self.lb, self.lb, AF.Sigmoid), reads=[self.b_lb], writes=[self.b_lb])
        P.op("dve", ts(self.oml, self.lb, -1.0, 1.0, ALU.mult, ALU.add), reads=[self.b_lb], writes=[self.b_lb])
        P.op("dve", ts(self.noml, self.lb, -1.0, None, ALU.add), reads=[self.b_lb], writes=[self.b_lb])

    def modulation(self):
        P, A, NB = self.P, self.A, self.NB
        A.mark()
        cT = A.alloc([KD, NB + 1], F32)
        b_cT = Buf()
        P.dma("sp", dma(cT, self.cT_d), writes=[b_cT])
        P.op("act", act(cT, cT, AF.Silu), reads=[b_cT], writes=[b_cT])
        bm = A.alloc([6 * D], F32, parts=1)
        b_bm = Buf()
        P.dma("sp", dma(bm, self.bmod_d), writes=[b_bm])
        modsb = A.alloc([6 * D], F32)
        b_mod = Buf()
        wms = [(A.alloc([KD, 512], F32), Buf()) for _ in range(2)]
        for ci in range(12):
            wm, bw = wms[ci % 2]
            P.dma("sp" if ci % 2 == 0 else "act", dma(wm, self.wmod_d[:, :, ci * 512:(ci + 1) * 512]), writes=[bw])
            ps, pb = self.bank()
            for k in range(KD):
                P.op("pe", mm(ps[0:NB + 1, :], cT[:, k, :], wm[:, k, :], k == 0, False), reads=[b_cT, bw], writes=[pb])
            P.op("pe", mm(ps[0:NB + 1, :], self.ones_f[0:1, 0:NB + 1], bm[0:1, ci * 512:(ci + 1) * 512], False, True),
                 reads=[self.b_cst, b_bm], writes=[pb])
            P.op("act", act(modsb[0:NB + 1, ci * 512:(ci + 1) * 512], ps[0:NB + 1, :], AF.Copy), reads=[pb], writes=[b_mod])
        self.b_MOD = Buf("MOD")
        P.dma("sp", dma(self.MOD_d, modsb[0:NB + 1, :]), reads=[b_mod], writes=[self.b_MOD])
        self.dump("mod", modsb[0:NB + 1, :], b_mod, [NB + 1, 6 * D])
        P.barrier()
        A.release()
        self.CA = A.alloc([D], F32)
        self.CB = A.alloc([D], F32)
        self.b_CA = Buf()
        self.b_CB = Buf()
        mt = self.MOD_d.tensor
        P.dma("sp", dma(self.CB, bcast_rows(mt, NB * 6 * D + 0 * D, D)), reads=[self.b_MOD], writes=[self.b_CB])
        P.dma("sp", dma(self.CA, bcast_rows(mt, NB * 6 * D + 1 * D, D)), reads=[self.b_MOD], writes=[self.b_CA])
        P.op("dve", stt(self.CA, self.CA, 1.0, self.n1g, ALU.add, ALU.mult), reads=[self.b_CA, self.b_n1g], writes=[self.b_CA])

    def norm_mod_tile(self, i, src, Abc, bA, Bbc, bB, dst, b_dst, hb_out=None):
        P = self.P
        xt, bx = self.xt[i % 2]
        P.dma("sp", dma(xt, src), writes=[bx])
        self.norm_mod_sb(i, xt, bx, Abc, bA, Bbc, bB, dst, b_dst)

    def norm_mod_sb(self, i, xt, bx, Abc, bA, Bbc, bB, dst, b_dst, hf_out=None, hb_out=None):
        P = self.P
        junk, bj = self.junk
        ss, bss = self.ssb[i % 2]
        hb, bhb = (self.hb[i % 2] if hb_out is None else hb_out)
        P.op("act", act(junk, xt, AF.Square), reads=[bx], writes=[bj])
        P.op("dve", rsum(ss, junk), reads=[bj], writes=[bss])
        P.op("act", act(ss, ss, AF.Sqrt, scale=1.0 / D, bias=self.eps_ap), reads=[bss, self.b_eps], writes=[bss])
        P.op("dve", lambda e: e.reciprocal(out=ss, in_=ss), reads=[bss], writes=[bss])
        P.op("dve", stt(junk, xt, ss, Abc, ALU.mult, ALU.mult), reads=[bx, bss, bA], writes=[bj])
        if hf_out is not None:
            hf, bhf = hf_out
            P.op("dve", tt(hf, junk, Bbc, ALU.add), reads=[bj, bB], writes=[bhf])
            P.op("act", act(hb, hf, AF.Copy), reads=[bhf], writes=[bhb])
        else:
            P.op("dve", tt(hb, junk, Bbc, ALU.add), reads=[bj, bB], writes=[bhb])
        ps, pb = self.bank()
        psb = ps.bitcast(BF16)
        for k in range(KD):
            P.op("pe", tr(psb[:, k * 128:(k + 1) * 128], hb[:, k * 128:(k + 1) * 128], self.identb),
                 reads=[bhb, self.b_cb], writes=[pb])
        P.op("act", act(dst, psb.rearrange("p (k t) -> p k t", k=KD), AF.Copy), reads=[pb], writes=[b_dst])

    def mixer_batch(self, b):
        P, A, NB = self.P, self.A, self.NB
        A.mark()
        mt = self.MOD_d.tensor
        A1 = A.alloc([D], F32)
        B1 = A.alloc([D], F32)
        bA1, bB1 = Buf(), Buf()
        P.dma("sp", dma(B1, bcast_rows(mt, b * 6 * D + 0 * D, D)), reads=[self.b_MOD], writes=[bB1])
        P.dma("sp", dma(A1, bcast_rows(mt, b * 6 * D + 1 * D, D)), reads=[self.b_MOD], writes=[bA1])
        P.op("dve", stt(A1, A1, 1.0, self.n1g, ALU.add, ALU.mult), reads=[bA1, self.b_n1g], writes=[bA1])
        hT = A.alloc([KD, T], BF16)
        b_hT = Buf("hT")
        yT = A.alloc([4, L], BF16)
        b_yT = Buf("yT")
        A.mark()
        self.xt = [(A.alloc([D], F32), Buf()) for _ in range(2)]
        self.junk = (A.alloc([D], F32), Buf())
        self.ssb = [(A.alloc([1], F32), Buf()) for _ in range(2)]
        self.hb = [(A.alloc([D], BF16), Buf()) for _ in range(2)]
        for j in range(NT):
            if j < 2:
                src = self.ctx_d[b * CTX + j * 128: b * CTX + (j + 1) * 128, :]
                self.norm_mod_tile(j, src, self.CA, self.b_CA, self.CB, self.b_CB, hT[:, :, j * 128:(j + 1) * 128], b_hT)
            else:
                src = self.x_d[b * L + (j - 2) * 128: b * L + (j - 1) * 128, :]
                self.norm_mod_tile(j, src, A1, bA1, B1, bB1, hT[:, :, j * 128:(j + 1) * 128], b_hT)
        self.b_HT = getattr(self, "b_HT", None) or Buf("HT")
        P.dma("sp", dma(self.HT_d[b].rearrange("p (k t) -> p k t", k=KD), hT[:, :, CTX:T]), reads=[b_hT], writes=[self.b_HT])
        if b == 0:
            self.dump("hT", hT, b_hT, [128, KD, T], BF16)
        P.barrier()
        A.release()
        if self.upto < 1:
            A.release()
            return
        self.hgrn2(b, hT, b_hT, yT, b_yT)
        self.b_YT = getattr(self, "b_YT", None) or Buf("YT")
        for hh in range(4):
            P.dma("sp", dma(self.YT_d[b, 4 + hh], yT[:, hh, :]), reads=[b_yT], writes=[self.b_YT])
        if b == 0:
            self.dump("yT_hg", yT, b_yT, [128, 4, L], BF16)
        P.barrier()
        A.release()

    def hgrn2(self, b, hT, b_hT, yT, b_yT):
        P, A = self.P, self.A
        A.mark()
        f32b = lambda: (A.alloc([T], F32), Buf())
        bf16b = lambda: (A.alloc([T], BF16), Buf())
        self.rs = A.alloc([T], F32)
        self.b_rs = Buf()
        P.op("pool", lambda e: e.memset(self.rs, 1.0), writes=[self.b_rs])
        rs3 = self.rs.rearrange("p (a b) -> p a b", b=64)
        P.op("pool", lambda e: e.memset(rs3[:, :, 0:1], 0.0), writes=[self.b_rs])
        t1, b_t1 = f32b()
        kk, b_kk = f32b()
        bb, b_bb = f32b()
        t2, b_t2 = f32b()
        qs, b_qs = bf16b()
        qm, b_qm = bf16b()
        km, b_km = bf16b()
        qbE, b_qbE = bf16b()
        qbO, b_qbO = bf16b()
        kdT, b_kdT = bf16b()
        P.op("pool", lambda e: e.memset(qbE, 0.0), writes=[b_qbE])
        P.op("pool", lambda e: e.memset(qbO, 0.0), writes=[b_qbO])
        kdTokE = A.alloc([NT, 128], BF16)
        kdTokO = A.alloc([NT, 128], BF16)
        b_kdTok = Buf()
        P.op("pool", lambda e: e.memset(kdTokE[64:128], 0.0), writes=[b_kdTok])
        P.op("pool", lambda e: e.memset(kdTokO[0:64], 0.0), writes=[b_kdTok])
        V = A.alloc([NT, 128], BF16)
        b_V = Buf()
        gsn = A.alloc([NLT, 128], BF16)
        b_gsn = Buf()
        of = A.alloc([NLT, 128], F32)
        b_of = Buf()
        dec = A.alloc([NCH], F32)
        b_dec = Buf()
        ws = [(A.alloc([KD, 128], BF16), Buf()) for _ in range(5)]
        Sf = [(A.alloc([128], F32), Buf()) for _ in range(2)]
        Sb = [(A.alloc([128], BF16), Buf()) for _ in range(2)]
        attT = [(A.alloc([128], BF16), Buf()) for _ in range(2)]
        otl = [(A.alloc([128], F32), Buf()) for _ in range(2)]
        oj = [(A.alloc([128], F32), Buf()) for _ in range(2)]
        ossb = [(A.alloc([1], F32), Buf()) for _ in range(2)]
        ytl = [(A.alloc([128], BF16), Buf()) for _ in range(2)]
        gtmp = [(A.alloc([128], F32), Buf()) for _ in range(2)]
        chunks = [(t0, min(512, T - t0)) for t0 in range(0, T, 512)]
        v3 = lambda ap: ap.rearrange("p (a b) -> p a b", b=64)
        for hh in range(4):
            cols = [1536 + s * 512 + hh * 128 for s in range(5)]
            for s in range(5):
                w, bw = ws[s]
                P.dma("pool", dma(w, self.win_d[:, :, cols[s]:cols[s] + 128]), writes=[bw])
            wq, wf, wb_, wi, wg = ws
            for j in range(NT):
                ps, pb = self.bank()
                for k in range(KD):
                    P.op("pe", mm(ps[:, 0:128], hT[:, k, j * 128:(j + 1) * 128], wi[0][:, k, :], k == 0, k == KD - 1),
                         reads=[b_hT, wi[1]], writes=[pb])
                P.op("act", act(V[:, j, :], ps[:, 0:128], AF.Copy), reads=[pb], writes=[b_V])
                if j >= 2:
                    ps2, pb2 = self.bank()
                    for k in range(KD):
                        P.op("pe", mm(ps2[:, 0:128], hT[:, k, j * 128:(j + 1) * 128], wg[0][:, k, :], k == 0, k == KD - 1),
                             reads=[b_hT, wg[1]], writes=[pb2])
                    gt, bgt = gtmp[j % 2]
                    P.op("act", act(gt, ps2[:, 0:128], AF.Silu), reads=[pb2], writes=[bgt])
                    P.op("dve", tt(gsn[:, j - 2, :], gt, self.hgng, ALU.mult), reads=[bgt, self.b_hgng], writes=[b_gsn])
            for (t0, n) in chunks:
                ps, pb = self.bank()
                for k in range(KD):
                    P.op("pe", mm(ps[:, 0:n], wq[0][:, k, :], hT[:, k, t0:t0 + n], k == 0, k == KD - 1),
                         reads=[b_hT, wq[1]], writes=[pb])
                P.op("act", act(qs[:, t0:t0 + n], ps[:, 0:n], AF.Silu), reads=[pb], writes=[b_qs])
            for d in range(2):
                wgate = wf if d == 0 else wb_
                for (t0, n) in chunks:
                    ps, pb = self.bank()
                    for k in range(KD):
                        P.op("pe", mm(ps[:, 0:n], wgate[0][:, k, :], hT[:, k, t0:t0 + n], k == 0, k == KD - 1),
                             reads=[b_hT, wgate[1]], writes=[pb])
                    P.op("act", act(t1[:, t0:t0 + n], ps[:, 0:n], AF.Sigmoid), reads=[pb], writes=[b_t1])
                P.op("dve", ts(kk, t1, self.noml[:, d, hh:hh + 1], self.oml[:, d, hh:hh + 1], ALU.mult, ALU.add),
                     reads=[b_t1, self.b_lb], writes=[b_kk])
                P.op("act", act(t1, kk, AF.Ln, scale=-1.0, bias=self.one_ap), reads=[b_kk, self.b_eps], writes=[b_t1])
                P.op("dve", lambda e: e.tensor_tensor_scan(out=bb, data0=self.rs, data1=t1, initial=0.0, op0=ALU.mult, op1=ALU.add),
                     reads=[self.b_rs, b_t1], writes=[b_bb])
                bb3, t13 = v3(bb), v3(t1)
                if d == 1:
                    P.op("dve", tt(t1, t1, bb, ALU.subtract), reads=[b_t1, b_bb], writes=[b_t1])
                    P.op("dve", tt(bb3, t13, bb3[:, :, 63:64].to_broadcast([128, NCH, 64]), ALU.add), reads=[b_t1, b_bb], writes=[b_bb])
                mid = 31 if d == 0 else 32
                last = 63 if d == 0 else 0
                P.op("dve", tt(t13, bb3, bb3[:, :, mid:mid + 1].to_broadcast([128, NCH, 64]), ALU.subtract), reads=[b_bb], writes=[b_t1])
                P.op("act", act(t2, t1, AF.Exp), reads=[b_t1], writes=[b_t2])
                P.op("dve", stt(qm, qs, HGS, t2, ALU.mult, ALU.mult), reads=[b_qs, b_t2], writes=[b_qm])
                P.op("act", act(t2, t1, AF.Exp, scale=-1.0), reads=[b_t1, b_qm], writes=[b_t2])
                P.op("dve", tt(km, kk, t2, ALU.mult), reads=[b_kk, b_t2], writes=[b_km])
                P.op("act", act(t2, bb, AF.Exp), reads=[b_bb, b_km], writes=[b_t2])
                t23, qs3, qbE3, qbO3 = v3(t2), v3(qs), v3(qbE), v3(qbO)
                P.op("dve", stt(qbE3[:, 0::2, :], qs3[:, 0::2, :], HGS, t23[:, 0::2, :], ALU.mult, ALU.mult),
                     reads=[b_qs, b_t2], writes=[b_qbE])
                P.op("dve", stt(qbO3[:, 1::2, :], qs3[:, 1::2, :], HGS, t23[:, 1::2, :], ALU.mult, ALU.mult),
                     reads=[b_qs, b_t2], writes=[b_qbO])
                P.op("dve", tt(t13, bb3[:, :, last:last + 1].to_broadcast([128, NCH, 64]), bb3, ALU.subtract), reads=[b_bb, b_t2], writes=[b_t1])
                P.op("act", act(t1, t1, AF.Exp), reads=[b_t1], writes=[b_t1])
                P.op("dve", tt(kdT, kk, t1, ALU.mult), reads=[b_kk, b_t1], writes=[b_kdT])
                P.op("act", act(dec, bb3[:, :, last], AF.Exp), reads=[b_bb], writes=[b_dec])
                for j0 in range(0, NT, 8):
                    nj = min(8, NT - j0)
                    ps, pb = self.bank()
                    psb = ps.bitcast(BF16)
                    for jj in range(nj):
                        j = j0 + jj
                        P.op("pe", tr(psb[:, jj * 128:(jj + 1) * 128], kdT[:, j * 128:(j + 1) * 128], self.identb),
                             reads=[b_kdT, self.b_cb], writes=[pb])
                    P.op("act", act(kdTokE[0:64, j0:j0 + nj, :], psb[0:64, 0:nj * 128].rearrange("p (a b) -> p a b", b=128), AF.Copy),
                         reads=[pb], writes=[b_kdTok])
                    P.op("act", act(kdTokO[64:128, j0:j0 + nj, :], psb[64:128, 0:nj * 128].rearrange("p (a b) -> p a b", b=128), AF.Copy),
                         reads=[pb], writes=[b_kdTok])
                si = 0
                P.op("pool", lambda e, s=Sf[0][0]: e.memset(s, 0.0), writes=[Sf[0][1]])
                P.op("pool", lambda e, s=Sb[0][0]: e.memset(s, 0.0), writes=[Sb[0][1]])
                order = list(range(NT)) if d == 0 else [1, 0] + list(range(NT - 1, 1, -1))
                mask = self.maskF if d == 0 else self.maskB
                for j in order:
                    lat = j >= 2
                    tsl = slice(j * 128, (j + 1) * 128)
                    halves = [0, 1] if d == 0 else [1, 0]
                    if lat:
                        psA, pbA = self.bank()
                        P.op("pe", mm(psA[:, 0:128], km[:, tsl], qm[:, tsl], True, True), reads=[b_km, b_qm], writes=[pbA])
                        at, bat = attT[j % 2]
                        P.op("dve", tt(at, psA[:, 0:128], mask, ALU.mult), reads=[pbA, self.b_cst], writes=[bat])
                        psO, pbO = self.bank()
                        P.op("pe", mm(psO[:, 0:128], at, V[:, j, :], True, False), reads=[bat, b_V], writes=[pbO])
                    for hi, h in enumerate(halves):
                        if lat:
                            qbx, b_qbx = (qbE, b_qbE) if h == 0 else (qbO, b_qbO)
                            P.op("pe", mm(psO[:, 0:128], qbx[:, tsl], Sb[si][0], False, hi == 1),
                                 reads=[b_qbx, Sb[si][1]], writes=[pbO])
                        psU, pbU = self.bank()
                        kdx = kdTokE if h == 0 else kdTokO
                        P.op("pe", mm(psU[:, 0:128], kdx[:, j, :], V[:, j, :], True, True), reads=[b_kdTok, b_V], writes=[pbU])
                        ch = 2 * j + h
                        P.op("dve", stt(Sb[1 - si][0], Sf[si][0], dec[:, ch:ch + 1], psU[:, 0:128], ALU.mult, ALU.add),
                             reads=[Sf[si][1], b_dec, pbU], writes=[Sb[1 - si][1]])
                        P.op("dve", stt(Sf[1 - si][0], Sf[si][0], dec[:, ch:ch + 1], psU[:, 0:128], ALU.mult, ALU.add),
                             reads=[Sf[si][1], b_dec, pbU], writes=[Sf[1 - si][1]])
                        si = 1 - si
                    if lat:
                        if d == 0:
                            P.op("act", act(of[:, j - 2, :], psO[:, 0:128], AF.Copy), reads=[pbO], writes=[b_of])
                        else:
                            o, bo = oj[j % 2]
                            P.op("dve", tt(o, psO[:, 0:128], of[:, j - 2, :], ALU.add), reads=[pbO, b_of], writes=[bo])
                            jk, bjk = otl[j % 2]
                            oss, boss = ossb[j % 2]
                            P.op("act", act(jk, o, AF.Square), reads=[bo], writes=[bjk])
                            P.op("dve", rsum(oss, jk), reads=[bjk], writes=[boss])
                            P.op("act", act(oss, oss, AF.Sqrt, scale=1.0 / 128, bias=self.eps_ap), reads=[boss, self.b_eps], writes=[boss])
                            P.op("dve", lambda e, oss=oss: e.reciprocal(out=oss, in_=oss), reads=[boss], writes=[boss])
                            yt, byt = ytl[j % 2]
                            P.op("dve", stt(yt, o, oss, gsn[:, j - 2, :], ALU.mult, ALU.mult), reads=[bo, boss, b_gsn], writes=[byt])
                            psT, pbT = self.bank()
                            psTb = psT.bitcast(BF16)
                            P.op("pe", tr(psTb[:, 0:128], yt, self.identb), reads=[byt, self.b_cb], writes=[pbT])
                            P.op("act", act(yT[:, hh, (j - 2) * 128:(j - 1) * 128], psTb[:, 0:128], AF.Copy), reads=[pbT], writes=[b_yT])
        P.barrier()
        A.release()

    def sin_da(self, out, arg, tmpa, tmpb, bufs):
        P = self.P
        b_out, b_arg, b_ta, b_tb = bufs
        P.op("act", act(tmpa, arg, AF.Sin, scale=0.5), reads=[b_arg], writes=[b_ta])
        P.op("act", act(tmpb, arg, AF.Sin, scale=0.25), reads=[b_arg], writes=[b_tb])
        P.op("dve", tt(tmpb, tmpb, tmpb, ALU.mult), reads=[b_tb], writes=[b_tb])
        P.op("dve", ts(tmpb, tmpb, -2.0, 1.0, ALU.mult, ALU.add), reads=[b_tb], writes=[b_tb])
        P.op("dve", stt(out, tmpa, 2.0, tmpb, ALU.mult, ALU.mult), reads=[b_ta, b_tb], writes=[b_out])

    def filters(self):
        P, A = self.P, self.A
        A.mark()
        ld = lambda shape, src, parts: (A.alloc(shape, F32, parts=parts), Buf())
        featsT, b_ft = ld([L], None, 33)
        P.dma("sp", dma(featsT, self.featsT_d), writes=[b_ft])
        fw1, b_fw1 = ld([64], None, 33)
        P.dma("sp", dma(fw1, self.fw1_d), writes=[b_fw1])
        fw2, b_fw2 = ld([64], None, 64)
        P.dma("sp", dma(fw2, self.fw2_d), writes=[b_fw2])
        fw3, b_fw3 = ld([2048], None, 64)
        P.dma("sp", dma(fw3, self.fw3_d), writes=[b_fw3])
        sm, b_sm = ld([8], None, 64)
        P.dma("sp", dma(sm[:, 0:1], self.fb1_d), writes=[b_sm])
        P.dma("sp", dma(sm[:, 1:2], self.fb2_d), writes=[b_sm])
        P.dma("sp", dma(sm[:, 2:3], self.freq_d), writes=[b_sm])
        P.op("dve", ts(sm[:, 3:5], sm[:, 0:2], sm[:, 2:3], None, ALU.mult), reads=[b_sm], writes=[b_sm])
        dl, b_dl = ld([512], None, 128)
        P.dma("sp", dma(dl, bcast_rows(self.deltas_d.tensor, 0, 512)), writes=[b_dl])
        tf, b_tf = ld([16], None, 128)
        P.dma("sp", dma(tf, self.tfrac_d), writes=[b_tf])
        dsk, b_dsk = ld([2, 4], None, 128)
        P.dma("sp", lambda e: e.dma_start(out=dsk, in_=self.hyd_d.rearrange("a (o g c) -> c (a o) g", o=2, g=4),
                                          allow_slow_non_contiguous=True), writes=[b_dsk])
        h1, b_h1 = ld([L], None, 64)
        h2, b_h2 = ld([L], None, 64)
        ta, b_ta = ld([L], None, 64)
        tb, b_tb = ld([L], None, 64)
        ar, b_ar = ld([L], None, 64)
        for layer in range(2):
            src, b_src, K_, w, b_w = (featsT, b_ft, 33, fw1, b_fw1) if layer == 0 else (h1, b_h1, 64, fw2, b_fw2)
            for q in range(4):
                ps, pb = self.bank()
                P.op("pe", mm(ps[0:64, :], w[0:K_, :], src[0:K_, q * 512:(q + 1) * 512], True, True), reads=[b_w, b_src], writes=[pb])
                P.op("act", act(ar[:, q * 512:(q + 1) * 512], ps[0:64, :], AF.Identity, scale=sm[:, 2:3], bias=sm[:, 3 + layer:4 + layer]),
                     reads=[pb, b_sm], writes=[b_ar])
            dst, b_dst = (h1, b_h1) if layer == 0 else (h2, b_h2)
            self.sin_da(dst, ar, ta, tb, (b_dst, b_ar, b_ta, b_tb))
        rinv = A.alloc([2, 512], F32)
        b_rinv = Buf()
        win = [(A.alloc([512], F32), Buf()) for _ in range(2)]
        winr = [(A.alloc([2, 512], F32), Buf()) for _ in range(2)]
        hw = [(A.alloc([512], F32), Buf()) for _ in range(2)]
        hn = [(A.alloc([512], BF16), Buf()) for _ in range(2)]
        GT = A.alloc([4, 2, 4096], BF16)
        b_GT = Buf("GT")
        P.op("pool", lambda e: e.memset(GT, 0.0), writes=[b_GT])
        accs = self.reserve(2)
        for i in range(16):
            wi_, bwi = win[i % 2]
            P.op("act", act(wi_, dl, AF.Exp, scale=tf[:, i:i + 1]), reads=[b_dl, b_tf], writes=[bwi])
            for o in range(2):
                for dr in range(2):
                    q = o * 2 + dr
                    ps, pb = self.bank()
                    P.op("pe", mm(ps[:, :], h2[0:64, i * 128:(i + 1) * 128], fw3[0:64, q * 512:(q + 1) * 512], True, True),
                         reads=[b_h2, b_fw3], writes=[pb])
                    hwt, bhw = hw[q % 2]
                    P.op("dve", tt(hwt, ps, wi_, ALU.mult), reads=[pb, bwi], writes=[bhw])
                    P.op("act", act(hwt, hwt, AF.Abs), reads=[bhw], writes=[bhw])
                    first = (i == 0 and dr == 0)
                    lastf = (i == 15 and dr == 1)
                    P.op("pe", mm(accs[o][0][:, :], self.ones_f, hwt, first, lastf), reads=[self.b_cst, bhw], writes=[accs[o][1]])
        for o in range(2):
            P.op("dve", lambda e, o=o: e.reciprocal(out=rinv[:, o, :], in_=accs[o][0][:, :]), reads=[accs[o][1]], writes=[b_rinv])
        self.unreserve(accs)
        GTv = GT
        for i in range(16):
            wi_, bwi = win[i % 2]
            wr, bwr = winr[i % 2]
            P.op("act", act(wi_, dl, AF.Exp, scale=tf[:, i:i + 1]), reads=[b_dl, b_tf], writes=[bwi])
            P.op("dve", tt(wr, rinv, wi_.unsqueeze(1).to_broadcast([128, 2, 512]), ALU.mult), reads=[bwi, b_rinv], writes=[bwr])
            for o in range(2):
                for dr in ([1, 0] if o == 0 else [0, 1]):
                    q = o * 2 + dr
                    useJ = (o == 0 and dr == 1) or (o == 1 and dr == 0)
                    ps, pb = self.bank()
                    P.op("pe", mm(ps[:, :], h2[0:64, i * 128:(i + 1) * 128], fw3[0:64, q * 512:(q + 1) * 512], True, True),
                         reads=[b_h2, b_fw3], writes=[pb])
                    hnt, bhn = hn[q % 2]
                    P.op("dve", tt(hnt, ps, wr[:, o, :], ALU.mult), reads=[pb, bwr], writes=[bhn])
                    pt, pbt = self.bank()
                    for cg in range(4):
                        P.op("pe", mm(pt[:, cg * 128:(cg + 1) * 128], hnt[:, cg * 128:(cg + 1) * 128], self.Jb if useJ else self.identb, True, True),
                             reads=[bhn, self.b_cb], writes=[pbt])
                    pt3 = pt.rearrange("p (g m) -> p g m", g=4)
                    if o == 0:
                        start = (1921 - 128 * i) if useJ else (2048 + 128 * i)
                    else:
                        start = (1920 - 128 * i) if useJ else (2047 + 128 * i)
                    if i == 0 and not useJ:
                        P.op("act", act(GTv[:, :, o, start + 1:start + 128], pt3[:, :, 1:128], AF.Copy), reads=[pbt], writes=[b_GT])
                        ctr, b_ctr = self.ctr_tmp = getattr(self, "ctr_tmp", None) or (A.alloc([4], F32), Buf())
                        P.op("dve", tt(ctr, pt3[:, :, 0], dsk[:, o, :], ALU.add), reads=[pbt, b_dsk], writes=[b_ctr])
                        P.op("dve", tt(GTv[:, :, o, start], GTv[:, :, o, start], ctr, ALU.add), reads=[b_ctr, b_GT], writes=[b_GT])
                    else:
                        P.op("act", act(GTv[:, :, o, start:start + 128], pt3, AF.Copy), reads=[pbt], writes=[b_GT])
        self.b_G = Buf("G")
        for o in range(2):
            for cg in range(4):
                P.dma("sp", dma(self.G_d[o, cg * 128:(cg + 1) * 128, :], GTv[:, cg, o, :]), reads=[b_GT], writes=[self.b_G])
        if "G" in self.dbg:
            for o in range(2):
                og = self.dout("dbg_G%d" % o, [128, 4, 4096], BF16)
                P.dma("sp", dma(og, GTv[:, :, o, :]), reads=[b_GT])
        P.barrier()
        A.release()

    def hyena_all(self):
        P, A, NB = self.P, self.A, self.NB
        NBI = NB * 16
        A.mark()
        bf = lambda: A.alloc([128, NB, 16], BF16)
        X1, X2r, Vr, Z = bf(), bf(), bf(), bf()
        b_in = [Buf() for _ in range(16)]
        b_Z = [Buf() for _ in range(16)]
        hTl = A.alloc([KD, L], BF16)
        b_hTl = Buf()
        W3 = A.alloc([KD, 384], BF16)
        b_W3 = Buf()
        wc = A.alloc([3, 384], F32)
        bc = A.alloc([384], F32)
        b_wc = Buf()
        pw = [(A.alloc([3, 384], BF16), Buf()) for _ in range(2)]
        NTZ = 3
        TZ = [[(A.alloc([3968], BF16), Buf()) for _ in range(NTZ)] for _ in range(2)]
        yTc = [(A.alloc([512], BF16), Buf()) for _ in range(2)]
        gt = self.G_d.tensor
        dq = ["sp", "act"]
        tzc = [0, 0]
        for cg in range(4):
            for s in range(3):
                c0 = s * 512 + cg * 128
                P.dma("pool", dma(W3[:, :, s * 128:(s + 1) * 128], self.win_d[:, :, c0:c0 + 128]), writes=[b_W3])
                for kk in range(3):
                    P.dma("sp", dma(wc[:, kk, s * 128:(s + 1) * 128], bcast_rows(self.hcw_d.tensor, kk * 1536 + c0, 128)), writes=[b_wc])
                P.dma("sp", dma(bc[:, s * 128:(s + 1) * 128], bcast_rows(self.hcb_d.tensor, c0, 128)), writes=[b_wc])
            for b in range(NB):
                P.dma("sp", dma(hTl, self.HT_d[b].rearrange("p (k t) -> p k t", k=KD)), reads=[self.b_HT], writes=[b_hTl])
                for i in range(NLT):
                    ps, pb = self.bank()
                    for k in range(KD):
                        P.op("pe", mm(ps[:, 0:384], hTl[:, k, i * 128:(i + 1) * 128], W3[:, k, :], k == 0, k == KD - 1),
                             reads=[b_hTl, b_W3], writes=[pb])
                    pwt, bpw = pw[i % 2]
                    for kk in range(3):
                        P.op("dve", tt(pwt[:, kk, :], ps[:, 0:384], wc[:, kk, :], ALU.mult), reads=[pb, b_wc], writes=[bpw])
                    pa, pba = self.bank()
                    pr, pbr = self.bank()
                    mats = [(self.Smb, self.SmRb), (self.identb, self.Jb), (self.Spb, self.SpRb)]
                    for kk in range(3):
                        P.op("pe", mm(pa[:, 0:128], mats[kk][0], pwt[:, kk, 0:128], kk == 0, kk == 2), reads=[bpw, self.b_cb], writes=[pba])
                    for kk in range(3):
                        P.op("pe", mm(pr[:, 0:256], mats[kk][1], pwt[:, kk, 128:384], kk == 0, kk == 2), reads=[bpw, self.b_cb], writes=[pbr])
                    P.op("dve", tt(X1[:, :, b, i], pa[:, 0:128], bc[:, 0:128], ALU.add), reads=[pba, b_wc], writes=b_in)
                    P.op("dve", tt(X2r[:, :, b, i], pr[:, 0:128], bc[:, 128:256], ALU.add), reads=[pbr, b_wc], writes=b_in)
                    P.op("dve", tt(Vr[:, :, b, i], pr[:, 128:256], bc[:, 256:384], ALU.add), reads=[pbr, b_wc], writes=b_in)
            deltas = [0] + [s * m for m in range(1, 16) for s in (1, -1)]

            def conv(o, g, src):
                ps, pb = self.bank()
                for cc in range(8):
                    c = g * 8 + cc
                    ch = cg * 128 + c
                    tz, btz = TZ[o][tzc[o] % NTZ]
                    tzc[o] += 1
                    off = (o * 512 + ch) * 4096 + (1 if o == 0 else 0)
                    P.dma(dq[(c + o) % 2], dma(tz, bass.AP(tensor=gt, offset=off, ap=[[1, 128], [1, 3968]])), reads=[self.b_G], writes=[btz])
                    psv = ps[:, cc * NBI:(cc + 1) * NBI].rearrange("p (b i) -> p b i", b=NB)
                    for di, dl_ in enumerate(deltas):
                        ilo, ihi = max(0, -dl_), min(16, 16 - dl_)
                        blk = (dl_ + 15) if o == 0 else (-dl_ + 15)
                        P.op("pe", mm(psv[:, :, ilo + dl_:ihi + dl_], tz[:, blk * 128:(blk + 1) * 128], src[:, c, :, ilo:ihi],
                                      di == 0, di == len(deltas) - 1),
                             reads=[btz, (b_in[g] if o == 0 else b_Z[g])], writes=[pb])
                return ps, pb

            def evac1(g, ps, pb):
                P.op("dve", tt(Z[:, g * 8:(g + 1) * 8, :, :].rearrange("p c b i -> p (c b i)"), ps[:, 0:8 * NBI],
                               X1[:, g * 8:(g + 1) * 8, :, :].rearrange("p c b i -> p (c b i)"), ALU.mult),
                     reads=[pb, b_in[g]], writes=[b_Z[g]])

            def evac2(g, ps, pb):
                P.op("dve", tt(Z[:, g * 8:(g + 1) * 8, :, :].rearrange("p c b i -> p (c b i)"), ps[:, 0:8 * NBI],
                               X2r[:, g * 8:(g + 1) * 8, :, :].rearrange("p c b i -> p (c b i)"), ALU.mult),
                     reads=[pb, b_in[g]], writes=[b_Z[g]])

            p1 = conv(0, 0, Vr)
            for g in range(16):
                evac1(g, *p1)
                if g + 1 < 16:
                    p1 = conv(0, g + 1, Vr)
                p2 = conv(1, g, Z)
                evac2(g, *p2)
            for b in range(NB):
                for i0 in range(0, NLT, 4):
                    ps, pb = self.bank()
                    for ii in range(4):
                        P.op("pe", mm(ps[:, ii * 128:(ii + 1) * 128], Z[:, :, b, i0 + ii], self.Jb, True, True), reads=b_Z + [self.b_cb], writes=[pb])
                    yt, byt = yTc[(i0 // 4) % 2]
                    P.op("act", act(yt, ps, AF.Copy), reads=[pb], writes=[byt])
                    P.dma("sp", dma(self.YT_d[b, cg, :, i0 * 128:(i0 + 4) * 128], yt), reads=[byt], writes=[self.b_YT])
                    if b == 0 and "yT_hy" in self.dbg:
                        if not hasattr(self, "dbg_hy"):
                            self.dbg_hy = self.dout("dbg_yT_hy", [128, 4, L], BF16)
                        P.dma("sp", dma(self.dbg_hy[:, cg, i0 * 128:(i0 + 4) * 128], yt), reads=[byt])
        P.barrier()
        A.release()

    def route_init(self):
        P, A, NB = self.P, self.A, self.NB
        NTT = NB * NLT
        self.destAll = A.alloc([NTT, 8], I32)
        self.wAll = A.alloc([NTT, 8], F32)
        self.b_dw = Buf("destw")
        self.cnt = A.alloc([NE], F32)
        self.b_cnt = Buf("cnt")
        P.op("pool", lambda e: e.memset(self.cnt, 0.0), writes=[self.b_cnt])
        self.b_rc = Buf("routeconst")
        self.E8All = A.alloc([NTT, 8], F32)
        self.P8All = A.alloc([NTT, 8], F32)
        self.W8raw = A.alloc([NTT, 8], F32)
        self.DestF = A.alloc([NTT, 8], F32)
        self.b_ov = Buf("ovstate")
        self.IDXG = A.alloc([OV], I32)
        self.b_idx = Buf("ovidx")
        self.b_H2 = Buf("H2")
        A.mark()
        self.iota3 = A.alloc([NE], F32)
        self.rbias = A.alloc([NE], F32)
        self.n2g = A.alloc([D], F32)
        P.dma("sp", dma(self.iota3, bcast_rows(self.iota_d.tensor, 0, NE)), writes=[self.b_rc])
        P.dma("sp", dma(self.rbias, bcast_rows(self.rb_d.tensor, 0, NE)), writes=[self.b_rc])
        P.dma("sp", dma(self.n2g, bcast_rows(self.n2g_d.tensor, 0, D)), writes=[self.b_rc])
        self.wr = A.alloc([KD, NE], F32)
        P.dma("sp", dma(self.wr, self.wr_d), writes=[self.b_rc])
        self.wout = A.alloc([KD, D], BF16)
        self.swgu = A.alloc([KD, 512], BF16)
        self.swd = A.alloc([2, D], BF16)
        self.b_wts = Buf("wts")
        for k in range(KD):
            P.dma("pool", dma(self.wout[:, k, :], self.wout_d[:, k, :]), writes=[self.b_wts])
        P.dma("pool", dma(self.swgu[:, :, 0:256], self.swg_d), writes=[self.b_wts])
        P.dma("pool", dma(self.swgu[:, :, 256:512], self.swu_d), writes=[self.b_wts])
        P.dma("pool", dma(self.swd, self.swd_d), writes=[self.b_wts])
        self.b_X1 = Buf("X1")
        self.b_XS = Buf("XS")

        def pre_pool(engine):
            self.bc_reg = engine.to_reg(NE * CAP - 1)
            self.bc_reg2 = engine.to_reg(NE * CAP + OV * 128 - 1)
        P.pre["pool"] = pre_pool

    def outproj_route(self, b):
        P, A, NB = self.P, self.A, self.NB
        A.mark()
        mt = self.MOD_d.tensor
        G1 = A.alloc([D], F32)
        A2 = A.alloc([D], F32)
        B2 = A.alloc([D], F32)
        G2 = A.alloc([D], F32)
        b_m = Buf()
        P.dma("sp", dma(G1, bcast_rows(mt, b * 6 * D + 2 * D, D)), reads=[self.b_MOD], writes=[b_m])
        P.dma("sp", dma(B2, bcast_rows(mt, b * 6 * D + 3 * D, D)), reads=[self.b_MOD], writes=[b_m])
        P.dma("sp", dma(A2, bcast_rows(mt, b * 6 * D + 4 * D, D)), reads=[self.b_MOD], writes=[b_m])
        P.dma("sp", dma(G2, bcast_rows(mt, b * 6 * D + 5 * D, D)), reads=[self.b_MOD], writes=[b_m])
        P.op("dve", stt(A2, A2, 1.0, self.n2g, ALU.add, ALU.mult), reads=[b_m, self.b_rc], writes=[b_m])
        self.junk = (A.alloc([D], F32), Buf())
        self.ssb = [(A.alloc([1], F32), Buf()) for _ in range(2)]
        yT4 = [(A.alloc([KD, 512], BF16), Buf()) for _ in range(2)]
        xts = [(A.alloc([D], F32), Buf()) for _ in range(2)]
        x1s = [(A.alloc([D], F32), Buf()) for _ in range(2)]
        tmpA = (A.alloc([D], F32), Buf())
        tmpB = (A.alloc([D], F32), Buf())
        hfs = [(A.alloc([D], F32), Buf()) for _ in range(2)]
        hbs = [(A.alloc([D], BF16), Buf()) for _ in range(2)]
        h2Tb = [(A.alloc([KD, 128], BF16), Buf()) for _ in range(2)]
        h2Tf = [(A.alloc([KD, 128], F32), Buf()) for _ in range(2)]
        scs = [(A.alloc([NE], F32), Buf()) for _ in range(2)]
        bias_ = [(A.alloc([NE], F32), Buf()) for _ in range(2)]
        msk = (A.alloc([NE], F32), Buf())
        sel = (A.alloc([NE], F32), Buf())
        selb = (A.alloc([NE], BF16), Buf())
        pos = (A.alloc([NE], F32), Buf())
        oh = (A.alloc([8, NE], F32), Buf())
        sm = (A.alloc([160], F32), Buf())
        dsc = [(A.alloc([8], I32), Buf()) for _ in range(2)]
        sg = (A.alloc([256], F32), Buf())
        hmid = (A.alloc([2, 128], BF16), Buf())
        smv = sm[0]
        b_s = sm[1]
        m8g = smv[:, 0:64].rearrange("p (g e) -> p g e", g=8)
        gs_ = smv[:, 64:72]
        m8 = smv[:, 72:80]
        gmask = smv[:, 80:88]
        gm1 = smv[:, 88:96]
        m8b = smv[:, 96:104]
        w8 = smv[:, 104:112]
        e8f = smv[:, 112:120]
        posk = smv[:, 120:128]
        dest = smv[:, 128:136]
        valid = smv[:, 136:144]
        t8 = smv[:, 144:152]
        ws1 = smv[:, 152:153]
        e8 = A.alloc([8], U32)
        def stage1(i):
            ti = b * NLT + i
            sc, bia = scs[i % 2], bias_[i % 2]
            tmp = tmpA
            tt_ = i % 4
            yt, byt = yT4[(i // 4) % 2]
            if tt_ == 0:
                P.dma("sp", dma(yt, self.YT_d[b].rearrange("k p t -> p k t")[:, :, i * 128:(i + 4) * 128]), reads=[self.b_YT], writes=[byt])
            xt, bx = xts[i % 2]
            P.dma("act", dma(xt, self.x_d[b * L + i * 128: b * L + (i + 1) * 128, :]), writes=[bx])
            pss = [self.bank(), self.bank()]
            for n in range(2):
                for k in range(KD):
                    P.op("pe", mm(pss[n][0][:, :], yt[:, k, tt_ * 128:(tt_ + 1) * 128], self.wout[:, k, n * 512:(n + 1) * 512], k == 0, k == KD - 1),
                         reads=[byt, self.b_wts], writes=[pss[n][1]])
            for n in range(2):
                P.op("dve", tt(tmp[0][:, n * 512:(n + 1) * 512], pss[n][0][:, :], G1[:, n * 512:(n + 1) * 512], ALU.mult),
                     reads=[pss[n][1], b_m], writes=[tmp[1]])
            x1, bx1 = x1s[i % 2]
            P.op("dve", tt(x1, tmp[0], xt, ALU.add), reads=[tmp[1], bx], writes=[bx1])
            if b == 0 and "x1" in self.dbg:
                if not hasattr(self, "dbg_x1"):
                    self.dbg_x1 = self.dout("dbg_x1", [L, D])
                P.dma("sp", dma(self.dbg_x1[i * 128:(i + 1) * 128, :], x1), reads=[bx1])
            hf, bhf = hfs[i % 2]
            hb, bhb = hbs[i % 2]
            hT2, bhT2 = h2Tb[i % 2]
            self.norm_mod_sb(i, x1, bx1, A2, b_m, B2, b_m, hT2, bhT2, hf_out=(hf, bhf), hb_out=(hb, bhb))
            if b == 0 and "hx2" in self.dbg:
                if not hasattr(self, "dbg_hx2"):
                    self.dbg_hx2 = self.dout("dbg_hx2", [L, D])
                P.dma("sp", dma(self.dbg_hx2[i * 128:(i + 1) * 128, :], hf), reads=[bhf])
            pf = [self.bank(), self.bank()]
            for k in range(KD):
                P.op("pe", tr(pf[k // 4][0][:, (k % 4) * 128:(k % 4 + 1) * 128], hf[:, k * 128:(k + 1) * 128], self.identf),
                     reads=[bhf, self.b_cst], writes=[pf[k // 4][1]])
            hTf, bhTf = h2Tf[i % 2]
            P.op("act", act(hTf[:, 0:4, :], pf[0][0].rearrange("p (k t) -> p k t", k=4), AF.Copy), reads=[pf[0][1]], writes=[bhTf])
            P.op("dve", cp(hTf[:, 4:8, :], pf[1][0].rearrange("p (k t) -> p k t", k=4)), reads=[pf[1][1]], writes=[bhTf])
            pr, pbr = self.bank()
            for k in range(KD):
                P.op("pe", mm(pr[:, 0:NE], hTf[:, k, :], self.wr[:, k, :], k == 0, k == KD - 1), reads=[bhTf, self.b_rc], writes=[pbr])
            P.op("act", act(sc[0], pr[:, 0:NE], AF.Sigmoid), reads=[pbr], writes=[sc[1]])
            P.op("dve", tt(bia[0], sc[0], self.rbias, ALU.add), reads=[sc[1], self.b_rc], writes=[bia[1]])
        def stage2(i):
            ti = b * NLT + i
            x1, bx1 = x1s[i % 2]
            hf, bhf = hfs[i % 2]
            hb, bhb = hbs[i % 2]
            hT2, bhT2 = h2Tb[i % 2]
            sc, bia = scs[i % 2], bias_[i % 2]
            tmp = tmpB
            bia3 = bia[0].rearrange("p (g e) -> p g e", g=8)
            for g in range(8):
                P.op("dve", lambda e, g=g: e.max(out=m8g[:, g, :], in_=bia3[:, g, :]), reads=[bia[1]], writes=[b_s])
            P.op("dve", tt(gs_, m8g[:, :, 0], m8g[:, :, 1], ALU.add), reads=[b_s], writes=[b_s])
            P.op("dve", lambda e: e.max(out=m8, in_=gs_), reads=[b_s], writes=[b_s])
            P.op("dve", ts(gmask, gs_, m8[:, 3:4], None, ALU.is_ge), reads=[b_s], writes=[b_s])
            P.op("dve", ts(gm1, gmask, -1.0, None, ALU.add), reads=[b_s], writes=[b_s])
            msk3 = msk[0].rearrange("p (g e) -> p g e", g=8)
            P.op("dve", tt(msk3, bia3, gmask.unsqueeze(2).to_broadcast([128, 8, 32]), ALU.mult), reads=[bia[1], b_s], writes=[msk[1]])
            P.op("dve", tt(msk3, msk3, gm1.unsqueeze(2).to_broadcast([128, 8, 32]), ALU.add), reads=[msk[1], b_s], writes=[msk[1]])
            P.op("dve", lambda e: e.max(out=m8b, in_=msk[0]), reads=[msk[1]], writes=[b_s])
            P.op("dve", ts(sel[0], msk[0], m8b[:, 7:8], None, ALU.is_ge), reads=[msk[1], b_s], writes=[sel[1]])
            P.op("act", act(selb[0], sel[0], AF.Copy), reads=[sel[1]], writes=[selb[1]])
            P.op("dve", tt(msk[0], sc[0], sel[0], ALU.mult), reads=[sc[1], sel[1]], writes=[msk[1]])
            P.op("dve", lambda e: e.max(out=w8, in_=msk[0]), reads=[msk[1]], writes=[b_s])
            P.op("dve", lambda e: e.max_index(out=e8, in_max=w8, in_values=msk[0]), reads=[msk[1], b_s], writes=[b_s])
            P.op("dve", cp(e8f, e8), reads=[b_s], writes=[b_s])
            P.op("dve", rsum(ws1, w8), reads=[b_s], writes=[b_s])
            P.op("dve", lambda e: e.reciprocal(out=ws1, in_=ws1), reads=[b_s], writes=[b_s])
            P.op("dve", ts(w8, w8, ws1, RSCALE, ALU.mult, ALU.mult), reads=[b_s], writes=[b_s])
            pp, pbp = self.bank()
            P.op("pe", mm(pp[:, 0:NE], self.ustrict_b, selb[0], True, True), reads=[selb[1], self.b_cb], writes=[pbp])
            pc, pbc = self.bank()
            P.op("pe", mm(pc[:, 0:NE], self.ones_b, selb[0], True, True), reads=[selb[1], self.b_cb], writes=[pbc])
            P.op("dve", tt(pos[0], pp[:, 0:NE], self.cnt, ALU.add), reads=[pbp, self.b_cnt], writes=[pos[1]])
            P.op("dve", tt(self.cnt, self.cnt, pc[:, 0:NE], ALU.add), reads=[pbc, pos[1]], writes=[self.b_cnt])
            P.op("dve", tt(oh[0], self.iota3.unsqueeze(1).to_broadcast([128, 8, NE]), e8f.unsqueeze(2).to_broadcast([128, 8, NE]), ALU.is_equal),
                 reads=[self.b_rc, b_s], writes=[oh[1]])
            P.op("dve", tt(oh[0], oh[0], pos[0].unsqueeze(1).to_broadcast([128, 8, NE]), ALU.mult), reads=[oh[1], pos[1]], writes=[oh[1]])
            P.op("dve", rsum(posk, oh[0]), reads=[oh[1]], writes=[b_s])
            P.op("dve", stt(dest, e8f, float(CAP), posk, ALU.mult, ALU.add), reads=[b_s], writes=[b_s])
            P.op("dve", ts(valid, posk, float(CAP), None, ALU.is_lt), reads=[b_s], writes=[b_s])
            P.op("dve", ts(t8, valid, -1.0e6, 1.0e6, ALU.mult, ALU.add), reads=[b_s], writes=[b_s])
            P.op("dve", tt(t8, t8, dest, ALU.add), reads=[b_s], writes=[b_s])
            di, bdi = dsc[i % 2]
            P.op("dve", cp(di, t8), reads=[b_s], writes=[bdi])
            P.op("dve", tt(self.DestF[:, ti, :], dest, valid, ALU.mult), reads=[b_s], writes=[self.b_ov])
            P.op("dve", tt(self.wAll[:, ti, :], w8, valid, ALU.mult), reads=[b_s], writes=[self.b_dw])
            P.op("dve", cp(self.E8All[:, ti, :], e8f), reads=[b_s], writes=[self.b_ov])
            P.op("dve", cp(self.P8All[:, ti, :], posk), reads=[b_s], writes=[self.b_ov])
            P.op("dve", cp(self.W8raw[:, ti, :], w8), reads=[b_s], writes=[self.b_ov])
            P.dma("act", dma(self.H2_d[ti * 128:(ti + 1) * 128, :], hb), reads=[bhb], writes=[self.b_H2])
            for k in range(8):
                P.dma("pool", lambda e, k=k, di=di, hb=hb: e.indirect_dma_start(
                    out=self.XS_d, out_offset=bass.IndirectOffsetOnAxis(ap=di[:, k:k + 1], axis=0), in_=hb, in_offset=None,
                    bounds_check=self.bc_reg, oob_is_err=False), reads=[bdi, bhb], writes=[self.b_XS])
            ph, pbh = self.bank()
            for fc in range(4):
                for k in range(KD):
                    P.op("pe", mm(ph[:, fc * 128:(fc + 1) * 128], self.swgu[:, k, fc * 128:(fc + 1) * 128], hT2[:, k, :], k == 0, k == KD - 1),
                         reads=[bhT2, self.b_wts], writes=[pbh])
            P.op("act", act(sg[0], ph[:, 0:256], AF.Silu), reads=[pbh], writes=[sg[1]])
            P.op("dve", tt(hmid[0].rearrange("p a b -> p (a b)"), sg[0], ph[:, 256:512], ALU.mult), reads=[sg[1], pbh], writes=[hmid[1]])
            pd = [self.bank(), self.bank()]
            for n in range(2):
                for fc in range(2):
                    P.op("pe", mm(pd[n][0][:, :], hmid[0][:, fc, :], self.swd[:, fc, n * 512:(n + 1) * 512], fc == 0, fc == 1),
                         reads=[hmid[1], self.b_wts], writes=[pd[n][1]])
            for n in range(2):
                P.op("dve", tt(tmp[0][:, n * 512:(n + 1) * 512], pd[n][0][:, :], G2[:, n * 512:(n + 1) * 512], ALU.mult),
                     reads=[pd[n][1], b_m], writes=[tmp[1]])
            P.op("dve", tt(x1, tmp[0], x1, ALU.add), reads=[tmp[1], bx1], writes=[bx1])
            P.dma("sp", dma(self.X1_d[ti * 128:(ti + 1) * 128, :], x1), reads=[bx1], writes=[self.b_X1])
        stage1(0)
        for i in range(NLT):
            if i + 1 < NLT:
                stage1(i + 1)
            stage2(i)
        P.barrier()
        A.release()

    def overflow_route(self):
        P, A, NB = self.P, self.A, self.NB
        NTT = NB * NLT
        A.mark()
        pidx = A.alloc([1], F32)
        b_c = Buf()
        P.dma("sp", dma(pidx, self.pidx_d), writes=[b_c])
        thr = A.alloc([1], F32)
        P.op("dve", ts(thr, pidx, 128.0, None, ALU.mult), reads=[b_c], writes=[b_c])
        ovc = A.alloc([NE], F32)
        b_o = Buf()
        P.op("dve", ts(ovc, self.cnt, -float(CAP), 0.0, ALU.add, ALU.max), reads=[self.b_cnt], writes=[b_o])
        cmpb = A.alloc([NE], BF16)
        P.op("dve", ts(cmpb, ovc, thr, None, ALU.is_gt), reads=[b_o, b_c], writes=[b_o])
        ps, pb = self.bank()
        P.op("pe", mm(ps[:, 0:NE], self.ones_b, cmpb, True, True), reads=[b_o, self.b_cb], writes=[pb])
        ovblk = A.alloc([NE], F32)
        ovend = A.alloc([NE], F32)
        ovs = A.alloc([NE], F32)
        onesr = A.alloc([NE], F32)
        b_e = Buf()
        P.op("act", act(ovblk, ps[:, 0:NE], AF.Copy), reads=[pb], writes=[b_e])
        P.op("pool", lambda e: e.memset(onesr, 1.0), writes=[b_e])
        P.op("dve", lambda e: e.tensor_tensor_scan(out=ovend, data0=onesr, data1=ovblk, initial=0.0, op0=ALU.mult, op1=ALU.add),
             reads=[b_e], writes=[b_e])
        P.op("dve", tt(ovs, ovend, ovblk, ALU.subtract), reads=[b_e], writes=[b_e])
        P.op("dve", ts(ovs, ovs, 128.0, None, ALU.mult), reads=[b_e], writes=[b_e])
        beRow = A.alloc([OV], F32)
        b_b = Buf()
        cmp3 = A.alloc([16, NE], F32)
        for c0 in range(0, OV, 16):
            P.op("dve", tt(cmp3, ovend.unsqueeze(1).to_broadcast([128, 16, NE]),
                           self.iota3[:, c0:c0 + 16].unsqueeze(2).to_broadcast([128, 16, NE]), ALU.is_le), reads=[b_e, self.b_rc], writes=[b_b])
            P.op("dve", rsum(beRow[:, c0:c0 + 16], cmp3), reads=[b_b], writes=[b_b])
        P.op("dve", ts(beRow, beRow, float(NE - 1), None, ALU.min), reads=[b_b], writes=[b_b])
        self.dump("be", beRow, b_b, [128, OV])
        idxf = A.alloc([OV], F32)
        P.op("dve", ts(idxf, beRow, 128.0, pidx, ALU.mult, ALU.add), reads=[b_b, b_c], writes=[b_b])
        P.op("dve", cp(self.IDXG, idxf), reads=[b_b], writes=[self.b_idx])
        oh = A.alloc([8, NE], F32)
        b_oh = Buf()
        sm = A.alloc([64], F32)
        b_s = Buf()
        ob8, ovslot, isov, ok, t8 = (sm[:, 8 * q:8 * q + 8] for q in range(5))
        di_ = [(A.alloc([8], I32), Buf()) for _ in range(2)]
        hbs = [(A.alloc([D], BF16), Buf()) for _ in range(2)]
        for ti in range(NTT):
            hb, bhb = hbs[ti % 2]
            P.dma("sp", dma(hb, self.H2_d[ti * 128:(ti + 1) * 128, :]), reads=[self.b_H2], writes=[bhb])
            P.op("dve", tt(oh, self.iota3.unsqueeze(1).to_broadcast([128, 8, NE]),
                           self.E8All[:, ti, :].unsqueeze(2).to_broadcast([128, 8, NE]), ALU.is_equal), reads=[self.b_rc, self.b_ov], writes=[b_oh])
            P.op("dve", tt(oh, oh, ovs.unsqueeze(1).to_broadcast([128, 8, NE]), ALU.mult), reads=[b_oh, b_e], writes=[b_oh])
            P.op("dve", rsum(ob8, oh), reads=[b_oh], writes=[b_s])
            P.op("dve", stt(ovslot, self.P8All[:, ti, :], -float(CAP), ob8, ALU.add, ALU.add), reads=[b_s, self.b_ov], writes=[b_s])
            P.op("dve", ts(isov, self.P8All[:, ti, :], float(CAP), None, ALU.is_ge), reads=[self.b_ov], writes=[b_s])
            P.op("dve", ts(ok, ovslot, float(OV * 128), None, ALU.is_lt), reads=[b_s], writes=[b_s])
            P.op("dve", tt(ok, ok, isov, ALU.mult), reads=[b_s], writes=[b_s])
            P.op("dve", ts(ovslot, ovslot, float(NE * CAP), None, ALU.add), reads=[b_s], writes=[b_s])
            P.op("dve", ts(t8, ok, -1.0e6, 1.0e6, ALU.mult, ALU.add), reads=[b_s], writes=[b_s])
            P.op("dve", tt(t8, t8, ovslot, ALU.add), reads=[b_s], writes=[b_s])
            di, bdi = di_[ti % 2]
            P.op("dve", cp(di, t8), reads=[b_s], writes=[bdi])
            P.op("dve", tt(ovslot, ovslot, ok, ALU.mult), reads=[b_s], writes=[b_s])
            P.op("dve", tt(t8, self.DestF[:, ti, :], ovslot, ALU.add), reads=[b_s, self.b_ov], writes=[b_s])
            P.op("dve", cp(self.destAll[:, ti, :], t8), reads=[b_s], writes=[self.b_dw])
            P.op("dve", tt(t8, self.W8raw[:, ti, :], ok, ALU.mult), reads=[b_s, self.b_ov], writes=[b_s])
            P.op("dve", tt(self.wAll[:, ti, :], self.wAll[:, ti, :], t8, ALU.add), reads=[b_s, self.b_dw], writes=[self.b_dw])
            for k in range(8):
                P.dma("pool", lambda e, k=k, di=di, hb=hb: e.indirect_dma_start(
                    out=self.XS_d, out_offset=bass.IndirectOffsetOnAxis(ap=di[:, k:k + 1], axis=0), in_=hb, in_offset=None,
                    bounds_check=self.bc_reg2, oob_is_err=False), reads=[bdi, bhb], writes=[self.b_XS])
        P.barrier()
        A.release()

    def experts(self):
        P, A = self.P, self.A
        self.dump("cnt", self.cnt, self.b_cnt, [128, NE])
        A.mark()
        xs4 = [(A.alloc([NBLK, D], BF16), Buf()) for _ in range(3)]
        xT = [(A.alloc([KD, CAP], BF16), Buf()) for _ in range(2)]
        wguf = [(A.alloc([KD, 512], F32), Buf()) for _ in range(3)]
        wdf = [(A.alloc([2, D], F32), Buf()) for _ in range(3)]
        wgub = [(A.alloc([KD, 512], BF16), Buf()) for _ in range(2)]
        wdb = [(A.alloc([2, D], BF16), Buf()) for _ in range(2)]
        sg = [(A.alloc([2, CAP], F32), Buf()) for _ in range(1)]
        hm = [(A.alloc([2, CAP], BF16), Buf()) for _ in range(2)]
        yo = [(A.alloc([D], BF16), Buf()) for _ in range(3)]
        self.b_Y = Buf("Y")

        def loads(e_):
            x4, bx4 = xs4[e_ % 3]
            P.dma("sp", dma(x4, self.XS_d[e_ * CAP:(e_ + 1) * CAP, :].rearrange("(s p) d -> p s d", p=128)), reads=[self.b_XS], writes=[bx4])
            wg, bwg = wguf[e_ % 3]
            wd, bwd = wdf[e_ % 3]
            P.dma("sp", dma(wg[:, :, 0:256], self.ewg_d[e_].rearrange("p (k f) -> p k f", k=KD)), writes=[bwg])
            P.dma("sp", dma(wg[:, :, 256:512], self.ewu_d[e_].rearrange("p (k f) -> p k f", k=KD)), writes=[bwg])
            P.dma("sp", dma(wd, self.ewd_d[e_].rearrange("p (k n) -> p k n", k=2)), writes=[bwd])

        def casts(e_):
            wg, bwg = wguf[e_ % 3]
            wd, bwd = wdf[e_ % 3]
            wgb, bwgb = wgub[e_ % 2]
            wdb_, bwdb = wdb[e_ % 2]
            P.op("dve", cp(wgb[:, 0:3, :], wg[:, 0:3, :]), reads=[bwg], writes=[bwgb])
            P.op("act", act(wgb[:, 3:6, :], wg[:, 3:6, :], AF.Copy), reads=[bwg], writes=[bwgb])
            P.op("pool", cp(wgb[:, 6:8, :], wg[:, 6:8, :]), reads=[bwg], writes=[bwgb])
            P.op("act", act(wdb_[:, 0, :], wd[:, 0, :], AF.Copy), reads=[bwd], writes=[bwdb])
            P.op("dve", cp(wdb_[:, 1, :], wd[:, 1, :]), reads=[bwd], writes=[bwdb])

        def transposes(e_):
            x4, bx4 = xs4[e_ % 3]
            xt_, bxt = xT[e_ % 2]
            for sb in range(NBLK):
                ps, pb = self.bank()
                psb = ps.bitcast(BF16)
                for k in range(KD):
                    P.op("pe", tr(psb[:, k * 128:(k + 1) * 128], x4[:, sb, k * 128:(k + 1) * 128], self.identb), reads=[bx4, self.b_cb], writes=[pb])
                src = psb.rearrange("p (k t) -> p k t", k=KD)
                P.op("dve", cp(xt_[:, :, sb * 128:(sb + 1) * 128], src), reads=[pb], writes=[bxt])

        loads(0)
        loads(1)
        casts(0)
        transposes(0)
        yc = 0
        for e_ in range(NE):
            if e_ + 2 < NE:
                loads(e_ + 2)
            if e_ + 1 < NE:
                casts(e_ + 1)
            wgb, bwgb = wgub[e_ % 2]
            wdb_, bwdb = wdb[e_ % 2]
            xt_, bxt = xT[e_ % 2]
            pg = [self.bank() for _ in range(4)]
            for fc in range(4):
                for k in range(KD):
                    P.op("pe", mm(pg[fc][0][:, 0:CAP], wgb[:, k, fc * 128:(fc + 1) * 128], xt_[:, k, :], k == 0, k == KD - 1),
                         reads=[bwgb, bxt], writes=[pg[fc][1]])
            sg_, bsg = sg[0]
            hm_, bhm = hm[e_ % 2]
            for fc in range(2):
                P.op("act", act(sg_[:, fc, :], pg[fc][0][:, 0:CAP], AF.Silu), reads=[pg[fc][1]], writes=[bsg])
                P.op("dve", tt(hm_[:, fc, :], sg_[:, fc, :], pg[2 + fc][0][:, 0:CAP], ALU.mult), reads=[bsg, pg[2 + fc][1]], writes=[bhm])
            if e_ + 1 < NE:
                transposes(e_ + 1)
            for sb in range(NBLK):
                pd = [self.bank(), self.bank()]
                for n in range(2):
                    for fc in range(2):
                        P.op("pe", mm(pd[n][0][:, :], hm_[:, fc, sb * 128:(sb + 1) * 128], wdb_[:, fc, n * 512:(n + 1) * 512], fc == 0, fc == 1),
                             reads=[bhm, bwdb], writes=[pd[n][1]])
                y_, by = yo[yc % 3]
                yc += 1
                P.op("act", act(y_[:, 0:512], pd[0][0][:, :], AF.Copy), reads=[pd[0][1]], writes=[by])
                P.op("act", act(y_[:, 512:1024], pd[1][0][:, :], AF.Copy), reads=[pd[1][1]], writes=[by])
                r0 = e_ * CAP + sb * 128
                P.dma("act", dma(self.Y_d[r0:r0 + 128, :], y_), reads=[by], writes=[self.b_Y])
        if OV > 0:
            ewg_rows = self.ewg_d.rearrange("e p n -> (e p) n")
            ewu_rows = self.ewu_d.rearrange("e p n -> (e p) n")
            ewd_rows = self.ewd_d.rearrange("e p n -> (e p) n")
            ovw = [(A.alloc([KD * 256], BF16), A.alloc([KD * 256], BF16), A.alloc([2 * D], BF16), Buf()) for _ in range(2)]

            def ovloads(j):
                x4, bx4 = xs4[j % 3]
                r0 = NE * CAP + j * 128
                P.dma("sp", dma(x4[:, 0, :], self.XS_d[r0:r0 + 128, :]), reads=[self.b_XS], writes=[bx4])
                og, ou, od, bw = ovw[j % 2]
                off = lambda j=j: bass.IndirectOffsetOnAxis(ap=self.IDXG[:, j:j + 1], axis=0)
                P.dma("pool", lambda e, og=og, off=off: e.indirect_dma_start(out=og, out_offset=None, in_=ewg_rows, in_offset=off()),
                      reads=[self.b_idx], writes=[bw])
                P.dma("pool", lambda e, ou=ou, off=off: e.indirect_dma_start(out=ou, out_offset=None, in_=ewu_rows, in_offset=off()),
                      reads=[self.b_idx], writes=[bw])
                P.dma("pool", lambda e, od=od, off=off: e.indirect_dma_start(out=od, out_offset=None, in_=ewd_rows, in_offset=off()),
                      reads=[self.b_idx], writes=[bw])

            ovloads(0)
            for j in range(OV):
                if j + 1 < OV:
                    ovloads(j + 1)
                x4, bx4 = xs4[j % 3]
                og, ou, od, bw = ovw[j % 2]
                og3 = og.rearrange("p (k f) -> p k f", k=KD)
                ou3 = ou.rearrange("p (k f) -> p k f", k=KD)
                od3 = od.rearrange("p (k n) -> p k n", k=2)
                xt_, bxt = xT[j % 2]
                ps, pb = self.bank()
                psb = ps.bitcast(BF16)
                for k in range(KD):
                    P.op("pe", tr(psb[:, k * 128:(k + 1) * 128], x4[:, 0, k * 128:(k + 1) * 128], self.identb), reads=[bx4, self.b_cb], writes=[pb])
                P.op("dve", cp(xt_[:, :, 0:128], psb.rearrange("p (k t) -> p k t", k=KD)), reads=[pb], writes=[bxt])
                pg, pbg = self.bank()
                for fc in range(4):
                    wsrc = og3 if fc < 2 else ou3
                    f0 = (fc % 2) * 128
                    for k in range(KD):
                        P.op("pe", mm(pg[:, fc * 128:(fc + 1) * 128], wsrc[:, k, f0:f0 + 128], xt_[:, k, 0:128], k == 0, k == KD - 1),
                             reads=[bw, bxt], writes=[pbg])
                sg_, bsg = sg[0]
                hm_, bhm = hm[j % 2]
                for fc in range(2):
                    P.op("act", act(sg_[:, fc, 0:128], pg[:, fc * 128:(fc + 1) * 128], AF.Silu), reads=[pbg], writes=[bsg])
                    P.op("dve", tt(hm_[:, fc, 0:128], sg_[:, fc, 0:128], pg[:, (2 + fc) * 128:(3 + fc) * 128], ALU.mult), reads=[bsg, pbg], writes=[bhm])
                pd = [self.bank(), self.bank()]
                for n in range(2):
                    for fc in range(2):
                        P.op("pe", mm(pd[n][0][:, :], hm_[:, fc, 0:128], od3[:, fc, n * 512:(n + 1) * 512], fc == 0, fc == 1),
                             reads=[bhm, bw], writes=[pd[n][1]])
                y_, by = yo[yc % 3]
                yc += 1
                P.op("act", act(y_[:, 0:512], pd[0][0][:, :], AF.Copy), reads=[pd[0][1]], writes=[by])
                P.op("act", act(y_[:, 512:1024], pd[1][0][:, :], AF.Copy), reads=[pd[1][1]], writes=[by])
                r0 = NE * CAP + j * 128
                P.dma("act", dma(self.Y_d[r0:r0 + 128, :], y_), reads=[by], writes=[self.b_Y])
        P.barrier()
        A.release()

    def combine(self):
        P, A, NB = self.P, self.A, self.NB
        A.mark()
        mt = self.MOD_d.tensor
        fing = A.alloc([D], F32)
        b_fg = Buf()
        P.dma("sp", dma(fing, bcast_rows(self.fing_d.tensor, 0, D)), writes=[b_fg])
        G2s = [(A.alloc([D], F32), Buf()) for _ in range(2)]
        base = [(A.alloc([D], F32), Buf()) for _ in range(2)]
        yk = [(A.alloc([D], BF16), Buf()) for _ in range(8)]
        acc = [(A.alloc([D], F32), Buf()) for _ in range(2)]
        junk = (A.alloc([D], F32), Buf())
        pre = [(A.alloc([D], F32), Buf()) for _ in range(2)]
        ssb = [(A.alloc([1], F32), Buf()) for _ in range(2)]
        ot = [(A.alloc([D], F32), Buf()) for _ in range(2)]
        for b in range(NB):
            G2, bG2 = G2s[b % 2]
            P.dma("sp", dma(G2, bcast_rows(mt, b * 6 * D + 5 * D, D)), reads=[self.b_MOD], writes=[bG2])
            for i in range(NLT):
                ti = b * NLT + i
                bs, bbs = base[i % 2]
                P.dma("sp", dma(bs, self.X1_d[ti * 128:(ti + 1) * 128, :]), reads=[self.b_X1], writes=[bbs])
                ac, bac = acc[i % 2]
                for k in range(8):
                    y_, by = yk[k]
                    P.dma("pool", lambda e, k=k, y_=y_, ti=ti: e.indirect_dma_start(
                        out=y_, out_offset=None, in_=self.Y_d, in_offset=bass.IndirectOffsetOnAxis(ap=self.destAll[:, ti, k:k + 1], axis=0)),
                        reads=[self.b_dw, self.b_Y], writes=[by])
                    if k == 0:
                        P.op("dve", ts(ac, y_, self.wAll[:, ti, 0:1], None, ALU.mult), reads=[by, self.b_dw], writes=[bac])
                    else:
                        P.op("dve", stt(ac, y_, self.wAll[:, ti, k:k + 1], ac, ALU.mult, ALU.add), reads=[by, self.b_dw, bac], writes=[bac])
                pa_, bpa = pre[0]
                pb_, bpb = pre[1]
                P.op("dve", tt(pa_, ac, G2, ALU.mult), reads=[bac, bG2], writes=[bpa])
                P.op("dve", tt(pb_, pa_, bs, ALU.add), reads=[bpa, bbs], writes=[bpb])
                ac, bac = pb_, bpb
                ss, bss = ssb[i % 2]
                P.op("act", act(junk[0], ac, AF.Square), reads=[bac], writes=[junk[1]])
                P.op("dve", rsum(ss, junk[0]), reads=[junk[1]], writes=[bss])
                P.op("act", act(ss, ss, AF.Sqrt, scale=1.0 / D, bias=self.eps_ap), reads=[bss, self.b_eps], writes=[bss])
                P.op("dve", lambda e, ss=ss: e.reciprocal(out=ss, in_=ss), reads=[bss], writes=[bss])
                o_, bo = ot[i % 2]
                P.op("dve", stt(o_, ac, ss, fing, ALU.mult, ALU.mult), reads=[bac, bss, b_fg], writes=[bo])
                P.dma("sp", dma(self.out_d[ti * 128:(ti + 1) * 128, :], o_), reads=[bo])
        A.release()


def const_tables():
    import math
    ident = np.eye(128, dtype=np.float32)
    J = ident[::-1].copy()
    s = np.arange(128)[:, None]
    c = np.arange(128)[None, :]
    same = (s // 64) == (c // 64)
    maskF = (same & (s <= c)).astype(np.float32)
    maskB = (same & (s >= c)).astype(np.float32)
    Sm = ((c == s + 1) & ((c % 64) != 0)).astype(np.float32)
    Sp = ((c == s - 1) & ((c % 64) != 63)).astype(np.float32)
    ustrict = (s < c).astype(np.float32)
    ones = np.ones((128, 128), np.float32)
    cst = np.stack([ident, J, maskF, maskB, Sm, Sp, ustrict, ones, Sm[:, ::-1], Sp[:, ::-1]], axis=1).astype(np.float32)
    f32 = np.float32
    pos = np.arange(L, dtype=f32)[:, None]
    t = pos / f32(L - 1)
    w = f32(2.0 * math.pi / L) * pos
    bands = np.linspace(1e-4, 15, 16, dtype=f32)[None, :]
    feats = np.concatenate([t, np.cos(bands * w), -np.sin(bands * w)], axis=-1).astype(f32)
    max_decay = math.log(1e-2) / 0.3
    min_decay = math.log(1e-2) / 1.5
    deltas = np.abs(np.linspace(min_decay, max_decay, 512, dtype=f32))[None, :].astype(f32)
    tfrac = (-(np.arange(L, dtype=f32) / f32(L - 1))).reshape(16, 128).T.copy()
    iota = np.arange(NE, dtype=f32)[None, :]
    pidx = np.arange(128, dtype=f32)[:, None].copy()
    return dict(cst=cst, featsT=np.ascontiguousarray(feats.T), deltas=deltas, tfrac=tfrac.astype(f32), iota=iota, pidx=pidx)


def prep_core(inp, core, NB, tables):
    f = lambda a: np.ascontiguousarray(a, dtype=np.float32)
    b0 = core * NB
    m = dict(tables)
    m["x"] = f(inp["x"][b0:b0 + NB].reshape(NB * L, D))
    m["ctx"] = f(inp["ctx"][b0:b0 + NB].reshape(NB * CTX, D))
    cc = np.concatenate([inp["c"][b0:b0 + NB], inp["c_ctx"][None, :]], axis=0)
    m["cT"] = f(cc.T.reshape(KD, 128, NB + 1).transpose(1, 0, 2))
    return m


def shared_inputs(inp):
    f = lambda a: np.ascontiguousarray(a, dtype=np.float32)
    m = {}
    m["w_mod"] = f(inp["w_mod"][0])
    m["b_mod"] = f(inp["b_mod"][0][None, :])
    m["norm1_g"] = f(inp["norm1_g"][0][None, :])
    m["norm2_g"] = f(inp["norm2_g"][0][None, :])
    m["final_g"] = f(inp["final_g"][None, :])
    m["w_in"] = f(inp["w_in"][0])
    m["w_out"] = f(inp["w_out"][0])
    m["hy_conv_w"] = f(inp["hy_conv_w"][0])
    m["hy_conv_b"] = f(inp["hy_conv_b"][0][None, :])
    m["hy_fw1"] = f(inp["hy_fw1"][0])
    m["hy_fb1"] = f(inp["hy_fb1"][0][:, None])
    m["hy_fw2"] = f(inp["hy_fw2"][0])
    m["hy_fb2"] = f(inp["hy_fb2"][0][:, None])
    m["hy_fw3"] = f(inp["hy_fw3"][0])
    m["hy_freq"] = f(inp["hy_freq"][0][:, None])
    m["hy_d"] = f(inp["hy_d"][0].reshape(1, 1024))
    lg = inp["hg_lb_logits"].reshape(2, 2, 4, 128)
    m["lbT"] = f(lg.transpose(3, 0, 1, 2).reshape(128, 16))
    m["hg_norm_g"] = f(inp["hg_norm_g"][0][None, :])
    m["w_router"] = f(inp["w_router"][0])
    m["router_bias"] = f(inp["router_bias"][0][None, :])
    m["ew_gate"] = f(np.asarray(inp["ew_gate"][0]).reshape(NE, KD, 128, 256).transpose(0, 2, 1, 3).reshape(NE, 128, KD * 256))
    m["ew_up"] = f(np.asarray(inp["ew_up"][0]).reshape(NE, KD, 128, 256).transpose(0, 2, 1, 3).reshape(NE, 128, KD * 256))
    m["ew_down"] = f(np.asarray(inp["ew_down"][0]).reshape(NE, 2, 128, D).transpose(0, 2, 1, 3).reshape(NE, 128, 2 * D))
    m["sw_gate"] = f(inp["sw_gate"][0])
    m["sw_up"] = f(inp["sw_up"][0])
    m["sw_down"] = f(inp["sw_down"][0])
    return m


def kernel(**inputs):
    NB = 4
    ncores = 8
    k = K(NB)
    nc = k.build()
    tables = const_tables()
    sh = shared_inputs(inputs)
    in_maps = []
    for c in range(ncores):
        m = prep_core(inputs, c, NB, tables)
        m.update(sh)
        in_maps.append(m)
    res = run_bass_kernel_spmd(nc, in_maps, core_ids=list(range(ncores)))
    outs = [np.asarray(r["out"], dtype=np.float32).reshape(NB, L, D) for r in res.results]
    return np.concatenate(outs, axis=0)
```

```python
import os
from contextlib import ExitStack
from concourse.bass_utils import run_bass_kernel_spmd
import numpy as np
import concourse.bass as bass
import concourse.mybir as mybir

F32 = mybir.dt.float32
BF16 = mybir.dt.bfloat16
I32 = mybir.dt.int32
U32 = mybir.dt.uint32
AF = mybir.ActivationFunctionType
ALU = mybir.AluOpType
AX = mybir.AxisListType


class Buf:
    __slots__ = ("name", "w", "r")

    def __init__(self, name=""):
        self.name = name
        self.w = None
        self.r = []


class Prog:
    COMPUTE = ("pe", "act", "dve", "pool")
    NDMA = {"sp": 10, "act": 4, "pool": 8}

    def __init__(self, nc, stack, same_engine_sync=True):
        self.nc = nc
        self.same = same_engine_sync
        self.ops = {e: [] for e in ("pe", "act", "dve", "pool", "sp")}
        self.sem = {}
        self.cnt = {}
        for e in self.COMPUTE:
            self.sem[e] = stack.enter_context(nc.semaphore("s_" + e))
            self.cnt[e] = 0
        self.dsem = {}
        self.dcnt = {}
        self.drr = {}
        for q, n in self.NDMA.items():
            for i in range(n):
                k = "d_%s_%d" % (q, i)
                self.sem[k] = stack.enter_context(nc.semaphore(k))
                self.cnt[k] = 0
            self.drr[q] = 0
        self.known = {e: {} for e in self.ops}
        self.nwaits = 0
        self.pre = {}

    def _deps(self, eng, reads, writes, is_dma=False):
        need = {}

        def add(tok):
            if tok is None:
                return
            k, v = tok
            if need.get(k, 0) < v:
                need[k] = v
        for b in reads:
            add(b.w)
        for b in writes:
            add(b.w)
            for t in b.r:
                add(t)
        waits = []
        kn = self.known[eng]
        for k, v in need.items():
            if k == eng and not self.same and not is_dma:
                continue
            if k == "pe" and eng == "pe":
                continue
            if kn.get(k, 0) >= v:
                continue
            kn[k] = v
            waits.append((k, v))
        self.nwaits += len(waits)
        return waits

    def _commit(self, tok, reads, writes):
        for b in reads:
            b.r.append(tok)
            if len(b.r) > 64:
                m = {}
                for k, v in b.r:
                    if m.get(k, 0) < v:
                        m[k] = v
                b.r = list(m.items())
        for b in writes:
            b.w = tok
            b.r = []

    def op(self, eng, fn, reads=(), writes=()):
        waits = self._deps(eng, reads, writes)
        self.cnt[eng] += 1
        tok = (eng, self.cnt[eng])
        self.ops[eng].append((waits, fn, (eng, 1)))
        self._commit(tok, reads, writes)
        return tok

    def dma(self, q, fn, reads=(), writes=()):
        n = self.NDMA[q]
        i = self.drr[q]
        self.drr[q] = (i + 1) % n
        k = "d_%s_%d" % (q, i)
        waits = self._deps(q, reads, writes, is_dma=True)
        prev = self.cnt[k]
        kn = self.known[q]
        if prev > 0 and kn.get(k, 0) < prev:
            kn[k] = prev
            waits.append((k, prev))
        self.cnt[k] += 16
        tok = (k, self.cnt[k])
        self.ops[q].append((waits, fn, (k, 16)))
        self._commit(tok, reads, writes)
        return tok

    def barrier_tokens(self):
        toks = []
        for k, v in self.cnt.items():
            if v > 0:
                toks.append((k, v))
        return toks

    def barrier(self):
        toks = self.barrier_tokens()
        for e in self.ops:
            kn = self.known[e]
            waits = []
            for k, v in toks:
                if k == e and e == "pe":
                    continue
                if kn.get(k, 0) < v:
                    kn[k] = v
                    waits.append((k, v))
            if waits:
                self.ops[e].append((waits, None, None))

    def final_wait(self, eng="sp"):
        toks = self.barrier_tokens()
        self.ops[eng].append(([(k, v) for k, v in toks], None, None))

    def emit(self):
        nc = self.nc
        engmap = {"pe": "tensor", "act": "scalar", "dve": "vector", "pool": "gpsimd", "sp": "sync"}
        with nc.Block() as block:
            for e, attr in engmap.items():
                lst = self.ops[e]

                def body(engine, lst=lst, e=e):
                    if e in self.pre:
                        self.pre[e](engine)
                    for waits, fn, inc in lst:
                        for k, v in waits:
                            engine.wait_ge(self.sem[k], v)
                        if fn is not None:
                            ins = fn(engine)
                            ins.then_inc(self.sem[inc[0]], inc[1])
                getattr(block, attr)(body)


class Arena:
    def __init__(self, nc, stack, nwords, name="arena"):
        self.t = stack.enter_context(nc.sbuf_tensor(name, [128, nwords], F32))
        self.n = nwords
        self.off = 0
        self.marks = []

    def alloc(self, shape, dtype, parts=128):
        n = int(np.prod(shape))
        if dtype == BF16:
            words = (n + 1) // 2
        else:
            words = n
        assert self.off + words <= self.n, "arena overflow %d + %d > %d" % (self.off, words, self.n)
        a = self.t[0:parts, self.off:self.off + words]
        self.off += words
        if dtype != F32:
            a = a.bitcast(dtype)
        if dtype == BF16 and n % 2 == 1:
            a = a[:, 0:n]
        if len(shape) > 1:
            names = " ".join("d%d" % i for i in range(len(shape)))
            kw = {"d%d" % i: int(s) for i, s in enumerate(shape)}
            a = a.rearrange("p (%s) -> p %s" % (names, names), **kw)
        return a

    def mark(self):
        self.marks.append(self.off)

    def release(self):
        self.off = self.marks.pop()

D = 1024
KD = 8
L = 2048
CTX = 256
T = L + CTX
NT = T // 128
NLT = L // 128
NCH = T // 64
HGS = 128.0 ** -0.5
EPS = 1e-6
NE = 256
CAP = int(os.environ.get('KCAP', '384'))
OV = 128
NBLK = CAP // 128
RSCALE = 2.5


def mm(out, lhsT, rhs, start, stop):
    return lambda e: e.matmul(out, lhsT, rhs, start=start, stop=stop)


def tr(out, in_, ident):
    return lambda e: e.transpose(out=out, in_=in_, identity=ident)


def act(out, in_, func, **kw):
    return lambda e: e.activation(out=out, in_=in_, func=func, **kw)


def tt(out, a, b, op):
    return lambda e: e.tensor_tensor(out=out, in0=a, in1=b, op=op)


def ts(out, a, s1, s2, op0, op1=None):
    if op1 is None:
        return lambda e: e.tensor_scalar(out=out, in0=a, scalar1=s1, scalar2=None, op0=op0)
    return lambda e: e.tensor_scalar(out=out, in0=a, scalar1=s1, scalar2=s2, op0=op0, op1=op1)


def stt(out, a, s, b, op0, op1):
    return lambda e: e.scalar_tensor_tensor(out=out, in0=a, scalar=s, in1=b, op0=op0, op1=op1)


def cp(out, in_):
    return lambda e: e.tensor_copy(out=out, in_=in_)


def rsum(out, in_):
    return lambda e: e.reduce_sum(out=out, in_=in_, axis=AX.X)


def dma(out, in_):
    return lambda e: e.dma_start(out=out, in_=in_)


def bcast_rows(dram_ap_tensor, offset, n, parts=128):
    return bass.AP(tensor=dram_ap_tensor, offset=offset, ap=[[0, parts], [1, n]])


class K:
    def __init__(self, NB, dbg=(), upto=4):
        self.NB = NB
        self.upto = upto
        self.cut = int(os.environ.get('KCUT', '0'))
        self.dbg = set(dbg)
        self.nc = bass.Bass("TRN2", target_bir_lowering=False)
        self.outs = []

    def din(self, name, shape, dtype=F32):
        return self.nc.dram_tensor(name, list(shape), dtype, kind="ExternalInput").ap()

    def dscr(self, name, shape, dtype):
        return self.nc.dram_tensor(name, list(shape), dtype, kind="Internal").ap()

    def dout(self, name, shape, dtype=F32):
        self.outs.append(name)
        return self.nc.dram_tensor(name, list(shape), dtype, kind="ExternalOutput").ap()

    def bank(self):
        self.bi = (self.bi + 1) % len(self.rot)
        return self.rot[self.bi]

    def reserve(self, n):
        got = [self.rot.pop() for _ in range(n)]
        self.bi = 0
        return got

    def unreserve(self, got):
        self.rot.extend(got)

    def dump(self, name, ap, buf, shape, dtype=F32):
        if name not in self.dbg:
            return
        o = self.dout("dbg_" + name, shape, dtype)
        self.P.dma("sp", dma(o, ap), reads=[buf])

    def build(self):
        nc = self.nc
        NB = self.NB
        with ExitStack() as st:
            self.st = st
            P = self.P = Prog(nc, st, same_engine_sync=(os.environ.get('KSAME', '1') == '1'))
            A = self.A = Arena(nc, st, 51500)
            self.banks = [(st.enter_context(nc.psum_tensor("pb%d" % i, [128, 512], F32))[:, :], Buf("pb%d" % i)) for i in range(8)]
            self.bi = 0
            self.rot = list(self.banks)
            self.declare_io()
            self.consts()
            self.modulation()
            if self.upto >= 2:
                self.filters()
            if self.upto >= 0.5:
                for b in range(NB):
                    self.mixer_batch(b)
            if self.upto >= 2:
                self.hyena_all()
            if self.upto >= 3:
                self.route_init()
                for b in range(NB):
                    self.outproj_route(b)
                self.overflow_route()
                P.barrier()
                self.A.release()
            if self.upto >= 4:
                self.experts()
                self.combine()
            P.final_wait("sp")
            P.emit()
        return nc

    def declare_io(self):
        NB = self.NB
        d = self.din
        self.x_d = d("x", [NB * L, D])
        self.ctx_d = d("ctx", [NB * CTX, D])
        self.cT_d = d("cT", [128, KD, NB + 1])
        self.wmod_d = d("w_mod", [D, 6 * D]).rearrange("(k p) n -> p k n", p=128)
        self.bmod_d = d("b_mod", [1, 6 * D])
        self.n1g_d = d("norm1_g", [1, D])
        self.n2g_d = d("norm2_g", [1, D])
        self.fing_d = d("final_g", [1, D])
        self.win_d = d("w_in", [D, 4096]).rearrange("(k p) n -> p k n", p=128)
        self.wout_d = d("w_out", [D, D]).rearrange("(k p) n -> p k n", p=128)
        self.hcw_d = d("hy_conv_w", [3, 1536])
        self.hcb_d = d("hy_conv_b", [1, 1536])
        self.fw1_d = d("hy_fw1", [33, 64])
        self.fb1_d = d("hy_fb1", [64, 1])
        self.fw2_d = d("hy_fw2", [64, 64])
        self.fb2_d = d("hy_fb2", [64, 1])
        self.fw3_d = d("hy_fw3", [64, 2048])
        self.freq_d = d("hy_freq", [64, 1])
        self.hyd_d = d("hy_d", [1, 1024])
        self.lbT_d = d("lbT", [128, 16])
        self.hgng_d = d("hg_norm_g", [1, 128])
        self.wr_d = d("w_router", [D, NE]).rearrange("(k p) n -> p k n", p=128)
        self.rb_d = d("router_bias", [1, NE])
        if self.upto >= 4:
            self.ewg_d = d("ew_gate", [NE, 128, KD * 256])
            self.ewu_d = d("ew_up", [NE, 128, KD * 256])
            self.ewd_d = d("ew_down", [NE, 128, 2 * D])
        self.swg_d = d("sw_gate", [D, 256]).rearrange("(k p) n -> p k n", p=128)
        self.swu_d = d("sw_up", [D, 256]).rearrange("(k p) n -> p k n", p=128)
        self.swd_d = d("sw_down", [256, D]).rearrange("(k p) n -> p k n", p=128)
        self.featsT_d = d("featsT", [33, L])
        self.cst_d = d("cst", [128, 10, 128])
        self.deltas_d = d("deltas", [1, 512])
        self.tfrac_d = d("tfrac", [128, 16])
        self.iota_d = d("iota", [1, NE])
        self.pidx_d = d("pidx", [128, 1])
        self.out_d = self.dout("out", [NB * L, D])
        self.MOD_d = self.dscr("MODs", [NB + 1, 6 * D], F32)
        self.HT_d = self.dscr("HTs", [NB, 128, KD * L], BF16)
        self.YT_d = self.dscr("YTs", [NB, 8, 128, L], BF16)
        self.G_d = self.dscr("Gs", [2, 512, 4096], BF16)
        self.X1_d = self.dscr("X1s", [NB * L, D], F32)
        self.XS_d = self.dscr("XSs", [NE * CAP + OV * 128, D], BF16)
        self.Y_d = self.dscr("Ys", [NE * CAP + OV * 128, D], BF16)
        self.H2_d = self.dscr("H2s", [NB * L, D], BF16)

    def consts(self):
        P, A = self.P, self.A
        cst = A.alloc([10, 128], F32)
        self.b_cst = Buf("cst")
        P.dma("sp", dma(cst, self.cst_d), writes=[self.b_cst])
        self.identf = cst[:, 0, :]
        self.Jf = cst[:, 1, :]
        self.maskF = cst[:, 2, :]
        self.maskB = cst[:, 3, :]
        self.ustrict_f = cst[:, 6, :]
        self.ones_f = cst[:, 7, :]
        cb = A.alloc([10, 128], BF16)
        self.b_cb = Buf("cb")
        P.op("dve", cp(cb, cst), reads=[self.b_cst], writes=[self.b_cb])
        self.identb = cb[:, 0, :]
        self.Jb = cb[:, 1, :]
        self.Smb = cb[:, 4, :]
        self.Spb = cb[:, 5, :]
        self.ustrict_b = cb[:, 6, :]
        self.ones_b = cb[:, 7, :]
        self.SmRb = cb[:, 8, :]
        self.SpRb = cb[:, 9, :]
        ce = A.alloc([2], F32)
        self.b_eps = Buf("eps")
        P.op("pool", lambda e: e.memset(ce[:, 0:1], EPS), writes=[self.b_eps])
        P.op("pool", lambda e: e.memset(ce[:, 1:2], 1.0), writes=[self.b_eps])
        self.eps_ap = ce[:, 0:1]
        self.one_ap = ce[:, 1:2]
        self.n1g = A.alloc([D], F32)
        self.b_n1g = Buf()
        P.dma("sp", dma(self.n1g, bcast_rows(self.n1g_d.tensor, 0, D)), writes=[self.b_n1g])
        self.hgng = A.alloc([128], F32)
        self.b_hgng = Buf()
        P.dma("sp", dma(self.hgng, bcast_rows(self.hgng_d.tensor, 0, 128)), writes=[self.b_hgng])
        lbt = A.alloc([2, 2, 4], F32)
        b_lbt = Buf()
        P.dma("sp", dma(lbt, self.lbT_d.rearrange("p (a b c) -> p a b c", a=2, b=2)), writes=[b_lbt])
        self.lb = A.alloc([2, 4], F32)
        self.oml = A.alloc([2, 4], F32)
        self.noml = A.alloc([2, 4], F32)
        self.b_lb = Buf()
        P.op("dve", tt(self.lb, lbt[:, :, 0, :], lbt[:, :, 1, :], ALU.subtract), reads=[b_lbt], writes=[self.b_lb])
        P.op("act", act(self.lb, self.lb, AF.Sigmoid), reads=[self.b_lb], writes=[self.b_lb])
        P.op("dve", ts(self.oml, self.lb, -1.0, 1.0, ALU.mult, ALU.add), reads=[self.b_lb], writes=[self.b_lb])
        P.op("dve", ts(self.noml, self.lb, -1.0, None, ALU.add), reads=[self.b_lb], writes=[self.b_lb])

    def modulation(self):
        P, A, NB = self.P, self.A, self.NB
        A.mark()
        cT = A.alloc([KD, NB + 1], F32)
        b_cT = Buf()
        P.dma("sp", dma(cT, self.cT_d), writes=[b_cT])
        P.op("act", act(cT, cT, AF.Silu), reads=[b_cT], writes=[b_cT])
        bm = A.alloc([6 * D], F32, parts=1)
        b_bm = Buf()
        P.dma("sp", dma(bm, self.bmod_d), writes=[b_bm])
        modsb = A.alloc([6 * D], F32)
        b_mod = Buf()
        wms = [(A.alloc([KD, 512], F32), Buf()) for _ in range(2)]
        for ci in range(12):
            wm, bw = wms[ci % 2]
            P.dma("sp" if ci % 2 == 0 else "act", dma(wm, self.wmod_d[:, :, ci * 512:(ci + 1) * 512]), writes=[bw])
            ps, pb = self.bank()
            for k in range(KD):
                P.op("pe", mm(ps[0:NB + 1, :], cT[:, k, :], wm[:, k, :], k == 0, False), reads=[b_cT, bw], writes=[pb])
            P.op("pe", mm(ps[0:NB + 1, :], self.ones_f[0:1, 0:NB + 1], bm[0:1, ci * 512:(ci + 1) * 512], False, True),
                 reads=[self.b_cst, b_bm], writes=[pb])
            P.op("act", act(modsb[0:NB + 1, ci * 512:(ci + 1) * 512], ps[0:NB + 1, :], AF.Copy), reads=[pb], writes=[b_mod])
        self.b_MOD = Buf("MOD")
        P.dma("sp", dma(self.MOD_d, modsb[0:NB + 1, :]), reads=[b_mod], writes=[self.b_MOD])
        self.dump("mod", modsb[0:NB + 1, :], b_mod, [NB + 1, 6 * D])
        P.barrier()
        A.release()
        self.CA = A.alloc([D], F32)
        self.CB = A.alloc([D], F32)
        self.b_CA = Buf()
        self.b_CB = Buf()
        mt = self.MOD_d.tensor
        P.dma("sp", dma(self.CB, bcast_rows(mt, NB * 6 * D + 0 * D, D)), reads=[self.b_MOD], writes=[self.b_CB])
        P.dma("sp", dma(self.CA, bcast_rows(mt, NB * 6 * D + 1 * D, D)), reads=[self.b_MOD], writes=[self.b_CA])
        P.op("dve", stt(self.CA, self.CA, 1.0, self.n1g, ALU.add, ALU.mult), reads=[self.b_CA, self.b_n1g], writes=[self.b_CA])

    def norm_mod_tile(self, i, src, Abc, bA, Bbc, bB, dst, b_dst, hb_out=None):
        P = self.P
        xt, bx = self.xt[i % 2]
        P.dma("sp", dma(xt, src), writes=[bx])
        self.norm_mod_sb(i, xt, bx, Abc, bA, Bbc, bB, dst, b_dst)

    def norm_mod_sb(self, i, xt, bx, Abc, bA, Bbc, bB, dst, b_dst, hf_out=None, hb_out=None):
        P = self.P
        junk, bj = self.junk
        ss, bss = self.ssb[i % 2]
        hb, bhb = (self.hb[i % 2] if hb_out is None else hb_out)
        P.op("act", act(junk, xt, AF.Square), reads=[bx], writes=[bj])
        P.op("dve", rsum(ss, junk), reads=[bj], writes=[bss])
        P.op("act", act(ss, ss, AF.Sqrt, scale=1.0 / D, bias=self.eps_ap), reads=[bss, self.b_eps], writes=[bss])
        P.op("dve", lambda e: e.reciprocal(out=ss, in_=ss), reads=[bss], writes=[bss])
        P.op("dve", stt(junk, xt, ss, Abc, ALU.mult, ALU.mult), reads=[bx, bss, bA], writes=[bj])
        if hf_out is not None:
            hf, bhf = hf_out
            P.op("dve", tt(hf, junk, Bbc, ALU.add), reads=[bj, bB], writes=[bhf])
            P.op("act", act(hb, hf, AF.Copy), reads=[bhf], writes=[bhb])
        else:
            P.op("dve", tt(hb, junk, Bbc, ALU.add), reads=[bj, bB], writes=[bhb])
        ps, pb = self.bank()
        psb = ps.bitcast(BF16)
        for k in range(KD):
            P.op("pe", tr(psb[:, k * 128:(k + 1) * 128], hb[:, k * 128:(k + 1) * 128], self.identb),
                 reads=[bhb, self.b_cb], writes=[pb])
        P.op("act", act(dst, psb.rearrange("p (k t) -> p k t", k=KD), AF.Copy), reads=[pb], writes=[b_dst])

    def mixer_batch(self, b):
        P, A, NB = self.P, self.A, self.NB
        A.mark()
        mt = self.MOD_d.tensor
        A1 = A.alloc([D], F32)
        B1 = A.alloc([D], F32)
        bA1, bB1 = Buf(), Buf()
        P.dma("sp", dma(B1, bcast_rows(mt, b * 6 * D + 0 * D, D)), reads=[self.b_MOD], writes=[bB1])
        P.dma("sp", dma(A1, bcast_rows(mt, b * 6 * D + 1 * D, D)), reads=[self.b_MOD], writes=[bA1])
        P.op("dve", stt(A1, A1, 1.0, self.n1g, ALU.add, ALU.mult), reads=[bA1, self.b_n1g], writes=[bA1])
        hT = A.alloc([KD, T], BF16)
        b_hT = Buf("hT")
        yT = A.alloc([4, L], BF16)
        b_yT = Buf("yT")
        A.mark()
        self.xt = [(A.alloc([D], F32), Buf()) for _ in range(2)]
        self.junk = (A.alloc([D], F32), Buf())
        self.ssb = [(A.alloc([1], F32), Buf()) for _ in range(2)]
        self.hb = [(A.alloc([D], BF16), Buf()) for _ in range(2)]
        for j in range(NT):
            if j < 2:
                src = self.ctx_d[b * CTX + j * 128: b * CTX + (j + 1) * 128, :]
                self.norm_mod_tile(j, src, self.CA, self.b_CA, self.CB, self.b_CB, hT[:, :, j * 128:(j + 1) * 128], b_hT)
            else:
                src = self.x_d[b * L + (j - 2) * 128: b * L + (j - 1) * 128, :]
                self.norm_mod_tile(j, src, A1, bA1, B1, bB1, hT[:, :, j * 128:(j + 1) * 128], b_hT)
        self.b_HT = getattr(self, "b_HT", None) or Buf("HT")
        P.dma("sp", dma(self.HT_d[b].rearrange("p (k t) -> p k t", k=KD), hT[:, :, CTX:T]), reads=[b_hT], writes=[self.b_HT])
        if b == 0:
            self.dump("hT", hT, b_hT, [128, KD, T], BF16)
        P.barrier()
        A.release()
        if self.upto < 1:
            A.release()
            return
        self.hgrn2(b, hT, b_hT, yT, b_yT)
        self.b_YT = getattr(self, "b_YT", None) or Buf("YT")
        for hh in range(4):
            P.dma("sp", dma(self.YT_d[b, 4 + hh], yT[:, hh, :]), reads=[b_yT], writes=[self.b_YT])
        if b == 0:
            self.dump("yT_hg", yT, b_yT, [128, 4, L], BF16)
        P.barrier()
        A.release()

    def hgrn2(self, b, hT, b_hT, yT, b_yT):
        P, A = self.P, self.A
        A.mark()
        f32b = lambda: (A.alloc([T], F32), Buf())
        bf16b = lambda: (A.alloc([T], BF16), Buf())
        self.rs = A.alloc([T], F32)
        self.b_rs = Buf()
        P.op("pool", lambda e: e.memset(self.rs, 1.0), writes=[self.b_rs])
        rs3 = self.rs.rearrange("p (a b) -> p a b", b=64)
        P.op("pool", lambda e: e.memset(rs3[:, :, 0:1], 0.0), writes=[self.b_rs])
        t1, b_t1 = f32b()
        kk, b_kk = f32b()
        bb, b_bb = f32b()
        t2, b_t2 = f32b()
        qs, b_qs = bf16b()
        qm, b_qm = bf16b()
        km, b_km = bf16b()
        qbE, b_qbE = bf16b()
        qbO, b_qbO = bf16b()
        kdT, b_kdT = bf16b()
        P.op("pool", lambda e: e.memset(qbE, 0.0), writes=[b_qbE])
        P.op("pool", lambda e: e.memset(qbO, 0.0), writes=[b_qbO])
        kdTokE = A.alloc([NT, 128], BF16)
        kdTokO = A.alloc([NT, 128], BF16)
        b_kdTok = Buf()
        P.op("pool", lambda e: e.memset(kdTokE[64:128], 0.0), writes=[b_kdTok])
        P.op("pool", lambda e: e.memset(kdTokO[0:64], 0.0), writes=[b_kdTok])
        V = A.alloc([NT, 128], BF16)
        b_V = Buf()
        gsn = A.alloc([NLT, 128], BF16)
        b_gsn = Buf()
        of = A.alloc([NLT, 128], F32)
        b_of = Buf()
        dec = A.alloc([NCH], F32)
        b_dec = Buf()
        ws = [(A.alloc([KD, 128], BF16), Buf()) for _ in range(5)]
        Sf = [(A.alloc([128], F32), Buf()) for _ in range(2)]
        Sb = [(A.alloc([128], BF16), Buf()) for _ in range(2)]
        attT = [(A.alloc([128], BF16), Buf()) for _ in range(2)]
        otl = [(A.alloc([128], F32), Buf()) for _ in range(2)]
        oj = [(A.alloc([128], F32), Buf()) for _ in range(2)]
        ossb = [(A.alloc([1], F32), Buf()) for _ in range(2)]
        ytl = [(A.alloc([128], BF16), Buf()) for _ in range(2)]
        gtmp = [(A.alloc([128], F32), Buf()) for _ in range(2)]
        chunks = [(t0, min(512, T - t0)) for t0 in range(0, T, 512)]
        v3 = lambda ap: ap.rearrange("p (a b) -> p a b", b=64)
        for hh in range(4):
            cols = [1536 + s * 512 + hh * 128 for s in range(5)]
            for s in range(5):
                w, bw = ws[s]
                P.dma("pool", dma(w, self.win_d[:, :, cols[s]:cols[s] + 128]), writes=[bw])
            wq, wf, wb_, wi, wg = ws
            for j in range(NT):
                ps, pb = self.bank()
                for k in range(KD):
                    P.op("pe", mm(ps[:, 0:128], hT[:, k, j * 128:(j + 1) * 128], wi[0][:, k, :], k == 0, k == KD - 1),
                         reads=[b_hT, wi[1]], writes=[pb])
                P.op("act", act(V[:, j, :], ps[:, 0:128], AF.Copy), reads=[pb], writes=[b_V])
                if j >= 2:
                    ps2, pb2 = self.bank()
                    for k in range(KD):
                        P.op("pe", mm(ps2[:, 0:128], hT[:, k, j * 128:(j + 1) * 128], wg[0][:, k, :], k == 0, k == KD - 1),
                             reads=[b_hT, wg[1]], writes=[pb2])
                    gt, bgt = gtmp[j % 2]
                    P.op("act", act(gt, ps2[:, 0:128], AF.Silu), reads=[pb2], writes=[bgt])
                    P.op("dve", tt(gsn[:, j - 2, :], gt, self.hgng, ALU.mult), reads=[bgt, self.b_hgng], writes=[b_gsn])
            for (t0, n) in chunks:
                ps, pb = self.bank()
                for k in range(KD):
                    P.op("pe", mm(ps[:, 0:n], wq[0][:, k, :], hT[:, k, t0:t0 + n], k == 0, k == KD - 1),
                         reads=[b_hT, wq[1]], writes=[pb])
                P.op("act", act(qs[:, t0:t0 + n], ps[:, 0:n], AF.Silu), reads=[pb], writes=[b_qs])
            for d in range(2):
                wgate = wf if d == 0 else wb_
                for (t0, n) in chunks:
                    ps, pb = self.bank()
                    for k in range(KD):
                        P.op("pe", mm(ps[:, 0:n], wgate[0][:, k, :], hT[:, k, t0:t0 + n], k == 0, k == KD - 1),
                             reads=[b_hT, wgate[1]], writes=[pb])
                    P.op("act", act(t1[:, t0:t0 + n], ps[:, 0:n], AF.Sigmoid), reads=[pb], writes=[b_t1])
                P.op("dve", ts(kk, t1, self.noml[:, d, hh:hh + 1], self.oml[:, d, hh:hh + 1], ALU.mult, ALU.add),
                     reads=[b_t1, self.b_lb], writes=[b_kk])
                P.op("act", act(t1, kk, AF.Ln, scale=-1.0, bias=self.one_ap), reads=[b_kk, self.b_eps], writes=[b_t1])
                P.op("dve", lambda e: e.tensor_tensor_scan(out=bb, data0=self.rs, data1=t1, initial=0.0, op0=ALU.mult, op1=ALU.add),
                     reads=[self.b_rs, b_t1], writes=[b_bb])
                bb3, t13 = v3(bb), v3(t1)
                if d == 1:
                    P.op("dve", tt(t1, t1, bb, ALU.subtract), reads=[b_t1, b_bb], writes=[b_t1])
                    P.op("dve", tt(bb3, t13, bb3[:, :, 63:64].to_broadcast([128, NCH, 64]), ALU.add), reads=[b_t1, b_bb], writes=[b_bb])
                mid = 31 if d == 0 else 32
                last = 63 if d == 0 else 0
                P.op("dve", tt(t13, bb3, bb3[:, :, mid:mid + 1].to_broadcast([128, NCH, 64]), ALU.subtract), reads=[b_bb], writes=[b_t1])
                P.op("act", act(t2, t1, AF.Exp), reads=[b_t1], writes=[b_t2])
                P.op("dve", stt(qm, qs, HGS, t2, ALU.mult, ALU.mult), reads=[b_qs, b_t2], writes=[b_qm])
                P.op("act", act(t2, t1, AF.Exp, scale=-1.0), reads=[b_t1, b_qm], writes=[b_t2])
                P.op("dve", tt(km, kk, t2, ALU.mult), reads=[b_kk, b_t2], writes=[b_km])
                P.op("act", act(t2, bb, AF.Exp), reads=[b_bb, b_km], writes=[b_t2])
                t23, qs3, qbE3, qbO3 = v3(t2), v3(qs), v3(qbE), v3(qbO)
                P.op("dve", stt(qbE3[:, 0::2, :], qs3[:, 0::2, :], HGS, t23[:, 0::2, :], ALU.mult, ALU.mult),
                     reads=[b_qs, b_t2], writes=[b_qbE])
                P.op("dve", stt(qbO3[:, 1::2, :], qs3[:, 1::2, :], HGS, t23[:, 1::2, :], ALU.mult, ALU.mult),
                     reads=[b_qs, b_t2], writes=[b_qbO])
                P.op("dve", tt(t13, bb3[:, :, last:last + 1].to_broadcast([128, NCH, 64]), bb3, ALU.subtract), reads=[b_bb, b_t2], writes=[b_t1])
                P.op("act", act(t1, t1, AF.Exp), reads=[b_t1], writes=[b_t1])
                P.op("dve", tt(kdT, kk, t1, ALU.mult), reads=[b_kk, b_t1], writes=[b_kdT])
                P.op("act", act(dec, bb3[:, :, last], AF.Exp), reads=[b_bb], writes=[b_dec])
                for j0 in range(0, NT, 8):
                    nj = min(8, NT - j0)
                    ps, pb = self.bank()
                    psb = ps.bitcast(BF16)
                    for jj in range(nj):
                        j = j0 + jj
                        P.op("pe", tr(psb[:, jj * 128:(jj + 1) * 128], kdT[:, j * 128:(j + 1) * 128], self.identb),
                             reads=[b_kdT, self.b_cb], writes=[pb])
                    P.op("act", act(kdTokE[0:64, j0:j0 + nj, :], psb[0:64, 0:nj * 128].rearrange("p (a b) -> p a b", b=128), AF.Copy),
                         reads=[pb], writes=[b_kdTok])
                    P.op("act", act(kdTokO[64:128, j0:j0 + nj, :], psb[64:128, 0:nj * 128].rearrange("p (a b) -> p a b", b=128), AF.Copy),
                         reads=[pb], writes=[b_kdTok])
                si = 0
                P.op("pool", lambda e, s=Sf[0][0]: e.memset(s, 0.0), writes=[Sf[0][1]])
                P.op("pool", lambda e, s=Sb[0][0]: e.memset(s, 0.0), writes=[Sb[0][1]])
                order = list(range(NT)) if d == 0 else [1, 0] + list(range(NT - 1, 1, -1))
                mask = self.maskF if d == 0 else self.maskB
                for j in order:
                    lat = j >= 2
                    tsl = slice(j * 128, (j + 1) * 128)
                    halves = [0, 1] if d == 0 else [1, 0]
                    if lat:
                        psA, pbA = self.bank()
                        P.op("pe", mm(psA[:, 0:128], km[:, tsl], qm[:, tsl], True, True), reads=[b_km, b_qm], writes=[pbA])
                        at, bat = attT[j % 2]
                        P.op("dve", tt(at, psA[:, 0:128], mask, ALU.mult), reads=[pbA, self.b_cst], writes=[bat])
                        psO, pbO = self.bank()
                        P.op("pe", mm(psO[:, 0:128], at, V[:, j, :], True, False), reads=[bat, b_V], writes=[pbO])
                    for hi, h in enumerate(halves):
                        if lat:
                            qbx, b_qbx = (qbE, b_qbE) if h == 0 else (qbO, b_qbO)
                            P.op("pe", mm(psO[:, 0:128], qbx[:, tsl], Sb[si][0], False, hi == 1),
                                 reads=[b_qbx, Sb[si][1]], writes=[pbO])
                        psU, pbU = self.bank()
                        kdx = kdTokE if h == 0 else kdTokO
                        P.op("pe", mm(psU[:, 0:128], kdx[:, j, :], V[:, j, :], True, True), reads=[b_kdTok, b_V], writes=[pbU])
                        ch = 2 * j + h
                        P.op("dve", stt(Sb[1 - si][0], Sf[si][0], dec[:, ch:ch + 1], psU[:, 0:128], ALU.mult, ALU.add),
                             reads=[Sf[si][1], b_dec, pbU], writes=[Sb[1 - si][1]])
                        P.op("dve", stt(Sf[1 - si][0], Sf[si][0], dec[:, ch:ch + 1], psU[:, 0:128], ALU.mult, ALU.add),
                             reads=[Sf[si][1], b_dec, pbU], writes=[Sf[1 - si][1]])
                        si = 1 - si
                    if lat:
                        if d == 0:
                            P.op("act", act(of[:, j - 2, :], psO[:, 0:128], AF.Copy), reads=[pbO], writes=[b_of])
                        else:
                            o, bo = oj[j % 2]
                            P.op("dve", tt(o, psO[:, 0:128], of[:, j - 2, :], ALU.add), reads=[pbO, b_of], writes=[bo])
                            jk, bjk = otl[j % 2]
                            oss, boss = ossb[j % 2]
                            P.op("act", act(jk, o, AF.Square), reads=[bo], writes=[bjk])
                            P.op("dve", rsum(oss, jk), reads=[bjk], writes=[boss])
                            P.op("act", act(oss, oss, AF.Sqrt, scale=1.0 / 128, bias=self.eps_ap), reads=[boss, self.b_eps], writes=[boss])
                            P.op("dve", lambda e, oss=oss: e.reciprocal(out=oss, in_=oss), reads=[boss], writes=[boss])
                            yt, byt = ytl[j % 2]
                            P.op("dve", stt(yt, o, oss, gsn[:, j - 2, :], ALU.mult, ALU.mult), reads=[bo, boss, b_gsn], writes=[byt])
                            psT, pbT = self.bank()
                            psTb = psT.bitcast(BF16)
                            P.op("pe", tr(psTb[:, 0:128], yt, self.identb), reads=[byt, self.b_cb], writes=[pbT])
                            P.op("act", act(yT[:, hh, (j - 2) * 128:(j - 1) * 128], psTb[:, 0:128], AF.Copy), reads=[pbT], writes=[b_yT])
        P.barrier()
        A.release()

    def sin_da(self, out, arg, tmpa, tmpb, bufs):
        P = self.P
        b_out, b_arg, b_ta, b_tb = bufs
        P.op("act", act(tmpa, arg, AF.Sin, scale=0.5), reads=[b_arg], writes=[b_ta])
        P.op("act", act(tmpb, arg, AF.Sin, scale=0.25), reads=[b_arg], writes=[b_tb])
        P.op("dve", tt(tmpb, tmpb, tmpb, ALU.mult), reads=[b_tb], writes=[b_tb])
        P.op("dve", ts(tmpb, tmpb, -2.0, 1.0, ALU.mult, ALU.add), reads=[b_tb], writes=[b_tb])
        P.op("dve", stt(out, tmpa, 2.0, tmpb, ALU.mult, ALU.mult), reads=[b_ta, b_tb], writes=[b_out])

    def filters(self):
        P, A = self.P, self.A
        A.mark()
        ld = lambda shape, src, parts: (A.alloc(shape, F32, parts=parts), Buf())
        featsT, b_ft = ld([L], None, 33)
        P.dma("sp", dma(featsT, self.featsT_d), writes=[b_ft])
        fw1, b_fw1 = ld([64], None, 33)
        P.dma("sp", dma(fw1, self.fw1_d), writes=[b_fw1])
        fw2, b_fw2 = ld([64], None, 64)
        P.dma("sp", dma(fw2, self.fw2_d), writes=[b_fw2])
        fw3, b_fw3 = ld([2048], None, 64)
        P.dma("sp", dma(fw3, self.fw3_d), writes=[b_fw3])
        sm, b_sm = ld([8], None, 64)
        P.dma("sp", dma(sm[:, 0:1], self.fb1_d), writes=[b_sm])
        P.dma("sp", dma(sm[:, 1:2], self.fb2_d), writes=[b_sm])
        P.dma("sp", dma(sm[:, 2:3], self.freq_d), writes=[b_sm])
        P.op("dve", ts(sm[:, 3:5], sm[:, 0:2], sm[:, 2:3], None, ALU.mult), reads=[b_sm], writes=[b_sm])
        dl, b_dl = ld([512], None, 128)
        P.dma("sp", dma(dl, bcast_rows(self.deltas_d.tensor, 0, 512)), writes=[b_dl])
        tf, b_tf = ld([16], None, 128)
        P.dma("sp", dma(tf, self.tfrac_d), writes=[b_tf])
        dsk, b_dsk = ld([2, 4], None, 128)
        P.dma("sp", lambda e: e.dma_start(out=dsk, in_=self.hyd_d.rearrange("a (o g c) -> c (a o) g", o=2, g=4),
                                          allow_slow_non_contiguous=True), writes=[b_dsk])
        h1, b_h1 = ld([L], None, 64)
        h2, b_h2 = ld([L], None, 64)
        ta, b_ta = ld([L], None, 64)
        tb, b_tb = ld([L], None, 64)
        ar, b_ar = ld([L], None, 64)
        for layer in range(2):
            src, b_src, K_, w, b_w = (featsT, b_ft, 33, fw1, b_fw1) if layer == 0 else (h1, b_h1, 64, fw2, b_fw2)
            for q in range(4):
                ps, pb = self.bank()
                P.op("pe", mm(ps[0:64, :], w[0:K_, :], src[0:K_, q * 512:(q + 1) * 512], True, True), reads=[b_w, b_src], writes=[pb])
                P.op("act", act(ar[:, q * 512:(q + 1) * 512], ps[0:64, :], AF.Identity, scale=sm[:, 2:3], bias=sm[:, 3 + layer:4 + layer]),
                     reads=[pb, b_sm], writes=[b_ar])
            dst, b_dst = (h1, b_h1) if layer == 0 else (h2, b_h2)
            self.sin_da(dst, ar, ta, tb, (b_dst, b_ar, b_ta, b_tb))
        rinv = A.alloc([2, 512], F32)
        b_rinv = Buf()
        win = [(A.alloc([512], F32), Buf()) for _ in range(2)]
        winr = [(A.alloc([2, 512], F32), Buf()) for _ in range(2)]
        hw = [(A.alloc([512], F32), Buf()) for _ in range(2)]
        hn = [(A.alloc([512], BF16), Buf()) for _ in range(2)]
        GT = A.alloc([4, 2, 4096], BF16)
        b_GT = Buf("GT")
        P.op("pool", lambda e: e.memset(GT, 0.0), writes=[b_GT])
        accs = self.reserve(2)
        for i in range(16):
            wi_, bwi = win[i % 2]
            P.op("act", act(wi_, dl, AF.Exp, scale=tf[:, i:i + 1]), reads=[b_dl, b_tf], writes=[bwi])
            for o in range(2):
                for dr in range(2):
                    q = o * 2 + dr
                    ps, pb = self.bank()
                    P.op("pe", mm(ps[:, :], h2[0:64, i * 128:(i + 1) * 128], fw3[0:64, q * 512:(q + 1) * 512], True, True),
                         reads=[b_h2, b_fw3], writes=[pb])
                    hwt, bhw = hw[q % 2]
                    P.op("dve", tt(hwt, ps, wi_, ALU.mult), reads=[pb, bwi], writes=[bhw])
                    P.op("act", act(hwt, hwt, AF.Abs), reads=[bhw], writes=[bhw])
                    first = (i == 0 and dr == 0)
                    lastf = (i == 15 and dr == 1)
                    P.op("pe", mm(accs[o][0][:, :], self.ones_f, hwt, first, lastf), reads=[self.b_cst, bhw], writes=[accs[o][1]])
        for o in range(2):
            P.op("dve", lambda e, o=o: e.reciprocal(out=rinv[:, o, :], in_=accs[o][0][:, :]), reads=[accs[o][1]], writes=[b_rinv])
        self.unreserve(accs)
        GTv = GT
        for i in range(16):
            wi_, bwi = win[i % 2]
            wr, bwr = winr[i % 2]
            P.op("act", act(wi_, dl, AF.Exp, scale=tf[:, i:i + 1]), reads=[b_dl, b_tf], writes=[bwi])
            P.op("dve", tt(wr, rinv, wi_.unsqueeze(1).to_broadcast([128, 2, 512]), ALU.mult), reads=[bwi, b_rinv], writes=[bwr])
            for o in range(2):
                for dr in ([1, 0] if o == 0 else [0, 1]):
                    q = o * 2 + dr
                    useJ = (o == 0 and dr == 1) or (o == 1 and dr == 0)
                    ps, pb = self.bank()
                    P.op("pe", mm(ps[:, :], h2[0:64, i * 128:(i + 1) * 128], fw3[0:64, q * 512:(q + 1) * 512], True, True),
                         reads=[b_h2, b_fw3], writes=[pb])
                    hnt, bhn = hn[q % 2]
                    P.op("dve", tt(hnt, ps, wr[:, o, :], ALU.mult), reads=[pb, bwr], writes=[bhn])
                    pt, pbt = self.bank()
                    for cg in range(4):
                        P.op("pe", mm(pt[:, cg * 128:(cg + 1) * 128], hnt[:, cg * 128:(cg + 1) * 128], self.Jb if useJ else self.identb, True, True),
                             reads=[bhn, self.b_cb], writes=[pbt])
                    pt3 = pt.rearrange("p (g m) -> p g m", g=4)
                    if o == 0:
                        start = (1921 - 128 * i) if useJ else (2048 + 128 * i)
                    else:
                        start = (1920 - 128 * i) if useJ else (2047 + 128 * i)
                    if i == 0 and not useJ:
                        P.op("act", act(GTv[:, :, o, start + 1:start + 128], pt3[:, :, 1:128], AF.Copy), reads=[pbt], writes=[b_GT])
                        ctr, b_ctr = self.ctr_tmp = getattr(self, "ctr_tmp", None) or (A.alloc([4], F32), Buf())
                        P.op("dve", tt(ctr, pt3[:, :, 0], dsk[:, o, :], ALU.add), reads=[pbt, b_dsk], writes=[b_ctr])
                        P.op("dve", tt(GTv[:, :, o, start], GTv[:, :, o, start], ctr, ALU.add), reads=[b_ctr, b_GT], writes=[b_GT])
                    else:
                        P.op("act", act(GTv[:, :, o, start:start + 128], pt3, AF.Copy), reads=[pbt], writes=[b_GT])
        self.b_G = Buf("G")
        for o in range(2):
            for cg in range(4):
                P.dma("sp", dma(self.G_d[o, cg * 128:(cg + 1) * 128, :], GTv[:, cg, o, :]), reads=[b_GT], writes=[self.b_G])
        if "G" in self.dbg:
            for o in range(2):
                og = self.dout("dbg_G%d" % o, [128, 4, 4096], BF16)
                P.dma("sp", dma(og, GTv[:, :, o, :]), reads=[b_GT])
        P.barrier()
        A.release()

    def hyena_all(self):
        P, A, NB = self.P, self.A, self.NB
        NBI = NB * 16
        A.mark()
        bf = lambda: A.alloc([128, NB, 16], BF16)
        X1, X2r, Vr, Z = bf(), bf(), bf(), bf()
        b_in = [Buf() for _ in range(16)]
        b_Z = [Buf() for _ in range(16)]
        hTl = A.alloc([KD, L], BF16)
        b_hTl = Buf()
        W3 = A.alloc([KD, 384], BF16)
        b_W3 = Buf()
        wc = A.alloc([3, 384], F32)
        bc = A.alloc([384], F32)
        b_wc = Buf()
        pw = [(A.alloc([3, 384], BF16), Buf()) for _ in range(2)]
        NTZ = 3
        TZ = [[(A.alloc([3968], BF16), Buf()) for _ in range(NTZ)] for _ in range(2)]
        yTc = [(A.alloc([512], BF16), Buf()) for _ in range(2)]
        gt = self.G_d.tensor
        dq = ["sp", "act"]
        tzc = [0, 0]
        for cg in range(4):
            for s in range(3):
                c0 = s * 512 + cg * 128
                P.dma("pool", dma(W3[:, :, s * 128:(s + 1) * 128], self.win_d[:, :, c0:c0 + 128]), writes=[b_W3])
                for kk in range(3):
                    P.dma("sp", dma(wc[:, kk, s * 128:(s + 1) * 128], bcast_rows(self.hcw_d.tensor, kk * 1536 + c0, 128)), writes=[b_wc])
                P.dma("sp", dma(bc[:, s * 128:(s + 1) * 128], bcast_rows(self.hcb_d.tensor, c0, 128)), writes=[b_wc])
            for b in range(NB):
                P.dma("sp", dma(hTl, self.HT_d[b].rearrange("p (k t) -> p k t", k=KD)), reads=[self.b_HT], writes=[b_hTl])
                for i in range(NLT):
                    ps, pb = self.bank()
                    for k in range(KD):
                        P.op("pe", mm(ps[:, 0:384], hTl[:, k, i * 128:(i + 1) * 128], W3[:, k, :], k == 0, k == KD - 1),
                             reads=[b_hTl, b_W3], writes=[pb])
                    pwt, bpw = pw[i % 2]
                    for kk in range(3):
                        P.op("dve", tt(pwt[:, kk, :], ps[:, 0:384], wc[:, kk, :], ALU.mult), reads=[pb, b_wc], writes=[bpw])
                    pa, pba = self.bank()
                    pr, pbr = self.bank()
                    mats = [(self.Smb, self.SmRb), (self.identb, self.Jb), (self.Spb, self.SpRb)]
                    for kk in range(3):
                        P.op("pe", mm(pa[:, 0:128], mats[kk][0], pwt[:, kk, 0:128], kk == 0, kk == 2), reads=[bpw, self.b_cb], writes=[pba])
                    for kk in range(3):
                        P.op("pe", mm(pr[:, 0:256], mats[kk][1], pwt[:, kk, 128:384], kk == 0, kk == 2), reads=[bpw, self.b_cb], writes=[pbr])
                    P.op("dve", tt(X1[:, :, b, i], pa[:, 0:128], bc[:, 0:128], ALU.add), reads=[pba, b_wc], writes=b_in)
                    P.op("dve", tt(X2r[:, :, b, i], pr[:, 0:128], bc[:, 128:256], ALU.add), reads=[pbr, b_wc], writes=b_in)
                    P.op("dve", tt(Vr[:, :, b, i], pr[:, 128:256], bc[:, 256:384], ALU.add), reads=[pbr, b_wc], writes=b_in)
            deltas = [0] + [s * m for m in range(1, 16) for s in (1, -1)]

            def conv(o, g, src):
                ps, pb = self.bank()
                for cc in range(8):
                    c = g * 8 + cc
                    ch = cg * 128 + c
                    tz, btz = TZ[o][tzc[o] % NTZ]
                    tzc[o] += 1
                    off = (o * 512 + ch) * 4096 + (1 if o == 0 else 0)
                    P.dma(dq[(c + o) % 2], dma(tz, bass.AP(tensor=gt, offset=off, ap=[[1, 128], [1, 3968]])), reads=[self.b_G], writes=[btz])
                    psv = ps[:, cc * NBI:(cc + 1) * NBI].rearrange("p (b i) -> p b i", b=NB)
                    for di, dl_ in enumerate(deltas):
                        ilo, ihi = max(0, -dl_), min(16, 16 - dl_)
                        blk = (dl_ + 15) if o == 0 else (-dl_ + 15)
                        P.op("pe", mm(psv[:, :, ilo + dl_:ihi + dl_], tz[:, blk * 128:(blk + 1) * 128], src[:, c, :, ilo:ihi],
                                      di == 0, di == len(deltas) - 1),
                             reads=[btz, (b_in[g] if o == 0 else b_Z[g])], writes=[pb])
                return ps, pb

            def evac1(g, ps, pb):
                P.op("dve", tt(Z[:, g * 8:(g + 1) * 8, :, :].rearrange("p c b i -> p (c b i)"), ps[:, 0:8 * NBI],
                               X1[:, g * 8:(g + 1) * 8, :, :].rearrange("p c b i -> p (c b i)"), ALU.mult),
                     reads=[pb, b_in[g]], writes=[b_Z[g]])

            def evac2(g, ps, pb):
                P.op("dve", tt(Z[:, g * 8:(g + 1) * 8, :, :].rearrange("p c b i -> p (c b i)"), ps[:, 0:8 * NBI],
                               X2r[:, g * 8:(g + 1) * 8, :, :].rearrange("p c b i -> p (c b i)"), ALU.mult),
                     reads=[pb, b_in[g]], writes=[b_Z[g]])

            p1 = conv(0, 0, Vr)
            for g in range(16):
                evac1(g, *p1)
                if g + 1 < 16:
                    p1 = conv(0, g + 1, Vr)
                p2 = conv(1, g, Z)
                evac2(g, *p2)
            for b in range(NB):
                for i0 in range(0, NLT, 4):
                    ps, pb = self.bank()
                    for ii in range(4):
                        P.op("pe", mm(ps[:, ii * 128:(ii + 1) * 128], Z[:, :, b, i0 + ii], self.Jb, True, True), reads=b_Z + [self.b_cb], writes=[pb])
                    yt, byt = yTc[(i0 // 4) % 2]
                    P.op("act", act(yt, ps, AF.Copy), reads=[pb], writes=[byt])
                    P.dma("sp", dma(self.YT_d[b, cg, :, i0 * 128:(i0 + 4) * 128], yt), reads=[byt], writes=[self.b_YT])
                    if b == 0 and "yT_hy" in self.dbg:
                        if not hasattr(self, "dbg_hy"):
                            self.dbg_hy = self.dout("dbg_yT_hy", [128, 4, L], BF16)
                        P.dma("sp", dma(self.dbg_hy[:, cg, i0 * 128:(i0 + 4) * 128], yt), reads=[byt])
        P.barrier()
        A.release()

    def route_init(self):
        P, A, NB = self.P, self.A, self.NB
        NTT = NB * NLT
        self.destAll = A.alloc([NTT, 8], I32)
        self.wAll = A.alloc([NTT, 8], F32)
        self.b_dw = Buf("destw")
        self.cnt = A.alloc([NE], F32)
        self.b_cnt = Buf("cnt")
        P.op("pool", lambda e: e.memset(self.cnt, 0.0), writes=[self.b_cnt])
        self.b_rc = Buf("routeconst")
        self.E8All = A.alloc([NTT, 8], F32)
        self.P8All = A.alloc([NTT, 8], F32)
        self.W8raw = A.alloc([NTT, 8], F32)
        self.DestF = A.alloc([NTT, 8], F32)
        self.b_ov = Buf("ovstate")
        self.IDXG = A.alloc([OV], I32)
        self.b_idx = Buf("ovidx")
        self.b_H2 = Buf("H2")
        A.mark()
        self.iota3 = A.alloc([NE], F32)
        self.rbias = A.alloc([NE], F32)
        self.n2g = A.alloc([D], F32)
        P.dma("sp", dma(self.iota3, bcast_rows(self.iota_d.tensor, 0, NE)), writes=[self.b_rc])
        P.dma("sp", dma(self.rbias, bcast_rows(self.rb_d.tensor, 0, NE)), writes=[self.b_rc])
        P.dma("sp", dma(self.n2g, bcast_rows(self.n2g_d.tensor, 0, D)), writes=[self.b_rc])
        self.wr = A.alloc([KD, NE], F32)
        P.dma("sp", dma(self.wr, self.wr_d), writes=[self.b_rc])
        self.wout = A.alloc([KD, D], BF16)
        self.swgu = A.alloc([KD, 512], BF16)
        self.swd = A.alloc([2, D], BF16)
        self.b_wts = Buf("wts")
        for k in range(KD):
            P.dma("pool", dma(self.wout[:, k, :], self.wout_d[:, k, :]), writes=[self.b_wts])
        P.dma("pool", dma(self.swgu[:, :, 0:256], self.swg_d), writes=[self.b_wts])
        P.dma("pool", dma(self.swgu[:, :, 256:512], self.swu_d), writes=[self.b_wts])
        P.dma("pool", dma(self.swd, self.swd_d), writes=[self.b_wts])
        self.b_X1 = Buf("X1")
        self.b_XS = Buf("XS")

        def pre_pool(engine):
            self.bc_reg = engine.to_reg(NE * CAP - 1)
            self.bc_reg2 = engine.to_reg(NE * CAP + OV * 128 - 1)
            self.bc_reg3 = engine.to_reg(NE * 128 - 1)
        P.pre["pool"] = pre_pool

    def outproj_route(self, b):
        P, A, NB = self.P, self.A, self.NB
        A.mark()
        mt = self.MOD_d.tensor
        G1 = A.alloc([D], F32)
        A2 = A.alloc([D], F32)
        B2 = A.alloc([D], F32)
        G2 = A.alloc([D], F32)
        b_m = Buf()
        P.dma("sp", dma(G1, bcast_rows(mt, b * 6 * D + 2 * D, D)), reads=[self.b_MOD], writes=[b_m])
        P.dma("sp", dma(B2, bcast_rows(mt, b * 6 * D + 3 * D, D)), reads=[self.b_MOD], writes=[b_m])
        P.dma("sp", dma(A2, bcast_rows(mt, b * 6 * D + 4 * D, D)), reads=[self.b_MOD], writes=[b_m])
        P.dma("sp", dma(G2, bcast_rows(mt, b * 6 * D + 5 * D, D)), reads=[self.b_MOD], writes=[b_m])
        P.op("dve", stt(A2, A2, 1.0, self.n2g, ALU.add, ALU.mult), reads=[b_m, self.b_rc], writes=[b_m])
        self.junk = (A.alloc([D], F32), Buf())
        self.ssb = [(A.alloc([1], F32), Buf()) for _ in range(2)]
        yT4 = [(A.alloc([KD, 512], BF16), Buf()) for _ in range(2)]
        xts = [(A.alloc([D], F32), Buf()) for _ in range(2)]
        x1s = [(A.alloc([D], F32), Buf()) for _ in range(2)]
        tmpA = (A.alloc([D], F32), Buf())
        tmpB = (A.alloc([D], F32), Buf())
        hfs = [(A.alloc([D], F32), Buf()) for _ in range(2)]
        hbs = [(A.alloc([D], BF16), Buf()) for _ in range(2)]
        h2Tb = [(A.alloc([KD, 128], BF16), Buf()) for _ in range(2)]
        h2Tf = [(A.alloc([KD, 128], F32), Buf()) for _ in range(2)]
        scs = [(A.alloc([NE], F32), Buf()) for _ in range(2)]
        bias_ = [(A.alloc([NE], F32), Buf()) for _ in range(2)]
        msk = (A.alloc([NE], F32), Buf())
        sel = (A.alloc([NE], F32), Buf())
        selb = (A.alloc([NE], BF16), Buf())
        pos = (A.alloc([NE], F32), Buf())
        oh = (A.alloc([8, NE], F32), Buf())
        sm = (A.alloc([160], F32), Buf())
        dsc = [(A.alloc([8], I32), Buf()) for _ in range(2)]
        sg = (A.alloc([256], F32), Buf())
        hmid = (A.alloc([2, 128], BF16), Buf())
        smv = sm[0]
        b_s = sm[1]
        m8g = smv[:, 0:64].rearrange("p (g e) -> p g e", g=8)
        gs_ = smv[:, 64:72]
        m8 = smv[:, 72:80]
        gmask = smv[:, 80:88]
        gm1 = smv[:, 88:96]
        m8b = smv[:, 96:104]
        w8 = smv[:, 104:112]
        e8f = smv[:, 112:120]
        posk = smv[:, 120:128]
        dest = smv[:, 128:136]
        valid = smv[:, 136:144]
        t8 = smv[:, 144:152]
        ws1 = smv[:, 152:153]
        e8 = A.alloc([8], U32)
        def stage1(i):
            ti = b * NLT + i
            sc, bia = scs[i % 2], bias_[i % 2]
            tmp = tmpA
            tt_ = i % 4
            yt, byt = yT4[(i // 4) % 2]
            if tt_ == 0:
                P.dma("sp", dma(yt, self.YT_d[b].rearrange("k p t -> p k t")[:, :, i * 128:(i + 4) * 128]), reads=[self.b_YT], writes=[byt])
            xt, bx = xts[i % 2]
            P.dma("act", dma(xt, self.x_d[b * L + i * 128: b * L + (i + 1) * 128, :]), writes=[bx])
            pss = [self.bank(), self.bank()]
            for n in range(2):
                for k in range(KD):
                    P.op("pe", mm(pss[n][0][:, :], yt[:, k, tt_ * 128:(tt_ + 1) * 128], self.wout[:, k, n * 512:(n + 1) * 512], k == 0, k == KD - 1),
                         reads=[byt, self.b_wts], writes=[pss[n][1]])
            for n in range(2):
                P.op("dve", tt(tmp[0][:, n * 512:(n + 1) * 512], pss[n][0][:, :], G1[:, n * 512:(n + 1) * 512], ALU.mult),
                     reads=[pss[n][1], b_m], writes=[tmp[1]])
            x1, bx1 = x1s[i % 2]
            P.op("dve", tt(x1, tmp[0], xt, ALU.add), reads=[tmp[1], bx], writes=[bx1])
            if b == 0 and "x1" in self.dbg:
                if not hasattr(self, "dbg_x1"):
                    self.dbg_x1 = self.dout("dbg_x1", [L, D])
                P.dma("sp", dma(self.dbg_x1[i * 128:(i + 1) * 128, :], x1), reads=[bx1])
            hf, bhf = hfs[i % 2]
            hb, bhb = hbs[i % 2]
            hT2, bhT2 = h2Tb[i % 2]
            self.norm_mod_sb(i, x1, bx1, A2, b_m, B2, b_m, hT2, bhT2, hf_out=(hf, bhf), hb_out=(hb, bhb))
            if b == 0 and "hx2" in self.dbg:
                if not hasattr(self, "dbg_hx2"):
                    self.dbg_hx2 = self.dout("dbg_hx2", [L, D])
                P.dma("sp", dma(self.dbg_hx2[i * 128:(i + 1) * 128, :], hf), reads=[bhf])
            pf = [self.bank(), self.bank()]
            for k in range(KD):
                P.op("pe", tr(pf[k // 4][0][:, (k % 4) * 128:(k % 4 + 1) * 128], hf[:, k * 128:(k + 1) * 128], self.identf),
                     reads=[bhf, self.b_cst], writes=[pf[k // 4][1]])
            hTf, bhTf = h2Tf[i % 2]
            P.op("act", act(hTf[:, 0:4, :], pf[0][0].rearrange("p (k t) -> p k t", k=4), AF.Copy), reads=[pf[0][1]], writes=[bhTf])
            P.op("dve", cp(hTf[:, 4:8, :], pf[1][0].rearrange("p (k t) -> p k t", k=4)), reads=[pf[1][1]], writes=[bhTf])
            pr, pbr = self.bank()
            for k in range(KD):
                P.op("pe", mm(pr[:, 0:NE], hTf[:, k, :], self.wr[:, k, :], k == 0, k == KD - 1), reads=[bhTf, self.b_rc], writes=[pbr])
            P.op("act", act(sc[0], pr[:, 0:NE], AF.Sigmoid), reads=[pbr], writes=[sc[1]])
            P.op("dve", tt(bia[0], sc[0], self.rbias, ALU.add), reads=[sc[1], self.b_rc], writes=[bia[1]])
        def stage2(i):
            ti = b * NLT + i
            x1, bx1 = x1s[i % 2]
            hf, bhf = hfs[i % 2]
            hb, bhb = hbs[i % 2]
            hT2, bhT2 = h2Tb[i % 2]
            sc, bia = scs[i % 2], bias_[i % 2]
            tmp = tmpB
            bia3 = bia[0].rearrange("p (g e) -> p g e", g=8)
            for g in range(8):
                P.op("dve", lambda e, g=g: e.max(out=m8g[:, g, :], in_=bia3[:, g, :]), reads=[bia[1]], writes=[b_s])
            P.op("dve", tt(gs_, m8g[:, :, 0], m8g[:, :, 1], ALU.add), reads=[b_s], writes=[b_s])
            P.op("dve", lambda e: e.max(out=m8, in_=gs_), reads=[b_s], writes=[b_s])
            P.op("dve", ts(gmask, gs_, m8[:, 3:4], None, ALU.is_ge), reads=[b_s], writes=[b_s])
            P.op("dve", ts(gm1, gmask, -1.0, None, ALU.add), reads=[b_s], writes=[b_s])
            msk3 = msk[0].rearrange("p (g e) -> p g e", g=8)
            P.op("dve", tt(msk3, bia3, gmask.unsqueeze(2).to_broadcast([128, 8, 32]), ALU.mult), reads=[bia[1], b_s], writes=[msk[1]])
            P.op("dve", tt(msk3, msk3, gm1.unsqueeze(2).to_broadcast([128, 8, 32]), ALU.add), reads=[msk[1], b_s], writes=[msk[1]])
            P.op("dve", lambda e: e.max(out=m8b, in_=msk[0]), reads=[msk[1]], writes=[b_s])
            P.op("dve", ts(sel[0], msk[0], m8b[:, 7:8], None, ALU.is_ge), reads=[msk[1], b_s], writes=[sel[1]])
            P.op("act", act(selb[0], sel[0], AF.Copy), reads=[sel[1]], writes=[selb[1]])
            P.op("dve", tt(msk[0], sc[0], sel[0], ALU.mult), reads=[sc[1], sel[1]], writes=[msk[1]])
            P.op("dve", lambda e: e.max(out=w8, in_=msk[0]), reads=[msk[1]], writes=[b_s])
            P.op("dve", lambda e: e.max_index(out=e8, in_max=w8, in_values=msk[0]), reads=[msk[1], b_s], writes=[b_s])
            P.op("dve", cp(e8f, e8), reads=[b_s], writes=[b_s])
            P.op("dve", rsum(ws1, w8), reads=[b_s], writes=[b_s])
            P.op("dve", lambda e: e.reciprocal(out=ws1, in_=ws1), reads=[b_s], writes=[b_s])
            P.op("dve", ts(w8, w8, ws1, RSCALE, ALU.mult, ALU.mult), reads=[b_s], writes=[b_s])
            pp, pbp = self.bank()
            P.op("pe", mm(pp[:, 0:NE], self.ustrict_b, selb[0], True, True), reads=[selb[1], self.b_cb], writes=[pbp])
            pc, pbc = self.bank()
            P.op("pe", mm(pc[:, 0:NE], self.ones_b, selb[0], True, True), reads=[selb[1], self.b_cb], writes=[pbc])
            P.op("dve", tt(pos[0], pp[:, 0:NE], self.cnt, ALU.add), reads=[pbp, self.b_cnt], writes=[pos[1]])
            P.op("dve", tt(self.cnt, self.cnt, pc[:, 0:NE], ALU.add), reads=[pbc, pos[1]], writes=[self.b_cnt])
            P.op("dve", tt(oh[0], self.iota3.unsqueeze(1).to_broadcast([128, 8, NE]), e8f.unsqueeze(2).to_broadcast([128, 8, NE]), ALU.is_equal),
                 reads=[self.b_rc, b_s], writes=[oh[1]])
            P.op("dve", tt(oh[0], oh[0], pos[0].unsqueeze(1).to_broadcast([128, 8, NE]), ALU.mult), reads=[oh[1], pos[1]], writes=[oh[1]])
            P.op("dve", rsum(posk, oh[0]), reads=[oh[1]], writes=[b_s])
            P.op("dve", stt(dest, e8f, float(CAP), posk, ALU.mult, ALU.add), reads=[b_s], writes=[b_s])
            P.op("dve", ts(valid, posk, float(CAP), None, ALU.is_lt), reads=[b_s], writes=[b_s])
            P.op("dve", ts(t8, valid, -1.0e6, 1.0e6, ALU.mult, ALU.add), reads=[b_s], writes=[b_s])
            P.op("dve", tt(t8, t8, dest, ALU.add), reads=[b_s], writes=[b_s])
            di, bdi = dsc[i % 2]
            P.op("dve", cp(di, t8), reads=[b_s], writes=[bdi])
            P.op("dve", tt(self.DestF[:, ti, :], dest, valid, ALU.mult), reads=[b_s], writes=[self.b_ov])
            P.op("dve", tt(self.wAll[:, ti, :], w8, valid, ALU.mult), reads=[b_s], writes=[self.b_dw])
            P.op("dve", cp(self.E8All[:, ti, :], e8f), reads=[b_s], writes=[self.b_ov])
            P.op("dve", cp(self.P8All[:, ti, :], posk), reads=[b_s], writes=[self.b_ov])
            P.op("dve", cp(self.W8raw[:, ti, :], w8), reads=[b_s], writes=[self.b_ov])
            P.dma("act", dma(self.H2_d[ti * 128:(ti + 1) * 128, :], hb), reads=[bhb], writes=[self.b_H2])
            for k in range(8):
                P.dma("pool", lambda e, k=k, di=di, hb=hb: e.indirect_dma_start(
                    out=self.XS_d, out_offset=bass.IndirectOffsetOnAxis(ap=di[:, k:k + 1], axis=0), in_=hb, in_offset=None,
                    bounds_check=self.bc_reg, oob_is_err=False), reads=[bdi, bhb], writes=[self.b_XS])
            ph, pbh = self.bank()
            for fc in range(4):
                for k in range(KD):
                    P.op("pe", mm(ph[:, fc * 128:(fc + 1) * 128], self.swgu[:, k, fc * 128:(fc + 1) * 128], hT2[:, k, :], k == 0, k == KD - 1),
                         reads=[bhT2, self.b_wts], writes=[pbh])
            P.op("act", act(sg[0], ph[:, 0:256], AF.Silu), reads=[pbh], writes=[sg[1]])
            P.op("dve", tt(hmid[0].rearrange("p a b -> p (a b)"), sg[0], ph[:, 256:512], ALU.mult), reads=[sg[1], pbh], writes=[hmid[1]])
            pd = [self.bank(), self.bank()]
            for n in range(2):
                for fc in range(2):
                    P.op("pe", mm(pd[n][0][:, :], hmid[0][:, fc, :], self.swd[:, fc, n * 512:(n + 1) * 512], fc == 0, fc == 1),
                         reads=[hmid[1], self.b_wts], writes=[pd[n][1]])
            for n in range(2):
                P.op("dve", tt(tmp[0][:, n * 512:(n + 1) * 512], pd[n][0][:, :], G2[:, n * 512:(n + 1) * 512], ALU.mult),
                     reads=[pd[n][1], b_m], writes=[tmp[1]])
            P.op("dve", tt(x1, tmp[0], x1, ALU.add), reads=[tmp[1], bx1], writes=[bx1])
            P.dma("sp", dma(self.X1_d[ti * 128:(ti + 1) * 128, :], x1), reads=[bx1], writes=[self.b_X1])
        stage1(0)
        for i in range(NLT):
            if i + 1 < NLT:
                stage1(i + 1)
            stage2(i)
        P.barrier()
        A.release()

    def overflow_route(self):
        P, A, NB = self.P, self.A, self.NB
        NTT = NB * NLT
        A.mark()
        pidx = A.alloc([1], F32)
        b_c = Buf()
        P.dma("sp", dma(pidx, self.pidx_d), writes=[b_c])
        thr = A.alloc([1], F32)
        P.op("dve", ts(thr, pidx, 128.0, None, ALU.mult), reads=[b_c], writes=[b_c])
        ovc = A.alloc([NE], F32)
        b_o = Buf()
        P.op("dve", ts(ovc, self.cnt, -float(CAP), 0.0, ALU.add, ALU.max), reads=[self.b_cnt], writes=[b_o])
        cmpb = A.alloc([NE], BF16)
        P.op("dve", ts(cmpb, ovc, thr, None, ALU.is_gt), reads=[b_o, b_c], writes=[b_o])
        ps, pb = self.bank()
        P.op("pe", mm(ps[:, 0:NE], self.ones_b, cmpb, True, True), reads=[b_o, self.b_cb], writes=[pb])
        ovblk = A.alloc([NE], F32)
        ovend = A.alloc([NE], F32)
        ovs = A.alloc([NE], F32)
        onesr = A.alloc([NE], F32)
        b_e = Buf()
        P.op("act", act(ovblk, ps[:, 0:NE], AF.Copy), reads=[pb], writes=[b_e])
        P.op("pool", lambda e: e.memset(onesr, 1.0), writes=[b_e])
        P.op("dve", lambda e: e.tensor_tensor_scan(out=ovend, data0=onesr, data1=ovblk, initial=0.0, op0=ALU.mult, op1=ALU.add),
             reads=[b_e], writes=[b_e])
        P.op("dve", tt(ovs, ovend, ovblk, ALU.subtract), reads=[b_e], writes=[b_e])
        P.op("dve", ts(ovs, ovs, 128.0, None, ALU.mult), reads=[b_e], writes=[b_e])
        beRow = A.alloc([OV], F32)
        b_b = Buf()
        cmp3 = A.alloc([16, NE], F32)
        for c0 in range(0, OV, 16):
            P.op("dve", tt(cmp3, ovend.unsqueeze(1).to_broadcast([128, 16, NE]),
                           self.iota3[:, c0:c0 + 16].unsqueeze(2).to_broadcast([128, 16, NE]), ALU.is_le), reads=[b_e, self.b_rc], writes=[b_b])
            P.op("dve", rsum(beRow[:, c0:c0 + 16], cmp3), reads=[b_b], writes=[b_b])
        self.dump("be", beRow, b_b, [128, OV])
        idxf = A.alloc([OV], F32)
        P.op("dve", ts(idxf, beRow, 128.0, pidx, ALU.mult, ALU.add), reads=[b_b, b_c], writes=[b_b])
        P.op("dve", cp(self.IDXG, idxf), reads=[b_b], writes=[self.b_idx])
        oh = A.alloc([8, NE], F32)
        b_oh = Buf()
        sm = A.alloc([64], F32)
        b_s = Buf()
        ob8, ovslot, isov, ok, t8 = (sm[:, 8 * q:8 * q + 8] for q in range(5))
        di_ = [(A.alloc([8], I32), Buf()) for _ in range(2)]
        hbs = [(A.alloc([D], BF16), Buf()) for _ in range(2)]
        for ti in range(NTT):
            hb, bhb = hbs[ti % 2]
            P.dma("sp", dma(hb, self.H2_d[ti * 128:(ti + 1) * 128, :]), reads=[self.b_H2], writes=[bhb])
            P.op("dve", tt(oh, self.iota3.unsqueeze(1).to_broadcast([128, 8, NE]),
                           self.E8All[:, ti, :].unsqueeze(2).to_broadcast([128, 8, NE]), ALU.is_equal), reads=[self.b_rc, self.b_ov], writes=[b_oh])
            P.op("dve", tt(oh, oh, ovs.unsqueeze(1).to_broadcast([128, 8, NE]), ALU.mult), reads=[b_oh, b_e], writes=[b_oh])
            P.op("dve", rsum(ob8, oh), reads=[b_oh], writes=[b_s])
            P.op("dve", stt(ovslot, self.P8All[:, ti, :], -float(CAP), ob8, ALU.add, ALU.add), reads=[b_s, self.b_ov], writes=[b_s])
            P.op("dve", ts(isov, self.P8All[:, ti, :], float(CAP), None, ALU.is_ge), reads=[self.b_ov], writes=[b_s])
            P.op("dve", ts(ok, ovslot, float(OV * 128), None, ALU.is_lt), reads=[b_s], writes=[b_s])
            P.op("dve", tt(ok, ok, isov, ALU.mult), reads=[b_s], writes=[b_s])
            P.op("dve", ts(ovslot, ovslot, float(NE * CAP), None, ALU.add), reads=[b_s], writes=[b_s])
            P.op("dve", ts(t8, ok, -1.0e6, 1.0e6, ALU.mult, ALU.add), reads=[b_s], writes=[b_s])
            P.op("dve", tt(t8, t8, ovslot, ALU.add), reads=[b_s], writes=[b_s])
            di, bdi = di_[ti % 2]
            P.op("dve", cp(di, t8), reads=[b_s], writes=[bdi])
            P.op("dve", tt(ovslot, ovslot, ok, ALU.mult), reads=[b_s], writes=[b_s])
            P.op("dve", tt(t8, self.DestF[:, ti, :], ovslot, ALU.add), reads=[b_s, self.b_ov], writes=[b_s])
            P.op("dve", cp(self.destAll[:, ti, :], t8), reads=[b_s], writes=[self.b_dw])
            P.op("dve", tt(t8, self.W8raw[:, ti, :], ok, ALU.mult), reads=[b_s, self.b_ov], writes=[b_s])
            P.op("dve", tt(self.wAll[:, ti, :], self.wAll[:, ti, :], t8, ALU.add), reads=[b_s, self.b_dw], writes=[self.b_dw])
            for k in range(8):
                P.dma("pool", lambda e, k=k, di=di, hb=hb: e.indirect_dma_start(
                    out=self.XS_d, out_offset=bass.IndirectOffsetOnAxis(ap=di[:, k:k + 1], axis=0), in_=hb, in_offset=None,
                    bounds_check=self.bc_reg2, oob_is_err=False), reads=[bdi, bhb], writes=[self.b_XS])
        P.barrier()
        A.release()

    def experts(self):
        P, A = self.P, self.A
        self.dump("cnt", self.cnt, self.b_cnt, [128, NE])
        A.mark()
        xs4 = [(A.alloc([NBLK, D], BF16), Buf()) for _ in range(3)]
        xT = [(A.alloc([KD, CAP], BF16), Buf()) for _ in range(2)]
        wguf = [(A.alloc([KD, 512], F32), Buf()) for _ in range(3)]
        wdf = [(A.alloc([2, D], F32), Buf()) for _ in range(3)]
        wgub = [(A.alloc([KD, 512], BF16), Buf()) for _ in range(2)]
        wdb = [(A.alloc([2, D], BF16), Buf()) for _ in range(2)]
        sg = [(A.alloc([2, CAP], F32), Buf()) for _ in range(1)]
        hm = [(A.alloc([2, CAP], BF16), Buf()) for _ in range(2)]
        yo = [(A.alloc([D], BF16), Buf()) for _ in range(3)]
        self.b_Y = Buf("Y")

        def loads(e_):
            x4, bx4 = xs4[e_ % 3]
            P.dma("sp", dma(x4, self.XS_d[e_ * CAP:(e_ + 1) * CAP, :].rearrange("(s p) d -> p s d", p=128)), reads=[self.b_XS], writes=[bx4])
            wg, bwg = wguf[e_ % 3]
            wd, bwd = wdf[e_ % 3]
            P.dma("sp", dma(wg[:, :, 0:256], self.ewg_d[e_].rearrange("p (k f) -> p k f", k=KD)), writes=[bwg])
            P.dma("sp", dma(wg[:, :, 256:512], self.ewu_d[e_].rearrange("p (k f) -> p k f", k=KD)), writes=[bwg])
            P.dma("sp", dma(wd, self.ewd_d[e_].rearrange("p (k n) -> p k n", k=2)), writes=[bwd])

        def casts(e_):
            wg, bwg = wguf[e_ % 3]
            wd, bwd = wdf[e_ % 3]
            wgb, bwgb = wgub[e_ % 2]
            wdb_, bwdb = wdb[e_ % 2]
            P.op("dve", cp(wgb[:, 0:3, :], wg[:, 0:3, :]), reads=[bwg], writes=[bwgb])
            P.op("act", act(wgb[:, 3:6, :], wg[:, 3:6, :], AF.Copy), reads=[bwg], writes=[bwgb])
            P.op("pool", cp(wgb[:, 6:8, :], wg[:, 6:8, :]), reads=[bwg], writes=[bwgb])
            P.op("act", act(wdb_[:, 0, :], wd[:, 0, :], AF.Copy), reads=[bwd], writes=[bwdb])
            P.op("dve", cp(wdb_[:, 1, :], wd[:, 1, :]), reads=[bwd], writes=[bwdb])

        def transposes(e_):
            x4, bx4 = xs4[e_ % 3]
            xt_, bxt = xT[e_ % 2]
            for sb in range(NBLK):
                ps, pb = self.bank()
                psb = ps.bitcast(BF16)
                for k in range(KD):
                    P.op("pe", tr(psb[:, k * 128:(k + 1) * 128], x4[:, sb, k * 128:(k + 1) * 128], self.identb), reads=[bx4, self.b_cb], writes=[pb])
                src = psb.rearrange("p (k t) -> p k t", k=KD)
                P.op("dve", cp(xt_[:, :, sb * 128:(sb + 1) * 128], src), reads=[pb], writes=[bxt])

        loads(0)
        loads(1)
        casts(0)
        transposes(0)
        yc = 0
        for e_ in range(NE):
            if e_ + 2 < NE:
                loads(e_ + 2)
            if e_ + 1 < NE:
                casts(e_ + 1)
            wgb, bwgb = wgub[e_ % 2]
            wdb_, bwdb = wdb[e_ % 2]
            xt_, bxt = xT[e_ % 2]
            pg = [self.bank() for _ in range(4)]
            for fc in range(4):
                for k in range(KD):
                    P.op("pe", mm(pg[fc][0][:, 0:CAP], wgb[:, k, fc * 128:(fc + 1) * 128], xt_[:, k, :], k == 0, k == KD - 1),
                         reads=[bwgb, bxt], writes=[pg[fc][1]])
            sg_, bsg = sg[0]
            hm_, bhm = hm[e_ % 2]
            for fc in range(2):
                P.op("act", act(sg_[:, fc, :], pg[fc][0][:, 0:CAP], AF.Silu), reads=[pg[fc][1]], writes=[bsg])
                P.op("dve", tt(hm_[:, fc, :], sg_[:, fc, :], pg[2 + fc][0][:, 0:CAP], ALU.mult), reads=[bsg, pg[2 + fc][1]], writes=[bhm])
            if e_ + 1 < NE:
                transposes(e_ + 1)
            for sb in range(NBLK):
                pd = [self.bank(), self.bank()]
                for n in range(2):
                    for fc in range(2):
                        P.op("pe", mm(pd[n][0][:, :], hm_[:, fc, sb * 128:(sb + 1) * 128], wdb_[:, fc, n * 512:(n + 1) * 512], fc == 0, fc == 1),
                             reads=[bhm, bwdb], writes=[pd[n][1]])
                y_, by = yo[yc % 3]
                yc += 1
                P.op("act", act(y_[:, 0:512], pd[0][0][:, :], AF.Copy), reads=[pd[0][1]], writes=[by])
                P.op("act", act(y_[:, 512:1024], pd[1][0][:, :], AF.Copy), reads=[pd[1][1]], writes=[by])
                r0 = e_ * CAP + sb * 128
                P.dma("act", dma(self.Y_d[r0:r0 + 128, :], y_), reads=[by], writes=[self.b_Y])
        if OV > 0:
            ewg_rows = self.ewg_d.rearrange("e p n -> (e p) n")
            ewu_rows = self.ewu_d.rearrange("e p n -> (e p) n")
            ewd_rows = self.ewd_d.rearrange("e p n -> (e p) n")
            ovw = [(A.alloc([KD * 256], BF16), A.alloc([KD * 256], BF16), A.alloc([2 * D], BF16), Buf()) for _ in range(2)]

            def ovloads(j):
                x4, bx4 = xs4[j % 3]
                r0 = NE * CAP + j * 128
                P.dma("sp", dma(x4[:, 0, :], self.XS_d[r0:r0 + 128, :]), reads=[self.b_XS], writes=[bx4])
                og, ou, od, bw = ovw[j % 2]
                off = lambda j=j: bass.IndirectOffsetOnAxis(ap=self.IDXG[:, j:j + 1], axis=0)
                P.dma("pool", lambda e, og=og, off=off: e.indirect_dma_start(out=og, out_offset=None, in_=ewg_rows, in_offset=off(), bounds_check=self.bc_reg3, oob_is_err=False),
                      reads=[self.b_idx], writes=[bw])
                P.dma("pool", lambda e, ou=ou, off=off: e.indirect_dma_start(out=ou, out_offset=None, in_=ewu_rows, in_offset=off(), bounds_check=self.bc_reg3, oob_is_err=False),
                      reads=[self.b_idx], writes=[bw])
                P.dma("pool", lambda e, od=od, off=off: e.indirect_dma_start(out=od, out_offset=None, in_=ewd_rows, in_offset=off(), bounds_check=self.bc_reg3, oob_is_err=False),
                      reads=[self.b_idx], writes=[bw])

            ovloads(0)
            for j in range(OV):
                if j + 1 < OV:
                    ovloads(j + 1)
                x4, bx4 = xs4[j % 3]
                og, ou, od, bw = ovw[j % 2]
                og3 = og.rearrange("p (k f) -> p k f", k=KD)
                ou3 = ou.rearrange("p (k f) -> p k f", k=KD)
                od3 = od.rearrange("p (k n) -> p k n", k=2)
                xt_, bxt = xT[j % 2]
                ps, pb = self.bank()
                psb = ps.bitcast(BF16)
                for k in range(KD):
                    P.op("pe", tr(psb[:, k * 128:(k + 1) * 128], x4[:, 0, k * 128:(k + 1) * 128], self.identb), reads=[bx4, self.b_cb], writes=[pb])
                P.op("dve", cp(xt_[:, :, 0:128], psb.rearrange("p (k t) -> p k t", k=KD)), reads=[pb], writes=[bxt])
                pg, pbg = self.bank()
                for fc in range(4):
                    wsrc = og3 if fc < 2 else ou3
                    f0 = (fc % 2) * 128
                    for k in range(KD):
                        P.op("pe", mm(pg[:, fc * 128:(fc + 1) * 128], wsrc[:, k, f0:f0 + 128], xt_[:, k, 0:128], k == 0, k == KD - 1),
                             reads=[bw, bxt], writes=[pbg])
                sg_, bsg = sg[0]
                hm_, bhm = hm[j % 2]
                for fc in range(2):
                    P.op("act", act(sg_[:, fc, 0:128], pg[:, fc * 128:(fc + 1) * 128], AF.Silu), reads=[pbg], writes=[bsg])
                    P.op("dve", tt(hm_[:, fc, 0:128], sg_[:, fc, 0:128], pg[:, (2 + fc) * 128:(3 + fc) * 128], ALU.mult), reads=[bsg, pbg], writes=[bhm])
                pd = [self.bank(), self.bank()]
                for n in range(2):
                    for fc in range(2):
                        P.op("pe", mm(pd[n][0][:, :], hm_[:, fc, 0:128], od3[:, fc, n * 512:(n + 1) * 512], fc == 0, fc == 1),
                             reads=[bhm, bw], writes=[pd[n][1]])
                y_, by = yo[yc % 3]
                yc += 1
                P.op("act", act(y_[:, 0:512], pd[0][0][:, :], AF.Copy), reads=[pd[0][1]], writes=[by])
                P.op("act", act(y_[:, 512:1024], pd[1][0][:, :], AF.Copy), reads=[pd[1][1]], writes=[by])
                r0 = NE * CAP + j * 128
                P.dma("act", dma(self.Y_d[r0:r0 + 128, :], y_), reads=[by], writes=[self.b_Y])
        P.barrier()
        A.release()

    def combine(self):
        P, A, NB = self.P, self.A, self.NB
        A.mark()
        mt = self.MOD_d.tensor
        fing = A.alloc([D], F32)
        b_fg = Buf()
        P.dma("sp", dma(fing, bcast_rows(self.fing_d.tensor, 0, D)), writes=[b_fg])
        G2s = [(A.alloc([D], F32), Buf()) for _ in range(2)]
        base = [(A.alloc([D], F32), Buf()) for _ in range(2)]
        yk = [(A.alloc([D], BF16), Buf()) for _ in range(8)]
        acc = [(A.alloc([D], F32), Buf()) for _ in range(2)]
        junk = (A.alloc([D], F32), Buf())
        pre = [(A.alloc([D], F32), Buf()) for _ in range(2)]
        ssb = [(A.alloc([1], F32), Buf()) for _ in range(2)]
        ot = [(A.alloc([D], F32), Buf()) for _ in range(2)]
        for b in range(NB):
            G2, bG2 = G2s[b % 2]
            P.dma("sp", dma(G2, bcast_rows(mt, b * 6 * D + 5 * D, D)), reads=[self.b_MOD], writes=[bG2])
            for i in range(NLT):
                ti = b * NLT + i
                bs, bbs = base[i % 2]
                P.dma("sp", dma(bs, self.X1_d[ti * 128:(ti + 1) * 128, :]), reads=[self.b_X1], writes=[bbs])
                ac, bac = acc[i % 2]
                for k in range(8):
                    y_, by = yk[k]
                    P.dma("pool", lambda e, k=k, y_=y_, ti=ti: e.indirect_dma_start(
                        out=y_, out_offset=None, in_=self.Y_d, in_offset=bass.IndirectOffsetOnAxis(ap=self.destAll[:, ti, k:k + 1], axis=0)),
                        reads=[self.b_dw, self.b_Y], writes=[by])
                    if k == 0:
                        P.op("dve", ts(ac, y_, self.wAll[:, ti, 0:1], None, ALU.mult), reads=[by, self.b_dw], writes=[bac])
                    else:
                        P.op("dve", stt(ac, y_, self.wAll[:, ti, k:k + 1], ac, ALU.mult, ALU.add), reads=[by, self.b_dw, bac], writes=[bac])
                pa_, bpa = pre[0]
                pb_, bpb = pre[1]
                P.op("dve", tt(pa_, ac, G2, ALU.mult), reads=[bac, bG2], writes=[bpa])
                P.op("dve", tt(pb_, pa_, bs, ALU.add), reads=[bpa, bbs], writes=[bpb])
                ac, bac = pb_, bpb
                ss, bss = ssb[i % 2]
                P.op("act", act(junk[0], ac, AF.Square), reads=[bac], writes=[junk[1]])
                P.op("dve", rsum(ss, junk[0]), reads=[junk[1]], writes=[bss])
                P.op("act", act(ss, ss, AF.Sqrt, scale=1.0 / D, bias=self.eps_ap), reads=[bss, self.b_eps], writes=[bss])
                P.op("dve", lambda e, ss=ss: e.reciprocal(out=ss, in_=ss), reads=[bss], writes=[bss])
                o_, bo = ot[i % 2]
                P.op("dve", stt(o_, ac, ss, fing, ALU.mult, ALU.mult), reads=[bac, bss, b_fg], writes=[bo])
                P.dma("sp", dma(self.out_d[ti * 128:(ti + 1) * 128, :], o_), reads=[bo])
        A.release()


def const_tables():
    import math
    ident = np.eye(128, dtype=np.float32)
    J = ident[::-1].copy()
    s = np.arange(128)[:, None]
    c = np.arange(128)[None, :]
    same = (s // 64) == (c // 64)
    maskF = (same & (s <= c)).astype(np.float32)
    maskB = (same & (s >= c)).astype(np.float32)
    Sm = ((c == s + 1) & ((c % 64) != 0)).astype(np.float32)
    Sp = ((c == s - 1) & ((c % 64) != 63)).astype(np.float32)
    ustrict = (s < c).astype(np.float32)
    ones = np.ones((128, 128), np.float32)
    cst = np.stack([ident, J, maskF, maskB, Sm, Sp, ustrict, ones, Sm[:, ::-1], Sp[:, ::-1]], axis=1).astype(np.float32)
    f32 = np.float32
    pos = np.arange(L, dtype=f32)[:, None]
    t = pos / f32(L - 1)
    w = f32(2.0 * math.pi / L) * pos
    bands = np.linspace(1e-4, 15, 16, dtype=f32)[None, :]
    feats = np.concatenate([t, np.cos(bands * w), -np.sin(bands * w)], axis=-1).astype(f32)
    max_decay = math.log(1e-2) / 0.3
    min_decay = math.log(1e-2) / 1.5
    deltas = np.abs(np.linspace(min_decay, max_decay, 512, dtype=f32))[None, :].astype(f32)
    tfrac = (-(np.arange(L, dtype=f32) / f32(L - 1))).reshape(16, 128).T.copy()
    iota = np.arange(NE, dtype=f32)[None, :]
    pidx = np.arange(128, dtype=f32)[:, None].copy()
    return dict(cst=cst, featsT=np.ascontiguousarray(feats.T), deltas=deltas, tfrac=tfrac.astype(f32), iota=iota, pidx=pidx)


def prep_core(inp, core, NB, tables):
    f = lambda a: np.ascontiguousarray(a, dtype=np.float32)
    b0 = core * NB
    m = dict(tables)
    m["x"] = f(inp["x"][b0:b0 + NB].reshape(NB * L, D))
    m["ctx"] = f(inp["ctx"][b0:b0 + NB].reshape(NB * CTX, D))
    cc = np.concatenate([inp["c"][b0:b0 + NB], inp["c_ctx"][None, :]], axis=0)
    m["cT"] = f(cc.T.reshape(KD, 128, NB + 1).transpose(1, 0, 2))
    return m


def shared_inputs(inp):
    f = lambda a: np.ascontiguousarray(a, dtype=np.float32)
    m = {}
    m["w_mod"] = f(inp["w_mod"][0])
    m["b_mod"] = f(inp["b_mod"][0][None, :])
    m["norm1_g"] = f(inp["norm1_g"][0][None, :])
    m["norm2_g"] = f(inp["norm2_g"][0][None, :])
    m["final_g"] = f(inp["final_g"][None, :])
    m["w_in"] = f(inp["w_in"][0])
    m["w_out"] = f(inp["w_out"][0])
    m["hy_conv_w"] = f(inp["hy_conv_w"][0])
    m["hy_conv_b"] = f(inp["hy_conv_b"][0][None, :])
    m["hy_fw1"] = f(inp["hy_fw1"][0])
    m["hy_fb1"] = f(inp["hy_fb1"][0][:, None])
    m["hy_fw2"] = f(inp["hy_fw2"][0])
    m["hy_fb2"] = f(inp["hy_fb2"][0][:, None])
    m["hy_fw3"] = f(inp["hy_fw3"][0])
    m["hy_freq"] = f(inp["hy_freq"][0][:, None])
    m["hy_d"] = f(inp["hy_d"][0].reshape(1, 1024))
    lg = inp["hg_lb_logits"].reshape(2, 2, 4, 128)
    m["lbT"] = f(lg.transpose(3, 0, 1, 2).reshape(128, 16))
    m["hg_norm_g"] = f(inp["hg_norm_g"][0][None, :])
    m["w_router"] = f(inp["w_router"][0])
    m["router_bias"] = f(inp["router_bias"][0][None, :])
    m["ew_gate"] = f(np.asarray(inp["ew_gate"][0]).reshape(NE, KD, 128, 256).transpose(0, 2, 1, 3).reshape(NE, 128, KD * 256))
    m["ew_up"] = f(np.asarray(inp["ew_up"][0]).reshape(NE, KD, 128, 256).transpose(0, 2, 1, 3).reshape(NE, 128, KD * 256))
    m["ew_down"] = f(np.asarray(inp["ew_down"][0]).reshape(NE, 2, 128, D).transpose(0, 2, 1, 3).reshape(NE, 128, 2 * D))
    m["sw_gate"] = f(inp["sw_gate"][0])
    m["sw_up"] = f(inp["sw_up"][0])
    m["sw_down"] = f(inp["sw_down"][0])
    return m


def kernel(**inputs):
    NB = 4
    ncores = 8
    k = K(NB)
    nc = k.build()
    tables = const_tables()
    sh = shared_inputs(inputs)
    in_maps = []
    for c in range(ncores):
        m = prep_core(inputs, c, NB, tables)
        m.update(sh)
        in_maps.append(m)
    res = run_bass_kernel_spmd(nc, in_maps, core_ids=list(range(ncores)))
    outs = [np.asarray(r["out"], dtype=np.float32).reshape(NB, L, D) for r in res.results]
    return np.concatenate(outs, axis=0)
```

```python
import os
from contextlib import ExitStack
from concourse.bass_utils import run_bass_kernel_spmd
import numpy as np
import concourse.bass as bass
import concourse.mybir as mybir

F32 = mybir.dt.float32
BF16 = mybir.dt.bfloat16
I32 = mybir.dt.int32
U32 = mybir.dt.uint32
AF = mybir.ActivationFunctionType
ALU = mybir.AluOpType
AX = mybir.AxisListType


class Buf:
    __slots__ = ("name", "w", "r")

    def __init__(self, name=""):
        self.name = name
        self.w = None
        self.r = []


class Prog:
    COMPUTE = ("pe", "act", "dve", "pool")
    NDMA = {"sp": 10, "act": 4, "pool": 8}

    def __init__(self, nc, stack, same_engine_sync=True):
        self.nc = nc
        self.same = same_engine_sync
        self.ops = {e: [] for e in ("pe", "act", "dve", "pool", "sp")}
        self.sem = {}
        self.cnt = {}
        for e in self.COMPUTE:
            self.sem[e] = stack.enter_context(nc.semaphore("s_" + e))
            self.cnt[e] = 0
        self.dsem = {}
        self.dcnt = {}
        self.drr = {}
        for q, n in self.NDMA.items():
            for i in range(n):
                k = "d_%s_%d" % (q, i)
                self.sem[k] = stack.enter_context(nc.semaphore(k))
                self.cnt[k] = 0
            self.drr[q] = 0
        self.known = {e: {} for e in self.ops}
        self.nwaits = 0
        self.pre = {}

    def _deps(self, eng, reads, writes, is_dma=False):
        need = {}

        def add(tok):
            if tok is None:
                return
            k, v = tok
            if need.get(k, 0) < v:
                need[k] = v
        for b in reads:
            add(b.w)
        for b in writes:
            add(b.w)
            for t in b.r:
                add(t)
        waits = []
        kn = self.known[eng]
        for k, v in need.items():
            if k == eng and not self.same and not is_dma:
                continue
            if k == "pe" and eng == "pe":
                continue
            if kn.get(k, 0) >= v:
                continue
            kn[k] = v
            waits.append((k, v))
        self.nwaits += len(waits)
        return waits

    def _commit(self, tok, reads, writes):
        for b in reads:
            b.r.append(tok)
            if len(b.r) > 64:
                m = {}
                for k, v in b.r:
                    if m.get(k, 0) < v:
                        m[k] = v
                b.r = list(m.items())
        for b in writes:
            b.w = tok
            b.r = []

    def op(self, eng, fn, reads=(), writes=()):
        waits = self._deps(eng, reads, writes)
        self.cnt[eng] += 1
        tok = (eng, self.cnt[eng])
        self.ops[eng].append((waits, fn, (eng, 1)))
        self._commit(tok, reads, writes)
        return tok

    def dma(self, q, fn, reads=(), writes=()):
        n = self.NDMA[q]
        i = self.drr[q]
        self.drr[q] = (i + 1) % n
        k = "d_%s_%d" % (q, i)
        waits = self._deps(q, reads, writes, is_dma=True)
        prev = self.cnt[k]
        kn = self.known[q]
        if prev > 0 and kn.get(k, 0) < prev:
            kn[k] = prev
            waits.append((k, prev))
        self.cnt[k] += 16
        tok = (k, self.cnt[k])
        self.ops[q].append((waits, fn, (k, 16)))
        self._commit(tok, reads, writes)
        return tok

    def barrier_tokens(self):
        toks = []
        for k, v in self.cnt.items():
            if v > 0:
                toks.append((k, v))
        return toks

    def barrier(self):
        toks = self.barrier_tokens()
        for e in self.ops:
            kn = self.known[e]
            waits = []
            for k, v in toks:
                if k == e and e == "pe":
                    continue
                if kn.get(k, 0) < v:
                    kn[k] = v
                    waits.append((k, v))
            if waits:
                self.ops[e].append((waits, None, None))

    def final_wait(self, eng="sp"):
        toks = self.barrier_tokens()
        self.ops[eng].append(([(k, v) for k, v in toks], None, None))

    def emit(self):
        nc = self.nc
        engmap = {"pe": "tensor", "act": "scalar", "dve": "vector", "pool": "gpsimd", "sp": "sync"}
        with nc.Block() as block:
            for e, attr in engmap.items():
                lst = self.ops[e]

                def body(engine, lst=lst, e=e):
                    if e in self.pre:
                        self.pre[e](engine)
                    for waits, fn, inc in lst:
                        for k, v in waits:
                            engine.wait_ge(self.sem[k], v)
                        if fn is not None:
                            ins = fn(engine)
                            ins.then_inc(self.sem[inc[0]], inc[1])
                getattr(block, attr)(body)


class Arena:
    def __init__(self, nc, stack, nwords, name="arena"):
        self.t = stack.enter_context(nc.sbuf_tensor(name, [128, nwords], F32))
        self.n = nwords
        self.off = 0
        self.marks = []

    def alloc(self, shape, dtype, parts=128):
        n = int(np.prod(shape))
        if dtype == BF16:
            words = (n + 1) // 2
        else:
            words = n
        assert self.off + words <= self.n, "arena overflow %d + %d > %d" % (self.off, words, self.n)
        a = self.t[0:parts, self.off:self.off + words]
        self.off += words
        if dtype != F32:
            a = a.bitcast(dtype)
        if dtype == BF16 and n % 2 == 1:
            a = a[:, 0:n]
        if len(shape) > 1:
            names = " ".join("d%d" % i for i in range(len(shape)))
            kw = {"d%d" % i: int(s) for i, s in enumerate(shape)}
            a = a.rearrange("p (%s) -> p %s" % (names, names), **kw)
        return a

    def mark(self):
        self.marks.append(self.off)

    def release(self):
        self.off = self.marks.pop()

D = 1024
KD = 8
L = 2048
CTX = 256
T = L + CTX
NT = T // 128
NLT = L // 128
NCH = T // 64
HGS = 128.0 ** -0.5
EPS = 1e-6
NE = 256
CAP = int(os.environ.get('KCAP', '384'))
OV = 96
NBLK = CAP // 128
RSCALE = 2.5


def mm(out, lhsT, rhs, start, stop):
    return lambda e: e.matmul(out, lhsT, rhs, start=start, stop=stop)


def tr(out, in_, ident):
    return lambda e: e.transpose(out=out, in_=in_, identity=ident)


def act(out, in_, func, **kw):
    return lambda e: e.activation(out=out, in_=in_, func=func, **kw)


def tt(out, a, b, op):
    return lambda e: e.tensor_tensor(out=out, in0=a, in1=b, op=op)


def ts(out, a, s1, s2, op0, op1=None):
    if op1 is None:
        return lambda e: e.tensor_scalar(out=out, in0=a, scalar1=s1, scalar2=None, op0=op0)
    return lambda e: e.tensor_scalar(out=out, in0=a, scalar1=s1, scalar2=s2, op0=op0, op1=op1)


def stt(out, a, s, b, op0, op1):
    return lambda e: e.scalar_tensor_tensor(out=out, in0=a, scalar=s, in1=b, op0=op0, op1=op1)


def cp(out, in_):
    return lambda e: e.tensor_copy(out=out, in_=in_)


def rsum(out, in_):
    return lambda e: e.reduce_sum(out=out, in_=in_, axis=AX.X)


def dma(out, in_):
    return lambda e: e.dma_start(out=out, in_=in_)


def bcast_rows(dram_ap_tensor, offset, n, parts=128):
    return bass.AP(tensor=dram_ap_tensor, offset=offset, ap=[[0, parts], [1, n]])


class K:
    def __init__(self, NB, dbg=(), upto=4):
        self.NB = NB
        self.upto = upto
        self.cut = int(os.environ.get('KCUT', '0'))
        self.dbg = set(dbg)
        self.nc = bass.Bass("TRN2", target_bir_lowering=False)
        self.outs = []

    def din(self, name, shape, dtype=F32):
        return self.nc.dram_tensor(name, list(shape), dtype, kind="ExternalInput").ap()

    def dscr(self, name, shape, dtype):
        return self.nc.dram_tensor(name, list(shape), dtype, kind="Internal").ap()

    def dout(self, name, shape, dtype=F32):
        self.outs.append(name)
        return self.nc.dram_tensor(name, list(shape), dtype, kind="ExternalOutput").ap()

    def bank(self):
        self.bi = (self.bi + 1) % len(self.rot)
        return self.rot[self.bi]

    def reserve(self, n):
        got = [self.rot.pop() for _ in range(n)]
        self.bi = 0
        return got

    def unreserve(self, got):
        self.rot.extend(got)

    def dump(self, name, ap, buf, shape, dtype=F32):
        if name not in self.dbg:
            return
        o = self.dout("dbg_" + name, shape, dtype)
        self.P.dma("sp", dma(o, ap), reads=[buf])

    def build(self):
        nc = self.nc
        NB = self.NB
        with ExitStack() as st:
            self.st = st
            P = self.P = Prog(nc, st, same_engine_sync=(os.environ.get('KSAME', '1') == '1'))
            A = self.A = Arena(nc, st, 51500)
            self.banks = [(st.enter_context(nc.psum_tensor("pb%d" % i, [128, 512], F32))[:, :], Buf("pb%d" % i)) for i in range(8)]
            self.bi = 0
            self.rot = list(self.banks)
            self.declare_io()
            self.consts()
            self.modulation()
            if self.upto >= 2:
                self.filters()
            if self.upto >= 0.5:
                for b in range(NB):
                    self.mixer_batch(b)
            if self.upto >= 2:
                self.hyena_all()
            if self.upto >= 3:
                self.route_init()
                for b in range(NB):
                    self.outproj_route(b)
                self.overflow_route()
                P.barrier()
                self.A.release()
            if self.upto >= 4:
                self.experts()
                self.combine()
            P.final_wait("sp")
            P.emit()
        return nc

    def declare_io(self):
        NB = self.NB
        d = self.din
        self.x_d = d("x", [NB * L, D])
        self.ctx_d = d("ctx", [NB * CTX, D])
        self.cT_d = d("cT", [128, KD, NB + 1])
        self.wmod_d = d("w_mod", [D, 6 * D]).rearrange("(k p) n -> p k n", p=128)
        self.bmod_d = d("b_mod", [1, 6 * D])
        self.n1g_d = d("norm1_g", [1, D])
        self.n2g_d = d("norm2_g", [1, D])
        self.fing_d = d("final_g", [1, D])
        self.win_d = d("w_in", [D, 4096]).rearrange("(k p) n -> p k n", p=128)
        self.wout_d = d("w_out", [D, D]).rearrange("(k p) n -> p k n", p=128)
        self.hcw_d = d("hy_conv_w", [3, 1536])
        self.hcb_d = d("hy_conv_b", [1, 1536])
        self.fw1_d = d("hy_fw1", [33, 64])
        self.fb1_d = d("hy_fb1", [64, 1])
        self.fw2_d = d("hy_fw2", [64, 64])
        self.fb2_d = d("hy_fb2", [64, 1])
        self.fw3_d = d("hy_fw3", [64, 2048])
        self.freq_d = d("hy_freq", [64, 1])
        self.hyd_d = d("hy_d", [1, 1024])
        self.lbT_d = d("lbT", [128, 16])
        self.hgng_d = d("hg_norm_g", [1, 128])
        self.wr_d = d("w_router", [D, NE]).rearrange("(k p) n -> p k n", p=128)
        self.rb_d = d("router_bias", [1, NE])
        if self.upto >= 4:
            self.ewg_d = d("ew_gate", [NE, 128, KD * 256])
            self.ewu_d = d("ew_up", [NE, 128, KD * 256])
            self.ewd_d = d("ew_down", [NE, 128, 2 * D])
        self.swg_d = d("sw_gate", [D, 256]).rearrange("(k p) n -> p k n", p=128)
        self.swu_d = d("sw_up", [D, 256]).rearrange("(k p) n -> p k n", p=128)
        self.swd_d = d("sw_down", [256, D]).rearrange("(k p) n -> p k n", p=128)
        self.featsT_d = d("featsT", [33, L])
        self.cst_d = d("cst", [128, 10, 128])
        self.deltas_d = d("deltas", [1, 512])
        self.tfrac_d = d("tfrac", [128, 16])
        self.iota_d = d("iota", [1, NE])
        self.pidx_d = d("pidx", [128, 1])
        self.out_d = self.dout("out", [NB * L, D])
        self.MOD_d = self.dscr("MODs", [NB + 1, 6 * D], F32)
        self.HT_d = self.dscr("HTs", [NB, 128, KD * L], BF16)
        self.YT_d = self.dscr("YTs", [NB, 8, 128, L], BF16)
        self.G_d = self.dscr("Gs", [2, 512, 4096], BF16)
        self.X1_d = self.dscr("X1s", [NB * L, D], F32)
        self.XS_d = self.dscr("XSs", [NE * CAP + OV * 128, D], BF16)
        self.Y_d = self.dscr("Ys", [NE * CAP + OV * 128, D], BF16)
        self.H2_d = self.dscr("H2s", [NB * L, D], BF16)

    def consts(self):
        P, A = self.P, self.A
        cst = A.alloc([10, 128], F32)
        self.b_cst = Buf("cst")
        P.dma("sp", dma(cst, self.cst_d), writes=[self.b_cst])
        self.identf = cst[:, 0, :]
        self.Jf = cst[:, 1, :]
        self.maskF = cst[:, 2, :]
        self.maskB = cst[:, 3, :]
        self.ustrict_f = cst[:, 6, :]
        self.ones_f = cst[:, 7, :]
        cb = A.alloc([10, 128], BF16)
        self.b_cb = Buf("cb")
        P.op("dve", cp(cb, cst), reads=[self.b_cst], writes=[self.b_cb])
        self.identb = cb[:, 0, :]
        self.Jb = cb[:, 1, :]
        self.Smb = cb[:, 4, :]
        self.Spb = cb[:, 5, :]
        self.ustrict_b = cb[:, 6, :]
        self.ones_b = cb[:, 7, :]
        self.SmRb = cb[:, 8, :]
        self.SpRb = cb[:, 9, :]
        ce = A.alloc([2], F32)
        self.b_eps = Buf("eps")
        P.op("pool", lambda e: e.memset(ce[:, 0:1], EPS), writes=[self.b_eps])
        P.op("pool", lambda e: e.memset(ce[:, 1:2], 1.0), writes=[self.b_eps])
        self.eps_ap = ce[:, 0:1]
        self.one_ap = ce[:, 1:2]
        self.n1g = A.alloc([D], F32)
        self.b_n1g = Buf()
        P.dma("sp", dma(self.n1g, bcast_rows(self.n1g_d.tensor, 0, D)), writes=[self.b_n1g])
        self.hgng = A.alloc([128], F32)
        self.b_hgng = Buf()
        P.dma("sp", dma(self.hgng, bcast_rows(self.hgng_d.tensor, 0, 128)), writes=[self.b_hgng])
        lbt = A.alloc([2, 2, 4], F32)
        b_lbt = Buf()
        P.dma("sp", dma(lbt, self.lbT_d.rearrange("p (a b c) -> p a b c", a=2, b=2)), writes=[b_lbt])
        self.lb = A.alloc([2, 4], F32)
        self.oml = A.alloc([2, 4], F32)
        self.noml = A.alloc([2, 4], F32)
        self.b_lb = Buf()
        P.op("dve", tt(self.lb, lbt[:, :, 0, :], lbt[:, :, 1, :], ALU.subtract), reads=[b_lbt], writes=[self.b_lb])
        P.op("act", act(self.lb, self.lb, AF.Sigmoid), reads=[self.b_lb], writes=[self.b_lb])
        P.op("dve", ts(self.oml, self.lb, -1.0, 1.0, ALU.mult, ALU.add), reads=[self.b_lb], writes=[self.b_lb])
        P.op("dve", ts(self.noml, self.lb, -1.0, None, ALU.add), reads=[self.b_lb], writes=[self.b_lb])

    def modulation(self):
        P, A, NB = self.P, self.A, self.NB
        A.mark()
        cT = A.alloc([KD, NB + 1], F32)
        b_cT = Buf()
        P.dma("sp", dma(cT, self.cT_d), writes=[b_cT])
        P.op("act", act(cT, cT, AF.Silu), reads=[b_cT], writes=[b_cT])
        bm = A.alloc([6 * D], F32, parts=1)
        b_bm = Buf()
        P.dma("sp", dma(bm, self.bmod_d), writes=[b_bm])
        modsb = A.alloc([6 * D], F32)
        b_mod = Buf()
        wms = [(A.alloc([KD, 512], F32), Buf()) for _ in range(2)]
        for ci in range(12):
            wm, bw = wms[ci % 2]
            P.dma("sp" if ci % 2 == 0 else "act", dma(wm, self.wmod_d[:, :, ci * 512:(ci + 1) * 512]), writes=[bw])
            ps, pb = self.bank()
            for k in range(KD):
                P.op("pe", mm(ps[0:NB + 1, :], cT[:, k, :], wm[:, k, :], k == 0, False), reads=[b_cT, bw], writes=[pb])
            P.op("pe", mm(ps[0:NB + 1, :], self.ones_f[0:1, 0:NB + 1], bm[0:1, ci * 512:(ci + 1) * 512], False, True),
                 reads=[self.b_cst, b_bm], writes=[pb])
            P.op("act", act(modsb[0:NB + 1, ci * 512:(ci + 1) * 512], ps[0:NB + 1, :], AF.Copy), reads=[pb], writes=[b_mod])
        self.b_MOD = Buf("MOD")
        P.dma("sp", dma(self.MOD_d, modsb[0:NB + 1, :]), reads=[b_mod], writes=[self.b_MOD])
        self.dump("mod", modsb[0:NB + 1, :], b_mod, [NB + 1, 6 * D])
        P.barrier()
        A.release()
        self.CA = A.alloc([D], F32)
        self.CB = A.alloc([D], F32)
        self.b_CA = Buf()
        self.b_CB = Buf()
        mt = self.MOD_d.tensor
        P.dma("sp", dma(self.CB, bcast_rows(mt, NB * 6 * D + 0 * D, D)), reads=[self.b_MOD], writes=[self.b_CB])
        P.dma("sp", dma(self.CA, bcast_rows(mt, NB * 6 * D + 1 * D, D)), reads=[self.b_MOD], writes=[self.b_CA])
        P.op("dve", stt(self.CA, self.CA, 1.0, self.n1g, ALU.add, ALU.mult), reads=[self.b_CA, self.b_n1g], writes=[self.b_CA])

    def norm_mod_tile(self, i, src, Abc, bA, Bbc, bB, dst, b_dst, hb_out=None):
        P = self.P
        xt, bx = self.xt[i % 2]
        P.dma("sp", dma(xt, src), writes=[bx])
        self.norm_mod_sb(i, xt, bx, Abc, bA, Bbc, bB, dst, b_dst)

    def norm_mod_sb(self, i, xt, bx, Abc, bA, Bbc, bB, dst, b_dst, hf_out=None, hb_out=None):
        P = self.P
        junk, bj = self.junk
        ss, bss = self.ssb[i % 2]
        hb, bhb = (self.hb[i % 2] if hb_out is None else hb_out)
        P.op("act", act(junk, xt, AF.Square), reads=[bx], writes=[bj])
        P.op("dve", rsum(ss, junk), reads=[bj], writes=[bss])
        P.op("act", act(ss, ss, AF.Sqrt, scale=1.0 / D, bias=self.eps_ap), reads=[bss, self.b_eps], writes=[bss])
        P.op("dve", lambda e: e.reciprocal(out=ss, in_=ss), reads=[bss], writes=[bss])
        P.op("dve", stt(junk, xt, ss, Abc, ALU.mult, ALU.mult), reads=[bx, bss, bA], writes=[bj])
        if hf_out is not None:
            hf, bhf = hf_out
            P.op("dve", tt(hf, junk, Bbc, ALU.add), reads=[bj, bB], writes=[bhf])
            P.op("act", act(hb, hf, AF.Copy), reads=[bhf], writes=[bhb])
        else:
            P.op("dve", tt(hb, junk, Bbc, ALU.add), reads=[bj, bB], writes=[bhb])
        ps, pb = self.bank()
        psb = ps.bitcast(BF16)
        for k in range(KD):
            P.op("pe", tr(psb[:, k * 128:(k + 1) * 128], hb[:, k * 128:(k + 1) * 128], self.identb),
                 reads=[bhb, self.b_cb], writes=[pb])
        P.op("act", act(dst, psb.rearrange("p (k t) -> p k t", k=KD), AF.Copy), reads=[pb], writes=[b_dst])

    def mixer_batch(self, b):
        P, A, NB = self.P, self.A, self.NB
        A.mark()
        mt = self.MOD_d.tensor
        A1 = A.alloc([D], F32)
        B1 = A.alloc([D], F32)
        bA1, bB1 = Buf(), Buf()
        P.dma("sp", dma(B1, bcast_rows(mt, b * 6 * D + 0 * D, D)), reads=[self.b_MOD], writes=[bB1])
        P.dma("sp", dma(A1, bcast_rows(mt, b * 6 * D + 1 * D, D)), reads=[self.b_MOD], writes=[bA1])
        P.op("dve", stt(A1, A1, 1.0, self.n1g, ALU.add, ALU.mult), reads=[bA1, self.b_n1g], writes=[bA1])
        hT = A.alloc([KD, T], BF16)
        b_hT = Buf("hT")
        yT = A.alloc([4, L], BF16)
        b_yT = Buf("yT")
        A.mark()
        self.xt = [(A.alloc([D], F32), Buf()) for _ in range(2)]
        self.junk = (A.alloc([D], F32), Buf())
        self.ssb = [(A.alloc([1], F32), Buf()) for _ in range(2)]
        self.hb = [(A.alloc([D], BF16), Buf()) for _ in range(2)]
        for j in range(NT):
            if j < 2:
                src = self.ctx_d[b * CTX + j * 128: b * CTX + (j + 1) * 128, :]
                self.norm_mod_tile(j, src, self.CA, self.b_CA, self.CB, self.b_CB, hT[:, :, j * 128:(j + 1) * 128], b_hT)
            else:
                src = self.x_d[b * L + (j - 2) * 128: b * L + (j - 1) * 128, :]
                self.norm_mod_tile(j, src, A1, bA1, B1, bB1, hT[:, :, j * 128:(j + 1) * 128], b_hT)
        self.b_HT = getattr(self, "b_HT", None) or Buf("HT")
        P.dma("sp", dma(self.HT_d[b].rearrange("p (k t) -> p k t", k=KD), hT[:, :, CTX:T]), reads=[b_hT], writes=[self.b_HT])
        if b == 0:
            self.dump("hT", hT, b_hT, [128, KD, T], BF16)
        P.barrier()
        A.release()
        if self.upto < 1:
            A.release()
            return
        self.hgrn2(b, hT, b_hT, yT, b_yT)
        self.b_YT = getattr(self, "b_YT", None) or Buf("YT")
        for hh in range(4):
            P.dma("sp", dma(self.YT_d[b, 4 + hh], yT[:, hh, :]), reads=[b_yT], writes=[self.b_YT])
        if b == 0:
            self.dump("yT_hg", yT, b_yT, [128, 4, L], BF16)
        P.barrier()
        A.release()

    def hgrn2(self, b, hT, b_hT, yT, b_yT):
        P, A = self.P, self.A
        A.mark()
        f32b = lambda: (A.alloc([T], F32), Buf())
        bf16b = lambda: (A.alloc([T], BF16), Buf())
        self.rs = A.alloc([T], F32)
        self.b_rs = Buf()
        P.op("pool", lambda e: e.memset(self.rs, 1.0), writes=[self.b_rs])
        rs3 = self.rs.rearrange("p (a b) -> p a b", b=64)
        P.op("pool", lambda e: e.memset(rs3[:, :, 0:1], 0.0), writes=[self.b_rs])
        t1, b_t1 = f32b()
        kk, b_kk = f32b()
        bb, b_bb = f32b()
        t2, b_t2 = f32b()
        qs, b_qs = bf16b()
        qm, b_qm = bf16b()
        km, b_km = bf16b()
        qbE, b_qbE = bf16b()
        qbO, b_qbO = bf16b()
        kdT, b_kdT = bf16b()
        P.op("pool", lambda e: e.memset(qbE, 0.0), writes=[b_qbE])
        P.op("pool", lambda e: e.memset(qbO, 0.0), writes=[b_qbO])
        kdTokE = A.alloc([NT, 128], BF16)
        kdTokO = A.alloc([NT, 128], BF16)
        b_kdTok = Buf()
        P.op("pool", lambda e: e.memset(kdTokE[64:128], 0.0), writes=[b_kdTok])
        P.op("pool", lambda e: e.memset(kdTokO[0:64], 0.0), writes=[b_kdTok])
        V = A.alloc([NT, 128], BF16)
        b_V = Buf()
        gsn = A.alloc([NLT, 128], BF16)
        b_gsn = Buf()
        of = A.alloc([NLT, 128], F32)
        b_of = Buf()
        dec = A.alloc([NCH], F32)
        b_dec = Buf()
        ws = [(A.alloc([KD, 128], BF16), Buf()) for _ in range(5)]
        Sf = [(A.alloc([128], F32), Buf()) for _ in range(2)]
        Sb = [(A.alloc([128], BF16), Buf()) for _ in range(2)]
        attT = [(A.alloc([128], BF16), Buf()) for _ in range(2)]
        otl = [(A.alloc([128], F32), Buf()) for _ in range(2)]
        oj = [(A.alloc([128], F32), Buf()) for _ in range(2)]
        ossb = [(A.alloc([1], F32), Buf()) for _ in range(2)]
        ytl = [(A.alloc([128], BF16), Buf()) for _ in range(2)]
        gtmp = [(A.alloc([128], F32), Buf()) for _ in range(2)]
        chunks = [(t0, min(512, T - t0)) for t0 in range(0, T, 512)]
        v3 = lambda ap: ap.rearrange("p (a b) -> p a b", b=64)
        for hh in range(4):
            cols = [1536 + s * 512 + hh * 128 for s in range(5)]
            for s in range(5):
                w, bw = ws[s]
                P.dma("pool", dma(w, self.win_d[:, :, cols[s]:cols[s] + 128]), writes=[bw])
            wq, wf, wb_, wi, wg = ws
            for j in range(NT):
                ps, pb = self.bank()
                for k in range(KD):
                    P.op("pe", mm(ps[:, 0:128], hT[:, k, j * 128:(j + 1) * 128], wi[0][:, k, :], k == 0, k == KD - 1),
                         reads=[b_hT, wi[1]], writes=[pb])
                P.op("act", act(V[:, j, :], ps[:, 0:128], AF.Copy), reads=[pb], writes=[b_V])
                if j >= 2:
                    ps2, pb2 = self.bank()
                    for k in range(KD):
                        P.op("pe", mm(ps2[:, 0:128], hT[:, k, j * 128:(j + 1) * 128], wg[0][:, k, :], k == 0, k == KD - 1),
                             reads=[b_hT, wg[1]], writes=[pb2])
                    gt, bgt = gtmp[j % 2]
                    P.op("act", act(gt, ps2[:, 0:128], AF.Silu), reads=[pb2], writes=[bgt])
                    P.op("dve", tt(gsn[:, j - 2, :], gt, self.hgng, ALU.mult), reads=[bgt, self.b_hgng], writes=[b_gsn])
            for (t0, n) in chunks:
                ps, pb = self.bank()
                for k in range(KD):
                    P.op("pe", mm(ps[:, 0:n], wq[0][:, k, :], hT[:, k, t0:t0 + n], k == 0, k == KD - 1),
                         reads=[b_hT, wq[1]], writes=[pb])
                P.op("act", act(qs[:, t0:t0 + n], ps[:, 0:n], AF.Silu), reads=[pb], writes=[b_qs])
            for d in range(2):
                wgate = wf if d == 0 else wb_
                for (t0, n) in chunks:
                    ps, pb = self.bank()
                    for k in range(KD):
                        P.op("pe", mm(ps[:, 0:n], wgate[0][:, k, :], hT[:, k, t0:t0 + n], k == 0, k == KD - 1),
                             reads=[b_hT, wgate[1]], writes=[pb])
                    P.op("act", act(t1[:, t0:t0 + n], ps[:, 0:n], AF.Sigmoid), reads=[pb], writes=[b_t1])
                P.op("dve", ts(kk, t1, self.noml[:, d, hh:hh + 1], self.oml[:, d, hh:hh + 1], ALU.mult, ALU.add),
                     reads=[b_t1, self.b_lb], writes=[b_kk])
                P.op("act", act(t1, kk, AF.Ln, scale=-1.0, bias=self.one_ap), reads=[b_kk, self.b_eps], writes=[b_t1])
                P.op("dve", lambda e: e.tensor_tensor_scan(out=bb, data0=self.rs, data1=t1, initial=0.0, op0=ALU.mult, op1=ALU.add),
                     reads=[self.b_rs, b_t1], writes=[b_bb])
                bb3, t13 = v3(bb), v3(t1)
                if d == 1:
                    P.op("dve", tt(t1, t1, bb, ALU.subtract), reads=[b_t1, b_bb], writes=[b_t1])
                    P.op("dve", tt(bb3, t13, bb3[:, :, 63:64].to_broadcast([128, NCH, 64]), ALU.add), reads=[b_t1, b_bb], writes=[b_bb])
                mid = 31 if d == 0 else 32
                last = 63 if d == 0 else 0
                P.op("dve", tt(t13, bb3, bb3[:, :, mid:mid + 1].to_broadcast([128, NCH, 64]), ALU.subtract), reads=[b_bb], writes=[b_t1])
                P.op("act", act(t2, t1, AF.Exp), reads=[b_t1], writes=[b_t2])
                P.op("dve", stt(qm, qs, HGS, t2, ALU.mult, ALU.mult), reads=[b_qs, b_t2], writes=[b_qm])
                P.op("act", act(t2, t1, AF.Exp, scale=-1.0), reads=[b_t1, b_qm], writes=[b_t2])
                P.op("dve", tt(km, kk, t2, ALU.mult), reads=[b_kk, b_t2], writes=[b_km])
                P.op("act", act(t2, bb, AF.Exp), reads=[b_bb, b_km], writes=[b_t2])
                t23, qs3, qbE3, qbO3 = v3(t2), v3(qs), v3(qbE), v3(qbO)
                P.op("dve", stt(qbE3[:, 0::2, :], qs3[:, 0::2, :], HGS, t23[:, 0::2, :], ALU.mult, ALU.mult),
                     reads=[b_qs, b_t2], writes=[b_qbE])
                P.op("dve", stt(qbO3[:, 1::2, :], qs3[:, 1::2, :], HGS, t23[:, 1::2, :], ALU.mult, ALU.mult),
                     reads=[b_qs, b_t2], writes=[b_qbO])
                P.op("dve", tt(t13, bb3[:, :, last:last + 1].to_broadcast([128, NCH, 64]), bb3, ALU.subtract), reads=[b_bb, b_t2], writes=[b_t1])
                P.op("act", act(t1, t1, AF.Exp), reads=[b_t1], writes=[b_t1])
                P.op("dve", tt(kdT, kk, t1, ALU.mult), reads=[b_kk, b_t1], writes=[b_kdT])
                P.op("act", act(dec, bb3[:, :, last], AF.Exp), reads=[b_bb], writes=[b_dec])
                for j0 in range(0, NT, 8):
                    nj = min(8, NT - j0)
                    ps, pb = self.bank()
                    psb = ps.bitcast(BF16)
                    for jj in range(nj):
                        j = j0 + jj
                        P.op("pe", tr(psb[:, jj * 128:(jj + 1) * 128], kdT[:, j * 128:(j + 1) * 128], self.identb),
                             reads=[b_kdT, self.b_cb], writes=[pb])
                    P.op("act", act(kdTokE[0:64, j0:j0 + nj, :], psb[0:64, 0:nj * 128].rearrange("p (a b) -> p a b", b=128), AF.Copy),
                         reads=[pb], writes=[b_kdTok])
                    P.op("act", act(kdTokO[64:128, j0:j0 + nj, :], psb[64:128, 0:nj * 128].rearrange("p (a b) -> p a b", b=128), AF.Copy),
                         reads=[pb], writes=[b_kdTok])
                si = 0
                P.op("pool", lambda e, s=Sf[0][0]: e.memset(s, 0.0), writes=[Sf[0][1]])
                P.op("pool", lambda e, s=Sb[0][0]: e.memset(s, 0.0), writes=[Sb[0][1]])
                order = list(range(NT)) if d == 0 else [1, 0] + list(range(NT - 1, 1, -1))
                mask = self.maskF if d == 0 else self.maskB
                for j in order:
                    lat = j >= 2
                    tsl = slice(j * 128, (j + 1) * 128)
                    halves = [0, 1] if d == 0 else [1, 0]
                    if lat:
                        psA, pbA = self.bank()
                        P.op("pe", mm(psA[:, 0:128], km[:, tsl], qm[:, tsl], True, True), reads=[b_km, b_qm], writes=[pbA])
                        at, bat = attT[j % 2]
                        P.op("dve", tt(at, psA[:, 0:128], mask, ALU.mult), reads=[pbA, self.b_cst], writes=[bat])
                        psO, pbO = self.bank()
                        P.op("pe", mm(psO[:, 0:128], at, V[:, j, :], True, False), reads=[bat, b_V], writes=[pbO])
                    for hi, h in enumerate(halves):
                        if lat:
                            qbx, b_qbx = (qbE, b_qbE) if h == 0 else (qbO, b_qbO)
                            P.op("pe", mm(psO[:, 0:128], qbx[:, tsl], Sb[si][0], False, hi == 1),
                                 reads=[b_qbx, Sb[si][1]], writes=[pbO])
                        psU, pbU = self.bank()
                        kdx = kdTokE if h == 0 else kdTokO
                        P.op("pe", mm(psU[:, 0:128], kdx[:, j, :], V[:, j, :], True, True), reads=[b_kdTok, b_V], writes=[pbU])
                        ch = 2 * j + h
                        P.op("dve", stt(Sb[1 - si][0], Sf[si][0], dec[:, ch:ch + 1], psU[:, 0:128], ALU.mult, ALU.add),
                             reads=[Sf[si][1], b_dec, pbU], writes=[Sb[1 - si][1]])
                        P.op("dve", stt(Sf[1 - si][0], Sf[si][0], dec[:, ch:ch + 1], psU[:, 0:128], ALU.mult, ALU.add),
                             reads=[Sf[si][1], b_dec, pbU], writes=[Sf[1 - si][1]])
                        si = 1 - si
                    if lat:
                        if d == 0:
                            P.op("act", act(of[:, j - 2, :], psO[:, 0:128], AF.Copy), reads=[pbO], writes=[b_of])
                        else:
                            o, bo = oj[j % 2]
                            P.op("dve", tt(o, psO[:, 0:128], of[:, j - 2, :], ALU.add), reads=[pbO, b_of], writes=[bo])
                            jk, bjk = otl[j % 2]
                            oss, boss = ossb[j % 2]
                            P.op("act", act(jk, o, AF.Square), reads=[bo], writes=[bjk])
                            P.op("dve", rsum(oss, jk), reads=[bjk], writes=[boss])
                            P.op("act", act(oss, oss, AF.Sqrt, scale=1.0 / 128, bias=self.eps_ap), reads=[boss, self.b_eps], writes=[boss])
                            P.op("dve", lambda e, oss=oss: e.reciprocal(out=oss, in_=oss), reads=[boss], writes=[boss])
                            yt, byt = ytl[j % 2]
                            P.op("dve", stt(yt, o, oss, gsn[:, j - 2, :], ALU.mult, ALU.mult), reads=[bo, boss, b_gsn], writes=[byt])
                            psT, pbT = self.bank()
                            psTb = psT.bitcast(BF16)
                            P.op("pe", tr(psTb[:, 0:128], yt, self.identb), reads=[byt, self.b_cb], writes=[pbT])
                            P.op("act", act(yT[:, hh, (j - 2) * 128:(j - 1) * 128], psTb[:, 0:128], AF.Copy), reads=[pbT], writes=[b_yT])
        P.barrier()
        A.release()

    def sin_da(self, out, arg, tmpa, tmpb, bufs):
        P = self.P
        b_out, b_arg, b_ta, b_tb = bufs
        P.op("act", act(tmpa, arg, AF.Sin, scale=0.5), reads=[b_arg], writes=[b_ta])
        P.op("act", act(tmpb, arg, AF.Sin, scale=0.25), reads=[b_arg], writes=[b_tb])
        P.op("dve", tt(tmpb, tmpb, tmpb, ALU.mult), reads=[b_tb], writes=[b_tb])
        P.op("dve", ts(tmpb, tmpb, -2.0, 1.0, ALU.mult, ALU.add), reads=[b_tb], writes=[b_tb])
        P.op("dve", stt(out, tmpa, 2.0, tmpb, ALU.mult, ALU.mult), reads=[b_ta, b_tb], writes=[b_out])

    def filters(self):
        P, A = self.P, self.A
        A.mark()
        ld = lambda shape, src, parts: (A.alloc(shape, F32, parts=parts), Buf())
        featsT, b_ft = ld([L], None, 33)
        P.dma("sp", dma(featsT, self.featsT_d), writes=[b_ft])
        fw1, b_fw1 = ld([64], None, 33)
        P.dma("sp", dma(fw1, self.fw1_d), writes=[b_fw1])
        fw2, b_fw2 = ld([64], None, 64)
        P.dma("sp", dma(fw2, self.fw2_d), writes=[b_fw2])
        fw3, b_fw3 = ld([2048], None, 64)
        P.dma("sp", dma(fw3, self.fw3_d), writes=[b_fw3])
        sm, b_sm = ld([8], None, 64)
        P.dma("sp", dma(sm[:, 0:1], self.fb1_d), writes=[b_sm])
        P.dma("sp", dma(sm[:, 1:2], self.fb2_d), writes=[b_sm])
        P.dma("sp", dma(sm[:, 2:3], self.freq_d), writes=[b_sm])
        P.op("dve", ts(sm[:, 3:5], sm[:, 0:2], sm[:, 2:3], None, ALU.mult), reads=[b_sm], writes=[b_sm])
        dl, b_dl = ld([512], None, 128)
        P.dma("sp", dma(dl, bcast_rows(self.deltas_d.tensor, 0, 512)), writes=[b_dl])
        tf, b_tf = ld([16], None, 128)
        P.dma("sp", dma(tf, self.tfrac_d), writes=[b_tf])
        dsk, b_dsk = ld([2, 4], None, 128)
        P.dma("sp", lambda e: e.dma_start(out=dsk, in_=self.hyd_d.rearrange("a (o g c) -> c (a o) g", o=2, g=4),
                                          allow_slow_non_contiguous=True), writes=[b_dsk])
        h1, b_h1 = ld([L], None, 64)
        h2, b_h2 = ld([L], None, 64)
        ta, b_ta = ld([L], None, 64)
        tb, b_tb = ld([L], None, 64)
        ar, b_ar = ld([L], None, 64)
        for layer in range(2):
            src, b_src, K_, w, b_w = (featsT, b_ft, 33, fw1, b_fw1) if layer == 0 else (h1, b_h1, 64, fw2, b_fw2)
            for q in range(4):
                ps, pb = self.bank()
                P.op("pe", mm(ps[0:64, :], w[0:K_, :], src[0:K_, q * 512:(q + 1) * 512], True, True), reads=[b_w, b_src], writes=[pb])
                P.op("act", act(ar[:, q * 512:(q + 1) * 512], ps[0:64, :], AF.Identity, scale=sm[:, 2:3], bias=sm[:, 3 + layer:4 + layer]),
                     reads=[pb, b_sm], writes=[b_ar])
            dst, b_dst = (h1, b_h1) if layer == 0 else (h2, b_h2)
            self.sin_da(dst, ar, ta, tb, (b_dst, b_ar, b_ta, b_tb))
        rinv = A.alloc([2, 512], F32)
        b_rinv = Buf()
        win = [(A.alloc([512], F32), Buf()) for _ in range(2)]
        winr = [(A.alloc([2, 512], F32), Buf()) for _ in range(2)]
        hw = [(A.alloc([512], F32), Buf()) for _ in range(2)]
        hn = [(A.alloc([512], BF16), Buf()) for _ in range(2)]
        GT = A.alloc([4, 2, 4096], BF16)
        b_GT = Buf("GT")
        P.op("pool", lambda e: e.memset(GT, 0.0), writes=[b_GT])
        accs = self.reserve(2)
        for i in range(16):
            wi_, bwi = win[i % 2]
            P.op("act", act(wi_, dl, AF.Exp, scale=tf[:, i:i + 1]), reads=[b_dl, b_tf], writes=[bwi])
            for o in range(2):
                for dr in range(2):
                    q = o * 2 + dr
                    ps, pb = self.bank()
                    P.op("pe", mm(ps[:, :], h2[0:64, i * 128:(i + 1) * 128], fw3[0:64, q * 512:(q + 1) * 512], True, True),
                         reads=[b_h2, b_fw3], writes=[pb])
                    hwt, bhw = hw[q % 2]
                    P.op("dve", tt(hwt, ps, wi_, ALU.mult), reads=[pb, bwi], writes=[bhw])
                    P.op("act", act(hwt, hwt, AF.Abs), reads=[bhw], writes=[bhw])
                    first = (i == 0 and dr == 0)
                    lastf = (i == 15 and dr == 1)
                    P.op("pe", mm(accs[o][0][:, :], self.ones_f, hwt, first, lastf), reads=[self.b_cst, bhw], writes=[accs[o][1]])
        for o in range(2):
            P.op("dve", lambda e, o=o: e.reciprocal(out=rinv[:, o, :], in_=accs[o][0][:, :]), reads=[accs[o][1]], writes=[b_rinv])
        self.unreserve(accs)
        GTv = GT
        for i in range(16):
            wi_, bwi = win[i % 2]
            wr, bwr = winr[i % 2]
            P.op("act", act(wi_, dl, AF.Exp, scale=tf[:, i:i + 1]), reads=[b_dl, b_tf], writes=[bwi])
            P.op("dve", tt(wr, rinv, wi_.unsqueeze(1).to_broadcast([128, 2, 512]), ALU.mult), reads=[bwi, b_rinv], writes=[bwr])
            for o in range(2):
                for dr in ([1, 0] if o == 0 else [0, 1]):
                    q = o * 2 + dr
                    useJ = (o == 0 and dr == 1) or (o == 1 and dr == 0)
                    ps, pb = self.bank()
                    P.op("pe", mm(ps[:, :], h2[0:64, i * 128:(i + 1) * 128], fw3[0:64, q * 512:(q + 1) * 512], True, True),
                         reads=[b_h2, b_fw3], writes=[pb])
                    hnt, bhn = hn[q % 2]
                    P.op("dve", tt(hnt, ps, wr[:, o, :], ALU.mult), reads=[pb, bwr], writes=[bhn])
                    pt, pbt = self.bank()
                    for cg in range(4):
                        P.op("pe", mm(pt[:, cg * 128:(cg + 1) * 128], hnt[:, cg * 128:(cg + 1) * 128], self.Jb if useJ else self.identb, True, True),
                             reads=[bhn, self.b_cb], writes=[pbt])
                    pt3 = pt.rearrange("p (g m) -> p g m", g=4)
                    if o == 0:
                        start = (1921 - 128 * i) if useJ else (2048 + 128 * i)
                    else:
                        start = (1920 - 128 * i) if useJ else (2047 + 128 * i)
                    if i == 0 and not useJ:
                        P.op("act", act(GTv[:, :, o, start + 1:start + 128], pt3[:, :, 1:128], AF.Copy), reads=[pbt], writes=[b_GT])
                        ctr, b_ctr = self.ctr_tmp = getattr(self, "ctr_tmp", None) or (A.alloc([4], F32), Buf())
                        P.op("dve", tt(ctr, pt3[:, :, 0], dsk[:, o, :], ALU.add), reads=[pbt, b_dsk], writes=[b_ctr])
                        P.op("dve", tt(GTv[:, :, o, start], GTv[:, :, o, start], ctr, ALU.add), reads=[b_ctr, b_GT], writes=[b_GT])
                    else:
                        P.op("act", act(GTv[:, :, o, start:start + 128], pt3, AF.Copy), reads=[pbt], writes=[b_GT])
        self.b_G = Buf("G")
        for o in range(2):
            for cg in range(4):
                P.dma("sp", dma(self.G_d[o, cg * 128:(cg + 1) * 128, :], GTv[:, cg, o, :]), reads=[b_GT], writes=[self.b_G])
        if "G" in self.dbg:
            for o in range(2):
                og = self.dout("dbg_G%d" % o, [128, 4, 4096], BF16)
                P.dma("sp", dma(og, GTv[:, :, o, :]), reads=[b_GT])
        P.barrier()
        A.release()

    def hyena_all(self):
        P, A, NB = self.P, self.A, self.NB
        NBI = NB * 16
        A.mark()
        bf = lambda: A.alloc([128, NB, 16], BF16)
        X1, X2r, Vr, Z = bf(), bf(), bf(), bf()
        b_in = [Buf() for _ in range(16)]
        b_Z = [Buf() for _ in range(16)]
        hTl = A.alloc([KD, L], BF16)
        b_hTl = Buf()
        W3 = A.alloc([KD, 384], BF16)
        b_W3 = Buf()
        wc = A.alloc([3, 384], F32)
        bc = A.alloc([384], F32)
        b_wc = Buf()
        pw = [(A.alloc([3, 384], BF16), Buf()) for _ in range(2)]
        NTZ = 3
        TZ = [[(A.alloc([3968], BF16), Buf()) for _ in range(NTZ)] for _ in range(2)]
        yTc = [(A.alloc([512], BF16), Buf()) for _ in range(2)]
        gt = self.G_d.tensor
        dq = ["sp", "act"]
        tzc = [0, 0]
        for cg in range(4):
            for s in range(3):
                c0 = s * 512 + cg * 128
                P.dma("pool", dma(W3[:, :, s * 128:(s + 1) * 128], self.win_d[:, :, c0:c0 + 128]), writes=[b_W3])
                for kk in range(3):
                    P.dma("sp", dma(wc[:, kk, s * 128:(s + 1) * 128], bcast_rows(self.hcw_d.tensor, kk * 1536 + c0, 128)), writes=[b_wc])
                P.dma("sp", dma(bc[:, s * 128:(s + 1) * 128], bcast_rows(self.hcb_d.tensor, c0, 128)), writes=[b_wc])
            for b in range(NB):
                P.dma("sp", dma(hTl, self.HT_d[b].rearrange("p (k t) -> p k t", k=KD)), reads=[self.b_HT], writes=[b_hTl])
                for i in range(NLT):
                    ps, pb = self.bank()
                    for k in range(KD):
                        P.op("pe", mm(ps[:, 0:384], hTl[:, k, i * 128:(i + 1) * 128], W3[:, k, :], k == 0, k == KD - 1),
                             reads=[b_hTl, b_W3], writes=[pb])
                    pwt, bpw = pw[i % 2]
                    for kk in range(3):
                        P.op("dve", tt(pwt[:, kk, :], ps[:, 0:384], wc[:, kk, :], ALU.mult), reads=[pb, b_wc], writes=[bpw])
                    pa, pba = self.bank()
                    pr, pbr = self.bank()
                    mats = [(self.Smb, self.SmRb), (self.identb, self.Jb), (self.Spb, self.SpRb)]
                    for kk in range(3):
                        P.op("pe", mm(pa[:, 0:128], mats[kk][0], pwt[:, kk, 0:128], kk == 0, kk == 2), reads=[bpw, self.b_cb], writes=[pba])
                    for kk in range(3):
                        P.op("pe", mm(pr[:, 0:256], mats[kk][1], pwt[:, kk, 128:384], kk == 0, kk == 2), reads=[bpw, self.b_cb], writes=[pbr])
                    P.op("dve", tt(X1[:, :, b, i], pa[:, 0:128], bc[:, 0:128], ALU.add), reads=[pba, b_wc], writes=b_in)
                    P.op("dve", tt(X2r[:, :, b, i], pr[:, 0:128], bc[:, 128:256], ALU.add), reads=[pbr, b_wc], writes=b_in)
                    P.op("dve", tt(Vr[:, :, b, i], pr[:, 128:256], bc[:, 256:384], ALU.add), reads=[pbr, b_wc], writes=b_in)
            deltas = [0] + [s * m for m in range(1, 16) for s in (1, -1)]

            def conv(o, g, src):
                ps, pb = self.bank()
                for cc in range(8):
                    c = g * 8 + cc
                    ch = cg * 128 + c
                    tz, btz = TZ[o][tzc[o] % NTZ]
                    tzc[o] += 1
                    off = (o * 512 + ch) * 4096 + (1 if o == 0 else 0)
                    P.dma(dq[(c + o) % 2], dma(tz, bass.AP(tensor=gt, offset=off, ap=[[1, 128], [1, 3968]])), reads=[self.b_G], writes=[btz])
                    psv = ps[:, cc * NBI:(cc + 1) * NBI].rearrange("p (b i) -> p b i", b=NB)
                    for di, dl_ in enumerate(deltas):
                        ilo, ihi = max(0, -dl_), min(16, 16 - dl_)
                        blk = (dl_ + 15) if o == 0 else (-dl_ + 15)
                        P.op("pe", mm(psv[:, :, ilo + dl_:ihi + dl_], tz[:, blk * 128:(blk + 1) * 128], src[:, c, :, ilo:ihi],
                                      di == 0, di == len(deltas) - 1),
                             reads=[btz, (b_in[g] if o == 0 else b_Z[g])], writes=[pb])
                return ps, pb

            def evac1(g, ps, pb):
                P.op("dve", tt(Z[:, g * 8:(g + 1) * 8, :, :].rearrange("p c b i -> p (c b i)"), ps[:, 0:8 * NBI],
                               X1[:, g * 8:(g + 1) * 8, :, :].rearrange("p c b i -> p (c b i)"), ALU.mult),
                     reads=[pb, b_in[g]], writes=[b_Z[g]])

            def evac2(g, ps, pb):
                P.op("dve", tt(Z[:, g * 8:(g + 1) * 8, :, :].rearrange("p c b i -> p (c b i)"), ps[:, 0:8 * NBI],
                               X2r[:, g * 8:(g + 1) * 8, :, :].rearrange("p c b i -> p (c b i)"), ALU.mult),
                     reads=[pb, b_in[g]], writes=[b_Z[g]])

            p1 = conv(0, 0, Vr)
            for g in range(16):
                evac1(g, *p1)
                if g + 1 < 16:
                    p1 = conv(0, g + 1, Vr)
                p2 = conv(1, g, Z)
                evac2(g, *p2)
            for b in range(NB):
                for i0 in range(0, NLT, 4):
                    ps, pb = self.bank()
                    for ii in range(4):
                        P.op("pe", mm(ps[:, ii * 128:(ii + 1) * 128], Z[:, :, b, i0 + ii], self.Jb, True, True), reads=b_Z + [self.b_cb], writes=[pb])
                    yt, byt = yTc[(i0 // 4) % 2]
                    P.op("act", act(yt, ps, AF.Copy), reads=[pb], writes=[byt])
                    P.dma("sp", dma(self.YT_d[b, cg, :, i0 * 128:(i0 + 4) * 128], yt), reads=[byt], writes=[self.b_YT])
                    if b == 0 and "yT_hy" in self.dbg:
                        if not hasattr(self, "dbg_hy"):
                            self.dbg_hy = self.dout("dbg_yT_hy", [128, 4, L], BF16)
                        P.dma("sp", dma(self.dbg_hy[:, cg, i0 * 128:(i0 + 4) * 128], yt), reads=[byt])
        P.barrier()
        A.release()

    def route_init(self):
        P, A, NB = self.P, self.A, self.NB
        NTT = NB * NLT
        self.destAll = A.alloc([NTT, 8], I32)
        self.wAll = A.alloc([NTT, 8], F32)
        self.b_dw = Buf("destw")
        self.cnt = A.alloc([NE], F32)
        self.b_cnt = Buf("cnt")
        P.op("pool", lambda e: e.memset(self.cnt, 0.0), writes=[self.b_cnt])
        self.b_rc = Buf("routeconst")
        self.E8All = A.alloc([NTT, 8], F32)
        self.P8All = A.alloc([NTT, 8], F32)
        self.W8raw = A.alloc([NTT, 8], F32)
        self.DestF = A.alloc([NTT, 8], F32)
        self.b_ov = Buf("ovstate")
        self.IDXG = A.alloc([OV], I32)
        self.b_idx = Buf("ovidx")
        self.b_H2 = Buf("H2")
        A.mark()
        self.iota3 = A.alloc([NE], F32)
        self.rbias = A.alloc([NE], F32)
        self.n2g = A.alloc([D], F32)
        P.dma("sp", dma(self.iota3, bcast_rows(self.iota_d.tensor, 0, NE)), writes=[self.b_rc])
        P.dma("sp", dma(self.rbias, bcast_rows(self.rb_d.tensor, 0, NE)), writes=[self.b_rc])
        P.dma("sp", dma(self.n2g, bcast_rows(self.n2g_d.tensor, 0, D)), writes=[self.b_rc])
        self.wr = A.alloc([KD, NE], F32)
        P.dma("sp", dma(self.wr, self.wr_d), writes=[self.b_rc])
        self.wout = A.alloc([KD, D], BF16)
        self.swgu = A.alloc([KD, 512], BF16)
        self.swd = A.alloc([2, D], BF16)
        self.b_wts = Buf("wts")
        for k in range(KD):
            P.dma("pool", dma(self.wout[:, k, :], self.wout_d[:, k, :]), writes=[self.b_wts])
        P.dma("pool", dma(self.swgu[:, :, 0:256], self.swg_d), writes=[self.b_wts])
        P.dma("pool", dma(self.swgu[:, :, 256:512], self.swu_d), writes=[self.b_wts])
        P.dma("pool", dma(self.swd, self.swd_d), writes=[self.b_wts])
        self.b_X1 = Buf("X1")
        self.b_XS = Buf("XS")

        def pre_pool(engine):
            self.bc_reg = engine.to_reg(NE * CAP - 1)
            self.bc_reg2 = engine.to_reg(NE * CAP + OV * 128 - 1)
            self.bc_reg3 = engine.to_reg(NE * 128 - 1)
        P.pre["pool"] = pre_pool

    def outproj_route(self, b):
        P, A, NB = self.P, self.A, self.NB
        A.mark()
        mt = self.MOD_d.tensor
        G1 = A.alloc([D], F32)
        A2 = A.alloc([D], F32)
        B2 = A.alloc([D], F32)
        G2 = A.alloc([D], F32)
        b_m = Buf()
        P.dma("sp", dma(G1, bcast_rows(mt, b * 6 * D + 2 * D, D)), reads=[self.b_MOD], writes=[b_m])
        P.dma("sp", dma(B2, bcast_rows(mt, b * 6 * D + 3 * D, D)), reads=[self.b_MOD], writes=[b_m])
        P.dma("sp", dma(A2, bcast_rows(mt, b * 6 * D + 4 * D, D)), reads=[self.b_MOD], writes=[b_m])
        P.dma("sp", dma(G2, bcast_rows(mt, b * 6 * D + 5 * D, D)), reads=[self.b_MOD], writes=[b_m])
        P.op("dve", stt(A2, A2, 1.0, self.n2g, ALU.add, ALU.mult), reads=[b_m, self.b_rc], writes=[b_m])
        self.junk = (A.alloc([D], F32), Buf())
        self.ssb = [(A.alloc([1], F32), Buf()) for _ in range(2)]
        yT4 = [(A.alloc([KD, 512], BF16), Buf()) for _ in range(2)]
        xts = [(A.alloc([D], F32), Buf()) for _ in range(2)]
        x1s = [(A.alloc([D], F32), Buf()) for _ in range(2)]
        tmpA = (A.alloc([D], F32), Buf())
        tmpB = (A.alloc([D], F32), Buf())
        hfs = [(A.alloc([D], F32), Buf()) for _ in range(2)]
        hbs = [(A.alloc([D], BF16), Buf()) for _ in range(2)]
        h2Tb = [(A.alloc([KD, 128], BF16), Buf()) for _ in range(2)]
        h2Tf = [(A.alloc([KD, 128], F32), Buf()) for _ in range(2)]
        scs = [(A.alloc([NE], F32), Buf()) for _ in range(2)]
        bias_ = [(A.alloc([NE], F32), Buf()) for _ in range(2)]
        msk = (A.alloc([NE], F32), Buf())
        sel = (A.alloc([NE], F32), Buf())
        selb = (A.alloc([NE], BF16), Buf())
        pos = (A.alloc([NE], F32), Buf())
        oh = (A.alloc([8, NE], F32), Buf())
        sm = (A.alloc([160], F32), Buf())
        dsc = [(A.alloc([8], I32), Buf()) for _ in range(2)]
        sg = (A.alloc([256], F32), Buf())
        hmid = (A.alloc([2, 128], BF16), Buf())
        smv = sm[0]
        b_s = sm[1]
        m8g = smv[:, 0:64].rearrange("p (g e) -> p g e", g=8)
        gs_ = smv[:, 64:72]
        m8 = smv[:, 72:80]
        gmask = smv[:, 80:88]
        gm1 = smv[:, 88:96]
        m8b = smv[:, 96:104]
        w8 = smv[:, 104:112]
        e8f = smv[:, 112:120]
        posk = smv[:, 120:128]
        dest = smv[:, 128:136]
        valid = smv[:, 136:144]
        t8 = smv[:, 144:152]
        ws1 = smv[:, 152:153]
        e8 = A.alloc([8], U32)
        def stage1(i):
            ti = b * NLT + i
            sc, bia = scs[i % 2], bias_[i % 2]
            tmp = tmpA
            tt_ = i % 4
            yt, byt = yT4[(i // 4) % 2]
            if tt_ == 0:
                P.dma("sp", dma(yt, self.YT_d[b].rearrange("k p t -> p k t")[:, :, i * 128:(i + 4) * 128]), reads=[self.b_YT], writes=[byt])
            xt, bx = xts[i % 2]
            P.dma("act", dma(xt, self.x_d[b * L + i * 128: b * L + (i + 1) * 128, :]), writes=[bx])
            pss = [self.bank(), self.bank()]
            for n in range(2):
                for k in range(KD):
                    P.op("pe", mm(pss[n][0][:, :], yt[:, k, tt_ * 128:(tt_ + 1) * 128], self.wout[:, k, n * 512:(n + 1) * 512], k == 0, k == KD - 1),
                         reads=[byt, self.b_wts], writes=[pss[n][1]])
            for n in range(2):
                P.op("dve", tt(tmp[0][:, n * 512:(n + 1) * 512], pss[n][0][:, :], G1[:, n * 512:(n + 1) * 512], ALU.mult),
                     reads=[pss[n][1], b_m], writes=[tmp[1]])
            x1, bx1 = x1s[i % 2]
            P.op("dve", tt(x1, tmp[0], xt, ALU.add), reads=[tmp[1], bx], writes=[bx1])
            if b == 0 and "x1" in self.dbg:
                if not hasattr(self, "dbg_x1"):
                    self.dbg_x1 = self.dout("dbg_x1", [L, D])
                P.dma("sp", dma(self.dbg_x1[i * 128:(i + 1) * 128, :], x1), reads=[bx1])
            hf, bhf = hfs[i % 2]
            hb, bhb = hbs[i % 2]
            hT2, bhT2 = h2Tb[i % 2]
            self.norm_mod_sb(i, x1, bx1, A2, b_m, B2, b_m, hT2, bhT2, hf_out=(hf, bhf), hb_out=(hb, bhb))
            if b == 0 and "hx2" in self.dbg:
                if not hasattr(self, "dbg_hx2"):
                    self.dbg_hx2 = self.dout("dbg_hx2", [L, D])
                P.dma("sp", dma(self.dbg_hx2[i * 128:(i + 1) * 128, :], hf), reads=[bhf])
            pf = [self.bank(), self.bank()]
            for k in range(KD):
                P.op("pe", tr(pf[k // 4][0][:, (k % 4) * 128:(k % 4 + 1) * 128], hf[:, k * 128:(k + 1) * 128], self.identf),
                     reads=[bhf, self.b_cst], writes=[pf[k // 4][1]])
            hTf, bhTf = h2Tf[i % 2]
            P.op("act", act(hTf[:, 0:4, :], pf[0][0].rearrange("p (k t) -> p k t", k=4), AF.Copy), reads=[pf[0][1]], writes=[bhTf])
            P.op("dve", cp(hTf[:, 4:8, :], pf[1][0].rearrange("p (k t) -> p k t", k=4)), reads=[pf[1][1]], writes=[bhTf])
            pr, pbr = self.bank()
            for k in range(KD):
                P.op("pe", mm(pr[:, 0:NE], hTf[:, k, :], self.wr[:, k, :], k == 0, k == KD - 1), reads=[bhTf, self.b_rc], writes=[pbr])
            P.op("act", act(sc[0], pr[:, 0:NE], AF.Sigmoid), reads=[pbr], writes=[sc[1]])
            P.op("dve", tt(bia[0], sc[0], self.rbias, ALU.add), reads=[sc[1], self.b_rc], writes=[bia[1]])
        def stage2(i):
            ti = b * NLT + i
            x1, bx1 = x1s[i % 2]
            hf, bhf = hfs[i % 2]
            hb, bhb = hbs[i % 2]
            hT2, bhT2 = h2Tb[i % 2]
            sc, bia = scs[i % 2], bias_[i % 2]
            tmp = tmpB
            bia3 = bia[0].rearrange("p (g e) -> p g e", g=8)
            for g in range(8):
                P.op("dve", lambda e, g=g: e.max(out=m8g[:, g, :], in_=bia3[:, g, :]), reads=[bia[1]], writes=[b_s])
            P.op("dve", tt(gs_, m8g[:, :, 0], m8g[:, :, 1], ALU.add), reads=[b_s], writes=[b_s])
            P.op("dve", lambda e: e.max(out=m8, in_=gs_), reads=[b_s], writes=[b_s])
            P.op("dve", ts(gmask, gs_, m8[:, 3:4], None, ALU.is_ge), reads=[b_s], writes=[b_s])
            P.op("dve", ts(gm1, gmask, -1.0, None, ALU.add), reads=[b_s], writes=[b_s])
            msk3 = msk[0].rearrange("p (g e) -> p g e", g=8)
            P.op("dve", tt(msk3, bia3, gmask.unsqueeze(2).to_broadcast([128, 8, 32]), ALU.mult), reads=[bia[1], b_s], writes=[msk[1]])
            P.op("dve", tt(msk3, msk3, gm1.unsqueeze(2).to_broadcast([128, 8, 32]), ALU.add), reads=[msk[1], b_s], writes=[msk[1]])
            P.op("dve", lambda e: e.max(out=m8b, in_=msk[0]), reads=[msk[1]], writes=[b_s])
            P.op("dve", ts(sel[0], msk[0], m8b[:, 7:8], None, ALU.is_ge), reads=[msk[1], b_s], writes=[sel[1]])
            P.op("act", act(selb[0], sel[0], AF.Copy), reads=[sel[1]], writes=[selb[1]])
            P.op("dve", tt(msk[0], sc[0], sel[0], ALU.mult), reads=[sc[1], sel[1]], writes=[msk[1]])
            P.op("dve", lambda e: e.max(out=w8, in_=msk[0]), reads=[msk[1]], writes=[b_s])
            P.op("dve", lambda e: e.max_index(out=e8, in_max=w8, in_values=msk[0]), reads=[msk[1], b_s], writes=[b_s])
            P.op("dve", cp(e8f, e8), reads=[b_s], writes=[b_s])
            P.op("dve", rsum(ws1, w8), reads=[b_s], writes=[b_s])
            P.op("dve", lambda e: e.reciprocal(out=ws1, in_=ws1), reads=[b_s], writes=[b_s])
            P.op("dve", ts(w8, w8, ws1, RSCALE, ALU.mult, ALU.mult), reads=[b_s], writes=[b_s])
            pp, pbp = self.bank()
            P.op("pe", mm(pp[:, 0:NE], self.ustrict_b, selb[0], True, True), reads=[selb[1], self.b_cb], writes=[pbp])
            pc, pbc = self.bank()
            P.op("pe", mm(pc[:, 0:NE], self.ones_b, selb[0], True, True), reads=[selb[1], self.b_cb], writes=[pbc])
            P.op("dve", tt(pos[0], pp[:, 0:NE], self.cnt, ALU.add), reads=[pbp, self.b_cnt], writes=[pos[1]])
            P.op("dve", tt(self.cnt, self.cnt, pc[:, 0:NE], ALU.add), reads=[pbc, pos[1]], writes=[self.b_cnt])
            P.op("dve", tt(oh[0], self.iota3.unsqueeze(1).to_broadcast([128, 8, NE]), e8f.unsqueeze(2).to_broadcast([128, 8, NE]), ALU.is_equal),
                 reads=[self.b_rc, b_s], writes=[oh[1]])
            P.op("dve", tt(oh[0], oh[0], pos[0].unsqueeze(1).to_broadcast([128, 8, NE]), ALU.mult), reads=[oh[1], pos[1]], writes=[oh[1]])
            P.op("dve", rsum(posk, oh[0]), reads=[oh[1]], writes=[b_s])
            P.op("dve", stt(dest, e8f, float(CAP), posk, ALU.mult, ALU.add), reads=[b_s], writes=[b_s])
            P.op("dve", ts(valid, posk, float(CAP), None, ALU.is_lt), reads=[b_s], writes=[b_s])
            P.op("dve", ts(t8, valid, -1.0e6, 1.0e6, ALU.mult, ALU.add), reads=[b_s], writes=[b_s])
            P.op("dve", tt(t8, t8, dest, ALU.add), reads=[b_s], writes=[b_s])
            di, bdi = dsc[i % 2]
            P.op("dve", cp(di, t8), reads=[b_s], writes=[bdi])
            P.op("dve", tt(self.DestF[:, ti, :], dest, valid, ALU.mult), reads=[b_s], writes=[self.b_ov])
            P.op("dve", tt(self.wAll[:, ti, :], w8, valid, ALU.mult), reads=[b_s], writes=[self.b_dw])
            P.op("dve", cp(self.E8All[:, ti, :], e8f), reads=[b_s], writes=[self.b_ov])
            P.op("dve", cp(self.P8All[:, ti, :], posk), reads=[b_s], writes=[self.b_ov])
            P.op("dve", cp(self.W8raw[:, ti, :], w8), reads=[b_s], writes=[self.b_ov])
            P.dma("act", dma(self.H2_d[ti * 128:(ti + 1) * 128, :], hb), reads=[bhb], writes=[self.b_H2])
            for k in range(8):
                P.dma("pool", lambda e, k=k, di=di, hb=hb: e.indirect_dma_start(
                    out=self.XS_d, out_offset=bass.IndirectOffsetOnAxis(ap=di[:, k:k + 1], axis=0), in_=hb, in_offset=None,
                    bounds_check=self.bc_reg, oob_is_err=False), reads=[bdi, bhb], writes=[self.b_XS])
            ph, pbh = self.bank()
            for fc in range(4):
                for k in range(KD):
                    P.op("pe", mm(ph[:, fc * 128:(fc + 1) * 128], self.swgu[:, k, fc * 128:(fc + 1) * 128], hT2[:, k, :], k == 0, k == KD - 1),
                         reads=[bhT2, self.b_wts], writes=[pbh])
            P.op("act", act(sg[0], ph[:, 0:256], AF.Silu), reads=[pbh], writes=[sg[1]])
            P.op("dve", tt(hmid[0].rearrange("p a b -> p (a b)"), sg[0], ph[:, 256:512], ALU.mult), reads=[sg[1], pbh], writes=[hmid[1]])
            pd = [self.bank(), self.bank()]
            for n in range(2):
                for fc in range(2):
                    P.op("pe", mm(pd[n][0][:, :], hmid[0][:, fc, :], self.swd[:, fc, n * 512:(n + 1) * 512], fc == 0, fc == 1),
                         reads=[hmid[1], self.b_wts], writes=[pd[n][1]])
            for n in range(2):
                P.op("dve", tt(tmp[0][:, n * 512:(n + 1) * 512], pd[n][0][:, :], G2[:, n * 512:(n + 1) * 512], ALU.mult),
                     reads=[pd[n][1], b_m], writes=[tmp[1]])
            P.op("dve", tt(x1, tmp[0], x1, ALU.add), reads=[tmp[1], bx1], writes=[bx1])
            P.dma("sp", dma(self.X1_d[ti * 128:(ti + 1) * 128, :], x1), reads=[bx1], writes=[self.b_X1])
        stage1(0)
        for i in range(NLT):
            if i + 1 < NLT:
                stage1(i + 1)
            stage2(i)
        P.barrier()
        A.release()

    def overflow_route(self):
        P, A, NB = self.P, self.A, self.NB
        NTT = NB * NLT
        A.mark()
        pidx = A.alloc([1], F32)
        b_c = Buf()
        P.dma("sp", dma(pidx, self.pidx_d), writes=[b_c])
        thr = A.alloc([1], F32)
        P.op("dve", ts(thr, pidx, 128.0, None, ALU.mult), reads=[b_c], writes=[b_c])
        ovc = A.alloc([NE], F32)
        b_o = Buf()
        P.op("dve", ts(ovc, self.cnt, -float(CAP), 0.0, ALU.add, ALU.max), reads=[self.b_cnt], writes=[b_o])
        cmpb = A.alloc([NE], BF16)
        P.op("dve", ts(cmpb, ovc, thr, None, ALU.is_gt), reads=[b_o, b_c], writes=[b_o])
        ps, pb = self.bank()
        P.op("pe", mm(ps[:, 0:NE], self.ones_b, cmpb, True, True), reads=[b_o, self.b_cb], writes=[pb])
        ovblk = A.alloc([NE], F32)
        ovend = A.alloc([NE], F32)
        ovs = A.alloc([NE], F32)
        onesr = A.alloc([NE], F32)
        b_e = Buf()
        P.op("act", act(ovblk, ps[:, 0:NE], AF.Copy), reads=[pb], writes=[b_e])
        P.op("pool", lambda e: e.memset(onesr, 1.0), writes=[b_e])
        P.op("dve", lambda e: e.tensor_tensor_scan(out=ovend, data0=onesr, data1=ovblk, initial=0.0, op0=ALU.mult, op1=ALU.add),
             reads=[b_e], writes=[b_e])
        P.op("dve", tt(ovs, ovend, ovblk, ALU.subtract), reads=[b_e], writes=[b_e])
        P.op("dve", ts(ovs, ovs, 128.0, None, ALU.mult), reads=[b_e], writes=[b_e])
        beRow = A.alloc([OV], F32)
        b_b = Buf()
        cmp3 = A.alloc([16, NE], F32)
        for c0 in range(0, OV, 16):
            P.op("dve", tt(cmp3, ovend.unsqueeze(1).to_broadcast([128, 16, NE]),
                           self.iota3[:, c0:c0 + 16].unsqueeze(2).to_broadcast([128, 16, NE]), ALU.is_le), reads=[b_e, self.b_rc], writes=[b_b])
            P.op("dve", rsum(beRow[:, c0:c0 + 16], cmp3), reads=[b_b], writes=[b_b])
        self.dump("be", beRow, b_b, [128, OV])
        idxf = A.alloc([OV], F32)
        P.op("dve", ts(idxf, beRow, 128.0, pidx, ALU.mult, ALU.add), reads=[b_b, b_c], writes=[b_b])
        P.op("dve", cp(self.IDXG, idxf), reads=[b_b], writes=[self.b_idx])
        oh = A.alloc([8, NE], F32)
        b_oh = Buf()
        sm = A.alloc([64], F32)
        b_s = Buf()
        ob8, ovslot, isov, ok, t8 = (sm[:, 8 * q:8 * q + 8] for q in range(5))
        di_ = [(A.alloc([8], I32), Buf()) for _ in range(2)]
        hbs = [(A.alloc([D], BF16), Buf()) for _ in range(2)]
        for ti in range(NTT):
            hb, bhb = hbs[ti % 2]
            P.dma("sp", dma(hb, self.H2_d[ti * 128:(ti + 1) * 128, :]), reads=[self.b_H2], writes=[bhb])
            P.op("dve", tt(oh, self.iota3.unsqueeze(1).to_broadcast([128, 8, NE]),
                           self.E8All[:, ti, :].unsqueeze(2).to_broadcast([128, 8, NE]), ALU.is_equal), reads=[self.b_rc, self.b_ov], writes=[b_oh])
            P.op("dve", tt(oh, oh, ovs.unsqueeze(1).to_broadcast([128, 8, NE]), ALU.mult), reads=[b_oh, b_e], writes=[b_oh])
            P.op("dve", rsum(ob8, oh), reads=[b_oh], writes=[b_s])
            P.op("dve", stt(ovslot, self.P8All[:, ti, :], -float(CAP), ob8, ALU.add, ALU.add), reads=[b_s, self.b_ov], writes=[b_s])
            P.op("dve", ts(isov, self.P8All[:, ti, :], float(CAP), None, ALU.is_ge), reads=[self.b_ov], writes=[b_s])
            P.op("dve", ts(ok, ovslot, float(OV * 128), None, ALU.is_lt), reads=[b_s], writes=[b_s])
            P.op("dve", tt(ok, ok, isov, ALU.mult), reads=[b_s], writes=[b_s])
            P.op("dve", ts(ovslot, ovslot, float(NE * CAP), None, ALU.add), reads=[b_s], writes=[b_s])
            P.op("dve", ts(t8, ok, -1.0e6, 1.0e6, ALU.mult, ALU.add), reads=[b_s], writes=[b_s])
            P.op("dve", tt(t8, t8, ovslot, ALU.add), reads=[b_s], writes=[b_s])
            di, bdi = di_[ti % 2]
            P.op("dve", cp(di, t8), reads=[b_s], writes=[bdi])
            P.op("dve", tt(ovslot, ovslot, ok, ALU.mult), reads=[b_s], writes=[b_s])
            P.op("dve", tt(t8, self.DestF[:, ti, :], ovslot, ALU.add), reads=[b_s, self.b_ov], writes=[b_s])
            P.op("dve", cp(self.destAll[:, ti, :], t8), reads=[b_s], writes=[self.b_dw])
            P.op("dve", tt(t8, self.W8raw[:, ti, :], ok, ALU.mult), reads=[b_s, self.b_ov], writes=[b_s])
            P.op("dve", tt(self.wAll[:, ti, :], self.wAll[:, ti, :], t8, ALU.add), reads=[b_s, self.b_dw], writes=[self.b_dw])
            for k in range(8):
                P.dma("pool", lambda e, k=k, di=di, hb=hb: e.indirect_dma_start(
                    out=self.XS_d, out_offset=bass.IndirectOffsetOnAxis(ap=di[:, k:k + 1], axis=0), in_=hb, in_offset=None,
                    bounds_check=self.bc_reg2, oob_is_err=False), reads=[bdi, bhb], writes=[self.b_XS])
        P.barrier()
        A.release()

    def experts(self):
        P, A = self.P, self.A
        self.dump("cnt", self.cnt, self.b_cnt, [128, NE])
        A.mark()
        xs4 = [(A.alloc([NBLK, D], BF16), Buf()) for _ in range(3)]
        xT = [(A.alloc([KD, CAP], BF16), Buf()) for _ in range(2)]
        wguf = [(A.alloc([KD, 512], F32), Buf()) for _ in range(3)]
        wdf = [(A.alloc([2, D], F32), Buf()) for _ in range(3)]
        wgub = [(A.alloc([KD, 512], BF16), Buf()) for _ in range(2)]
        wdb = [(A.alloc([2, D], BF16), Buf()) for _ in range(2)]
        sg = [(A.alloc([2, CAP], F32), Buf()) for _ in range(1)]
        hm = [(A.alloc([2, CAP], BF16), Buf()) for _ in range(2)]
        yo = [(A.alloc([D], BF16), Buf()) for _ in range(3)]
        self.b_Y = Buf("Y")

        def loads(e_):
            x4, bx4 = xs4[e_ % 3]
            P.dma("sp", dma(x4, self.XS_d[e_ * CAP:(e_ + 1) * CAP, :].rearrange("(s p) d -> p s d", p=128)), reads=[self.b_XS], writes=[bx4])
            wg, bwg = wguf[e_ % 3]
            wd, bwd = wdf[e_ % 3]
            P.dma("sp", dma(wg[:, :, 0:256], self.ewg_d[e_].rearrange("p (k f) -> p k f", k=KD)), writes=[bwg])
            P.dma("sp", dma(wg[:, :, 256:512], self.ewu_d[e_].rearrange("p (k f) -> p k f", k=KD)), writes=[bwg])
            P.dma("sp", dma(wd, self.ewd_d[e_].rearrange("p (k n) -> p k n", k=2)), writes=[bwd])

        def casts(e_):
            wg, bwg = wguf[e_ % 3]
            wd, bwd = wdf[e_ % 3]
            wgb, bwgb = wgub[e_ % 2]
            wdb_, bwdb = wdb[e_ % 2]
            P.op("dve", cp(wgb[:, 0:3, :], wg[:, 0:3, :]), reads=[bwg], writes=[bwgb])
            P.op("act", act(wgb[:, 3:6, :], wg[:, 3:6, :], AF.Copy), reads=[bwg], writes=[bwgb])
            P.op("pool", cp(wgb[:, 6:8, :], wg[:, 6:8, :]), reads=[bwg], writes=[bwgb])
            P.op("act", act(wdb_[:, 0, :], wd[:, 0, :], AF.Copy), reads=[bwd], writes=[bwdb])
            P.op("dve", cp(wdb_[:, 1, :], wd[:, 1, :]), reads=[bwd], writes=[bwdb])

        def transposes(e_):
            x4, bx4 = xs4[e_ % 3]
            xt_, bxt = xT[e_ % 2]
            for sb in range(NBLK):
                ps, pb = self.bank()
                psb = ps.bitcast(BF16)
                for k in range(KD):
                    P.op("pe", tr(psb[:, k * 128:(k + 1) * 128], x4[:, sb, k * 128:(k + 1) * 128], self.identb), reads=[bx4, self.b_cb], writes=[pb])
                src = psb.rearrange("p (k t) -> p k t", k=KD)
                P.op("dve", cp(xt_[:, :, sb * 128:(sb + 1) * 128], src), reads=[pb], writes=[bxt])

        loads(0)
        loads(1)
        casts(0)
        transposes(0)
        yc = 0
        for e_ in range(NE):
            if e_ + 2 < NE:
                loads(e_ + 2)
            if e_ + 1 < NE:
                casts(e_ + 1)
            wgb, bwgb = wgub[e_ % 2]
            wdb_, bwdb = wdb[e_ % 2]
            xt_, bxt = xT[e_ % 2]
            pg = [self.bank() for _ in range(4)]
            for fc in range(4):
                for k in range(KD):
                    P.op("pe", mm(pg[fc][0][:, 0:CAP], wgb[:, k, fc * 128:(fc + 1) * 128], xt_[:, k, :], k == 0, k == KD - 1),
                         reads=[bwgb, bxt], writes=[pg[fc][1]])
            sg_, bsg = sg[0]
            hm_, bhm = hm[e_ % 2]
            for fc in range(2):
                P.op("act", act(sg_[:, fc, :], pg[fc][0][:, 0:CAP], AF.Silu), reads=[pg[fc][1]], writes=[bsg])
                P.op("dve", tt(hm_[:, fc, :], sg_[:, fc, :], pg[2 + fc][0][:, 0:CAP], ALU.mult), reads=[bsg, pg[2 + fc][1]], writes=[bhm])
            if e_ + 1 < NE:
                transposes(e_ + 1)
            for sb in range(NBLK):
                pd = [self.bank(), self.bank()]
                for n in range(2):
                    for fc in range(2):
                        P.op("pe", mm(pd[n][0][:, :], hm_[:, fc, sb * 128:(sb + 1) * 128], wdb_[:, fc, n * 512:(n + 1) * 512], fc == 0, fc == 1),
                             reads=[bhm, bwdb], writes=[pd[n][1]])
                y_, by = yo[yc % 3]
                yc += 1
                P.op("act", act(y_[:, 0:512], pd[0][0][:, :], AF.Copy), reads=[pd[0][1]], writes=[by])
                P.op("act", act(y_[:, 512:1024], pd[1][0][:, :], AF.Copy), reads=[pd[1][1]], writes=[by])
                r0 = e_ * CAP + sb * 128
                P.dma("act", dma(self.Y_d[r0:r0 + 128, :], y_), reads=[by], writes=[self.b_Y])
        if OV > 0:
            ewg_rows = self.ewg_d.rearrange("e p n -> (e p) n")
            ewu_rows = self.ewu_d.rearrange("e p n -> (e p) n")
            ewd_rows = self.ewd_d.rearrange("e p n -> (e p) n")
            ovw = [(A.alloc([KD * 256], BF16), A.alloc([KD * 256], BF16), A.alloc([2 * D], BF16), Buf()) for _ in range(2)]

            def ovloads(j):
                x4, bx4 = xs4[j % 3]
                r0 = NE * CAP + j * 128
                P.dma("sp", dma(x4[:, 0, :], self.XS_d[r0:r0 + 128, :]), reads=[self.b_XS], writes=[bx4])
                og, ou, od, bw = ovw[j % 2]
                off = lambda j=j: bass.IndirectOffsetOnAxis(ap=self.IDXG[:, j:j + 1], axis=0)
                P.dma("pool", lambda e, og=og, off=off: e.indirect_dma_start(out=og, out_offset=None, in_=ewg_rows, in_offset=off(), bounds_check=self.bc_reg3, oob_is_err=False),
                      reads=[self.b_idx], writes=[bw])
                P.dma("pool", lambda e, ou=ou, off=off: e.indirect_dma_start(out=ou, out_offset=None, in_=ewu_rows, in_offset=off(), bounds_check=self.bc_reg3, oob_is_err=False),
                      reads=[self.b_idx], writes=[bw])
                P.dma("pool", lambda e, od=od, off=off: e.indirect_dma_start(out=od, out_offset=None, in_=ewd_rows, in_offset=off(), bounds_check=self.bc_reg3, oob_is_err=False),
                      reads=[self.b_idx], writes=[bw])

            ovloads(0)
            for j in range(OV):
                if j + 1 < OV:
                    ovloads(j + 1)
                x4, bx4 = xs4[j % 3]
                og, ou, od, bw = ovw[j % 2]
                og3 = og.rearrange("p (k f) -> p k f", k=KD)
                ou3 = ou.rearrange("p (k f) -> p k f", k=KD)
                od3 = od.rearrange("p (k n) -> p k n", k=2)
                xt_, bxt = xT[j % 2]
                ps, pb = self.bank()
                psb = ps.bitcast(BF16)
                for k in range(KD):
                    P.op("pe", tr(psb[:, k * 128:(k + 1) * 128], x4[:, 0, k * 128:(k + 1) * 128], self.identb), reads=[bx4, self.b_cb], writes=[pb])
                P.op("dve", cp(xt_[:, :, 0:128], psb.rearrange("p (k t) -> p k t", k=KD)), reads=[pb], writes=[bxt])
                pg, pbg = self.bank()
                for fc in range(4):
                    wsrc = og3 if fc < 2 else ou3
                    f0 = (fc % 2) * 128
                    for k in range(KD):
                        P.op("pe", mm(pg[:, fc * 128:(fc + 1) * 128], wsrc[:, k, f0:f0 + 128], xt_[:, k, 0:128], k == 0, k == KD - 1),
                             reads=[bw, bxt], writes=[pbg])
                sg_, bsg = sg[0]
                hm_, bhm = hm[j % 2]
                for fc in range(2):
                    P.op("act", act(sg_[:, fc, 0:128], pg[:, fc * 128:(fc + 1) * 128], AF.Silu), reads=[pbg], writes=[bsg])
                    P.op("dve", tt(hm_[:, fc, 0:128], sg_[:, fc, 0:128], pg[:, (2 + fc) * 128:(3 + fc) * 128], ALU.mult), reads=[bsg, pbg], writes=[bhm])
                pd = [self.bank(), self.bank()]
                for n in range(2):
                    for fc in range(2):
                        P.op("pe", mm(pd[n][0][:, :], hm_[:, fc, 0:128], od3[:, fc, n * 512:(n + 1) * 512], fc == 0, fc == 1),
                             reads=[bhm, bw], writes=[pd[n][1]])
                y_, by = yo[yc % 3]
                yc += 1
                P.op("act", act(y_[:, 0:512], pd[0][0][:, :], AF.Copy), reads=[pd[0][1]], writes=[by])
                P.op("act", act(y_[:, 512:1024], pd[1][0][:, :], AF.Copy), reads=[pd[1][1]], writes=[by])
                r0 = NE * CAP + j * 128
                P.dma("act", dma(self.Y_d[r0:r0 + 128, :], y_), reads=[by], writes=[self.b_Y])
        P.barrier()
        A.release()

    def combine(self):
        P, A, NB = self.P, self.A, self.NB
        A.mark()
        mt = self.MOD_d.tensor
        fing = A.alloc([D], F32)
        b_fg = Buf()
        P.dma("sp", dma(fing, bcast_rows(self.fing_d.tensor, 0, D)), writes=[b_fg])
        G2s = [(A.alloc([D], F32), Buf()) for _ in range(2)]
        base = [(A.alloc([D], F32), Buf()) for _ in range(2)]
        yk = [(A.alloc([D], BF16), Buf()) for _ in range(8)]
        acc = [(A.alloc([D], F32), Buf()) for _ in range(2)]
        junk = (A.alloc([D], F32), Buf())
        pre = [(A.alloc([D], F32), Buf()) for _ in range(2)]
        ssb = [(A.alloc([1], F32), Buf()) for _ in range(2)]
        ot = [(A.alloc([D], F32), Buf()) for _ in range(2)]
        for b in range(NB):
            G2, bG2 = G2s[b % 2]
            P.dma("sp", dma(G2, bcast_rows(mt, b * 6 * D + 5 * D, D)), reads=[self.b_MOD], writes=[bG2])
            for i in range(NLT):
                ti = b * NLT + i
                bs, bbs = base[i % 2]
                P.dma("sp", dma(bs, self.X1_d[ti * 128:(ti + 1) * 128, :]), reads=[self.b_X1], writes=[bbs])
                ac, bac = acc[i % 2]
                for k in range(8):
                    y_, by = yk[k]
                    P.dma("pool", lambda e, k=k, y_=y_, ti=ti: e.indirect_dma_start(
                        out=y_, out_offset=None, in_=self.Y_d, in_offset=bass.IndirectOffsetOnAxis(ap=self.destAll[:, ti, k:k + 1], axis=0)),
                        reads=[self.b_dw, self.b_Y], writes=[by])
                    if k == 0:
                        P.op("dve", ts(ac, y_, self.wAll[:, ti, 0:1], None, ALU.mult), reads=[by, self.b_dw], writes=[bac])
                    else:
                        P.op("dve", stt(ac, y_, self.wAll[:, ti, k:k + 1], ac, ALU.mult, ALU.add), reads=[by, self.b_dw, bac], writes=[bac])
                pa_, bpa = pre[0]
                pb_, bpb = pre[1]
                P.op("dve", tt(pa_, ac, G2, ALU.mult), reads=[bac, bG2], writes=[bpa])
                P.op("dve", tt(pb_, pa_, bs, ALU.add), reads=[bpa, bbs], writes=[bpb])
                ac, bac = pb_, bpb
                ss, bss = ssb[i % 2]
                P.op("act", act(junk[0], ac, AF.Square), reads=[bac], writes=[junk[1]])
                P.op("dve", rsum(ss, junk[0]), reads=[junk[1]], writes=[bss])
                P.op("act", act(ss, ss, AF.Sqrt, scale=1.0 / D, bias=self.eps_ap), reads=[bss, self.b_eps], writes=[bss])
                P.op("dve", lambda e, ss=ss: e.reciprocal(out=ss, in_=ss), reads=[bss], writes=[bss])
                o_, bo = ot[i % 2]
                P.op("dve", stt(o_, ac, ss, fing, ALU.mult, ALU.mult), reads=[bac, bss, b_fg], writes=[bo])
                P.dma("sp", dma(self.out_d[ti * 128:(ti + 1) * 128, :], o_), reads=[bo])
        A.release()


def const_tables():
    import math
    ident = np.eye(128, dtype=np.float32)
    J = ident[::-1].copy()
    s = np.arange(128)[:, None]
    c = np.arange(128)[None, :]
    same = (s // 64) == (c // 64)
    maskF = (same & (s <= c)).astype(np.float32)
    maskB = (same & (s >= c)).astype(np.float32)
    Sm = ((c == s + 1) & ((c % 64) != 0)).astype(np.float32)
    Sp = ((c == s - 1) & ((c % 64) != 63)).astype(np.float32)
    ustrict = (s < c).astype(np.float32)
    ones = np.ones((128, 128), np.float32)
    cst = np.stack([ident, J, maskF, maskB, Sm, Sp, ustrict, ones, Sm[:, ::-1], Sp[:, ::-1]], axis=1).astype(np.float32)
    f32 = np.float32
    pos = np.arange(L, dtype=f32)[:, None]
    t = pos / f32(L - 1)
    w = f32(2.0 * math.pi / L) * pos
    bands = np.linspace(1e-4, 15, 16, dtype=f32)[None, :]
    feats = np.concatenate([t, np.cos(bands * w), -np.sin(bands * w)], axis=-1).astype(f32)
    max_decay = math.log(1e-2) / 0.3
    min_decay = math.log(1e-2) / 1.5
    deltas = np.abs(np.linspace(min_decay, max_decay, 512, dtype=f32))[None, :].astype(f32)
    tfrac = (-(np.arange(L, dtype=f32) / f32(L - 1))).reshape(16, 128).T.copy()
    iota = np.arange(NE, dtype=f32)[None, :]
    pidx = np.arange(128, dtype=f32)[:, None].copy()
    return dict(cst=cst, featsT=np.ascontiguousarray(feats.T), deltas=deltas, tfrac=tfrac.astype(f32), iota=iota, pidx=pidx)


def prep_core(inp, core, NB, tables):
    f = lambda a: np.ascontiguousarray(a, dtype=np.float32)
    b0 = core * NB
    m = dict(tables)
    m["x"] = f(inp["x"][b0:b0 + NB].reshape(NB * L, D))
    m["ctx"] = f(inp["ctx"][b0:b0 + NB].reshape(NB * CTX, D))
    cc = np.concatenate([inp["c"][b0:b0 + NB], inp["c_ctx"][None, :]], axis=0)
    m["cT"] = f(cc.T.reshape(KD, 128, NB + 1).transpose(1, 0, 2))
    return m


def shared_inputs(inp):
    f = lambda a: np.ascontiguousarray(a, dtype=np.float32)
    m = {}
    m["w_mod"] = f(inp["w_mod"][0])
    m["b_mod"] = f(inp["b_mod"][0][None, :])
    m["norm1_g"] = f(inp["norm1_g"][0][None, :])
    m["norm2_g"] = f(inp["norm2_g"][0][None, :])
    m["final_g"] = f(inp["final_g"][None, :])
    m["w_in"] = f(inp["w_in"][0])
    m["w_out"] = f(inp["w_out"][0])
    m["hy_conv_w"] = f(inp["hy_conv_w"][0])
    m["hy_conv_b"] = f(inp["hy_conv_b"][0][None, :])
    m["hy_fw1"] = f(inp["hy_fw1"][0])
    m["hy_fb1"] = f(inp["hy_fb1"][0][:, None])
    m["hy_fw2"] = f(inp["hy_fw2"][0])
    m["hy_fb2"] = f(inp["hy_fb2"][0][:, None])
    m["hy_fw3"] = f(inp["hy_fw3"][0])
    m["hy_freq"] = f(inp["hy_freq"][0][:, None])
    m["hy_d"] = f(inp["hy_d"][0].reshape(1, 1024))
    lg = inp["hg_lb_logits"].reshape(2, 2, 4, 128)
    m["lbT"] = f(lg.transpose(3, 0, 1, 2).reshape(128, 16))
    m["hg_norm_g"] = f(inp["hg_norm_g"][0][None, :])
    m["w_router"] = f(inp["w_router"][0])
    m["router_bias"] = f(inp["router_bias"][0][None, :])
    m["ew_gate"] = f(np.asarray(inp["ew_gate"][0]).reshape(NE, KD, 128, 256).transpose(0, 2, 1, 3).reshape(NE, 128, KD * 256))
    m["ew_up"] = f(np.asarray(inp["ew_up"][0]).reshape(NE, KD, 128, 256).transpose(0, 2, 1, 3).reshape(NE, 128, KD * 256))
    m["ew_down"] = f(np.asarray(inp["ew_down"][0]).reshape(NE, 2, 128, D).transpose(0, 2, 1, 3).reshape(NE, 128, 2 * D))
    m["sw_gate"] = f(inp["sw_gate"][0])
    m["sw_up"] = f(inp["sw_up"][0])
    m["sw_down"] = f(inp["sw_down"][0])
    return m


def kernel(**inputs):
    NB = 4
    ncores = 8
    k = K(NB)
    nc = k.build()
    tables = const_tables()
    sh = shared_inputs(inputs)
    in_maps = []
    for c in range(ncores):
        m = prep_core(inputs, c, NB, tables)
        m.update(sh)
        in_maps.append(m)
    res = run_bass_kernel_spmd(nc, in_maps, core_ids=list(range(ncores)))
    outs = [np.asarray(r["out"], dtype=np.float32).reshape(NB, L, D) for r in res.results]
    return np.concatenate(outs, axis=0)
```
